# Optimizing a Trainium2 kernel written in Bass

```python
import math
import jax, jax.numpy as jnp
from jax import lax
import numpy as np

D_MODEL = 1024
BATCH = 4
SEQ = 8192
DEPTH = 1

ATTN_WIDTH = D_MODEL // 2
CONV_WIDTH = D_MODEL - ATTN_WIDTH
HEAD_DIM = 64
N_HEADS = ATTN_WIDTH // HEAD_DIM
CONV_GROUPS = 8
MOBA_BLOCK = 256
MOBA_TOPK = 3
QUERY_CHUNK = 32
CONV_K = 3
D_FF = 256 * math.ceil(8 * D_MODEL / 3 / 256)
IN_COLS = 3 * ATTN_WIDTH + 3 * CONV_WIDTH
RMS_EPS = 1e-6
NEG_BIG = -1e30

kernel_name = "hymba_moba_shortconv_convffn_adaln"


def rmsnorm(x, g):
    xf = x.astype(jnp.float32)
    y = xf * lax.rsqrt(jnp.mean(xf * xf, axis=-1, keepdims=True) + RMS_EPS)
    return (y * g.astype(jnp.float32)).astype(x.dtype)


def causal_dwconv(x, w, b):
    K = w.shape[0]
    S = x.shape[1]
    xp = jnp.pad(x, ((0, 0), (K - 1, 0), (0, 0)))
    y = xp[:, 0:S] * w[0]
    for kk in range(1, K):
        y = y + xp[:, kk:kk + S] * w[kk]
    return y + b


def alibi_slopes(n_heads):
    return 2.0 ** (-8.0 * jnp.arange(1, n_heads + 1, dtype=jnp.float32) / n_heads)


def moba_attention(q, k, v, slopes):
    B, H, S, dh = q.shape
    nb = -(-S // MOBA_BLOCK)
    s_pad = nb * MOBA_BLOCK
    pad = ((0, 0), (0, 0), (0, s_pad - S), (0, 0))
    k_blocks = jnp.pad(k, pad).reshape(B, H, nb, MOBA_BLOCK, dh)
    v_blocks = jnp.pad(v, pad).reshape(B, H, nb, MOBA_BLOCK, dh)
    k_mean = jnp.mean(k_blocks.astype(jnp.float32), axis=3)
    n_sel = min(MOBA_TOPK, nb)
    scale = dh ** -0.5
    n_chunks = S // QUERY_CHUNK
    bi = jnp.arange(B)[:, None, None, None]
    hi = jnp.arange(H)[None, :, None, None]
    blk_pos = jnp.arange(MOBA_BLOCK)

    def one_chunk(ci):
        start = ci * QUERY_CHUNK
        qc = lax.dynamic_slice_in_dim(q, start, QUERY_CHUNK, axis=2).astype(jnp.float32)
        t = start + jnp.arange(QUERY_CHUNK)
        blk = start // MOBA_BLOCK
        gate = jnp.einsum('bhqd,bhnd->bhqn', qc, k_mean)
        gate = jnp.where(jnp.arange(nb) < blk, gate, NEG_BIG)
        _, idx = lax.top_k(gate, n_sel)
        sel_ok = idx < blk
        ks = k_blocks[bi, hi, idx].astype(jnp.float32)
        vs = v_blocks[bi, hi, idx].astype(jnp.float32)
        s_sel = jnp.einsum('bhqd,bhqnkd->bhqnk', qc, ks) * scale
        pos_sel = idx[..., None] * MOBA_BLOCK + blk_pos
        dist_sel = (t[None, None, :, None, None] - pos_sel).astype(jnp.float32)
        s_sel = s_sel - slopes[None, :, None, None, None] * dist_sel
        s_sel = jnp.where(sel_ok[..., None], s_sel, NEG_BIG)
        k_own = lax.dynamic_index_in_dim(k_blocks, blk, axis=2, keepdims=False).astype(jnp.float32)
        v_own = lax.dynamic_index_in_dim(v_blocks, blk, axis=2, keepdims=False).astype(jnp.float32)
        s_own = jnp.einsum('bhqd,bhkd->bhqk', qc, k_own) * scale
        dist_own = (t[:, None] - (blk * MOBA_BLOCK + blk_pos)[None, :]).astype(jnp.float32)
        s_own = s_own - slopes[None, :, None, None] * dist_own
        s_own = jnp.where(dist_own >= 0, s_own, NEG_BIG)
        scores = jnp.concatenate(
            [s_sel.reshape(B, H, QUERY_CHUNK, n_sel * MOBA_BLOCK), s_own], axis=-1)
        p = jax.nn.softmax(scores, axis=-1)
        p_sel = p[..., :n_sel * MOBA_BLOCK].reshape(B, H, QUERY_CHUNK, n_sel, MOBA_BLOCK)
        p_own = p[..., n_sel * MOBA_BLOCK:]
        out = (jnp.einsum('bhqnk,bhqnkd->bhqd', p_sel, vs)
               + jnp.einsum('bhqk,bhkd->bhqd', p_own, v_own))
        return out.astype(q.dtype)

    outs = lax.map(one_chunk, jnp.arange(n_chunks))
    return outs.transpose(1, 2, 0, 3, 4).reshape(B, H, S, dh)


def setup_inputs(seed: int = 0) -> dict:
    key = jax.random.key(seed)
    ks = jax.random.split(key, 20)
    f32 = jnp.float32

    def nrm(k, shape, scale):
        return jax.random.normal(k, shape, f32) * scale

    L = DEPTH
    return {
        "x": nrm(ks[0], (BATCH, SEQ, D_MODEL), 1.0),
        "c": nrm(ks[1], (BATCH, D_MODEL), 1.0),
        "w_ada": nrm(ks[2], (L, D_MODEL, 6 * D_MODEL), 0.5 * D_MODEL ** -0.5),
        "b_ada": nrm(ks[3], (L, 6 * D_MODEL), 0.01),
        "g_mix": 1.0 + nrm(ks[4], (L, D_MODEL), 0.02),
        "w_in": nrm(ks[5], (L, D_MODEL, IN_COLS), D_MODEL ** -0.5),
        "conv_w": nrm(ks[6], (L, CONV_K, CONV_WIDTH), CONV_K ** -0.5),
        "conv_b": nrm(ks[7], (L, CONV_WIDTH), 0.01),
        "g_attn_out": 1.0 + nrm(ks[8], (L, ATTN_WIDTH), 0.02),
        "g_conv_out": 1.0 + nrm(ks[9], (L, CONV_WIDTH), 0.02),
        "w_out": nrm(ks[10], (L, D_MODEL, D_MODEL), D_MODEL ** -0.5),
        "g_ffn": 1.0 + nrm(ks[11], (L, D_MODEL), 0.02),
        "w_up": nrm(ks[12], (L, D_MODEL, 2 * D_FF), D_MODEL ** -0.5),
        "ffn_conv_w": nrm(ks[13], (L, CONV_K, 2 * D_FF), CONV_K ** -0.5),
        "ffn_conv_b": nrm(ks[14], (L, 2 * D_FF), 0.01),
        "w_down": nrm(ks[15], (L, D_FF, D_MODEL), D_FF ** -0.5),
        "g_final": 1.0 + nrm(ks[16], (D_MODEL,), 0.02),
    }


def reference(x, c, w_ada, b_ada, g_mix, w_in, conv_w, conv_b, g_attn_out, g_conv_out,
              w_out, g_ffn, w_up, ffn_conv_w, ffn_conv_b, w_down, g_final):
    B, S, D = x.shape
    slopes = alibi_slopes(N_HEADS)
    A, C = ATTN_WIDTH, CONV_WIDTH
    for l in range(DEPTH):
        mod = (jax.nn.silu(c) @ w_ada[l] + b_ada[l]).astype(x.dtype)
        sh1, sc1, gt1, sh2, sc2, gt2 = jnp.split(mod[:, None, :], 6, axis=-1)

        h = rmsnorm(x, g_mix[l]) * (1.0 + sc1) + sh1
        proj = h @ w_in[l]
        q, k, v, u, c_gate, b_gate = jnp.split(
            proj, [A, 2 * A, 3 * A, 3 * A + C, 3 * A + 2 * C], axis=-1)

        def heads(t):
            return t.reshape(B, S, N_HEADS, HEAD_DIM).transpose(0, 2, 1, 3)

        attn = moba_attention(heads(q), heads(k), heads(v), slopes)
        attn = attn.transpose(0, 2, 1, 3).reshape(B, S, A)
        conv = b_gate * causal_dwconv(c_gate * u, conv_w[l], conv_b[l])
        mixed = jnp.concatenate([rmsnorm(attn, g_attn_out[l]),
                                 rmsnorm(conv, g_conv_out[l])], axis=-1)
        x = x + gt1 * (mixed @ w_out[l])

        h = rmsnorm(x, g_ffn[l]) * (1.0 + sc2) + sh2
        up = causal_dwconv(h @ w_up[l], ffn_conv_w[l], ffn_conv_b[l])
        val, gte = jnp.split(up, 2, axis=-1)
        x = x + gt2 * ((jax.nn.silu(gte) * val) @ w_down[l])
    return rmsnorm(x, g_final)
```

```python
import numpy as np
from contextlib import ExitStack
import ml_dtypes
import concourse.bass as bass
import concourse.mybir as mybir
from concourse.bass_utils import run_bass_kernel_spmd

F32 = mybir.dt.float32
BF16 = mybir.dt.bfloat16
AF = mybir.ActivationFunctionType
ALU = mybir.AluOpType
AX = mybir.AxisListType

D = 1024
S = 8192
NH = 8
DFF = 2816
NQB = 17
NQT = 34
QTOK = NQB * 256
EPS = 1e-6
NEG = -30000.0
NCORES = 8
PHASES = 3
DEBUG = False


class Tok:
    __slots__ = ("sem", "val")

    def __init__(self, sem, val):
        self.sem = sem
        self.val = val


class Slot:
    def __init__(self, name=""):
        self.name = name
        self.w = None
        self.r = {}
        self.dsem = None
        self.dcnt = 0


class Builder:
    ENG = ("pe", "act", "dve", "pool", "sp")

    def __init__(self, nc, es):
        self.nc = nc
        self.es = es
        self.q = {}
        for n in self.ENG:
            sem = es.enter_context(nc.semaphore("sem_" + n))
            self.q[n] = dict(sem=sem, cnt=0, ops=[], waited={})
        self.dslots = []

    def _waits(self, en, deps):
        q = self.q[en]
        need = {}
        for t in deps:
            if t is None:
                continue
            if en == "pe" and t.sem is q["sem"]:
                continue
            k = id(t.sem)
            if q["waited"].get(k, 0) >= t.val:
                continue
            if k not in need or need[k].val < t.val:
                need[k] = t
        for k, t in need.items():
            q["waited"][k] = t.val
        return [(t.sem, t.val) for t in need.values()]

    @staticmethod
    def _deps(reads, writes, deps):
        d = list(deps)
        for s in reads:
            d.append(s.w)
        for s in writes:
            d.extend(s.r.values())
            d.append(s.w)
        return d

    @staticmethod
    def _update(tok, reads, writes):
        for s in writes:
            s.w = tok
            s.r = {}
        for s in reads:
            k = id(tok.sem)
            if k not in s.r or s.r[k].val < tok.val:
                s.r[k] = tok

    def op(self, en, fn, reads=(), writes=(), deps=(), inc=True):
        q = self.q[en]
        waits = self._waits(en, self._deps(reads, writes, deps))
        tok = None
        if inc:
            q["cnt"] += 1
            tok = Tok(q["sem"], q["cnt"])
        else:
            assert not reads and not writes
        sem = q["sem"]

        def emit(e, waits=waits, fn=fn, inc=inc, sem=sem):
            for (sm, v) in waits:
                e.wait_ge(sm, v)
            ins = fn(e)
            if inc:
                ins.then_inc(sem, 1)

        q["ops"].append(emit)
        if tok is not None:
            self._update(tok, reads, writes)
        return tok

    def pe_group(self, fns, reads=(), writes=()):
        q = self.q["pe"]
        waits = self._waits("pe", self._deps(reads, writes, ()))
        q["cnt"] += 1
        tok = Tok(q["sem"], q["cnt"])
        sem = q["sem"]
        n = len(fns)

        def emit(e, waits=waits, fns=fns, sem=sem, n=n):
            for (sm, v) in waits:
                e.wait_ge(sm, v)
            for i, f in enumerate(fns):
                ins = f(e)
                if i == n - 1:
                    ins.then_inc(sem, 1)

        q["ops"].append(emit)
        self._update(tok, reads, writes)
        return tok

    def dma(self, en, out, in_, sem_slot, reads=(), writes=(), deps=(), **kw):
        q = self.q[en]
        sl = sem_slot
        if sl.dsem is None:
            sl.dsem = self.es.enter_context(self.nc.semaphore("dsem%d" % len(self.dslots)))
            self.dslots.append(sl)
        waits = self._waits(en, self._deps(reads, writes, deps))
        sl.dcnt += 16
        tok = Tok(sl.dsem, sl.dcnt)
        dsem = sl.dsem

        def emit(e, waits=waits, out=out, in_=in_, dsem=dsem, kw=kw):
            for (sm, v) in waits:
                e.wait_ge(sm, v)
            e.dma_start(out=out, in_=in_, **kw).then_inc(dsem, 16)

        q["ops"].append(emit)
        self._update(tok, reads, writes)
        return tok

    def emit_block(self):
        nc = self.nc
        finals = [(s.dsem, s.dcnt) for s in self.dslots if s.dcnt > 0]
        pe_fin = (self.q["pe"]["sem"], self.q["pe"]["cnt"])

        def fin(e, finals=finals):
            for sm, v in finals:
                e.wait_ge(sm, v)

        self.q["sp"]["ops"].append(fin)
        with nc.Block() as blk:
            for n, meth in (("pe", blk.tensor), ("act", blk.scalar), ("dve", blk.vector),
                            ("pool", blk.gpsimd), ("sp", blk.sync)):
                ops = self.q[n]["ops"]
                if ops:
                    def body(e, ops=ops):
                        for f in ops:
                            f(e)
                    meth(body)
                self.q[n]["ops"] = []


def mm(out, lhsT, rhs, start, stop):
    return lambda e: e.matmul(out, lhsT=lhsT, rhs=rhs, start=start, stop=stop)


def tr(out, in_, ident):
    return lambda e: e.transpose(out, in_, ident)


def build_program():
    nc = bass.Bass("TRN2", target_bir_lowering=False)

    def din(name, shape, dt=F32):
        return nc.dram_tensor(name, list(shape), dt, kind="ExternalInput").ap()

    def dscr(name, shape, dt):
        return nc.dram_tensor(name, list(shape), dt, kind="ExternalOutput" if DEBUG else "Internal").ap()

    xs = din("xs", [S, D])
    cT = din("cT", [128, 8])
    w_ada = din("w_ada", [D, 6 * D])
    b_ada = din("b_ada", [1, 6 * D])
    gmix = din("gmix", [128, 8])
    w_in = din("w_in", [D, 3072])
    convw = din("convw", [128, 12])
    convb = din("convb", [128, 4])
    gattn = din("gattn", [64, 8])
    gconv = din("gconv", [128, 4])
    w_out = din("w_out", [D, D])
    gffn = din("gffn", [128, 8])
    w_up = din("w_up", [D, 2 * DFF])
    fcw = din("fcw", [128, 132])
    fcb = din("fcb", [128, 44])
    w_down = din("w_down", [DFF, D])
    gfin = din("gfin", [128, D])
    ident_d = din("ident", [128, 128], BF16)
    kstat = din("kstat", [35, S], BF16)
    cmask = din("cmask", [128, 512], BF16)
    gvb_d = din("gvb", [128, NQB * 32])
    gvb2_d = din("gvb2", [128, NQB * 32])
    qstat = din("qstat", [NH, 128, NQT * 99], BF16)
    hv_d = din("hv", [128, 1])
    out = nc.dram_tensor("out", [4096, D], F32, kind="ExternalOutput").ap()

    kT_s = dscr("kT_s", [NH, 64, S], BF16)
    qT_s = dscr("qT_s", [NH, 64, QTOK], BF16)
    at_s = dscr("at_s", [NH, 64, QTOK], BF16)
    cg_s = dscr("cg_s", [4, 128, QTOK], BF16)
    mod_s = dscr("mod_s", [1, 6 * D], F32)

    es = ExitStack()
    with es:
        B = Builder(nc, es)

        def sb(st, name, shape, dt):
            return st.enter_context(nc.sbuf_tensor("sb_" + name, list(shape), dt))

        def ps(st, name, shape, dt):
            return st.enter_context(nc.psum_tensor("ps_" + name, list(shape), dt))

        ident = sb(es, "ident", [128, 128], BF16)
        ones_b = sb(es, "ones_b", [128, 1], BF16)
        ones_f = sb(es, "ones_f", [128, 64], F32)
        epsc = sb(es, "epsc", [128, 1], F32)
        hv = sb(es, "hv", [128, 1], F32)
        G1 = sb(es, "G1", [128, 8], F32)
        G2 = sb(es, "G2", [128, 8], F32)
        modT = sb(es, "modT", [128, 48], F32)
        gmix_sb = sb(es, "gmix_sb", [128, 8], F32)
        gffn_sb = sb(es, "gffn_sb", [128, 8], F32)
        convw_sb = sb(es, "convw_sb", [128, 12], F32)
        convb_sb = sb(es, "convb_sb", [128, 4], F32)
        gconv_sb = sb(es, "gconv_sb", [128, 4], F32)
        gattn_sb = sb(es, "gattn_sb", [64, 8], F32)
        fcw_sb = sb(es, "fcw_sb", [128, 132], F32)
        fcb_sb = sb(es, "fcb_sb", [128, 44], F32)
        ssa = sb(es, "ssa", [128, NQT], F32)
        ssc = sb(es, "ssc", [128, NQT], F32)
        S_const = Slot("const")
        S_ssa = Slot("ssa")
        S_ssc = Slot("ssc")
        S_G = Slot("G")
        S_mods = Slot("mods")

        for dst, src in ((ident, ident_d), (hv, hv_d), (gmix_sb, gmix), (gffn_sb, gffn), (convw_sb, convw),
                         (convb_sb, convb), (gconv_sb, gconv), (gattn_sb, gattn), (fcw_sb, fcw), (fcb_sb, fcb)):
            B.dma("sp", dst[:], src, S_const, writes=[S_const])
        B.op("dve", lambda e: e.memset(ones_b[:], 1.0), writes=[S_const])
        B.op("dve", lambda e: e.memset(ones_f[:], 1.0), writes=[S_const])
        B.op("dve", lambda e: e.memset(epsc[:], EPS), writes=[S_const])

        with ExitStack() as p0:
            c_sb = sb(p0, "c_sb", [128, 8], F32)
            sc_b = sb(p0, "sc_b", [128, 8], BF16)
            brow = sb(p0, "brow", [1, 6 * D], F32)
            modrow = sb(p0, "modrow", [1, 6 * D], F32)
            wa = [sb(p0, "wa%d" % i, [128, 8, 512], BF16) for i in range(2)]
            pm = [ps(p0, "pm%d" % i, [128, 512], F32) for i in range(2)]
            S_c, S_scb, S_brow, S_modrow, S_modT = Slot(), Slot(), Slot(), Slot(), Slot()
            S_wa = [Slot(), Slot()]
            S_pm = [Slot(), Slot()]
            B.dma("sp", c_sb[:], cT, S_c, writes=[S_c])
            B.dma("sp", brow[:], b_ada, S_brow, writes=[S_brow])
            B.op("act", lambda e: e.activation(out=sc_b[:], in_=c_sb[:], func=AF.Silu), reads=[S_c], writes=[S_scb])
            wav = w_ada.rearrange("(c p) n -> p c n", p=128)
            for g in range(12):
                k = g % 2
                B.dma("pool", wa[k][:], wav[:, :, g * 512:(g + 1) * 512], S_wa[k], writes=[S_wa[k]])
                B.pe_group([mm(pm[k][0:1, :], sc_b[:, c:c + 1], wa[k][:, c, :], c == 0, c == 7) for c in range(8)],
                           reads=[S_wa[k], S_scb], writes=[S_pm[k]])
                B.op("dve", lambda e, k=k, g=g: e.tensor_tensor(out=modrow[0:1, g * 512:(g + 1) * 512], in0=pm[k][0:1, :],
                                                                 in1=brow[0:1, g * 512:(g + 1) * 512], op=ALU.add),
                     reads=[S_pm[k], S_brow], writes=[S_modrow])
            B.dma("sp", mod_s, modrow[:], S_mods, reads=[S_modrow], writes=[S_mods])
            B.dma("sp", modT[:], mod_s.rearrange("o (k p) -> (o p) k", p=128), S_modT, reads=[S_mods], writes=[S_modT],
                  allow_slow_non_contiguous=True)
            B.op("dve", lambda e: e.scalar_tensor_tensor(out=G1[:], in0=modT[:, 8:16], scalar=1.0, in1=gmix_sb[:],
                                                         op0=ALU.add, op1=ALU.mult), reads=[S_modT, S_const], writes=[S_G])
            B.op("dve", lambda e: e.scalar_tensor_tensor(out=G2[:], in0=modT[:, 32:40], scalar=1.0, in1=gffn_sb[:],
                                                         op0=ALU.add, op1=ALU.mult), reads=[S_modT, S_const], writes=[S_G])
            B.emit_block()
        sh1 = modT[:, 0:8]
        sh2 = modT[:, 24:32]

        with ExitStack() as p12:
          if PHASES >= 1:
            V_all = sb(p12, "V_all", [128, 64, NH, 65], BF16)
            S_V = Slot("V")
            with ExitStack() as p1:
                w_in_sb = sb(p1, "w_in_sb", [128, 8, 3072], BF16)
                S_win = Slot("win")
                wiv = w_in.rearrange("(c p) n -> p c n", p=128)
                for c in range(8):
                    B.dma("pool", w_in_sb[:, c, :], wiv[:, c, :], S_win, writes=[S_win])
                xt = [sb(p1, "xt%d" % i, [128, 2, D], F32) for i in range(2)]
                xn = [sb(p1, "xn%d" % i, [128, 2, D], BF16) for i in range(2)]
                hT = [sb(p1, "hT%d" % i, [128, 8, 258], BF16) for i in range(2)]
                junk = sb(p1, "junk", [128, D], BF16)
                ss = [sb(p1, "ss%d" % i, [128, 2], F32) for i in range(2)]
                rt = [sb(p1, "rt%d" % i, [128, 2], F32) for i in range(2)]
                rstd = [sb(p1, "rstd%d" % i, [128, 2], F32) for i in range(2)]
                kst = [sb(p1, "kst%d" % i, [128, 4, 256], BF16) for i in range(2)]
                qst = [sb(p1, "qst%d" % i, [128, 4, 256], BF16) for i in range(2)]
                cgs = [sb(p1, "cgs%d" % i, [128, 4, 256], BF16) for i in range(2)]
                sqc = sb(p1, "sqc", [128, 4, 256], BF16)
                u_sb = [sb(p1, "u_sb%d" % i, [128, 258], F32) for i in range(2)]
                zb = [sb(p1, "zb%d" % i, [128, 258], F32) for i in range(2)]
                y0 = [sb(p1, "y0%d" % i, [128, 256], F32) for i in range(2)]
                y1 = [sb(p1, "y1%d" % i, [128, 256], F32) for i in range(2)]
                y2 = [sb(p1, "y2%d" % i, [128, 256], F32) for i in range(2)]
                cv = [sb(p1, "cv%d" % i, [128, 256], F32) for i in range(2)]
                tp = ps(p1, "tp", [128, 8, 256], BF16)
                pp = [ps(p1, "pp%d" % i, [128, 512], F32) for i in range(5)]
                pss = ps(p1, "pss", [128, 512], F32)
                S_xt = [Slot(), Slot()]
                S_xn = [Slot(), Slot()]
                S_hT = [Slot(), Slot()]
                S_junk = Slot()
                S_ss = [Slot(), Slot()]
                S_rt = [Slot(), Slot()]
                S_rstd = [Slot(), Slot()]
                S_kst = [Slot(), Slot()]
                S_qst = [Slot(), Slot()]
                S_cgs = [Slot(), Slot()]
                S_sqc = Slot()
                S_u = [Slot(), Slot()]
                S_z = [Slot(), Slot()]
                S_y0 = [Slot(), Slot()]
                S_y1 = [Slot(), Slot()]
                S_y2 = [Slot(), Slot()]
                S_cv = [Slot(), Slot()]
                S_tp = Slot()
                S_pp = [Slot() for _ in range(5)]
                S_pss = Slot()
                S_kT = Slot("kT_s")
                S_qT = Slot("qT_s")
                S_cgd = Slot("cg_s")
                ppi = [0]

                def next_pp():
                    i = ppi[0] % 5
                    ppi[0] += 1
                    return pp[i], S_pp[i]

                B.op("pool", lambda e: e.memset(V_all[:, :, :, 64:65], 1.0), writes=[S_V])
                for i in range(2):
                    B.op("pool", lambda e, i=i: e.memset(hT[i][:, :, 0:2], 0.0), writes=[S_hT[i]])
                xsv = xs.rearrange("(n t p) d -> n p t d", t=2, p=128)
                kTv = kT_s.rearrange("(pr two) d t -> (two d) pr t", two=2)
                qTv = qT_s.rearrange("(pr two) d t -> (two d) pr t", two=2)
                cgv = cg_s.rearrange("c p t -> p c t")
                evac_rr = [0]

                def evac(out_ap, in_ap, reads, writes, scale=None):
                    evac_rr[0] += 1
                    if evac_rr[0] % 2 == 0:
                        if scale is None:
                            return B.op("act", lambda e: e.activation(out=out_ap, in_=in_ap, func=AF.Copy), reads=reads, writes=writes)
                        return B.op("act", lambda e: e.activation(out=out_ap, in_=in_ap, func=AF.Copy, scale=scale), reads=reads, writes=writes)
                    if scale is None:
                        return B.op("dve", lambda e: e.tensor_copy(out=out_ap, in_=in_ap), reads=reads, writes=writes)
                    return B.op("dve", lambda e: e.tensor_scalar(out=out_ap, in0=in_ap, scalar1=scale, scalar2=None, op0=ALU.mult),
                                reads=reads, writes=writes)

                def front1(s):
                    k = s % 2
                    B.dma("sp", xt[k][:], xsv[s], S_xt[k], writes=[S_xt[k]])
                    for t in range(2):
                        B.op("act", lambda e, k=k, t=t: e.activation(out=junk[:], in_=xt[k][:, t, :], func=AF.Square,
                                                                     accum_out=ss[k][:, t:t + 1]),
                             reads=[S_xt[k]], writes=[S_junk, S_ss[k]])
                    B.op("act", lambda e, k=k: e.activation(out=rt[k][:], in_=ss[k][:], func=AF.Sqrt, scale=1.0 / D, bias=epsc[:]),
                         reads=[S_ss[k], S_const], writes=[S_rt[k]])
                    B.op("dve", lambda e, k=k: e.reciprocal(out=rstd[k][:], in_=rt[k][:]), reads=[S_rt[k]], writes=[S_rstd[k]])
                    for t in range(2):
                        B.op("dve", lambda e, k=k, t=t: e.tensor_scalar(out=xn[k][:, t, :], in0=xt[k][:, t, :], scalar1=rstd[k][:, t:t + 1],
                                                                        scalar2=None, op0=ALU.mult),
                             reads=[S_xt[k], S_rstd[k]], writes=[S_xn[k]])
                    B.pe_group([tr(tp[:, c, t * 128:(t + 1) * 128], xn[k][:, t, c * 128:(c + 1) * 128], ident[:])
                                for t in range(2) for c in range(8)], reads=[S_xn[k], S_const], writes=[S_tp])
                    for c in range(8):
                        B.op("act", lambda e, k=k, c=c: e.activation(out=hT[k][:, c, 2:258], in_=tp[:, c, :], func=AF.Identity,
                                                                     scale=G1[:, c:c + 1], bias=sh1[:, c:c + 1]),
                             reads=[S_tp, S_G], writes=[S_hT[k]])
                    if s == 16:
                        B.op("dve", lambda e, k=k: e.tensor_scalar(out=hT[k][:, :, 0:2], in0=hT[1 - k][:, :, 256:258], scalar1=hv[:, 0:1],
                                                                   scalar2=None, op0=ALU.mult),
                             reads=[S_hT[1 - k], S_const], writes=[S_hT[k]])
                    elif s > 16:
                        B.op("dve", lambda e, k=k: e.tensor_copy(out=hT[k][:, :, 0:2], in_=hT[1 - k][:, :, 256:258]),
                             reads=[S_hT[1 - k]], writes=[S_hT[k]])

                def back1(s):
                    k = s % 2
                    for pr in range(4):
                        pt_, sp_ = next_pp()
                        B.pe_group([mm(pt_[:, 0:256], w_in_sb[:, c, 512 + pr * 128:512 + (pr + 1) * 128], hT[k][:, c, 2:258], c == 0, c == 7)
                                    for c in range(8)], reads=[S_win, S_hT[k]], writes=[sp_])
                        evac(kst[k][:, pr, :], pt_[:, 0:256], [sp_], [S_kst[k]])
                    B.dma("sp", kTv[:, :, s * 256:(s + 1) * 256], kst[k][:], S_kst[k], reads=[S_kst[k]], writes=[S_kT])
                    for t in range(2):
                        pt_, sp_ = next_pp()
                        B.pe_group([mm(pt_[:, :], hT[k][:, c, 2 + t * 128:2 + (t + 1) * 128], w_in_sb[:, c, 1024:1536], c == 0, c == 7)
                                    for c in range(8)], reads=[S_win, S_hT[k]], writes=[sp_])
                        evac(V_all[:, 2 * s + t, :, 0:64], pt_[:, :].rearrange("p (h d) -> p h d", d=64), [sp_], [S_V])
                    if s < 15:
                        return
                    qb = s - 15
                    for pr in range(4):
                        pt_, sp_ = next_pp()
                        B.pe_group([mm(pt_[:, 0:256], w_in_sb[:, c, pr * 128:(pr + 1) * 128], hT[k][:, c, 2:258], c == 0, c == 7)
                                    for c in range(8)], reads=[S_win, S_hT[k]], writes=[sp_])
                        evac(qst[k][:, pr, :], pt_[:, 0:256], [sp_], [S_qst[k]], scale=0.125)
                    B.dma("sp", qTv[:, :, qb * 256:(qb + 1) * 256], qst[k][:], S_qst[k], reads=[S_qst[k]], writes=[S_qT])
                    for ch in range(4):
                        j = ch % 2
                        pu_, spu = next_pp()
                        B.pe_group([mm(pu_[:, 0:258], w_in_sb[:, c, 1536 + ch * 128:1536 + (ch + 1) * 128], hT[k][:, c, 0:258], c == 0, c == 7)
                                    for c in range(8)], reads=[S_win, S_hT[k]], writes=[spu])
                        pc_, spc = next_pp()
                        B.pe_group([mm(pc_[:, 0:258], w_in_sb[:, c, 2048 + ch * 128:2048 + (ch + 1) * 128], hT[k][:, c, 0:258], c == 0, c == 7)
                                    for c in range(8)], reads=[S_win, S_hT[k]], writes=[spc])
                        pb_, spb = next_pp()
                        B.pe_group([mm(pb_[:, 0:256], w_in_sb[:, c, 2560 + ch * 128:2560 + (ch + 1) * 128], hT[k][:, c, 2:258], c == 0, c == 7)
                                    for c in range(8)], reads=[S_win, S_hT[k]], writes=[spb])
                        B.op("act", lambda e, j=j, pu_=pu_: e.activation(out=u_sb[j][:], in_=pu_[:, 0:258], func=AF.Copy),
                             reads=[spu], writes=[S_u[j]])
                        B.op("dve", lambda e, j=j, pc_=pc_: e.tensor_tensor(out=zb[j][:], in0=pc_[:, 0:258], in1=u_sb[j][:], op=ALU.mult),
                             reads=[spc, S_u[j]], writes=[S_z[j]])
                        B.op("dve", lambda e, j=j, ch=ch: e.tensor_scalar(out=y0[j][:], in0=zb[j][:, 2:258], scalar1=convw_sb[:, 8 + ch:9 + ch],
                                                                          scalar2=convb_sb[:, ch:ch + 1], op0=ALU.mult, op1=ALU.add),
                             reads=[S_z[j], S_const], writes=[S_y0[j]])
                        B.op("dve", lambda e, j=j, ch=ch: e.scalar_tensor_tensor(out=y1[j][:], in0=zb[j][:, 1:257], scalar=convw_sb[:, 4 + ch:5 + ch],
                                                                                 in1=y0[j][:], op0=ALU.mult, op1=ALU.add),
                             reads=[S_z[j], S_y0[j], S_const], writes=[S_y1[j]])
                        B.op("dve", lambda e, j=j, ch=ch: e.scalar_tensor_tensor(out=y2[j][:], in0=zb[j][:, 0:256], scalar=convw_sb[:, ch:ch + 1],
                                                                                 in1=y1[j][:], op0=ALU.mult, op1=ALU.add),
                             reads=[S_z[j], S_y1[j], S_const], writes=[S_y2[j]])
                        B.op("dve", lambda e, j=j, pb_=pb_: e.tensor_tensor(out=cv[j][:], in0=pb_[:, 0:256], in1=y2[j][:], op=ALU.mult),
                             reads=[spb, S_y2[j]], writes=[S_cv[j]])
                        B.op("act", lambda e, j=j, ch=ch: e.activation(out=sqc[:, ch, :], in_=cv[j][:], func=AF.Square),
                             reads=[S_cv[j]], writes=[S_sqc])
                        B.op("act", lambda e, j=j, ch=ch, k=k: e.activation(out=cgs[k][:, ch, :], in_=cv[j][:], func=AF.Copy,
                                                                           scale=gconv_sb[:, ch:ch + 1]),
                             reads=[S_cv[j], S_const], writes=[S_cgs[k]])
                    for t in range(2):
                        B.pe_group([mm(pss[:, t:t + 1], sqc[:, ch, t * 128:(t + 1) * 128], ones_b[:, 0:1], ch == 0, ch == 3) for ch in range(4)],
                                   reads=[S_sqc, S_const], writes=[S_pss])
                    B.op("dve", lambda e, qb=qb: e.tensor_copy(out=ssc[:, 2 * qb:2 * qb + 2], in_=pss[:, 0:2]), reads=[S_pss], writes=[S_ssc])
                    B.dma("sp", cgv[:, :, qb * 256:(qb + 1) * 256], cgs[k][:], S_cgs[k], reads=[S_cgs[k]], writes=[S_cgd])

                front1(0)
                for s in range(32):
                    if s + 1 < 32:
                        front1(s + 1)
                    back1(s)
                B.emit_block()

            with ExitStack() as p2:
              if PHASES >= 2:
                kaug = [sb(p2, "kaug%d" % i, [99, S], BF16) for i in range(2)]
                qaug = [sb(p2, "qaug%d" % i, [99, QTOK], BF16) for i in range(2)]
                qa = [sb(p2, "qa%d" % i, [128, NQT, 99], BF16) for i in range(2)]
                atst = [sb(p2, "atst%d" % i, [64, QTOK], BF16) for i in range(2)]
                gvb = sb(p2, "gvb", [128, NQB, 32], F32)
                gvb2 = sb(p2, "gvb2", [128, NQB, 32], F32)
                cm = sb(p2, "cm", [128, 2, 256], BF16)
                km_f = sb(p2, "km_f", [64, 32], F32)
                km_b = [sb(p2, "km_b%d" % i, [64, 32], BF16) for i in range(2)]
                gm = [sb(p2, "gm%d" % i, [128, 32], F32) for i in range(2)]
                t8 = [sb(p2, "t8%d" % i, [128, 8], F32) for i in range(2)]
                tsel = [sb(p2, "tsel%d" % i, [128, 32], F32) for i in range(2)]
                pT = [sb(p2, "pT%d" % i, [128, 2, 256], BF16) for i in range(3)]
                rc = [sb(p2, "rc%d" % i, [128, 256], F32) for i in range(2)]
                bcs = [sb(p2, "bcs%d" % i, [64, 256], F32) for i in range(2)]
                atf = [sb(p2, "atf%d" % i, [64, 256], F32) for i in range(2)]
                sqa = [sb(p2, "sqa%d" % i, [64, 256], BF16) for i in range(2)]
                sp = [ps(p2, "sp%d" % i, [128, 2, 256], F32) for i in range(3)]
                po = [ps(p2, "po%d" % i, [128, 512], F32) for i in range(2)]
                pgt = ps(p2, "pgt", [128, 512], F32)
                pbc = ps(p2, "pbc", [128, 512], F32)
                ptt_t = ps(p2, "ptt", [128, 1024], BF16)
                ptt = ptt_t[:, 0:128]
                S_kaug = [Slot(), Slot()]
                S_kstat = Slot()
                S_qq = [Slot(), Slot()]
                S_qm = [Slot(), Slot()]
                S_qa = [Slot(), Slot()]
                S_atst = [Slot(), Slot()]
                S_c2 = Slot()
                S_kmf = Slot()
                S_kmb = [Slot(), Slot()]
                S_gm = [Slot(), Slot()]
                S_t8 = [Slot(), Slot()]
                S_tsel = [Slot(), Slot()]
                S_pT = [Slot() for _ in range(3)]
                S_rc = [Slot(), Slot()]
                S_bcs = [Slot(), Slot()]
                S_atf = [Slot(), Slot()]
                S_sqa = [Slot(), Slot()]
                S_sp = [Slot() for _ in range(3)]
                S_po = [Slot(), Slot()]
                S_pgt, S_ptt, S_pbc = Slot(), Slot(), Slot()
                S_pss2 = S_pbc
                S_atd = Slot("at_s")

                B.dma("sp", gvb[:].rearrange("p a b -> p (a b)"), gvb_d, S_c2, writes=[S_c2])
                B.dma("sp", gvb2[:].rearrange("p a b -> p (a b)"), gvb2_d, S_c2, writes=[S_c2])
                B.dma("sp", cm[:].rearrange("p a b -> p (a b)"), cmask, S_c2, writes=[S_c2])
                for i in range(2):
                    B.dma("sp", kaug[i][64:99, :], kstat, S_kstat, writes=[S_kstat])

                def load_head(h):
                    hb = h % 2
                    B.dma("sp", kaug[hb][0:64, :], kT_s[h], S_kaug[hb], reads=[S_kT], writes=[S_kaug[hb]])
                    B.dma("sp", qaug[hb][0:64, :], qT_s[h], S_qq[hb], reads=[S_qT], writes=[S_qq[hb]])
                    B.dma("sp", qa[hb][:].rearrange("p a b -> p (a b)"), qstat[h], S_qa[hb], writes=[S_qa[hb]])

                def prep_head(h):
                    hb = h % 2
                    B.op("dve", lambda e: e.tensor_reduce(out=km_f[:], in_=kaug[hb][0:64, :].rearrange("p (j c) -> p j c", c=256),
                                                          axis=AX.X, op=ALU.add),
                         reads=[S_kaug[hb]], writes=[S_kmf])
                    B.op("dve", lambda e: e.tensor_scalar(out=km_b[hb][:], in0=km_f[:], scalar1=1.0 / 256, scalar2=None, op0=ALU.mult),
                         reads=[S_kmf], writes=[S_kmb[hb]])

                def mask_tile(h, qt):
                    hb = h % 2
                    qb = qt // 2
                    g2 = qt % 2
                    B.pe_group([mm(pgt[:, 0:32], qaug[hb][0:64, qt * 128:(qt + 1) * 128], km_b[hb][:, :], True, True)],
                               reads=[S_qq[hb], S_kmb[hb]], writes=[S_pgt])
                    B.op("dve", lambda e: e.tensor_tensor(out=gm[g2][:], in0=pgt[:, 0:32], in1=gvb[:, qb, :], op=ALU.add),
                         reads=[S_pgt, S_c2], writes=[S_gm[g2]])
                    B.op("dve", lambda e: e.max(out=t8[g2][:], in_=gm[g2][:]), reads=[S_gm[g2]], writes=[S_t8[g2]])
                    B.op("dve", lambda e: e.tensor_scalar(out=tsel[g2][:], in0=gm[g2][:], scalar1=t8[g2][:, 2:3], scalar2=-NEG,
                                                          op0=ALU.is_ge, op1=ALU.mult),
                         reads=[S_gm[g2], S_t8[g2]], writes=[S_tsel[g2]])
                    B.op("dve", lambda e: e.tensor_tensor(out=qa[hb][:, qt, 64:96], in0=tsel[g2][:], in1=gvb2[:, qb, :], op=ALU.add),
                         reads=[S_tsel[g2], S_c2], writes=[S_qa[hb]])
                    B.pe_group([tr(ptt[0:99, :], qa[hb][:, qt, :], ident[:])], reads=[S_qa[hb], S_const], writes=[S_ptt])
                    B.op("act", lambda e: e.activation(out=qaug[hb][64:99, qt * 128:(qt + 1) * 128], in_=ptt[64:99, :], func=AF.Copy),
                         reads=[S_ptt], writes=[S_qm[hb]])

                sctr = [0]

                def s_mm(h, qb, j):
                    hb = h % 2
                    sB = 15 + qb
                    qc = slice(qb * 256, (qb + 1) * 256)
                    i = sctr[0] % 3
                    sctr[0] += 1
                    fns = []
                    for kt in range(2):
                        fns.append(mm(sp[i][:, kt, :], kaug[hb][0:99, (2 * j + kt) * 128:(2 * j + kt + 1) * 128], qaug[hb][0:99, qc],
                                      True, j != sB))
                        if j == sB:
                            fns.append(mm(sp[i][:, kt, :], ident[:], cm[:, kt, :], False, True))
                    B.pe_group(fns, reads=[S_kaug[hb], S_kstat, S_qq[hb], S_qm[hb], S_c2, S_const], writes=[S_sp[i]])
                    B.op("act", lambda e: e.activation(out=pT[i][:], in_=sp[i][:], func=AF.Exp), reads=[S_sp[i]], writes=[S_pT[i]])
                    return i

                def pv_mm(h, qb, j, i):
                    sB = 15 + qb
                    ob = qb % 2
                    B.pe_group([mm(po[ob][0:65, 0:256], V_all[:, 2 * j + kt, h, :], pT[i][:, kt, :], (j == 0 and kt == 0), (j == sB and kt == 1))
                                for kt in range(2)], reads=[S_V, S_pT[i]], writes=[S_po[ob]])

                def tail_a(h, qb):
                    ob = qb % 2
                    B.op("dve", lambda e: e.reciprocal(out=rc[ob][64:65, :], in_=po[ob][64:65, 0:256]), reads=[S_po[ob]], writes=[S_rc[ob]])

                def tail_b(h, qb):
                    ob = qb % 2
                    hb = h % 2
                    qc = slice(qb * 256, (qb + 1) * 256)
                    B.pe_group([mm(pbc[0:64, 0:256], ones_f[64:65, 0:64], rc[ob][64:65, :], True, True)],
                               reads=[S_rc[ob], S_const], writes=[S_pbc])
                    B.op("act", lambda e: e.activation(out=bcs[ob][:], in_=pbc[0:64, 0:256], func=AF.Copy), reads=[S_pbc], writes=[S_bcs[ob]])
                    B.op("dve", lambda e: e.tensor_tensor(out=atf[ob][:], in0=po[ob][0:64, 0:256], in1=bcs[ob][:], op=ALU.mult),
                         reads=[S_po[ob], S_bcs[ob]], writes=[S_atf[ob]])
                    B.op("act", lambda e: e.activation(out=sqa[ob][:], in_=atf[ob][:], func=AF.Square), reads=[S_atf[ob]], writes=[S_sqa[ob]])
                    B.op("act", lambda e: e.activation(out=atst[hb][:, qc], in_=atf[ob][:], func=AF.Copy, scale=gattn_sb[:, h:h + 1]),
                         reads=[S_atf[ob], S_const], writes=[S_atst[hb]])

                def tail_c(h, qb):
                    ob = qb % 2
                    B.pe_group([mm(pbc[:, 256 + t:257 + t], sqa[ob][:, t * 128:(t + 1) * 128], ones_b[0:64, 0:1], True, True) for t in range(2)],
                               reads=[S_sqa[ob], S_const], writes=[S_pss2])
                    if h == 0:
                        B.op("dve", lambda e: e.tensor_copy(out=ssa[:, 2 * qb:2 * qb + 2], in_=pbc[:, 256:258]), reads=[S_pss2], writes=[S_ssa])
                    else:
                        B.op("dve", lambda e: e.tensor_tensor(out=ssa[:, 2 * qb:2 * qb + 2], in0=pbc[:, 256:258], in1=ssa[:, 2 * qb:2 * qb + 2],
                                                              op=ALU.add),
                             reads=[S_pss2, S_ssa], writes=[S_ssa])

                load_head(0)
                prep_head(0)
                for qt in range(NQT):
                    mask_tile(0, qt)
                for h in range(NH):
                    hb = h % 2
                    if h + 1 < NH:
                        load_head(h + 1)
                        prep_head(h + 1)
                    deferred = []
                    for qb in range(NQB):
                        sB = 15 + qb
                        prev = s_mm(h, qb, 0)
                        for j in range(1, sB + 1):
                            cur = s_mm(h, qb, j)
                            pv_mm(h, qb, j - 1, prev)
                            prev = cur
                            for (tj, fn) in list(deferred):
                                if tj <= j:
                                    fn()
                                    deferred.remove((tj, fn))
                            if h + 1 < NH and j in (5, 9):
                                mask_tile(h + 1, 2 * qb + (0 if j == 5 else 1))
                        pv_mm(h, qb, sB, prev)
                        assert not deferred
                        tail_a(h, qb)
                        deferred.append((2, lambda h=h, qb=qb: tail_b(h, qb)))
                        deferred.append((5, lambda h=h, qb=qb: tail_c(h, qb)))
                    for (tj, fn) in deferred:
                        fn()
                    B.dma("sp", at_s[h], atst[hb][:], S_atst[hb], reads=[S_atst[hb]], writes=[S_atd])
                B.emit_block()

        with ExitStack() as p3:
          if PHASES >= 3:
            wo = sb(p3, "wo", [128, 8, D], BF16)
            wu = sb(p3, "wu", [128, 8, 2 * DFF], BF16)
            wd = sb(p3, "wd", [128, 22, D], BF16)
            x1 = [sb(p3, "x1%d" % i, [128, 2, D], F32) for i in range(2)]
            atb = sb(p3, "atb", [128, 4, 256], BF16)
            cgb = sb(p3, "cgb", [128, 4, 256], BF16)
            xn2 = sb(p3, "xn2", [128, 2, D], BF16)
            h2T = [sb(p3, "h2T%d" % i, [128, 8, 258], BF16) for i in range(2)]
            yb = [sb(p3, "yb%d" % i, [128, 256], F32) for i in range(6)]
            actT = sb(p3, "actT", [128, 22, 256], BF16)
            gfb = sb(p3, "gfb", [128, D], F32)
            rsa = sb(p3, "rsa", [128, NQT], F32)
            rsc = sb(p3, "rsc", [128, NQT], F32)
            ssf = sb(p3, "ssf", [128, 2], F32)
            rtf = sb(p3, "rtf", [128, 2], F32)
            rsf = sb(p3, "rsf", [128, 2], F32)
            ssb = sb(p3, "ssb", [128, 2], F32)
            rtb = sb(p3, "rtb", [128, 2], F32)
            rsb = sb(p3, "rsb", [128, 2], F32)
            pu = [ps(p3, "pu%d" % i, [128, 512], F32) for i in range(4)]
            pd = [ps(p3, "pd%d" % i, [128, 512], F32) for i in range(2)]
            pac = [ps(p3, "pac%d" % i, [128, 512], F32) for i in range(2)]
            S_wo, S_wu, S_wd = Slot(), Slot(), Slot()
            S_x1 = [Slot(), Slot()]
            S_atb, S_cgb, S_xn2 = Slot(), Slot(), Slot()
            S_h2T = [Slot(), Slot()]
            S_yb = [Slot() for _ in range(6)]
            S_actT, S_gfb, S_rs = Slot(), Slot(), Slot()
            S_ssf, S_rtf, S_rsf, S_ssb, S_rtb, S_rsb = Slot(), Slot(), Slot(), Slot(), Slot(), Slot()
            S_pu = [Slot() for _ in range(4)]
            S_pd = [Slot(), Slot()]
            S_pac = [Slot(), Slot()]
            gtb = x1[1][:, 0, :]
            S_gtb = S_x1[1]

            wov = w_out.rearrange("(c p) n -> p c n", p=128)
            wuv = w_up.rearrange("(c p) n -> p c n", p=128)
            wdv = w_down.rearrange("(c p) n -> p c n", p=128)
            for c in range(8):
                B.dma("pool", wo[:, c, :], wov[:, c, :], S_wo, writes=[S_wo])
            for c in range(8):
                B.dma("pool", wu[:, c, :], wuv[:, c, :], S_wu, writes=[S_wu])
            for c0 in range(0, 22, 2):
                B.dma("pool", wd[:, c0:c0 + 2, :], wdv[:, c0:c0 + 2, :], S_wd, writes=[S_wd])
            B.dma("sp", gfb[:], gfin, S_gfb, writes=[S_gfb])
            mod_t = mod_s.tensor
            B.dma("sp", gtb, bass.AP(mod_t, 2048, [[0, 128], [1, D]]), S_gtb, reads=[S_mods], writes=[S_gtb])
            for c in range(8):
                B.op("pool", lambda e, c=c: e.tensor_tensor(out=wo[:, c, :], in0=wo[:, c, :], in1=gtb, op=ALU.mult),
                     reads=[S_gtb], writes=[S_wo])
            B.dma("sp", gtb, bass.AP(mod_t, 5120, [[0, 128], [1, D]]), S_gtb, reads=[S_mods], writes=[S_gtb])
            for c in range(22):
                B.op("pool", lambda e, c=c: e.tensor_tensor(out=wd[:, c, :], in0=wd[:, c, :], in1=gtb, op=ALU.mult),
                     reads=[S_gtb], writes=[S_wd])
            for src_, dst, ssl in ((ssa, rsa, S_ssa), (ssc, rsc, S_ssc)):
                B.op("act", lambda e, src_=src_, dst=dst: e.activation(out=dst[:], in_=src_[:], func=AF.Sqrt, scale=1.0 / 512, bias=epsc[:]),
                     reads=[ssl, S_const], writes=[S_rs])
                B.op("dve", lambda e, dst=dst: e.reciprocal(out=dst[:], in_=dst[:]), reads=[S_rs], writes=[S_rs])
            for i in range(2):
                B.op("dve", lambda e, i=i: e.memset(h2T[i][:, :, 0:2], 0.0), writes=[S_h2T[i]])

            xsv = xs.rearrange("(n t p) d -> n p t d", t=2, p=128)
            outv = out.rearrange("(n t p) d -> n p t d", t=2, p=128)
            atv = at_s.rearrange("(pr two) d t -> (two d) pr t", two=2)
            cgv = cg_s.rearrange("c p t -> p c t")
            puc = [0]
            ybc = [0]
            tpv = [pd[i][:, :].bitcast(BF16).rearrange("p (c t) -> p c t", t=256) for i in range(2)]

            def next_yb():
                i = ybc[0] % 6
                ybc[0] += 1
                return yb[i], S_yb[i]

            def front3(qb):
                k = qb % 2
                sl = 15 + qb
                qc = slice(qb * 256, (qb + 1) * 256)
                B.dma("sp", x1[k][:], xsv[sl], S_x1[k], writes=[S_x1[k]])
                B.dma("sp", atb[:], atv[:, :, qc], S_atb, reads=[S_atd], writes=[S_atb])
                B.dma("sp", cgb[:], cgv[:, :, qc], S_cgb, reads=[S_cgd], writes=[S_cgb])
                for t in range(2):
                    tile = 2 * qb + t
                    for n in range(2):
                        ns = slice(n * 512, (n + 1) * 512)
                        B.pe_group([mm(pac[0][:, :], atb[:, c, t * 128:(t + 1) * 128], wo[:, c, ns], c == 0, c == 3) for c in range(4)],
                                   reads=[S_atb, S_wo], writes=[S_pac[0]])
                        B.pe_group([mm(pac[1][:, :], cgb[:, c, t * 128:(t + 1) * 128], wo[:, 4 + c, ns], c == 0, c == 3) for c in range(4)],
                                   reads=[S_cgb, S_wo], writes=[S_pac[1]])
                        B.op("dve", lambda e, t=t, ns=ns, tile=tile: e.scalar_tensor_tensor(
                            out=x1[k][:, t, ns], in0=pac[0][:, :], scalar=rsa[:, tile:tile + 1], in1=x1[k][:, t, ns], op0=ALU.mult, op1=ALU.add),
                            reads=[S_pac[0], S_rs], writes=[S_x1[k]])
                        B.op("dve", lambda e, t=t, ns=ns, tile=tile: e.scalar_tensor_tensor(
                            out=x1[k][:, t, ns], in0=pac[1][:, :], scalar=rsc[:, tile:tile + 1], in1=x1[k][:, t, ns], op0=ALU.mult, op1=ALU.add),
                            reads=[S_pac[1], S_rs], writes=[S_x1[k]])
                for t in range(2):
                    B.op("act", lambda e, t=t: e.activation(out=xn2[:, t, :], in_=x1[k][:, t, :], func=AF.Square, accum_out=ssf[:, t:t + 1]),
                         reads=[S_x1[k]], writes=[S_xn2, S_ssf])
                B.op("act", lambda e: e.activation(out=rtf[:], in_=ssf[:], func=AF.Sqrt, scale=1.0 / D, bias=epsc[:]),
                     reads=[S_ssf, S_const], writes=[S_rtf])
                B.op("dve", lambda e: e.reciprocal(out=rsf[:], in_=rtf[:]), reads=[S_rtf], writes=[S_rsf])
                for t in range(2):
                    B.op("dve", lambda e, t=t: e.tensor_scalar(out=xn2[:, t, :], in0=x1[k][:, t, :], scalar1=rsf[:, t:t + 1], scalar2=None,
                                                               op0=ALU.mult),
                         reads=[S_x1[k], S_rsf], writes=[S_xn2])
                for half in range(2):
                    B.pe_group([tr(tpv[half][:, c, t * 128:(t + 1) * 128], xn2[:, t, (half * 4 + c) * 128:(half * 4 + c + 1) * 128], ident[:])
                                for t in range(2) for c in range(4)], reads=[S_xn2, S_const], writes=[S_pd[half]])
                    for c in range(4):
                        cc = half * 4 + c
                        B.op("act", lambda e, c=c, cc=cc, half=half: e.activation(out=h2T[k][:, cc, 2:258], in_=tpv[half][:, c, :],
                                                                                 func=AF.Identity, scale=G2[:, cc:cc + 1], bias=sh2[:, cc:cc + 1]),
                             reads=[S_pd[half], S_G], writes=[S_h2T[k]])
                if qb == 1:
                    B.op("dve", lambda e: e.tensor_scalar(out=h2T[k][:, :, 0:2], in0=h2T[1 - k][:, :, 256:258], scalar1=hv[:, 0:1],
                                                          scalar2=None, op0=ALU.mult),
                         reads=[S_h2T[1 - k], S_const], writes=[S_h2T[k]])
                elif qb > 1:
                    B.op("dve", lambda e: e.tensor_copy(out=h2T[k][:, :, 0:2], in_=h2T[1 - k][:, :, 256:258]),
                         reads=[S_h2T[1 - k]], writes=[S_h2T[k]])

            def back3(qb):
                k = qb % 2
                for v in range(22):
                    res = []
                    for col0 in (v * 128, DFF + v * 128):
                        ch = col0 // 128
                        i = puc[0] % 4
                        puc[0] += 1
                        B.pe_group([mm(pu[i][:, 0:258], wu[:, c, col0:col0 + 128], h2T[k][:, c, 0:258], c == 0, c == 7) for c in range(8)],
                                   reads=[S_wu, S_h2T[k]], writes=[S_pu[i]])
                        ya, sya = next_yb()
                        B.op("act", lambda e, i=i, ya=ya, ch=ch: e.activation(out=ya[:], in_=pu[i][:, 2:258], func=AF.Identity,
                                                                             scale=fcw_sb[:, 88 + ch:89 + ch], bias=fcb_sb[:, ch:ch + 1]),
                             reads=[S_pu[i], S_const], writes=[sya])
                        ybb, syb = next_yb()
                        B.op("dve", lambda e, i=i, ya=ya, ybb=ybb, ch=ch: e.scalar_tensor_tensor(
                            out=ybb[:], in0=pu[i][:, 1:257], scalar=fcw_sb[:, 44 + ch:45 + ch], in1=ya[:], op0=ALU.mult, op1=ALU.add),
                            reads=[S_pu[i], sya, S_const], writes=[syb])
                        yc, syc = next_yb()
                        B.op("dve", lambda e, i=i, ybb=ybb, yc=yc, ch=ch: e.scalar_tensor_tensor(
                            out=yc[:], in0=pu[i][:, 0:256], scalar=fcw_sb[:, ch:ch + 1], in1=ybb[:], op0=ALU.mult, op1=ALU.add),
                            reads=[S_pu[i], syb, S_const], writes=[syc])
                        res.append((yc, syc))
                    (yv, syv), (yg, syg) = res
                    B.op("act", lambda e, yg=yg: e.activation(out=yg[:], in_=yg[:], func=AF.Silu), reads=[syg], writes=[syg])
                    B.op("dve", lambda e, yv=yv, yg=yg, v=v: e.tensor_tensor(out=actT[:, v, :], in0=yv[:], in1=yg[:], op=ALU.mult),
                         reads=[syv, syg], writes=[S_actT])
                for t in range(2):
                    for n in range(2):
                        B.pe_group([mm(pd[n][:, :], actT[:, v, t * 128:(t + 1) * 128], wd[:, v, n * 512:(n + 1) * 512], v == 0, v == 21)
                                    for v in range(22)], reads=[S_actT, S_wd], writes=[S_pd[n]])
                        B.op("dve", lambda e, t=t, n=n: e.tensor_tensor(out=x1[k][:, t, n * 512:(n + 1) * 512], in0=pd[n][:, :],
                                                                        in1=x1[k][:, t, n * 512:(n + 1) * 512], op=ALU.add),
                             reads=[S_pd[n]], writes=[S_x1[k]])
                for t in range(2):
                    B.op("act", lambda e, t=t: e.activation(out=xn2[:, t, :], in_=x1[k][:, t, :], func=AF.Square, accum_out=ssb[:, t:t + 1]),
                         reads=[S_x1[k]], writes=[S_xn2, S_ssb])
                B.op("act", lambda e: e.activation(out=rtb[:], in_=ssb[:], func=AF.Sqrt, scale=1.0 / D, bias=epsc[:]),
                     reads=[S_ssb, S_const], writes=[S_rtb])
                B.op("dve", lambda e: e.reciprocal(out=rsb[:], in_=rtb[:]), reads=[S_rtb], writes=[S_rsb])
                for t in range(2):
                    B.op("dve", lambda e, t=t: e.scalar_tensor_tensor(out=x1[k][:, t, :], in0=x1[k][:, t, :], scalar=rsb[:, t:t + 1],
                                                                      in1=gfb[:], op0=ALU.mult, op1=ALU.mult),
                         reads=[S_rsb, S_gfb], writes=[S_x1[k]])
                B.dma("sp", outv[qb - 1], x1[k][:], S_x1[k], reads=[S_x1[k]])

            front3(0)
            front3(1)
            for qb in range(1, NQB):
                if qb + 1 < NQB:
                    front3(qb + 1)
                back3(qb)
            B.emit_block()
    return nc


_NC_CACHE = {}


def _consts(half):
    bf = ml_dtypes.bfloat16
    ident = np.eye(128, dtype=np.float32).astype(bf)
    key = np.arange(S)
    kstat = np.zeros((35, S), np.float32)
    kstat[key // 256, key] = 1.0
    kstat[32] = key % 256
    kstat[33] = key // 256
    kstat[34] = 1.0
    kstat = kstat.astype(bf)
    p = np.arange(128)[:, None, None]
    kt = np.arange(2)[None, :, None]
    a = np.arange(256)[None, None, :]
    cmask = np.where(kt * 128 + p <= a, 0.0, NEG).astype(np.float32).reshape(128, 512).astype(bf)
    gvb = np.zeros((NQB, 32), np.float32)
    gvb2 = np.zeros((NQB, 32), np.float32)
    for qb in range(NQB):
        sB = 15 + qb
        for j in range(32):
            valid = (j < sB) and (half == 1 or j >= 16)
            gvb[qb, j] = 0.0 if valid else -1.0e9
            gvb2[qb, j] = NEG if valid else 2 * NEG
        gvb[qb, sB] = -3.0e9
        gvb2[qb, sB] = 0.0
    gvb = np.ascontiguousarray(np.broadcast_to(gvb.reshape(1, -1), (128, NQB * 32))).astype(np.float32)
    gvb2 = np.ascontiguousarray(np.broadcast_to(gvb2.reshape(1, -1), (128, NQB * 32))).astype(np.float32)
    qstat = np.zeros((NH, 128, NQT, 99), np.float32)
    for h in range(NH):
        slope = 2.0 ** (-(h + 1))
        for qt in range(NQT):
            sB = 15 + qt // 2
            apos = (qt % 2) * 128 + np.arange(128)
            qstat[h, :, qt, 96] = slope
            qstat[h, :, qt, 97] = 256.0 * slope
            qstat[h, :, qt, 98] = -slope * (256.0 * sB + apos)
    qstat = qstat.reshape(NH, 128, NQT * 99).astype(bf)
    hv = np.full((128, 1), float(half), np.float32)
    return dict(ident=ident, kstat=kstat, cmask=cmask, gvb=gvb, gvb2=gvb2, qstat=qstat, hv=hv)


def _pc(v, nchunk):
    return np.ascontiguousarray(np.asarray(v, np.float32).reshape(nchunk, 128).T)


def kernel(x, c, w_ada, b_ada, g_mix, w_in, conv_w, conv_b, g_attn_out, g_conv_out, w_out, g_ffn,
           w_up, ffn_conv_w, ffn_conv_b, w_down, g_final):
    f = lambda a: np.ascontiguousarray(np.asarray(a, dtype=np.float32))
    x = f(x)
    c = f(c)
    if "nc" not in _NC_CACHE:
        _NC_CACHE["nc"] = build_program()
    nc = _NC_CACHE["nc"]
    shared = dict(
        w_ada=f(w_ada[0]), b_ada=f(b_ada[0]).reshape(1, -1), gmix=_pc(g_mix[0], 8), w_in=f(w_in[0]),
        convw=np.ascontiguousarray(np.concatenate([_pc(conv_w[0][kk], 4) for kk in range(3)], axis=1)),
        convb=_pc(conv_b[0], 4),
        gattn=np.ascontiguousarray(f(g_attn_out[0]).reshape(8, 64).T),
        gconv=_pc(g_conv_out[0], 4), w_out=f(w_out[0]), gffn=_pc(g_ffn[0], 8), w_up=f(w_up[0]),
        fcw=np.ascontiguousarray(np.concatenate([_pc(ffn_conv_w[0][kk], 44) for kk in range(3)], axis=1)),
        fcb=_pc(ffn_conv_b[0], 44), w_down=f(w_down[0]),
        gfin=np.ascontiguousarray(np.broadcast_to(f(g_final).reshape(1, -1), (128, D))),
    )
    cst = [_consts(0), _consts(1)]
    in_maps = []
    for i in range(NCORES):
        b, half = i // 2, i % 2
        if half == 1:
            xs = x[b]
        else:
            xs = np.concatenate([np.zeros((4096, D), np.float32), x[b, :4096]], axis=0)
        m = dict(shared)
        m.update(cst[half])
        m["xs"] = np.ascontiguousarray(xs)
        m["cT"] = _pc(c[b], 8)
        in_maps.append(m)
    res = run_bass_kernel_spmd(nc, in_maps, core_ids=list(range(NCORES)))
    outp = np.empty((4, S, D), np.float32)
    for i in range(NCORES):
        b, half = i // 2, i % 2
        outp[b, half * 4096:(half + 1) * 4096] = res.results[i]["out"]
    return outp
```

```python
import numpy as np
from contextlib import ExitStack
import ml_dtypes
import concourse.bass as bass
import concourse.mybir as mybir
from concourse.bass_utils import run_bass_kernel_spmd

F32 = mybir.dt.float32
BF16 = mybir.dt.bfloat16
AF = mybir.ActivationFunctionType
ALU = mybir.AluOpType
AX = mybir.AxisListType

D = 1024
S = 8192
NH = 8
DFF = 2816
NQB = 17
NQT = 34
QTOK = NQB * 256
EPS = 1e-6
NEG = -30000.0
NCORES = 8
PHASES = 3
DEBUG = False


class Tok:
    __slots__ = ("sem", "val")

    def __init__(self, sem, val):
        self.sem = sem
        self.val = val


class Slot:
    def __init__(self, name=""):
        self.name = name
        self.w = None
        self.r = {}
        self.dsem = None
        self.dcnt = 0


class Builder:
    ENG = ("pe", "act", "dve", "pool", "sp")

    def __init__(self, nc, es):
        self.nc = nc
        self.es = es
        self.q = {}
        for n in self.ENG:
            sem = es.enter_context(nc.semaphore("sem_" + n))
            self.q[n] = dict(sem=sem, cnt=0, ops=[], waited={})
        self.dslots = []

    def _waits(self, en, deps):
        q = self.q[en]
        need = {}
        for t in deps:
            if t is None:
                continue
            if en == "pe" and t.sem is q["sem"]:
                continue
            k = id(t.sem)
            if q["waited"].get(k, 0) >= t.val:
                continue
            if k not in need or need[k].val < t.val:
                need[k] = t
        for k, t in need.items():
            q["waited"][k] = t.val
        return [(t.sem, t.val) for t in need.values()]

    @staticmethod
    def _deps(reads, writes, deps):
        d = list(deps)
        for s in reads:
            d.append(s.w)
        for s in writes:
            d.extend(s.r.values())
            d.append(s.w)
        return d

    @staticmethod
    def _update(tok, reads, writes):
        for s in writes:
            s.w = tok
            s.r = {}
        for s in reads:
            k = id(tok.sem)
            if k not in s.r or s.r[k].val < tok.val:
                s.r[k] = tok

    def op(self, en, fn, reads=(), writes=(), deps=(), inc=True):
        q = self.q[en]
        waits = self._waits(en, self._deps(reads, writes, deps))
        tok = None
        if inc:
            q["cnt"] += 1
            tok = Tok(q["sem"], q["cnt"])
        else:
            assert not reads and not writes
        sem = q["sem"]

        def emit(e, waits=waits, fn=fn, inc=inc, sem=sem):
            for (sm, v) in waits:
                e.wait_ge(sm, v)
            ins = fn(e)
            if inc:
                ins.then_inc(sem, 1)

        q["ops"].append(emit)
        if tok is not None:
            self._update(tok, reads, writes)
        return tok

    def pe_group(self, fns, reads=(), writes=()):
        q = self.q["pe"]
        waits = self._waits("pe", self._deps(reads, writes, ()))
        q["cnt"] += 1
        tok = Tok(q["sem"], q["cnt"])
        sem = q["sem"]
        n = len(fns)

        def emit(e, waits=waits, fns=fns, sem=sem, n=n):
            for (sm, v) in waits:
                e.wait_ge(sm, v)
            for i, f in enumerate(fns):
                ins = f(e)
                if i == n - 1:
                    ins.then_inc(sem, 1)

        q["ops"].append(emit)
        self._update(tok, reads, writes)
        return tok

    def dma(self, en, out, in_, sem_slot, reads=(), writes=(), deps=(), **kw):
        q = self.q[en]
        sl = sem_slot
        if sl.dsem is None:
            sl.dsem = self.es.enter_context(self.nc.semaphore("dsem%d" % len(self.dslots)))
            self.dslots.append(sl)
        waits = self._waits(en, self._deps(reads, writes, deps))
        sl.dcnt += 16
        tok = Tok(sl.dsem, sl.dcnt)
        dsem = sl.dsem

        def emit(e, waits=waits, out=out, in_=in_, dsem=dsem, kw=kw):
            for (sm, v) in waits:
                e.wait_ge(sm, v)
            e.dma_start(out=out, in_=in_, **kw).then_inc(dsem, 16)

        q["ops"].append(emit)
        self._update(tok, reads, writes)
        return tok

    def emit_block(self):
        nc = self.nc
        finals = [(s.dsem, s.dcnt) for s in self.dslots if s.dcnt > 0]
        pe_fin = (self.q["pe"]["sem"], self.q["pe"]["cnt"])

        def fin(e, finals=finals):
            for sm, v in finals:
                e.wait_ge(sm, v)

        self.q["sp"]["ops"].append(fin)
        with nc.Block() as blk:
            for n, meth in (("pe", blk.tensor), ("act", blk.scalar), ("dve", blk.vector),
                            ("pool", blk.gpsimd), ("sp", blk.sync)):
                ops = self.q[n]["ops"]
                if ops:
                    def body(e, ops=ops):
                        for f in ops:
                            f(e)
                    meth(body)
                self.q[n]["ops"] = []


def mm(out, lhsT, rhs, start, stop):
    return lambda e: e.matmul(out, lhsT=lhsT, rhs=rhs, start=start, stop=stop)


def tr(out, in_, ident):
    return lambda e: e.transpose(out, in_, ident)


def build_program():
    nc = bass.Bass("TRN2", target_bir_lowering=False)

    def din(name, shape, dt=F32):
        return nc.dram_tensor(name, list(shape), dt, kind="ExternalInput").ap()

    def dscr(name, shape, dt):
        return nc.dram_tensor(name, list(shape), dt, kind="ExternalOutput" if DEBUG else "Internal").ap()

    xs = din("xs", [S, D])
    cT = din("cT", [128, 8])
    w_ada = din("w_ada", [D, 6 * D])
    b_ada = din("b_ada", [1, 6 * D])
    gmix = din("gmix", [128, 8])
    w_in = din("w_in", [D, 3072])
    convw = din("convw", [128, 12])
    convb = din("convb", [128, 4])
    gattn = din("gattn", [64, 8])
    gconv = din("gconv", [128, 4])
    w_out = din("w_out", [D, D])
    gffn = din("gffn", [128, 8])
    w_up = din("w_up", [D, 2 * DFF])
    fcw = din("fcw", [128, 132])
    fcb = din("fcb", [128, 44])
    w_down = din("w_down", [DFF, D])
    gfin = din("gfin", [128, D])
    ident_d = din("ident", [128, 128], BF16)
    kstat = din("kstat", [35, S], BF16)
    cmask = din("cmask", [128, 512], BF16)
    gvb_d = din("gvb", [128, NQB * 32])
    gvb2_d = din("gvb2", [128, NQB * 32])
    qstat = din("qstat", [NH, 128, NQT * 99], BF16)
    hv_d = din("hv", [128, 1])
    out = nc.dram_tensor("out", [4096, D], F32, kind="ExternalOutput").ap()

    kT_s = dscr("kT_s", [NH, 64, S], BF16)
    qT_s = dscr("qT_s", [NH, 64, QTOK], BF16)
    at_s = dscr("at_s", [NH, 64, QTOK], BF16)
    cg_s = dscr("cg_s", [4, 128, QTOK], BF16)
    mod_s = dscr("mod_s", [1, 6 * D], F32)

    es = ExitStack()
    with es:
        B = Builder(nc, es)

        def sb(st, name, shape, dt):
            return st.enter_context(nc.sbuf_tensor("sb_" + name, list(shape), dt))

        def ps(st, name, shape, dt):
            return st.enter_context(nc.psum_tensor("ps_" + name, list(shape), dt))

        ident = sb(es, "ident", [128, 128], BF16)
        ones_b = sb(es, "ones_b", [128, 1], BF16)
        ones_f = sb(es, "ones_f", [128, 64], F32)
        epsc = sb(es, "epsc", [128, 1], F32)
        hv = sb(es, "hv", [128, 1], F32)
        G1 = sb(es, "G1", [128, 8], F32)
        G2 = sb(es, "G2", [128, 8], F32)
        modT = sb(es, "modT", [128, 48], F32)
        gmix_sb = sb(es, "gmix_sb", [128, 8], F32)
        gffn_sb = sb(es, "gffn_sb", [128, 8], F32)
        convw_sb = sb(es, "convw_sb", [128, 12], F32)
        convb_sb = sb(es, "convb_sb", [128, 4], F32)
        gconv_sb = sb(es, "gconv_sb", [128, 4], F32)
        gattn_sb = sb(es, "gattn_sb", [64, 8], F32)
        fcw_sb = sb(es, "fcw_sb", [128, 132], F32)
        fcb_sb = sb(es, "fcb_sb", [128, 44], F32)
        ssa = sb(es, "ssa", [128, NQT], F32)
        ssc = sb(es, "ssc", [128, NQT], F32)
        S_const = Slot("const")
        S_ssa = Slot("ssa")
        S_ssc = Slot("ssc")
        S_G = Slot("G")
        S_mods = Slot("mods")

        for dst, src in ((ident, ident_d), (hv, hv_d), (gmix_sb, gmix), (gffn_sb, gffn), (convw_sb, convw),
                         (convb_sb, convb), (gconv_sb, gconv), (gattn_sb, gattn), (fcw_sb, fcw), (fcb_sb, fcb)):
            B.dma("sp", dst[:], src, S_const, writes=[S_const])
        B.op("dve", lambda e: e.memset(ones_b[:], 1.0), writes=[S_const])
        B.op("dve", lambda e: e.memset(ones_f[:], 1.0), writes=[S_const])
        B.op("dve", lambda e: e.memset(epsc[:], EPS), writes=[S_const])

        with ExitStack() as p0:
            c_sb = sb(p0, "c_sb", [128, 8], F32)
            sc_b = sb(p0, "sc_b", [128, 8], BF16)
            brow = sb(p0, "brow", [1, 6 * D], F32)
            modrow = sb(p0, "modrow", [1, 6 * D], F32)
            wa = [sb(p0, "wa%d" % i, [128, 8, 512], BF16) for i in range(2)]
            pm = [ps(p0, "pm%d" % i, [128, 512], F32) for i in range(2)]
            S_c, S_scb, S_brow, S_modrow, S_modT = Slot(), Slot(), Slot(), Slot(), Slot()
            S_wa = [Slot(), Slot()]
            S_pm = [Slot(), Slot()]
            B.dma("sp", c_sb[:], cT, S_c, writes=[S_c])
            B.dma("sp", brow[:], b_ada, S_brow, writes=[S_brow])
            B.op("act", lambda e: e.activation(out=sc_b[:], in_=c_sb[:], func=AF.Silu), reads=[S_c], writes=[S_scb])
            wav = w_ada.rearrange("(c p) n -> p c n", p=128)
            for g in range(12):
                k = g % 2
                B.dma("pool", wa[k][:], wav[:, :, g * 512:(g + 1) * 512], S_wa[k], writes=[S_wa[k]])
                B.pe_group([mm(pm[k][0:1, :], sc_b[:, c:c + 1], wa[k][:, c, :], c == 0, c == 7) for c in range(8)],
                           reads=[S_wa[k], S_scb], writes=[S_pm[k]])
                B.op("dve", lambda e, k=k, g=g: e.tensor_tensor(out=modrow[0:1, g * 512:(g + 1) * 512], in0=pm[k][0:1, :],
                                                                 in1=brow[0:1, g * 512:(g + 1) * 512], op=ALU.add),
                     reads=[S_pm[k], S_brow], writes=[S_modrow])
            B.dma("sp", mod_s, modrow[:], S_mods, reads=[S_modrow], writes=[S_mods])
            B.dma("sp", modT[:], mod_s.rearrange("o (k p) -> (o p) k", p=128), S_modT, reads=[S_mods], writes=[S_modT],
                  allow_slow_non_contiguous=True)
            B.op("dve", lambda e: e.scalar_tensor_tensor(out=G1[:], in0=modT[:, 8:16], scalar=1.0, in1=gmix_sb[:],
                                                         op0=ALU.add, op1=ALU.mult), reads=[S_modT, S_const], writes=[S_G])
            B.op("dve", lambda e: e.scalar_tensor_tensor(out=G2[:], in0=modT[:, 32:40], scalar=1.0, in1=gffn_sb[:],
                                                         op0=ALU.add, op1=ALU.mult), reads=[S_modT, S_const], writes=[S_G])
            B.emit_block()
        sh1 = modT[:, 0:8]
        sh2 = modT[:, 24:32]

        with ExitStack() as p12:
          if PHASES >= 1:
            V_all = sb(p12, "V_all", [128, 64, NH, 65], BF16)
            S_V = Slot("V")
            with ExitStack() as p1:
                w_in_sb = sb(p1, "w_in_sb", [128, 8, 3072], BF16)
                S_win = Slot("win")
                wiv = w_in.rearrange("(c p) n -> p c n", p=128)
                for c in range(8):
                    B.dma("pool", w_in_sb[:, c, :], wiv[:, c, :], S_win, writes=[S_win])
                xt = [sb(p1, "xt%d" % i, [128, 2, D], F32) for i in range(2)]
                xn = [sb(p1, "xn%d" % i, [128, 2, D], BF16) for i in range(2)]
                hT = [sb(p1, "hT%d" % i, [128, 8, 258], BF16) for i in range(2)]
                junk = sb(p1, "junk", [128, D], BF16)
                ss = [sb(p1, "ss%d" % i, [128, 2], F32) for i in range(2)]
                rt = [sb(p1, "rt%d" % i, [128, 2], F32) for i in range(2)]
                rstd = [sb(p1, "rstd%d" % i, [128, 2], F32) for i in range(2)]
                kst = [sb(p1, "kst%d" % i, [128, 4, 256], BF16) for i in range(2)]
                qst = [sb(p1, "qst%d" % i, [128, 4, 256], BF16) for i in range(2)]
                cgs = [sb(p1, "cgs%d" % i, [128, 4, 256], BF16) for i in range(2)]
                sqc = sb(p1, "sqc", [128, 4, 256], BF16)
                u_sb = [sb(p1, "u_sb%d" % i, [128, 258], F32) for i in range(2)]
                zb = [sb(p1, "zb%d" % i, [128, 258], F32) for i in range(2)]
                y0 = [sb(p1, "y0%d" % i, [128, 256], F32) for i in range(2)]
                y1 = [sb(p1, "y1%d" % i, [128, 256], F32) for i in range(2)]
                y2 = [sb(p1, "y2%d" % i, [128, 256], F32) for i in range(2)]
                cv = [sb(p1, "cv%d" % i, [128, 256], F32) for i in range(2)]
                tp = ps(p1, "tp", [128, 8, 256], BF16)
                pp = [ps(p1, "pp%d" % i, [128, 512], F32) for i in range(5)]
                pss = ps(p1, "pss", [128, 512], F32)
                S_xt = [Slot(), Slot()]
                S_xn = [Slot(), Slot()]
                S_hT = [Slot(), Slot()]
                S_junk = Slot()
                S_ss = [Slot(), Slot()]
                S_rt = [Slot(), Slot()]
                S_rstd = [Slot(), Slot()]
                S_kst = [Slot(), Slot()]
                S_qst = [Slot(), Slot()]
                S_cgs = [Slot(), Slot()]
                S_sqc = Slot()
                S_u = [Slot(), Slot()]
                S_z = [Slot(), Slot()]
                S_y0 = [Slot(), Slot()]
                S_y1 = [Slot(), Slot()]
                S_y2 = [Slot(), Slot()]
                S_cv = [Slot(), Slot()]
                S_tp = Slot()
                S_pp = [Slot() for _ in range(5)]
                S_pss = Slot()
                S_kT = Slot("kT_s")
                S_qT = Slot("qT_s")
                S_cgd = Slot("cg_s")
                ppi = [0]

                def next_pp():
                    i = ppi[0] % 5
                    ppi[0] += 1
                    return pp[i], S_pp[i]

                B.op("pool", lambda e: e.memset(V_all[:, :, :, 64:65], 1.0), writes=[S_V])
                for i in range(2):
                    B.op("pool", lambda e, i=i: e.memset(hT[i][:, :, 0:2], 0.0), writes=[S_hT[i]])
                xsv = xs.rearrange("(n t p) d -> n p t d", t=2, p=128)
                kTv = kT_s.rearrange("(pr two) d t -> (two d) pr t", two=2)
                qTv = qT_s.rearrange("(pr two) d t -> (two d) pr t", two=2)
                cgv = cg_s.rearrange("c p t -> p c t")
                evac_rr = [0]

                def evac(out_ap, in_ap, reads, writes, scale=None):
                    evac_rr[0] += 1
                    if evac_rr[0] % 2 == 0:
                        if scale is None:
                            return B.op("act", lambda e: e.activation(out=out_ap, in_=in_ap, func=AF.Copy), reads=reads, writes=writes)
                        return B.op("act", lambda e: e.activation(out=out_ap, in_=in_ap, func=AF.Copy, scale=scale), reads=reads, writes=writes)
                    if scale is None:
                        return B.op("dve", lambda e: e.tensor_copy(out=out_ap, in_=in_ap), reads=reads, writes=writes)
                    return B.op("dve", lambda e: e.tensor_scalar(out=out_ap, in0=in_ap, scalar1=scale, scalar2=None, op0=ALU.mult),
                                reads=reads, writes=writes)

                def f1a(s):
                    k = s % 2
                    B.dma("sp", xt[k][:], xsv[s], S_xt[k], writes=[S_xt[k]])
                    for t in range(2):
                        B.op("act", lambda e, k=k, t=t: e.activation(out=junk[:], in_=xt[k][:, t, :], func=AF.Square,
                                                                     accum_out=ss[k][:, t:t + 1]),
                             reads=[S_xt[k]], writes=[S_junk, S_ss[k]])
                    B.op("act", lambda e, k=k: e.activation(out=rt[k][:], in_=ss[k][:], func=AF.Sqrt, scale=1.0 / D, bias=epsc[:]),
                         reads=[S_ss[k], S_const], writes=[S_rt[k]])
                    B.op("dve", lambda e, k=k: e.reciprocal(out=rstd[k][:], in_=rt[k][:]), reads=[S_rt[k]], writes=[S_rstd[k]])
                    for t in range(2):
                        B.op("dve", lambda e, k=k, t=t: e.tensor_scalar(out=xn[k][:, t, :], in0=xt[k][:, t, :], scalar1=rstd[k][:, t:t + 1],
                                                                        scalar2=None, op0=ALU.mult),
                             reads=[S_xt[k], S_rstd[k]], writes=[S_xn[k]])

                def f1b(s):
                    k = s % 2
                    B.pe_group([tr(tp[:, c, t * 128:(t + 1) * 128], xn[k][:, t, c * 128:(c + 1) * 128], ident[:])
                                for t in range(2) for c in range(8)], reads=[S_xn[k], S_const], writes=[S_tp])
                    for c in range(8):
                        if c % 2 == 0:
                            B.op("act", lambda e, k=k, c=c: e.activation(out=hT[k][:, c, 2:258], in_=tp[:, c, :], func=AF.Identity,
                                                                         scale=G1[:, c:c + 1], bias=sh1[:, c:c + 1]),
                                 reads=[S_tp, S_G], writes=[S_hT[k]])
                        else:
                            B.op("dve", lambda e, k=k, c=c: e.tensor_scalar(out=hT[k][:, c, 2:258], in0=tp[:, c, :], scalar1=G1[:, c:c + 1],
                                                                            scalar2=sh1[:, c:c + 1], op0=ALU.mult, op1=ALU.add),
                                 reads=[S_tp, S_G], writes=[S_hT[k]])
                    if s == 16:
                        B.op("dve", lambda e, k=k: e.tensor_scalar(out=hT[k][:, :, 0:2], in0=hT[1 - k][:, :, 256:258], scalar1=hv[:, 0:1],
                                                                   scalar2=None, op0=ALU.mult),
                             reads=[S_hT[1 - k], S_const], writes=[S_hT[k]])
                    elif s > 16:
                        B.op("dve", lambda e, k=k: e.tensor_copy(out=hT[k][:, :, 0:2], in_=hT[1 - k][:, :, 256:258]),
                             reads=[S_hT[1 - k]], writes=[S_hT[k]])

                def back1a(s):
                    k = s % 2
                    for pr in range(4):
                        pt_, sp_ = next_pp()
                        B.pe_group([mm(pt_[:, 0:256], w_in_sb[:, c, 512 + pr * 128:512 + (pr + 1) * 128], hT[k][:, c, 2:258], c == 0, c == 7)
                                    for c in range(8)], reads=[S_win, S_hT[k]], writes=[sp_])
                        evac(kst[k][:, pr, :], pt_[:, 0:256], [sp_], [S_kst[k]])
                    B.dma("sp", kTv[:, :, s * 256:(s + 1) * 256], kst[k][:], S_kst[k], reads=[S_kst[k]], writes=[S_kT])
                    for t in range(2):
                        pt_, sp_ = next_pp()
                        B.pe_group([mm(pt_[:, :], hT[k][:, c, 2 + t * 128:2 + (t + 1) * 128], w_in_sb[:, c, 1024:1536], c == 0, c == 7)
                                    for c in range(8)], reads=[S_win, S_hT[k]], writes=[sp_])
                        evac(V_all[:, 2 * s + t, :, 0:64], pt_[:, :].rearrange("p (h d) -> p h d", d=64), [sp_], [S_V])

                def back1b(s):
                    k = s % 2
                    if s < 15:
                        return
                    qb = s - 15
                    for pr in range(4):
                        pt_, sp_ = next_pp()
                        B.pe_group([mm(pt_[:, 0:256], w_in_sb[:, c, pr * 128:(pr + 1) * 128], hT[k][:, c, 2:258], c == 0, c == 7)
                                    for c in range(8)], reads=[S_win, S_hT[k]], writes=[sp_])
                        evac(qst[k][:, pr, :], pt_[:, 0:256], [sp_], [S_qst[k]], scale=0.125)
                    B.dma("sp", qTv[:, :, qb * 256:(qb + 1) * 256], qst[k][:], S_qst[k], reads=[S_qst[k]], writes=[S_qT])
                    for ch in range(4):
                        j = ch % 2
                        pu_, spu = next_pp()
                        B.pe_group([mm(pu_[:, 0:258], w_in_sb[:, c, 1536 + ch * 128:1536 + (ch + 1) * 128], hT[k][:, c, 0:258], c == 0, c == 7)
                                    for c in range(8)], reads=[S_win, S_hT[k]], writes=[spu])
                        pc_, spc = next_pp()
                        B.pe_group([mm(pc_[:, 0:258], w_in_sb[:, c, 2048 + ch * 128:2048 + (ch + 1) * 128], hT[k][:, c, 0:258], c == 0, c == 7)
                                    for c in range(8)], reads=[S_win, S_hT[k]], writes=[spc])
                        pb_, spb = next_pp()
                        B.pe_group([mm(pb_[:, 0:256], w_in_sb[:, c, 2560 + ch * 128:2560 + (ch + 1) * 128], hT[k][:, c, 2:258], c == 0, c == 7)
                                    for c in range(8)], reads=[S_win, S_hT[k]], writes=[spb])
                        B.op("act", lambda e, j=j, pu_=pu_: e.activation(out=u_sb[j][:], in_=pu_[:, 0:258], func=AF.Copy),
                             reads=[spu], writes=[S_u[j]])
                        B.op("dve", lambda e, j=j, pc_=pc_: e.tensor_tensor(out=zb[j][:], in0=pc_[:, 0:258], in1=u_sb[j][:], op=ALU.mult),
                             reads=[spc, S_u[j]], writes=[S_z[j]])
                        B.op("dve", lambda e, j=j, ch=ch: e.tensor_scalar(out=y0[j][:], in0=zb[j][:, 2:258], scalar1=convw_sb[:, 8 + ch:9 + ch],
                                                                          scalar2=convb_sb[:, ch:ch + 1], op0=ALU.mult, op1=ALU.add),
                             reads=[S_z[j], S_const], writes=[S_y0[j]])
                        B.op("dve", lambda e, j=j, ch=ch: e.scalar_tensor_tensor(out=y1[j][:], in0=zb[j][:, 1:257], scalar=convw_sb[:, 4 + ch:5 + ch],
                                                                                 in1=y0[j][:], op0=ALU.mult, op1=ALU.add),
                             reads=[S_z[j], S_y0[j], S_const], writes=[S_y1[j]])
                        B.op("dve", lambda e, j=j, ch=ch: e.scalar_tensor_tensor(out=y2[j][:], in0=zb[j][:, 0:256], scalar=convw_sb[:, ch:ch + 1],
                                                                                 in1=y1[j][:], op0=ALU.mult, op1=ALU.add),
                             reads=[S_z[j], S_y1[j], S_const], writes=[S_y2[j]])
                        B.op("dve", lambda e, j=j, pb_=pb_: e.tensor_tensor(out=cv[j][:], in0=pb_[:, 0:256], in1=y2[j][:], op=ALU.mult),
                             reads=[spb, S_y2[j]], writes=[S_cv[j]])
                        B.op("act", lambda e, j=j, ch=ch: e.activation(out=sqc[:, ch, :], in_=cv[j][:], func=AF.Square),
                             reads=[S_cv[j]], writes=[S_sqc])
                        B.op("act", lambda e, j=j, ch=ch, k=k: e.activation(out=cgs[k][:, ch, :], in_=cv[j][:], func=AF.Copy,
                                                                           scale=gconv_sb[:, ch:ch + 1]),
                             reads=[S_cv[j], S_const], writes=[S_cgs[k]])
                    for t in range(2):
                        B.pe_group([mm(pss[:, t:t + 1], sqc[:, ch, t * 128:(t + 1) * 128], ones_b[:, 0:1], ch == 0, ch == 3) for ch in range(4)],
                                   reads=[S_sqc, S_const], writes=[S_pss])
                    B.op("dve", lambda e, qb=qb: e.tensor_copy(out=ssc[:, 2 * qb:2 * qb + 2], in_=pss[:, 0:2]), reads=[S_pss], writes=[S_ssc])
                    B.dma("sp", cgv[:, :, qb * 256:(qb + 1) * 256], cgs[k][:], S_cgs[k], reads=[S_cgs[k]], writes=[S_cgd])

                f1a(0)
                f1b(0)
                f1a(1)
                for s in range(32):
                    back1a(s)
                    if s + 1 < 32:
                        f1b(s + 1)
                    if s + 2 < 32:
                        f1a(s + 2)
                    back1b(s)
                B.emit_block()

            with ExitStack() as p2:
              if PHASES >= 2:
                kaug = [sb(p2, "kaug%d" % i, [99, S], BF16) for i in range(2)]
                qaug = [sb(p2, "qaug%d" % i, [99, QTOK], BF16) for i in range(2)]
                qa = [sb(p2, "qa%d" % i, [128, NQT, 99], BF16) for i in range(2)]
                atst = [sb(p2, "atst%d" % i, [64, QTOK], BF16) for i in range(2)]
                gvb = sb(p2, "gvb", [128, NQB, 32], F32)
                gvb2 = sb(p2, "gvb2", [128, NQB, 32], F32)
                cm = sb(p2, "cm", [128, 2, 256], BF16)
                km_f = sb(p2, "km_f", [64, 32], F32)
                km_b = [sb(p2, "km_b%d" % i, [64, 32], BF16) for i in range(2)]
                gm = [sb(p2, "gm%d" % i, [128, 32], F32) for i in range(2)]
                t8 = [sb(p2, "t8%d" % i, [128, 8], F32) for i in range(2)]
                tsel = [sb(p2, "tsel%d" % i, [128, 32], F32) for i in range(2)]
                pT = [sb(p2, "pT%d" % i, [128, 2, 256], BF16) for i in range(3)]
                rc = [sb(p2, "rc%d" % i, [128, 256], F32) for i in range(4)]
                pos = [sb(p2, "pos%d" % i, [128, 256], F32) for i in range(4)]
                atf = [sb(p2, "atf%d" % i, [64, 256], F32) for i in range(4)]
                sqa = [sb(p2, "sqa%d" % i, [64, 256], BF16) for i in range(4)]
                sp = [ps(p2, "sp%d" % i, [128, 2, 256], F32) for i in range(3)]
                po = [ps(p2, "po%d" % i, [128, 512], F32) for i in range(2)]
                pgt = ps(p2, "pgt", [128, 512], F32)
                pbc = ps(p2, "pbc", [128, 512], F32)
                ptt_t = ps(p2, "ptt", [128, 1024], BF16)
                ptt = ptt_t[:, 0:128]
                S_kaug = [Slot(), Slot()]
                S_kstat = Slot()
                S_qq = [Slot(), Slot()]
                S_qm = [Slot(), Slot()]
                S_qa = [Slot(), Slot()]
                S_atst = [Slot(), Slot()]
                S_c2 = Slot()
                S_kmf = Slot()
                S_kmb = [Slot(), Slot()]
                S_gm = [Slot(), Slot()]
                S_t8 = [Slot(), Slot()]
                S_tsel = [Slot(), Slot()]
                S_pT = [Slot() for _ in range(3)]
                S_rc = [Slot() for _ in range(4)]
                S_pos = [Slot() for _ in range(4)]
                S_atf = [Slot() for _ in range(4)]
                S_sqa = [Slot() for _ in range(4)]
                S_sp = [Slot() for _ in range(3)]
                S_po = [Slot(), Slot()]
                S_pgt, S_ptt, S_pbc = Slot(), Slot(), Slot()
                S_pss2 = S_pbc
                S_atd = Slot("at_s")

                B.dma("sp", gvb[:].rearrange("p a b -> p (a b)"), gvb_d, S_c2, writes=[S_c2])
                B.dma("sp", gvb2[:].rearrange("p a b -> p (a b)"), gvb2_d, S_c2, writes=[S_c2])
                B.dma("sp", cm[:].rearrange("p a b -> p (a b)"), cmask, S_c2, writes=[S_c2])
                for i in range(2):
                    B.dma("sp", kaug[i][64:99, :], kstat, S_kstat, writes=[S_kstat])

                def load_head(h):
                    hb = h % 2
                    B.dma("sp", kaug[hb][0:64, :], kT_s[h], S_kaug[hb], reads=[S_kT], writes=[S_kaug[hb]])
                    B.dma("sp", qaug[hb][0:64, :], qT_s[h], S_qq[hb], reads=[S_qT], writes=[S_qq[hb]])
                    B.dma("sp", qa[hb][:].rearrange("p a b -> p (a b)"), qstat[h], S_qa[hb], writes=[S_qa[hb]])

                def prep_head(h):
                    hb = h % 2
                    B.op("dve", lambda e: e.tensor_reduce(out=km_f[:], in_=kaug[hb][0:64, :].rearrange("p (j c) -> p j c", c=256),
                                                          axis=AX.X, op=ALU.add),
                         reads=[S_kaug[hb]], writes=[S_kmf])
                    B.op("dve", lambda e: e.tensor_scalar(out=km_b[hb][:], in0=km_f[:], scalar1=1.0 / 256, scalar2=None, op0=ALU.mult),
                         reads=[S_kmf], writes=[S_kmb[hb]])

                def mask_a(h, qt):
                    hb = h % 2
                    qb = qt // 2
                    g2 = qt % 2
                    B.pe_group([mm(pgt[:, 0:32], qaug[hb][0:64, qt * 128:(qt + 1) * 128], km_b[hb][:, :], True, True)],
                               reads=[S_qq[hb], S_kmb[hb]], writes=[S_pgt])
                    B.op("dve", lambda e: e.tensor_tensor(out=gm[g2][:], in0=pgt[:, 0:32], in1=gvb[:, qb, :], op=ALU.add),
                         reads=[S_pgt, S_c2], writes=[S_gm[g2]])
                    B.op("dve", lambda e: e.max(out=t8[g2][:], in_=gm[g2][:]), reads=[S_gm[g2]], writes=[S_t8[g2]])
                    B.op("dve", lambda e: e.tensor_scalar(out=tsel[g2][:], in0=gm[g2][:], scalar1=t8[g2][:, 2:3], scalar2=-NEG,
                                                          op0=ALU.is_ge, op1=ALU.mult),
                         reads=[S_gm[g2], S_t8[g2]], writes=[S_tsel[g2]])
                    B.op("dve", lambda e: e.tensor_tensor(out=qa[hb][:, qt, 64:96], in0=tsel[g2][:], in1=gvb2[:, qb, :], op=ALU.add),
                         reads=[S_tsel[g2], S_c2], writes=[S_qa[hb]])

                def mask_b(h, qt):
                    hb = h % 2
                    B.pe_group([tr(ptt[0:99, :], qa[hb][:, qt, :], ident[:])], reads=[S_qa[hb], S_const], writes=[S_ptt])
                    B.op("act", lambda e: e.activation(out=qaug[hb][64:99, qt * 128:(qt + 1) * 128], in_=ptt[64:99, :], func=AF.Copy),
                         reads=[S_ptt], writes=[S_qm[hb]])

                sctr = [0]
                KEEP = []
                for h_ in range(NH):
                    slope_ = 2.0 ** (-(h_ + 1))
                    kk_ = 0
                    while kk_ < 31 and slope_ * (kk_ * 256 + 1) < 80.0:
                        kk_ += 1
                    KEEP.append(kk_)

                def s_mm(h, qb, j):
                    hb = h % 2
                    sB = 15 + qb
                    qc = slice(qb * 256, (qb + 1) * 256)
                    i = sctr[0] % 3
                    sctr[0] += 1
                    fns = []
                    for kt in range(2):
                        fns.append(mm(sp[i][:, kt, :], kaug[hb][0:99, (2 * j + kt) * 128:(2 * j + kt + 1) * 128], qaug[hb][0:99, qc],
                                      True, j != sB))
                        if j == sB:
                            fns.append(mm(sp[i][:, kt, :], ident[:], cm[:, kt, :], False, True))
                    B.pe_group(fns, reads=[S_kaug[hb], S_kstat, S_qq[hb], S_qm[hb], S_c2, S_const], writes=[S_sp[i]])
                    B.op("act", lambda e: e.activation(out=pT[i][:], in_=sp[i][:], func=AF.Exp), reads=[S_sp[i]], writes=[S_pT[i]])
                    return i

                def pv_mm(h, qb, j, i, first, last):
                    ob = qb % 2
                    B.pe_group([mm(po[ob][0:65, 0:256], V_all[:, 2 * j + kt, h, :], pT[i][:, kt, :], (first and kt == 0), (last and kt == 1))
                                for kt in range(2)], reads=[S_V, S_pT[i]], writes=[S_po[ob]])

                tctr = [0]

                def tail_a(h, qb):
                    ob = qb % 2
                    r = tctr[0] % 4
                    tctr[0] += 1
                    B.op("act", lambda e: e.activation(out=pos[r][0:65, :], in_=po[ob][0:65, 0:256], func=AF.Copy), reads=[S_po[ob]], writes=[S_pos[r]])
                    B.op("dve", lambda e: e.reciprocal(out=rc[r][64:65, :], in_=pos[r][64:65, :]), reads=[S_pos[r]], writes=[S_rc[r]])
                    return r

                def tail_b(h, qb, r):
                    hb = h % 2
                    qc = slice(qb * 256, (qb + 1) * 256)
                    B.pe_group([mm(pbc[0:64, 0:256], ones_f[64:65, 0:64], rc[r][64:65, :], True, True)],
                               reads=[S_rc[r], S_const], writes=[S_pbc])
                    B.op("dve", lambda e: e.tensor_tensor(out=atf[r][:], in0=pbc[0:64, 0:256], in1=pos[r][0:64, :], op=ALU.mult),
                         reads=[S_pbc, S_pos[r]], writes=[S_atf[r]])
                    B.op("act", lambda e: e.activation(out=sqa[r][:], in_=atf[r][:], func=AF.Square), reads=[S_atf[r]], writes=[S_sqa[r]])
                    B.op("act", lambda e: e.activation(out=atst[hb][:, qc], in_=atf[r][:], func=AF.Copy, scale=gattn_sb[:, h:h + 1]),
                         reads=[S_atf[r], S_const], writes=[S_atst[hb]])

                def tail_c(h, qb, r):
                    B.pe_group([mm(pbc[:, 256 + t:257 + t], sqa[r][:, t * 128:(t + 1) * 128], ones_b[0:64, 0:1], True, True) for t in range(2)],
                               reads=[S_sqa[r], S_const], writes=[S_pss2])
                    if h == 0:
                        B.op("dve", lambda e: e.tensor_copy(out=ssa[:, 2 * qb:2 * qb + 2], in_=pbc[:, 256:258]), reads=[S_pss2], writes=[S_ssa])
                    else:
                        B.op("dve", lambda e: e.tensor_tensor(out=ssa[:, 2 * qb:2 * qb + 2], in0=pbc[:, 256:258], in1=ssa[:, 2 * qb:2 * qb + 2],
                                                              op=ALU.add),
                             reads=[S_pss2, S_ssa], writes=[S_ssa])

                def blocks_of(h, qb):
                    sB = 15 + qb
                    return list(range(max(0, sB - KEEP[h]), sB + 1))

                load_head(0)
                prep_head(0)
                for qt in range(NQT):
                    mask_a(0, qt)
                    mask_b(0, qt)
                for h in range(NH):
                    hb = h % 2
                    if h + 1 < NH:
                        load_head(h + 1)
                        prep_head(h + 1)
                    total_iter = sum(len(blocks_of(h, qb)) for qb in range(NQB))
                    sched = {}
                    if h + 1 < NH:
                        step = max(1, (total_iter - 8) // NQT)
                        for qt in range(NQT):
                            ia = min(total_iter - 1, 1 + qt * step)
                            ib = min(total_iter - 1, ia + 3)
                            sched.setdefault(ia, []).append(lambda qt=qt: mask_a(h + 1, qt))
                            sched.setdefault(ib, []).append(lambda qt=qt: mask_b(h + 1, qt))
                    it = 0
                    tails = []
                    for qb in range(NQB):
                        js = blocks_of(h, qb)
                        n = len(js)
                        ids = {0: s_mm(h, qb, js[0])}
                        if n > 1:
                            ids[1] = s_mm(h, qb, js[1])
                        for i in range(n):
                            if i + 2 < n:
                                ids[i + 2] = s_mm(h, qb, js[i + 2])
                            pv_mm(h, qb, js[i], ids[i], i == 0, i == n - 1)
                            for fn in sched.pop(it, []):
                                fn()
                            for item in list(tails):
                                if item[0] <= it:
                                    item[1]()
                                    tails.remove(item)
                            it += 1
                        r = tail_a(h, qb)
                        tails.append((it + 5, lambda h=h, qb=qb, r=r: tail_b(h, qb, r)))
                        tails.append((it + 10, lambda h=h, qb=qb, r=r: tail_c(h, qb, r)))
                    for key in sorted(sched):
                        for fn in sched[key]:
                            fn()
                    for item in tails:
                        item[1]()
                    B.dma("sp", at_s[h], atst[hb][:], S_atst[hb], reads=[S_atst[hb]], writes=[S_atd])
                B.emit_block()

        with ExitStack() as p3:
          if PHASES >= 3:
            wo = sb(p3, "wo", [128, 8, D], BF16)
            wu = sb(p3, "wu", [128, 8, 2 * DFF], BF16)
            wd = sb(p3, "wd", [128, 22, D], BF16)
            x1 = [sb(p3, "x1%d" % i, [128, 2, D], F32) for i in range(2)]
            atb = sb(p3, "atb", [128, 4, 256], BF16)
            cgb = sb(p3, "cgb", [128, 4, 256], BF16)
            xn2 = sb(p3, "xn2", [128, 2, D], BF16)
            h2T = [sb(p3, "h2T%d" % i, [128, 8, 258], BF16) for i in range(2)]
            yb = [sb(p3, "yb%d" % i, [128, 256], F32) for i in range(6)]
            actT = sb(p3, "actT", [128, 22, 256], BF16)
            gfb = sb(p3, "gfb", [128, D], F32)
            rsa = sb(p3, "rsa", [128, NQT], F32)
            rsc = sb(p3, "rsc", [128, NQT], F32)
            ssf = sb(p3, "ssf", [128, 2], F32)
            rtf = sb(p3, "rtf", [128, 2], F32)
            rsf = sb(p3, "rsf", [128, 2], F32)
            ssb = sb(p3, "ssb", [128, 2], F32)
            rtb = sb(p3, "rtb", [128, 2], F32)
            rsb = sb(p3, "rsb", [128, 2], F32)
            pu = [ps(p3, "pu%d" % i, [128, 512], F32) for i in range(4)]
            pd = [ps(p3, "pd%d" % i, [128, 512], F32) for i in range(2)]
            pac = [ps(p3, "pac%d" % i, [128, 512], F32) for i in range(2)]
            S_wo, S_wu, S_wd = Slot(), Slot(), Slot()
            S_x1 = [Slot(), Slot()]
            S_atb, S_cgb, S_xn2 = Slot(), Slot(), Slot()
            S_h2T = [Slot(), Slot()]
            S_yb = [Slot() for _ in range(6)]
            S_actT, S_gfb, S_rs = Slot(), Slot(), Slot()
            S_ssf, S_rtf, S_rsf, S_ssb, S_rtb, S_rsb = Slot(), Slot(), Slot(), Slot(), Slot(), Slot()
            S_pu = [Slot() for _ in range(4)]
            S_pd = [Slot(), Slot()]
            S_pac = [Slot(), Slot()]
            gtb = x1[1][:, 0, :]
            S_gtb = S_x1[1]

            wov = w_out.rearrange("(c p) n -> p c n", p=128)
            wuv = w_up.rearrange("(c p) n -> p c n", p=128)
            wdv = w_down.rearrange("(c p) n -> p c n", p=128)
            for c in range(8):
                B.dma("pool", wo[:, c, :], wov[:, c, :], S_wo, writes=[S_wo])
            for c in range(8):
                B.dma("pool", wu[:, c, :], wuv[:, c, :], S_wu, writes=[S_wu])
            for c0 in range(0, 22, 2):
                B.dma("pool", wd[:, c0:c0 + 2, :], wdv[:, c0:c0 + 2, :], S_wd, writes=[S_wd])
            B.dma("sp", gfb[:], gfin, S_gfb, writes=[S_gfb])
            mod_t = mod_s.tensor
            B.dma("sp", gtb, bass.AP(mod_t, 2048, [[0, 128], [1, D]]), S_gtb, reads=[S_mods], writes=[S_gtb])
            for c in range(8):
                B.op("pool", lambda e, c=c: e.tensor_tensor(out=wo[:, c, :], in0=wo[:, c, :], in1=gtb, op=ALU.mult),
                     reads=[S_gtb], writes=[S_wo])
            B.dma("sp", gtb, bass.AP(mod_t, 5120, [[0, 128], [1, D]]), S_gtb, reads=[S_mods], writes=[S_gtb])
            for c in range(22):
                B.op("pool", lambda e, c=c: e.tensor_tensor(out=wd[:, c, :], in0=wd[:, c, :], in1=gtb, op=ALU.mult),
                     reads=[S_gtb], writes=[S_wd])
            for src_, dst, ssl in ((ssa, rsa, S_ssa), (ssc, rsc, S_ssc)):
                B.op("act", lambda e, src_=src_, dst=dst: e.activation(out=dst[:], in_=src_[:], func=AF.Sqrt, scale=1.0 / 512, bias=epsc[:]),
                     reads=[ssl, S_const], writes=[S_rs])
                B.op("dve", lambda e, dst=dst: e.reciprocal(out=dst[:], in_=dst[:]), reads=[S_rs], writes=[S_rs])
            for i in range(2):
                B.op("dve", lambda e, i=i: e.memset(h2T[i][:, :, 0:2], 0.0), writes=[S_h2T[i]])

            xsv = xs.rearrange("(n t p) d -> n p t d", t=2, p=128)
            outv = out.rearrange("(n t p) d -> n p t d", t=2, p=128)
            atv = at_s.rearrange("(pr two) d t -> (two d) pr t", two=2)
            cgv = cg_s.rearrange("c p t -> p c t")
            puc = [0]
            ybc = [0]
            tpv = [pd[i][:, :].bitcast(BF16).rearrange("p (c t) -> p c t", t=256) for i in range(2)]

            def next_yb():
                i = ybc[0] % 6
                ybc[0] += 1
                return yb[i], S_yb[i]

            def f3a(qb):
                k = qb % 2
                sl = 15 + qb
                qc = slice(qb * 256, (qb + 1) * 256)
                B.dma("sp", x1[k][:], xsv[sl], S_x1[k], writes=[S_x1[k]])
                B.dma("sp", atb[:], atv[:, :, qc], S_atb, reads=[S_atd], writes=[S_atb])
                B.dma("sp", cgb[:], cgv[:, :, qc], S_cgb, reads=[S_cgd], writes=[S_cgb])
                for t in range(2):
                    tile = 2 * qb + t
                    for n in range(2):
                        ns = slice(n * 512, (n + 1) * 512)
                        B.pe_group([mm(pac[0][:, :], atb[:, c, t * 128:(t + 1) * 128], wo[:, c, ns], c == 0, c == 3) for c in range(4)],
                                   reads=[S_atb, S_wo], writes=[S_pac[0]])
                        B.pe_group([mm(pac[1][:, :], cgb[:, c, t * 128:(t + 1) * 128], wo[:, 4 + c, ns], c == 0, c == 3) for c in range(4)],
                                   reads=[S_cgb, S_wo], writes=[S_pac[1]])
                        B.op("dve", lambda e, t=t, ns=ns, tile=tile: e.scalar_tensor_tensor(
                            out=x1[k][:, t, ns], in0=pac[0][:, :], scalar=rsa[:, tile:tile + 1], in1=x1[k][:, t, ns], op0=ALU.mult, op1=ALU.add),
                            reads=[S_pac[0], S_rs], writes=[S_x1[k]])
                        B.op("dve", lambda e, t=t, ns=ns, tile=tile: e.scalar_tensor_tensor(
                            out=x1[k][:, t, ns], in0=pac[1][:, :], scalar=rsc[:, tile:tile + 1], in1=x1[k][:, t, ns], op0=ALU.mult, op1=ALU.add),
                            reads=[S_pac[1], S_rs], writes=[S_x1[k]])
                for t in range(2):
                    B.op("act", lambda e, t=t: e.activation(out=xn2[:, t, :], in_=x1[k][:, t, :], func=AF.Square, accum_out=ssf[:, t:t + 1]),
                         reads=[S_x1[k]], writes=[S_xn2, S_ssf])
                B.op("act", lambda e: e.activation(out=rtf[:], in_=ssf[:], func=AF.Sqrt, scale=1.0 / D, bias=epsc[:]),
                     reads=[S_ssf, S_const], writes=[S_rtf])
                B.op("dve", lambda e: e.reciprocal(out=rsf[:], in_=rtf[:]), reads=[S_rtf], writes=[S_rsf])
                for t in range(2):
                    B.op("dve", lambda e, t=t: e.tensor_scalar(out=xn2[:, t, :], in0=x1[k][:, t, :], scalar1=rsf[:, t:t + 1], scalar2=None,
                                                               op0=ALU.mult),
                         reads=[S_x1[k], S_rsf], writes=[S_xn2])

            def f3b(qb):
                k = qb % 2
                for half in range(2):
                    B.pe_group([tr(tpv[half][:, c, t * 128:(t + 1) * 128], xn2[:, t, (half * 4 + c) * 128:(half * 4 + c + 1) * 128], ident[:])
                                for t in range(2) for c in range(4)], reads=[S_xn2, S_const], writes=[S_pd[half]])
                    for c in range(4):
                        cc = half * 4 + c
                        B.op("act", lambda e, c=c, cc=cc, half=half: e.activation(out=h2T[k][:, cc, 2:258], in_=tpv[half][:, c, :],
                                                                                 func=AF.Identity, scale=G2[:, cc:cc + 1], bias=sh2[:, cc:cc + 1]),
                             reads=[S_pd[half], S_G], writes=[S_h2T[k]])
                if qb == 1:
                    B.op("dve", lambda e: e.tensor_scalar(out=h2T[k][:, :, 0:2], in0=h2T[1 - k][:, :, 256:258], scalar1=hv[:, 0:1],
                                                          scalar2=None, op0=ALU.mult),
                         reads=[S_h2T[1 - k], S_const], writes=[S_h2T[k]])
                elif qb > 1:
                    B.op("dve", lambda e: e.tensor_copy(out=h2T[k][:, :, 0:2], in_=h2T[1 - k][:, :, 256:258]),
                         reads=[S_h2T[1 - k]], writes=[S_h2T[k]])

            def back3(qb):
                k = qb % 2
                for v in range(22):
                    if qb + 1 < NQB and v == 3:
                        f3a(qb + 1)
                    if qb + 1 < NQB and v == 14:
                        f3b(qb + 1)
                    res = []
                    for col0 in (v * 128, DFF + v * 128):
                        ch = col0 // 128
                        i = puc[0] % 4
                        puc[0] += 1
                        B.pe_group([mm(pu[i][:, 0:258], wu[:, c, col0:col0 + 128], h2T[k][:, c, 0:258], c == 0, c == 7) for c in range(8)],
                                   reads=[S_wu, S_h2T[k]], writes=[S_pu[i]])
                        ya, sya = next_yb()
                        B.op("act", lambda e, i=i, ya=ya, ch=ch: e.activation(out=ya[:], in_=pu[i][:, 2:258], func=AF.Identity,
                                                                             scale=fcw_sb[:, 88 + ch:89 + ch], bias=fcb_sb[:, ch:ch + 1]),
                             reads=[S_pu[i], S_const], writes=[sya])
                        ybb, syb = next_yb()
                        B.op("dve", lambda e, i=i, ya=ya, ybb=ybb, ch=ch: e.scalar_tensor_tensor(
                            out=ybb[:], in0=pu[i][:, 1:257], scalar=fcw_sb[:, 44 + ch:45 + ch], in1=ya[:], op0=ALU.mult, op1=ALU.add),
                            reads=[S_pu[i], sya, S_const], writes=[syb])
                        yc, syc = next_yb()
                        B.op("dve", lambda e, i=i, ybb=ybb, yc=yc, ch=ch: e.scalar_tensor_tensor(
                            out=yc[:], in0=pu[i][:, 0:256], scalar=fcw_sb[:, ch:ch + 1], in1=ybb[:], op0=ALU.mult, op1=ALU.add),
                            reads=[S_pu[i], syb, S_const], writes=[syc])
                        res.append((yc, syc))
                    (yv, syv), (yg, syg) = res
                    B.op("act", lambda e, yg=yg: e.activation(out=yg[:], in_=yg[:], func=AF.Silu), reads=[syg], writes=[syg])
                    B.op("pool", lambda e, yv=yv, yg=yg, v=v: e.tensor_tensor(out=actT[:, v, :], in0=yv[:], in1=yg[:], op=ALU.mult),
                         reads=[syv, syg], writes=[S_actT])
                for t in range(2):
                    for n in range(2):
                        B.pe_group([mm(pd[n][:, :], actT[:, v, t * 128:(t + 1) * 128], wd[:, v, n * 512:(n + 1) * 512], v == 0, v == 21)
                                    for v in range(22)], reads=[S_actT, S_wd], writes=[S_pd[n]])
                        B.op("dve", lambda e, t=t, n=n: e.tensor_tensor(out=x1[k][:, t, n * 512:(n + 1) * 512], in0=pd[n][:, :],
                                                                        in1=x1[k][:, t, n * 512:(n + 1) * 512], op=ALU.add),
                             reads=[S_pd[n]], writes=[S_x1[k]])
                for t in range(2):
                    B.op("act", lambda e, t=t: e.activation(out=xn2[:, t, :], in_=x1[k][:, t, :], func=AF.Square, accum_out=ssb[:, t:t + 1]),
                         reads=[S_x1[k]], writes=[S_xn2, S_ssb])
                B.op("act", lambda e: e.activation(out=rtb[:], in_=ssb[:], func=AF.Sqrt, scale=1.0 / D, bias=epsc[:]),
                     reads=[S_ssb, S_const], writes=[S_rtb])
                B.op("dve", lambda e: e.reciprocal(out=rsb[:], in_=rtb[:]), reads=[S_rtb], writes=[S_rsb])
                for t in range(2):
                    B.op("dve", lambda e, t=t: e.scalar_tensor_tensor(out=x1[k][:, t, :], in0=x1[k][:, t, :], scalar=rsb[:, t:t + 1],
                                                                      in1=gfb[:], op0=ALU.mult, op1=ALU.mult),
                         reads=[S_rsb, S_gfb], writes=[S_x1[k]])
                B.dma("sp", outv[qb - 1], x1[k][:], S_x1[k], reads=[S_x1[k]])

            f3a(0)
            f3b(0)
            f3a(1)
            f3b(1)
            for qb in range(1, NQB):
                back3(qb)
            B.emit_block()
    return nc


_NC_CACHE = {}


def _consts(half):
    bf = ml_dtypes.bfloat16
    ident = np.eye(128, dtype=np.float32).astype(bf)
    key = np.arange(S)
    kstat = np.zeros((35, S), np.float32)
    kstat[key // 256, key] = 1.0
    kstat[32] = key % 256
    kstat[33] = key // 256
    kstat[34] = 1.0
    kstat = kstat.astype(bf)
    p = np.arange(128)[:, None, None]
    kt = np.arange(2)[None, :, None]
    a = np.arange(256)[None, None, :]
    cmask = np.where(kt * 128 + p <= a, 0.0, NEG).astype(np.float32).reshape(128, 512).astype(bf)
    gvb = np.zeros((NQB, 32), np.float32)
    gvb2 = np.zeros((NQB, 32), np.float32)
    for qb in range(NQB):
        sB = 15 + qb
        for j in range(32):
            valid = (j < sB) and (half == 1 or j >= 16)
            gvb[qb, j] = 0.0 if valid else -1.0e9
            gvb2[qb, j] = NEG if valid else 2 * NEG
        gvb[qb, sB] = -3.0e9
        gvb2[qb, sB] = 0.0
    gvb = np.ascontiguousarray(np.broadcast_to(gvb.reshape(1, -1), (128, NQB * 32))).astype(np.float32)
    gvb2 = np.ascontiguousarray(np.broadcast_to(gvb2.reshape(1, -1), (128, NQB * 32))).astype(np.float32)
    qstat = np.zeros((NH, 128, NQT, 99), np.float32)
    for h in range(NH):
        slope = 2.0 ** (-(h + 1))
        for qt in range(NQT):
            sB = 15 + qt // 2
            apos = (qt % 2) * 128 + np.arange(128)
            qstat[h, :, qt, 96] = slope
            qstat[h, :, qt, 97] = 256.0 * slope
            qstat[h, :, qt, 98] = -slope * (256.0 * sB + apos)
    qstat = qstat.reshape(NH, 128, NQT * 99).astype(bf)
    hv = np.full((128, 1), float(half), np.float32)
    return dict(ident=ident, kstat=kstat, cmask=cmask, gvb=gvb, gvb2=gvb2, qstat=qstat, hv=hv)


def _pc(v, nchunk):
    return np.ascontiguousarray(np.asarray(v, np.float32).reshape(nchunk, 128).T)


def kernel(x, c, w_ada, b_ada, g_mix, w_in, conv_w, conv_b, g_attn_out, g_conv_out, w_out, g_ffn,
           w_up, ffn_conv_w, ffn_conv_b, w_down, g_final):
    f = lambda a: np.ascontiguousarray(np.asarray(a, dtype=np.float32))
    x = f(x)
    c = f(c)
    if "nc" not in _NC_CACHE:
        _NC_CACHE["nc"] = build_program()
    nc = _NC_CACHE["nc"]
    shared = dict(
        w_ada=f(w_ada[0]), b_ada=f(b_ada[0]).reshape(1, -1), gmix=_pc(g_mix[0], 8), w_in=f(w_in[0]),
        convw=np.ascontiguousarray(np.concatenate([_pc(conv_w[0][kk], 4) for kk in range(3)], axis=1)),
        convb=_pc(conv_b[0], 4),
        gattn=np.ascontiguousarray(f(g_attn_out[0]).reshape(8, 64).T),
        gconv=_pc(g_conv_out[0], 4), w_out=f(w_out[0]), gffn=_pc(g_ffn[0], 8), w_up=f(w_up[0]),
        fcw=np.ascontiguousarray(np.concatenate([_pc(ffn_conv_w[0][kk], 44) for kk in range(3)], axis=1)),
        fcb=_pc(ffn_conv_b[0], 44), w_down=f(w_down[0]),
        gfin=np.ascontiguousarray(np.broadcast_to(f(g_final).reshape(1, -1), (128, D))),
    )
    cst = [_consts(0), _consts(1)]
    in_maps = []
    for i in range(NCORES):
        b, half = i // 2, i % 2
        if half == 1:
            xs = x[b]
        else:
            xs = np.concatenate([np.zeros((4096, D), np.float32), x[b, :4096]], axis=0)
        m = dict(shared)
        m.update(cst[half])
        m["xs"] = np.ascontiguousarray(xs)
        m["cT"] = _pc(c[b], 8)
        in_maps.append(m)
    res = run_bass_kernel_spmd(nc, in_maps, core_ids=list(range(NCORES)))
    outp = np.empty((4, S, D), np.float32)
    for i in range(NCORES):
        b, half = i // 2, i % 2
        outp[b, half * 4096:(half + 1) * 4096] = res.results[i]["out"]
    return outp
```

```python
import numpy as np
from contextlib import ExitStack
import ml_dtypes
import concourse.bass as bass
import concourse.mybir as mybir
from concourse.bass_utils import run_bass_kernel_spmd

F32 = mybir.dt.float32
BF16 = mybir.dt.bfloat16
AF = mybir.ActivationFunctionType
ALU = mybir.AluOpType
AX = mybir.AxisListType

D = 1024
S = 8192
NH = 8
DFF = 2816
NQB = 17
NQT = 34
QTOK = NQB * 256
EPS = 1e-6
NEG = -30000.0
NCORES = 8
PHASES = 3
DEBUG = False


class Tok:
    __slots__ = ("sem", "val")

    def __init__(self, sem, val):
        self.sem = sem
        self.val = val


class Slot:
    def __init__(self, name=""):
        self.name = name
        self.w = None
        self.r = {}
        self.dsem = None
        self.dcnt = 0


class Builder:
    ENG = ("pe", "act", "dve", "pool", "sp")

    def __init__(self, nc, es):
        self.nc = nc
        self.es = es
        self.q = {}
        for n in self.ENG:
            sem = es.enter_context(nc.semaphore("sem_" + n))
            self.q[n] = dict(sem=sem, cnt=0, ops=[], waited={})
        self.dslots = []

    def _waits(self, en, deps):
        q = self.q[en]
        need = {}
        for t in deps:
            if t is None:
                continue
            if en == "pe" and t.sem is q["sem"]:
                continue
            k = id(t.sem)
            if q["waited"].get(k, 0) >= t.val:
                continue
            if k not in need or need[k].val < t.val:
                need[k] = t
        for k, t in need.items():
            q["waited"][k] = t.val
        return [(t.sem, t.val) for t in need.values()]

    @staticmethod
    def _deps(reads, writes, deps):
        d = list(deps)
        for s in reads:
            d.append(s.w)
        for s in writes:
            d.extend(s.r.values())
            d.append(s.w)
        return d

    @staticmethod
    def _update(tok, reads, writes):
        for s in writes:
            s.w = tok
            s.r = {}
        for s in reads:
            k = id(tok.sem)
            if k not in s.r or s.r[k].val < tok.val:
                s.r[k] = tok

    def op(self, en, fn, reads=(), writes=(), deps=(), inc=True):
        q = self.q[en]
        waits = self._waits(en, self._deps(reads, writes, deps))
        tok = None
        if inc:
            q["cnt"] += 1
            tok = Tok(q["sem"], q["cnt"])
        else:
            assert not reads and not writes
        sem = q["sem"]

        def emit(e, waits=waits, fn=fn, inc=inc, sem=sem):
            for (sm, v) in waits:
                e.wait_ge(sm, v)
            ins = fn(e)
            if inc:
                ins.then_inc(sem, 1)

        q["ops"].append(emit)
        if tok is not None:
            self._update(tok, reads, writes)
        return tok

    def pe_group(self, fns, reads=(), writes=()):
        q = self.q["pe"]
        waits = self._waits("pe", self._deps(reads, writes, ()))
        q["cnt"] += 1
        tok = Tok(q["sem"], q["cnt"])
        sem = q["sem"]
        n = len(fns)

        def emit(e, waits=waits, fns=fns, sem=sem, n=n):
            for (sm, v) in waits:
                e.wait_ge(sm, v)
            for i, f in enumerate(fns):
                ins = f(e)
                if i == n - 1:
                    ins.then_inc(sem, 1)

        q["ops"].append(emit)
        self._update(tok, reads, writes)
        return tok

    def dma(self, en, out, in_, sem_slot, reads=(), writes=(), deps=(), **kw):
        q = self.q[en]
        sl = sem_slot
        if sl.dsem is None:
            sl.dsem = self.es.enter_context(self.nc.semaphore("dsem%d" % len(self.dslots)))
            self.dslots.append(sl)
        waits = self._waits(en, self._deps(reads, writes, deps))
        sl.dcnt += 16
        tok = Tok(sl.dsem, sl.dcnt)
        dsem = sl.dsem

        def emit(e, waits=waits, out=out, in_=in_, dsem=dsem, kw=kw):
            for (sm, v) in waits:
                e.wait_ge(sm, v)
            e.dma_start(out=out, in_=in_, **kw).then_inc(dsem, 16)

        q["ops"].append(emit)
        self._update(tok, reads, writes)
        return tok

    def emit_block(self):
        nc = self.nc
        finals = [(s.dsem, s.dcnt) for s in self.dslots if s.dcnt > 0]
        pe_fin = (self.q["pe"]["sem"], self.q["pe"]["cnt"])

        def fin(e, finals=finals):
            for sm, v in finals:
                e.wait_ge(sm, v)

        self.q["sp"]["ops"].append(fin)
        with nc.Block() as blk:
            for n, meth in (("pe", blk.tensor), ("act", blk.scalar), ("dve", blk.vector),
                            ("pool", blk.gpsimd), ("sp", blk.sync)):
                ops = self.q[n]["ops"]
                if ops:
                    def body(e, ops=ops):
                        for f in ops:
                            f(e)
                    meth(body)
                self.q[n]["ops"] = []


def mm(out, lhsT, rhs, start, stop):
    return lambda e: e.matmul(out, lhsT=lhsT, rhs=rhs, start=start, stop=stop)


def tr(out, in_, ident):
    return lambda e: e.transpose(out, in_, ident)


def build_program():
    nc = bass.Bass("TRN2", target_bir_lowering=False)

    def din(name, shape, dt=F32):
        return nc.dram_tensor(name, list(shape), dt, kind="ExternalInput").ap()

    def dscr(name, shape, dt):
        return nc.dram_tensor(name, list(shape), dt, kind="ExternalOutput" if DEBUG else "Internal").ap()

    xs = din("xs", [S, D])
    cT = din("cT", [128, 8])
    w_ada = din("w_ada", [D, 6 * D])
    b_ada = din("b_ada", [1, 6 * D])
    gmix = din("gmix", [128, 8])
    w_in = din("w_in", [D, 3072])
    convw = din("convw", [128, 12])
    convb = din("convb", [128, 4])
    gattn = din("gattn", [64, 8])
    gconv = din("gconv", [128, 4])
    w_out = din("w_out", [D, D])
    gffn = din("gffn", [128, 8])
    w_up = din("w_up", [D, 2 * DFF])
    fcw = din("fcw", [128, 132])
    fcb = din("fcb", [128, 44])
    w_down = din("w_down", [DFF, D])
    gfin = din("gfin", [128, D])
    ident_d = din("ident", [128, 128], BF16)
    kstat = din("kstat", [35, S], BF16)
    cmask = din("cmask", [128, 512], BF16)
    gvb_d = din("gvb", [128, NQB * 32])
    gvb2_d = din("gvb2", [128, NQB * 32])
    qstat = din("qstat", [NH, 128, NQT * 99], BF16)
    hv_d = din("hv", [128, 1])
    out = nc.dram_tensor("out", [4096, D], F32, kind="ExternalOutput").ap()

    kT_s = dscr("kT_s", [NH, 64, S], BF16)
    qT_s = dscr("qT_s", [NH, 64, QTOK], BF16)
    at_s = dscr("at_s", [NH, 64, QTOK], BF16)
    cg_s = dscr("cg_s", [4, 128, QTOK], BF16)
    mod_s = dscr("mod_s", [1, 6 * D], F32)

    es = ExitStack()
    with es:
        B = Builder(nc, es)

        def sb(st, name, shape, dt):
            return st.enter_context(nc.sbuf_tensor("sb_" + name, list(shape), dt))

        def ps(st, name, shape, dt):
            return st.enter_context(nc.psum_tensor("ps_" + name, list(shape), dt))

        ident = sb(es, "ident", [128, 128], BF16)
        ones_b = sb(es, "ones_b", [128, 1], BF16)
        ones_f = sb(es, "ones_f", [128, 64], F32)
        epsc = sb(es, "epsc", [128, 1], F32)
        hv = sb(es, "hv", [128, 1], F32)
        G1 = sb(es, "G1", [128, 8], F32)
        G2 = sb(es, "G2", [128, 8], F32)
        modT = sb(es, "modT", [128, 48], F32)
        gmix_sb = sb(es, "gmix_sb", [128, 8], F32)
        gffn_sb = sb(es, "gffn_sb", [128, 8], F32)
        convw_sb = sb(es, "convw_sb", [128, 12], F32)
        convb_sb = sb(es, "convb_sb", [128, 4], F32)
        gconv_sb = sb(es, "gconv_sb", [128, 4], F32)
        gattn_sb = sb(es, "gattn_sb", [64, 8], F32)
        fcw_sb = sb(es, "fcw_sb", [128, 132], F32)
        fcb_sb = sb(es, "fcb_sb", [128, 44], F32)
        ssa = sb(es, "ssa", [128, NQT], F32)
        ssc = sb(es, "ssc", [128, NQT], F32)
        S_const = Slot("const")
        S_ssa = Slot("ssa")
        S_ssc = Slot("ssc")
        S_G = Slot("G")
        S_mods = Slot("mods")

        for dst, src in ((ident, ident_d), (hv, hv_d), (gmix_sb, gmix), (gffn_sb, gffn), (convw_sb, convw),
                         (convb_sb, convb), (gconv_sb, gconv), (gattn_sb, gattn), (fcw_sb, fcw), (fcb_sb, fcb)):
            B.dma("sp", dst[:], src, S_const, writes=[S_const])
        B.op("dve", lambda e: e.memset(ones_b[:], 1.0), writes=[S_const])
        B.op("dve", lambda e: e.memset(ones_f[:], 1.0), writes=[S_const])
        B.op("dve", lambda e: e.memset(epsc[:], EPS), writes=[S_const])

        with ExitStack() as p0:
            c_sb = sb(p0, "c_sb", [128, 8], F32)
            sc_b = sb(p0, "sc_b", [128, 8], BF16)
            brow = sb(p0, "brow", [1, 6 * D], F32)
            modrow = sb(p0, "modrow", [1, 6 * D], F32)
            wa = [sb(p0, "wa%d" % i, [128, 8, 512], BF16) for i in range(2)]
            pm = [ps(p0, "pm%d" % i, [128, 512], F32) for i in range(2)]
            S_c, S_scb, S_brow, S_modrow, S_modT = Slot(), Slot(), Slot(), Slot(), Slot()
            S_wa = [Slot(), Slot()]
            S_pm = [Slot(), Slot()]
            B.dma("sp", c_sb[:], cT, S_c, writes=[S_c])
            B.dma("sp", brow[:], b_ada, S_brow, writes=[S_brow])
            B.op("act", lambda e: e.activation(out=sc_b[:], in_=c_sb[:], func=AF.Silu), reads=[S_c], writes=[S_scb])
            wav = w_ada.rearrange("(c p) n -> p c n", p=128)
            for g in range(12):
                k = g % 2
                B.dma("pool", wa[k][:], wav[:, :, g * 512:(g + 1) * 512], S_wa[k], writes=[S_wa[k]])
                B.pe_group([mm(pm[k][0:1, :], sc_b[:, c:c + 1], wa[k][:, c, :], c == 0, c == 7) for c in range(8)],
                           reads=[S_wa[k], S_scb], writes=[S_pm[k]])
                B.op("dve", lambda e, k=k, g=g: e.tensor_tensor(out=modrow[0:1, g * 512:(g + 1) * 512], in0=pm[k][0:1, :],
                                                                 in1=brow[0:1, g * 512:(g + 1) * 512], op=ALU.add),
                     reads=[S_pm[k], S_brow], writes=[S_modrow])
            B.dma("sp", mod_s, modrow[:], S_mods, reads=[S_modrow], writes=[S_mods])
            B.dma("sp", modT[:], mod_s.rearrange("o (k p) -> (o p) k", p=128), S_modT, reads=[S_mods], writes=[S_modT],
                  allow_slow_non_contiguous=True)
            B.op("dve", lambda e: e.scalar_tensor_tensor(out=G1[:], in0=modT[:, 8:16], scalar=1.0, in1=gmix_sb[:],
                                                         op0=ALU.add, op1=ALU.mult), reads=[S_modT, S_const], writes=[S_G])
            B.op("dve", lambda e: e.scalar_tensor_tensor(out=G2[:], in0=modT[:, 32:40], scalar=1.0, in1=gffn_sb[:],
                                                         op0=ALU.add, op1=ALU.mult), reads=[S_modT, S_const], writes=[S_G])
            B.emit_block()
        sh1 = modT[:, 0:8]
        sh2 = modT[:, 24:32]

        with ExitStack() as p12:
          if PHASES >= 1:
            V_all = sb(p12, "V_all", [128, 64, NH, 65], BF16)
            S_V = Slot("V")
            with ExitStack() as p1:
                w_in_sb = sb(p1, "w_in_sb", [128, 8, 3072], BF16)
                S_win = Slot("win")
                wiv = w_in.rearrange("(c p) n -> p c n", p=128)
                for c in range(8):
                    B.dma("pool", w_in_sb[:, c, :], wiv[:, c, :], S_win, writes=[S_win])
                xt = [sb(p1, "xt%d" % i, [128, 2, D], F32) for i in range(2)]
                xn = [sb(p1, "xn%d" % i, [128, 2, D], BF16) for i in range(2)]
                hT = [sb(p1, "hT%d" % i, [128, 8, 258], BF16) for i in range(2)]
                junk = sb(p1, "junk", [128, D], BF16)
                ss = [sb(p1, "ss%d" % i, [128, 2], F32) for i in range(2)]
                rt = [sb(p1, "rt%d" % i, [128, 2], F32) for i in range(2)]
                rstd = [sb(p1, "rstd%d" % i, [128, 2], F32) for i in range(2)]
                kst = [sb(p1, "kst%d" % i, [128, 4, 256], BF16) for i in range(2)]
                qst = [sb(p1, "qst%d" % i, [128, 4, 256], BF16) for i in range(2)]
                cgs = [sb(p1, "cgs%d" % i, [128, 4, 256], BF16) for i in range(2)]
                sqc = sb(p1, "sqc", [128, 4, 256], BF16)
                u_sb = [sb(p1, "u_sb%d" % i, [128, 258], F32) for i in range(2)]
                zb = [sb(p1, "zb%d" % i, [128, 258], F32) for i in range(2)]
                y0 = [sb(p1, "y0%d" % i, [128, 256], F32) for i in range(2)]
                y1 = [sb(p1, "y1%d" % i, [128, 256], F32) for i in range(2)]
                y2 = [sb(p1, "y2%d" % i, [128, 256], F32) for i in range(2)]
                cv = [sb(p1, "cv%d" % i, [128, 256], F32) for i in range(2)]
                tp = ps(p1, "tp", [128, 8, 256], BF16)
                pp = [ps(p1, "pp%d" % i, [128, 512], F32) for i in range(5)]
                pss = ps(p1, "pss", [128, 512], F32)
                S_xt = [Slot(), Slot()]
                S_xn = [Slot(), Slot()]
                S_hT = [Slot(), Slot()]
                S_junk = Slot()
                S_ss = [Slot(), Slot()]
                S_rt = [Slot(), Slot()]
                S_rstd = [Slot(), Slot()]
                S_kst = [Slot(), Slot()]
                S_qst = [Slot(), Slot()]
                S_cgs = [Slot(), Slot()]
                S_sqc = Slot()
                S_u = [Slot(), Slot()]
                S_z = [Slot(), Slot()]
                S_y0 = [Slot(), Slot()]
                S_y1 = [Slot(), Slot()]
                S_y2 = [Slot(), Slot()]
                S_cv = [Slot(), Slot()]
                S_tp = Slot()
                S_pp = [Slot() for _ in range(5)]
                S_pss = Slot()
                S_kT = Slot("kT_s")
                S_qT = Slot("qT_s")
                S_cgd = Slot("cg_s")
                ppi = [0]

                def next_pp():
                    i = ppi[0] % 5
                    ppi[0] += 1
                    return pp[i], S_pp[i]

                B.op("pool", lambda e: e.memset(V_all[:, :, :, 64:65], 1.0), writes=[S_V])
                for i in range(2):
                    B.op("pool", lambda e, i=i: e.memset(hT[i][:, :, 0:2], 0.0), writes=[S_hT[i]])
                xsv = xs.rearrange("(n t p) d -> n p t d", t=2, p=128)
                kTv = kT_s.rearrange("(pr two) d t -> (two d) pr t", two=2)
                qTv = qT_s.rearrange("(pr two) d t -> (two d) pr t", two=2)
                cgv = cg_s.rearrange("c p t -> p c t")
                evac_rr = [0]

                def evac(out_ap, in_ap, reads, writes, scale=None):
                    evac_rr[0] += 1
                    if evac_rr[0] % 2 == 0:
                        if scale is None:
                            return B.op("act", lambda e: e.activation(out=out_ap, in_=in_ap, func=AF.Copy), reads=reads, writes=writes)
                        return B.op("act", lambda e: e.activation(out=out_ap, in_=in_ap, func=AF.Copy, scale=scale), reads=reads, writes=writes)
                    if scale is None:
                        return B.op("dve", lambda e: e.tensor_copy(out=out_ap, in_=in_ap), reads=reads, writes=writes)
                    return B.op("dve", lambda e: e.tensor_scalar(out=out_ap, in0=in_ap, scalar1=scale, scalar2=None, op0=ALU.mult),
                                reads=reads, writes=writes)

                def f1a(s):
                    k = s % 2
                    B.dma("sp", xt[k][:], xsv[s], S_xt[k], writes=[S_xt[k]])
                    for t in range(2):
                        B.op("act", lambda e, k=k, t=t: e.activation(out=junk[:], in_=xt[k][:, t, :], func=AF.Square,
                                                                     accum_out=ss[k][:, t:t + 1]),
                             reads=[S_xt[k]], writes=[S_junk, S_ss[k]])
                    B.op("act", lambda e, k=k: e.activation(out=rt[k][:], in_=ss[k][:], func=AF.Sqrt, scale=1.0 / D, bias=epsc[:]),
                         reads=[S_ss[k], S_const], writes=[S_rt[k]])
                    B.op("dve", lambda e, k=k: e.reciprocal(out=rstd[k][:], in_=rt[k][:]), reads=[S_rt[k]], writes=[S_rstd[k]])
                    for t in range(2):
                        B.op("dve", lambda e, k=k, t=t: e.tensor_scalar(out=xn[k][:, t, :], in0=xt[k][:, t, :], scalar1=rstd[k][:, t:t + 1],
                                                                        scalar2=None, op0=ALU.mult),
                             reads=[S_xt[k], S_rstd[k]], writes=[S_xn[k]])

                def f1b(s):
                    k = s % 2
                    B.pe_group([tr(tp[:, c, t * 128:(t + 1) * 128], xn[k][:, t, c * 128:(c + 1) * 128], ident[:])
                                for t in range(2) for c in range(8)], reads=[S_xn[k], S_const], writes=[S_tp])
                    for c in range(8):
                        if c % 2 == 0:
                            B.op("act", lambda e, k=k, c=c: e.activation(out=hT[k][:, c, 2:258], in_=tp[:, c, :], func=AF.Identity,
                                                                         scale=G1[:, c:c + 1], bias=sh1[:, c:c + 1]),
                                 reads=[S_tp, S_G], writes=[S_hT[k]])
                        else:
                            B.op("dve", lambda e, k=k, c=c: e.tensor_scalar(out=hT[k][:, c, 2:258], in0=tp[:, c, :], scalar1=G1[:, c:c + 1],
                                                                            scalar2=sh1[:, c:c + 1], op0=ALU.mult, op1=ALU.add),
                                 reads=[S_tp, S_G], writes=[S_hT[k]])
                    if s == 16:
                        B.op("dve", lambda e, k=k: e.tensor_scalar(out=hT[k][:, :, 0:2], in0=hT[1 - k][:, :, 256:258], scalar1=hv[:, 0:1],
                                                                   scalar2=None, op0=ALU.mult),
                             reads=[S_hT[1 - k], S_const], writes=[S_hT[k]])
                    elif s > 16:
                        B.op("dve", lambda e, k=k: e.tensor_copy(out=hT[k][:, :, 0:2], in_=hT[1 - k][:, :, 256:258]),
                             reads=[S_hT[1 - k]], writes=[S_hT[k]])

                def back1a(s):
                    k = s % 2
                    for pr in range(4):
                        pt_, sp_ = next_pp()
                        B.pe_group([mm(pt_[:, 0:256], w_in_sb[:, c, 512 + pr * 128:512 + (pr + 1) * 128], hT[k][:, c, 2:258], c == 0, c == 7)
                                    for c in range(8)], reads=[S_win, S_hT[k]], writes=[sp_])
                        evac(kst[k][:, pr, :], pt_[:, 0:256], [sp_], [S_kst[k]])
                    B.dma("sp", kTv[:, :, s * 256:(s + 1) * 256], kst[k][:], S_kst[k], reads=[S_kst[k]], writes=[S_kT])
                    for t in range(2):
                        pt_, sp_ = next_pp()
                        B.pe_group([mm(pt_[:, :], hT[k][:, c, 2 + t * 128:2 + (t + 1) * 128], w_in_sb[:, c, 1024:1536], c == 0, c == 7)
                                    for c in range(8)], reads=[S_win, S_hT[k]], writes=[sp_])
                        evac(V_all[:, 2 * s + t, :, 0:64], pt_[:, :].rearrange("p (h d) -> p h d", d=64), [sp_], [S_V])

                def back1b(s):
                    k = s % 2
                    if s < 15:
                        return
                    qb = s - 15
                    for pr in range(4):
                        pt_, sp_ = next_pp()
                        B.pe_group([mm(pt_[:, 0:256], w_in_sb[:, c, pr * 128:(pr + 1) * 128], hT[k][:, c, 2:258], c == 0, c == 7)
                                    for c in range(8)], reads=[S_win, S_hT[k]], writes=[sp_])
                        evac(qst[k][:, pr, :], pt_[:, 0:256], [sp_], [S_qst[k]], scale=0.125)
                    B.dma("sp", qTv[:, :, qb * 256:(qb + 1) * 256], qst[k][:], S_qst[k], reads=[S_qst[k]], writes=[S_qT])
                    for ch in range(4):
                        j = ch % 2
                        pu_, spu = next_pp()
                        B.pe_group([mm(pu_[:, 0:258], w_in_sb[:, c, 1536 + ch * 128:1536 + (ch + 1) * 128], hT[k][:, c, 0:258], c == 0, c == 7)
                                    for c in range(8)], reads=[S_win, S_hT[k]], writes=[spu])
                        pc_, spc = next_pp()
                        B.pe_group([mm(pc_[:, 0:258], w_in_sb[:, c, 2048 + ch * 128:2048 + (ch + 1) * 128], hT[k][:, c, 0:258], c == 0, c == 7)
                                    for c in range(8)], reads=[S_win, S_hT[k]], writes=[spc])
                        pb_, spb = next_pp()
                        B.pe_group([mm(pb_[:, 0:256], w_in_sb[:, c, 2560 + ch * 128:2560 + (ch + 1) * 128], hT[k][:, c, 2:258], c == 0, c == 7)
                                    for c in range(8)], reads=[S_win, S_hT[k]], writes=[spb])
                        B.op("act", lambda e, j=j, pu_=pu_: e.activation(out=u_sb[j][:], in_=pu_[:, 0:258], func=AF.Copy),
                             reads=[spu], writes=[S_u[j]])
                        B.op("dve", lambda e, j=j, pc_=pc_: e.tensor_tensor(out=zb[j][:], in0=pc_[:, 0:258], in1=u_sb[j][:], op=ALU.mult),
                             reads=[spc, S_u[j]], writes=[S_z[j]])
                        B.op("dve", lambda e, j=j, ch=ch: e.tensor_scalar(out=y0[j][:], in0=zb[j][:, 2:258], scalar1=convw_sb[:, 8 + ch:9 + ch],
                                                                          scalar2=convb_sb[:, ch:ch + 1], op0=ALU.mult, op1=ALU.add),
                             reads=[S_z[j], S_const], writes=[S_y0[j]])
                        B.op("dve", lambda e, j=j, ch=ch: e.scalar_tensor_tensor(out=y1[j][:], in0=zb[j][:, 1:257], scalar=convw_sb[:, 4 + ch:5 + ch],
                                                                                 in1=y0[j][:], op0=ALU.mult, op1=ALU.add),
                             reads=[S_z[j], S_y0[j], S_const], writes=[S_y1[j]])
                        B.op("dve", lambda e, j=j, ch=ch: e.scalar_tensor_tensor(out=y2[j][:], in0=zb[j][:, 0:256], scalar=convw_sb[:, ch:ch + 1],
                                                                                 in1=y1[j][:], op0=ALU.mult, op1=ALU.add),
                             reads=[S_z[j], S_y1[j], S_const], writes=[S_y2[j]])
                        B.op("dve", lambda e, j=j, pb_=pb_: e.tensor_tensor(out=cv[j][:], in0=pb_[:, 0:256], in1=y2[j][:], op=ALU.mult),
                             reads=[spb, S_y2[j]], writes=[S_cv[j]])
                        B.op("act", lambda e, j=j, ch=ch: e.activation(out=sqc[:, ch, :], in_=cv[j][:], func=AF.Square),
                             reads=[S_cv[j]], writes=[S_sqc])
                        B.op("act", lambda e, j=j, ch=ch, k=k: e.activation(out=cgs[k][:, ch, :], in_=cv[j][:], func=AF.Copy,
                                                                           scale=gconv_sb[:, ch:ch + 1]),
                             reads=[S_cv[j], S_const], writes=[S_cgs[k]])
                    for t in range(2):
                        B.pe_group([mm(pss[:, t:t + 1], sqc[:, ch, t * 128:(t + 1) * 128], ones_b[:, 0:1], ch == 0, ch == 3) for ch in range(4)],
                                   reads=[S_sqc, S_const], writes=[S_pss])
                    B.op("dve", lambda e, qb=qb: e.tensor_copy(out=ssc[:, 2 * qb:2 * qb + 2], in_=pss[:, 0:2]), reads=[S_pss], writes=[S_ssc])
                    B.dma("sp", cgv[:, :, qb * 256:(qb + 1) * 256], cgs[k][:], S_cgs[k], reads=[S_cgs[k]], writes=[S_cgd])

                f1a(0)
                f1b(0)
                f1a(1)
                for s in range(32):
                    if s + 1 < 32:
                        f1b(s + 1)
                    if s + 2 < 32:
                        f1a(s + 2)
                    back1a(s)
                    back1b(s)
                B.emit_block()

            with ExitStack() as p2:
              if PHASES >= 2:
                kaug = [sb(p2, "kaug%d" % i, [99, S], BF16) for i in range(2)]
                qaug = [sb(p2, "qaug%d" % i, [99, QTOK], BF16) for i in range(2)]
                qa = [sb(p2, "qa%d" % i, [128, NQT, 99], BF16) for i in range(2)]
                atst = [sb(p2, "atst%d" % i, [64, QTOK], BF16) for i in range(2)]
                gvb = sb(p2, "gvb", [128, NQB, 32], F32)
                gvb2 = sb(p2, "gvb2", [128, NQB, 32], F32)
                cm = sb(p2, "cm", [128, 2, 256], BF16)
                km_f = sb(p2, "km_f", [64, 32], F32)
                km_b = [sb(p2, "km_b%d" % i, [64, 32], BF16) for i in range(2)]
                gm = [sb(p2, "gm%d" % i, [128, 32], F32) for i in range(2)]
                t8 = [sb(p2, "t8%d" % i, [128, 8], F32) for i in range(2)]
                tsel = [sb(p2, "tsel%d" % i, [128, 32], F32) for i in range(2)]
                pT = [sb(p2, "pT%d" % i, [128, 2, 256], BF16) for i in range(3)]
                rc = [sb(p2, "rc%d" % i, [128, 256], F32) for i in range(4)]
                pos = [sb(p2, "pos%d" % i, [128, 256], F32) for i in range(4)]
                atf = [sb(p2, "atf%d" % i, [64, 256], F32) for i in range(4)]
                sqa = [sb(p2, "sqa%d" % i, [64, 256], BF16) for i in range(4)]
                sp = [ps(p2, "sp%d" % i, [128, 2, 256], F32) for i in range(3)]
                po = [ps(p2, "po%d" % i, [128, 512], F32) for i in range(2)]
                pgt = ps(p2, "pgt", [128, 512], F32)
                pbc = ps(p2, "pbc", [128, 512], F32)
                ptt_t = ps(p2, "ptt", [128, 1024], BF16)
                ptt = ptt_t[:, 0:128]
                S_kaug = [Slot(), Slot()]
                S_kstat = Slot()
                S_qq = [Slot(), Slot()]
                S_qm = [Slot(), Slot()]
                S_qa = [Slot(), Slot()]
                S_atst = [Slot(), Slot()]
                S_c2 = Slot()
                S_kmf = Slot()
                S_kmb = [Slot(), Slot()]
                S_gm = [Slot(), Slot()]
                S_t8 = [Slot(), Slot()]
                S_tsel = [Slot(), Slot()]
                S_pT = [Slot() for _ in range(3)]
                S_rc = [Slot() for _ in range(4)]
                S_pos = [Slot() for _ in range(4)]
                S_atf = [Slot() for _ in range(4)]
                S_sqa = [Slot() for _ in range(4)]
                S_sp = [Slot() for _ in range(3)]
                S_po = [Slot(), Slot()]
                S_pgt, S_ptt, S_pbc = Slot(), Slot(), Slot()
                S_pss2 = S_pbc
                S_atd = Slot("at_s")

                B.dma("sp", gvb[:].rearrange("p a b -> p (a b)"), gvb_d, S_c2, writes=[S_c2])
                B.dma("sp", gvb2[:].rearrange("p a b -> p (a b)"), gvb2_d, S_c2, writes=[S_c2])
                B.dma("sp", cm[:].rearrange("p a b -> p (a b)"), cmask, S_c2, writes=[S_c2])
                for i in range(2):
                    B.dma("sp", kaug[i][64:99, :], kstat, S_kstat, writes=[S_kstat])

                def load_head(h):
                    hb = h % 2
                    B.dma("sp", kaug[hb][0:64, :], kT_s[h], S_kaug[hb], reads=[S_kT], writes=[S_kaug[hb]])
                    B.dma("sp", qaug[hb][0:64, :], qT_s[h], S_qq[hb], reads=[S_qT], writes=[S_qq[hb]])
                    B.dma("sp", qa[hb][:].rearrange("p a b -> p (a b)"), qstat[h], S_qa[hb], writes=[S_qa[hb]])

                def prep_head(h):
                    hb = h % 2
                    B.op("dve", lambda e: e.tensor_reduce(out=km_f[:], in_=kaug[hb][0:64, :].rearrange("p (j c) -> p j c", c=256),
                                                          axis=AX.X, op=ALU.add),
                         reads=[S_kaug[hb]], writes=[S_kmf])
                    B.op("dve", lambda e: e.tensor_scalar(out=km_b[hb][:], in0=km_f[:], scalar1=1.0 / 256, scalar2=None, op0=ALU.mult),
                         reads=[S_kmf], writes=[S_kmb[hb]])

                def mask_a(h, qt):
                    hb = h % 2
                    qb = qt // 2
                    g2 = qt % 2
                    B.pe_group([mm(pgt[:, 0:32], qaug[hb][0:64, qt * 128:(qt + 1) * 128], km_b[hb][:, :], True, True)],
                               reads=[S_qq[hb], S_kmb[hb]], writes=[S_pgt])
                    B.op("dve", lambda e: e.tensor_tensor(out=gm[g2][:], in0=pgt[:, 0:32], in1=gvb[:, qb, :], op=ALU.add),
                         reads=[S_pgt, S_c2], writes=[S_gm[g2]])
                    B.op("dve", lambda e: e.max(out=t8[g2][:], in_=gm[g2][:]), reads=[S_gm[g2]], writes=[S_t8[g2]])
                    B.op("dve", lambda e: e.tensor_scalar(out=tsel[g2][:], in0=gm[g2][:], scalar1=t8[g2][:, 2:3], scalar2=-NEG,
                                                          op0=ALU.is_ge, op1=ALU.mult),
                         reads=[S_gm[g2], S_t8[g2]], writes=[S_tsel[g2]])
                    B.op("dve", lambda e: e.tensor_tensor(out=qa[hb][:, qt, 64:96], in0=tsel[g2][:], in1=gvb2[:, qb, :], op=ALU.add),
                         reads=[S_tsel[g2], S_c2], writes=[S_qa[hb]])

                def mask_b(h, qt):
                    hb = h % 2
                    B.pe_group([tr(ptt[0:99, :], qa[hb][:, qt, :], ident[:])], reads=[S_qa[hb], S_const], writes=[S_ptt])
                    B.op("dve", lambda e: e.tensor_copy(out=qaug[hb][64:99, qt * 128:(qt + 1) * 128], in_=ptt[64:99, :]),
                         reads=[S_ptt], writes=[S_qm[hb]])

                sctr = [0]
                KEEP = []
                for h_ in range(NH):
                    slope_ = 2.0 ** (-(h_ + 1))
                    kk_ = 0
                    while kk_ < 31 and slope_ * (kk_ * 256 + 1) < 80.0:
                        kk_ += 1
                    KEEP.append(kk_)

                def s_mm(h, qb, j):
                    hb = h % 2
                    sB = 15 + qb
                    qc = slice(qb * 256, (qb + 1) * 256)
                    i = sctr[0] % 3
                    sctr[0] += 1
                    fns = []
                    for kt in range(2):
                        fns.append(mm(sp[i][:, kt, :], kaug[hb][0:99, (2 * j + kt) * 128:(2 * j + kt + 1) * 128], qaug[hb][0:99, qc],
                                      True, j != sB))
                        if j == sB:
                            fns.append(mm(sp[i][:, kt, :], ident[:], cm[:, kt, :], False, True))
                    B.pe_group(fns, reads=[S_kaug[hb], S_kstat, S_qq[hb], S_qm[hb], S_c2, S_const], writes=[S_sp[i]])
                    B.op("act", lambda e: e.activation(out=pT[i][:], in_=sp[i][:], func=AF.Exp), reads=[S_sp[i]], writes=[S_pT[i]])
                    return i

                def pv_mm(h, qb, j, i, first, last):
                    ob = qb % 2
                    B.pe_group([mm(po[ob][0:65, 0:256], V_all[:, 2 * j + kt, h, :], pT[i][:, kt, :], (first and kt == 0), (last and kt == 1))
                                for kt in range(2)], reads=[S_V, S_pT[i]], writes=[S_po[ob]])

                tctr = [0]

                def tail_a(h, qb):
                    ob = qb % 2
                    r = tctr[0] % 4
                    tctr[0] += 1
                    B.op("dve", lambda e: e.tensor_copy(out=pos[r][0:65, :], in_=po[ob][0:65, 0:256]), reads=[S_po[ob]], writes=[S_pos[r]])
                    B.op("dve", lambda e: e.reciprocal(out=rc[r][64:65, :], in_=pos[r][64:65, :]), reads=[S_pos[r]], writes=[S_rc[r]])
                    return r

                def tail_b(h, qb, r):
                    hb = h % 2
                    qc = slice(qb * 256, (qb + 1) * 256)
                    B.pe_group([mm(pbc[0:64, 0:256], ones_f[64:65, 0:64], rc[r][64:65, :], True, True)],
                               reads=[S_rc[r], S_const], writes=[S_pbc])
                    B.op("dve", lambda e: e.tensor_tensor(out=atf[r][:], in0=pbc[0:64, 0:256], in1=pos[r][0:64, :], op=ALU.mult),
                         reads=[S_pbc, S_pos[r]], writes=[S_atf[r]])
                    B.op("pool", lambda e: e.tensor_tensor(out=sqa[r][:], in0=atf[r][:], in1=atf[r][:], op=ALU.mult), reads=[S_atf[r]], writes=[S_sqa[r]])
                    B.op("pool", lambda e: e.tensor_scalar(out=atst[hb][:, qc], in0=atf[r][:], scalar1=gattn_sb[:, h:h + 1], scalar2=None, op0=ALU.mult),
                         reads=[S_atf[r], S_const], writes=[S_atst[hb]])

                def tail_c(h, qb, r):
                    B.pe_group([mm(pbc[:, 256 + t:257 + t], sqa[r][:, t * 128:(t + 1) * 128], ones_b[0:64, 0:1], True, True) for t in range(2)],
                               reads=[S_sqa[r], S_const], writes=[S_pss2])
                    if h == 0:
                        B.op("dve", lambda e: e.tensor_copy(out=ssa[:, 2 * qb:2 * qb + 2], in_=pbc[:, 256:258]), reads=[S_pss2], writes=[S_ssa])
                    else:
                        B.op("dve", lambda e: e.tensor_tensor(out=ssa[:, 2 * qb:2 * qb + 2], in0=pbc[:, 256:258], in1=ssa[:, 2 * qb:2 * qb + 2],
                                                              op=ALU.add),
                             reads=[S_pss2, S_ssa], writes=[S_ssa])

                def blocks_of(h, qb):
                    sB = 15 + qb
                    return list(range(max(0, sB - KEEP[h]), sB + 1))

                load_head(0)
                prep_head(0)
                for qt in range(NQT):
                    mask_a(0, qt)
                    mask_b(0, qt)
                for h in range(NH):
                    hb = h % 2
                    if h + 1 < NH:
                        load_head(h + 1)
                        prep_head(h + 1)
                    total_iter = sum(len(blocks_of(h, qb)) for qb in range(NQB))
                    sched = {}
                    if h + 1 < NH:
                        step = max(1, (total_iter - 8) // NQT)
                        for qt in range(NQT):
                            ia = min(total_iter - 1, 1 + qt * step)
                            ib = min(total_iter - 1, ia + 3)
                            sched.setdefault(ia, []).append(lambda qt=qt: mask_a(h + 1, qt))
                            sched.setdefault(ib, []).append(lambda qt=qt: mask_b(h + 1, qt))
                    it = 0
                    tails = []
                    for qb in range(NQB):
                        js = blocks_of(h, qb)
                        n = len(js)
                        ids = {0: s_mm(h, qb, js[0])}
                        if n > 1:
                            ids[1] = s_mm(h, qb, js[1])
                        for i in range(n):
                            if i + 2 < n:
                                ids[i + 2] = s_mm(h, qb, js[i + 2])
                            pv_mm(h, qb, js[i], ids[i], i == 0, i == n - 1)
                            for fn in sched.pop(it, []):
                                fn()
                            for item in list(tails):
                                if item[0] <= it:
                                    item[1]()
                                    tails.remove(item)
                            it += 1
                        r = tail_a(h, qb)
                        tails.append((it + 5, lambda h=h, qb=qb, r=r: tail_b(h, qb, r)))
                        tails.append((it + 10, lambda h=h, qb=qb, r=r: tail_c(h, qb, r)))
                    for key in sorted(sched):
                        for fn in sched[key]:
                            fn()
                    for item in tails:
                        item[1]()
                    B.dma("sp", at_s[h], atst[hb][:], S_atst[hb], reads=[S_atst[hb]], writes=[S_atd])
                B.emit_block()

        with ExitStack() as p3:
          if PHASES >= 3:
            wo = sb(p3, "wo", [128, 8, D], BF16)
            wu = sb(p3, "wu", [128, 8, 2 * DFF], BF16)
            wd = sb(p3, "wd", [128, 22, D], BF16)
            x1 = [sb(p3, "x1%d" % i, [128, 2, D], F32) for i in range(2)]
            atb = sb(p3, "atb", [128, 4, 256], BF16)
            cgb = sb(p3, "cgb", [128, 4, 256], BF16)
            xn2 = sb(p3, "xn2", [128, 2, D], BF16)
            h2T = [sb(p3, "h2T%d" % i, [128, 8, 258], BF16) for i in range(2)]
            YA = [sb(p3, "YA%d" % i, [128, 256], F32) for i in range(2)]
            YB = [sb(p3, "YB%d" % i, [128, 256], F32) for i in range(2)]
            YV = [sb(p3, "YV%d" % i, [128, 256], F32) for i in range(2)]
            YG = [sb(p3, "YG%d" % i, [128, 256], F32) for i in range(2)]
            actT = sb(p3, "actT", [128, 22, 256], BF16)
            gfb = sb(p3, "gfb", [128, D], F32)
            rsa = sb(p3, "rsa", [128, NQT], F32)
            rsc = sb(p3, "rsc", [128, NQT], F32)
            ssf = sb(p3, "ssf", [128, 2], F32)
            rtf = sb(p3, "rtf", [128, 2], F32)
            rsf = sb(p3, "rsf", [128, 2], F32)
            ssb = sb(p3, "ssb", [128, 2], F32)
            rtb = sb(p3, "rtb", [128, 2], F32)
            rsb = sb(p3, "rsb", [128, 2], F32)
            pu = [ps(p3, "pu%d" % i, [128, 512], F32) for i in range(4)]
            pd = [ps(p3, "pd%d" % i, [128, 512], F32) for i in range(2)]
            pac = [ps(p3, "pac%d" % i, [128, 512], F32) for i in range(2)]
            S_wo, S_wu, S_wd = Slot(), Slot(), Slot()
            S_x1 = [Slot(), Slot()]
            S_atb, S_cgb, S_xn2 = Slot(), Slot(), Slot()
            S_h2T = [Slot(), Slot()]
            S_YA = [Slot() for _ in range(2)]
            S_YB = [Slot() for _ in range(2)]
            S_YV = [Slot(), Slot()]
            S_YG = [Slot(), Slot()]
            S_actT, S_gfb, S_rs = Slot(), Slot(), Slot()
            S_ssf, S_rtf, S_rsf, S_ssb, S_rtb, S_rsb = Slot(), Slot(), Slot(), Slot(), Slot(), Slot()
            S_pu = [Slot() for _ in range(4)]
            S_pd = [Slot(), Slot()]
            S_pac = [Slot(), Slot()]
            gtb = x1[1][:, 0, :]
            S_gtb = S_x1[1]

            wov = w_out.rearrange("(c p) n -> p c n", p=128)
            wuv = w_up.rearrange("(c p) n -> p c n", p=128)
            wdv = w_down.rearrange("(c p) n -> p c n", p=128)
            for c in range(8):
                B.dma("pool", wo[:, c, :], wov[:, c, :], S_wo, writes=[S_wo])
            for c in range(8):
                B.dma("pool", wu[:, c, :], wuv[:, c, :], S_wu, writes=[S_wu])
            for c0 in range(0, 22, 2):
                B.dma("pool", wd[:, c0:c0 + 2, :], wdv[:, c0:c0 + 2, :], S_wd, writes=[S_wd])
            B.dma("sp", gfb[:], gfin, S_gfb, writes=[S_gfb])
            mod_t = mod_s.tensor
            B.dma("sp", gtb, bass.AP(mod_t, 2048, [[0, 128], [1, D]]), S_gtb, reads=[S_mods], writes=[S_gtb])
            for c in range(8):
                B.op("pool", lambda e, c=c: e.tensor_tensor(out=wo[:, c, :], in0=wo[:, c, :], in1=gtb, op=ALU.mult),
                     reads=[S_gtb], writes=[S_wo])
            B.dma("sp", gtb, bass.AP(mod_t, 5120, [[0, 128], [1, D]]), S_gtb, reads=[S_mods], writes=[S_gtb])
            for c in range(22):
                B.op("pool", lambda e, c=c: e.tensor_tensor(out=wd[:, c, :], in0=wd[:, c, :], in1=gtb, op=ALU.mult),
                     reads=[S_gtb], writes=[S_wd])
            for src_, dst, ssl in ((ssa, rsa, S_ssa), (ssc, rsc, S_ssc)):
                B.op("act", lambda e, src_=src_, dst=dst: e.activation(out=dst[:], in_=src_[:], func=AF.Sqrt, scale=1.0 / 512, bias=epsc[:]),
                     reads=[ssl, S_const], writes=[S_rs])
                B.op("dve", lambda e, dst=dst: e.reciprocal(out=dst[:], in_=dst[:]), reads=[S_rs], writes=[S_rs])
            for i in range(2):
                B.op("dve", lambda e, i=i: e.memset(h2T[i][:, :, 0:2], 0.0), writes=[S_h2T[i]])

            xsv = xs.rearrange("(n t p) d -> n p t d", t=2, p=128)
            outv = out.rearrange("(n t p) d -> n p t d", t=2, p=128)
            atv = at_s.rearrange("(pr two) d t -> (two d) pr t", two=2)
            cgv = cg_s.rearrange("c p t -> p c t")
            puc = [0]
            ybc = [0]
            yac = [0]
            tpv = [pd[i][:, :].bitcast(BF16).rearrange("p (c t) -> p c t", t=256) for i in range(2)]

            def next_yb():
                i = ybc[0] % 6
                ybc[0] += 1
                return yb[i], S_yb[i]

            def f3a(qb):
                k = qb % 2
                sl = 15 + qb
                qc = slice(qb * 256, (qb + 1) * 256)
                B.dma("sp", x1[k][:], xsv[sl], S_x1[k], writes=[S_x1[k]])
                B.dma("sp", atb[:], atv[:, :, qc], S_atb, reads=[S_atd], writes=[S_atb])
                B.dma("sp", cgb[:], cgv[:, :, qc], S_cgb, reads=[S_cgd], writes=[S_cgb])
                for t in range(2):
                    tile = 2 * qb + t
                    for n in range(2):
                        ns = slice(n * 512, (n + 1) * 512)
                        B.pe_group([mm(pac[0][:, :], atb[:, c, t * 128:(t + 1) * 128], wo[:, c, ns], c == 0, c == 3) for c in range(4)],
                                   reads=[S_atb, S_wo], writes=[S_pac[0]])
                        B.pe_group([mm(pac[1][:, :], cgb[:, c, t * 128:(t + 1) * 128], wo[:, 4 + c, ns], c == 0, c == 3) for c in range(4)],
                                   reads=[S_cgb, S_wo], writes=[S_pac[1]])
                        B.op("dve", lambda e, t=t, ns=ns, tile=tile: e.scalar_tensor_tensor(
                            out=x1[k][:, t, ns], in0=pac[0][:, :], scalar=rsa[:, tile:tile + 1], in1=x1[k][:, t, ns], op0=ALU.mult, op1=ALU.add),
                            reads=[S_pac[0], S_rs], writes=[S_x1[k]])
                        B.op("dve", lambda e, t=t, ns=ns, tile=tile: e.scalar_tensor_tensor(
                            out=x1[k][:, t, ns], in0=pac[1][:, :], scalar=rsc[:, tile:tile + 1], in1=x1[k][:, t, ns], op0=ALU.mult, op1=ALU.add),
                            reads=[S_pac[1], S_rs], writes=[S_x1[k]])
                for t in range(2):
                    B.op("act", lambda e, t=t: e.activation(out=xn2[:, t, :], in_=x1[k][:, t, :], func=AF.Square, accum_out=ssf[:, t:t + 1]),
                         reads=[S_x1[k]], writes=[S_xn2, S_ssf])
                B.op("act", lambda e: e.activation(out=rtf[:], in_=ssf[:], func=AF.Sqrt, scale=1.0 / D, bias=epsc[:]),
                     reads=[S_ssf, S_const], writes=[S_rtf])
                B.op("dve", lambda e: e.reciprocal(out=rsf[:], in_=rtf[:]), reads=[S_rtf], writes=[S_rsf])
                for t in range(2):
                    B.op("dve", lambda e, t=t: e.tensor_scalar(out=xn2[:, t, :], in0=x1[k][:, t, :], scalar1=rsf[:, t:t + 1], scalar2=None,
                                                               op0=ALU.mult),
                         reads=[S_x1[k], S_rsf], writes=[S_xn2])

            def f3b(qb):
                k = qb % 2
                for half in range(2):
                    B.pe_group([tr(tpv[half][:, c, t * 128:(t + 1) * 128], xn2[:, t, (half * 4 + c) * 128:(half * 4 + c + 1) * 128], ident[:])
                                for t in range(2) for c in range(4)], reads=[S_xn2, S_const], writes=[S_pd[half]])
                    for c in range(4):
                        cc = half * 4 + c
                        B.op("act", lambda e, c=c, cc=cc, half=half: e.activation(out=h2T[k][:, cc, 2:258], in_=tpv[half][:, c, :],
                                                                                 func=AF.Identity, scale=G2[:, cc:cc + 1], bias=sh2[:, cc:cc + 1]),
                             reads=[S_pd[half], S_G], writes=[S_h2T[k]])
                if qb == 1:
                    B.op("dve", lambda e: e.tensor_scalar(out=h2T[k][:, :, 0:2], in0=h2T[1 - k][:, :, 256:258], scalar1=hv[:, 0:1],
                                                          scalar2=None, op0=ALU.mult),
                         reads=[S_h2T[1 - k], S_const], writes=[S_h2T[k]])
                elif qb > 1:
                    B.op("dve", lambda e: e.tensor_copy(out=h2T[k][:, :, 0:2], in_=h2T[1 - k][:, :, 256:258]),
                         reads=[S_h2T[1 - k]], writes=[S_h2T[k]])

            def back3(qb):
                k = qb % 2
                for v in range(22):
                    if qb + 1 < NQB and v == 3:
                        f3a(qb + 1)
                    if qb + 1 < NQB and v == 14:
                        f3b(qb + 1)
                    p2_ = v % 2
                    chs = (v, 22 + v)
                    pis = []
                    for ch in chs:
                        i = puc[0] % 4
                        puc[0] += 1
                        pis.append(i)
                        B.pe_group([mm(pu[i][:, 0:258], wu[:, c, ch * 128:(ch + 1) * 128], h2T[k][:, c, 0:258], c == 0, c == 7) for c in range(8)],
                                   reads=[S_wu, S_h2T[k]], writes=[S_pu[i]])
                    yas = []
                    for w_, (ch, i) in enumerate(zip(chs, pis)):
                        ai = yac[0] % 2
                        yac[0] += 1
                        yas.append(ai)
                        B.op("act", lambda e, i=i, ai=ai, ch=ch: e.activation(out=YA[ai][:], in_=pu[i][:, 2:258], func=AF.Identity,
                                                                             scale=fcw_sb[:, 88 + ch:89 + ch], bias=fcb_sb[:, ch:ch + 1]),
                             reads=[S_pu[i], S_const], writes=[S_YA[ai]])
                    ybs = []
                    for w_, (ch, i) in enumerate(zip(chs, pis)):
                        bi = ybc[0] % 2
                        ybc[0] += 1
                        ybs.append(bi)
                        ai = yas[w_]
                        B.op("dve", lambda e, i=i, ai=ai, bi=bi, ch=ch: e.scalar_tensor_tensor(
                            out=YB[bi][:], in0=pu[i][:, 1:257], scalar=fcw_sb[:, 44 + ch:45 + ch], in1=YA[ai][:], op0=ALU.mult, op1=ALU.add),
                            reads=[S_pu[i], S_YA[ai], S_const], writes=[S_YB[bi]])
                    outs = ((YV[p2_], S_YV[p2_]), (YG[p2_], S_YG[p2_]))
                    for w_, (ch, i) in enumerate(zip(chs, pis)):
                        bi = ybs[w_]
                        yo, syo = outs[w_]
                        B.op("dve", lambda e, i=i, bi=bi, yo=yo, ch=ch: e.scalar_tensor_tensor(
                            out=yo[:], in0=pu[i][:, 0:256], scalar=fcw_sb[:, ch:ch + 1], in1=YB[bi][:], op0=ALU.mult, op1=ALU.add),
                            reads=[S_pu[i], S_YB[bi], S_const], writes=[syo])
                    B.op("act", lambda e, p2_=p2_: e.activation(out=YG[p2_][:], in_=YG[p2_][:], func=AF.Silu), reads=[S_YG[p2_]], writes=[S_YG[p2_]])
                    B.op("pool", lambda e, p2_=p2_, v=v: e.tensor_tensor(out=actT[:, v, :], in0=YV[p2_][:], in1=YG[p2_][:], op=ALU.mult),
                         reads=[S_YV[p2_], S_YG[p2_]], writes=[S_actT])
                for t in range(2):
                    for n in range(2):
                        B.pe_group([mm(pd[n][:, :], actT[:, v, t * 128:(t + 1) * 128], wd[:, v, n * 512:(n + 1) * 512], v == 0, v == 21)
                                    for v in range(22)], reads=[S_actT, S_wd], writes=[S_pd[n]])
                        B.op("dve", lambda e, t=t, n=n: e.tensor_tensor(out=x1[k][:, t, n * 512:(n + 1) * 512], in0=pd[n][:, :],
                                                                        in1=x1[k][:, t, n * 512:(n + 1) * 512], op=ALU.add),
                             reads=[S_pd[n]], writes=[S_x1[k]])
                for t in range(2):
                    B.op("act", lambda e, t=t: e.activation(out=xn2[:, t, :], in_=x1[k][:, t, :], func=AF.Square, accum_out=ssb[:, t:t + 1]),
                         reads=[S_x1[k]], writes=[S_xn2, S_ssb])
                B.op("act", lambda e: e.activation(out=rtb[:], in_=ssb[:], func=AF.Sqrt, scale=1.0 / D, bias=epsc[:]),
                     reads=[S_ssb, S_const], writes=[S_rtb])
                B.op("dve", lambda e: e.reciprocal(out=rsb[:], in_=rtb[:]), reads=[S_rtb], writes=[S_rsb])
                for t in range(2):
                    B.op("dve", lambda e, t=t: e.scalar_tensor_tensor(out=x1[k][:, t, :], in0=x1[k][:, t, :], scalar=rsb[:, t:t + 1],
                                                                      in1=gfb[:], op0=ALU.mult, op1=ALU.mult),
                         reads=[S_rsb, S_gfb], writes=[S_x1[k]])
                B.dma("sp", outv[qb - 1], x1[k][:], S_x1[k], reads=[S_x1[k]])

            f3a(0)
            f3b(0)
            f3a(1)
            f3b(1)
            for qb in range(1, NQB):
                back3(qb)
            B.emit_block()
    return nc


_NC_CACHE = {}


def _consts(half):
    bf = ml_dtypes.bfloat16
    ident = np.eye(128, dtype=np.float32).astype(bf)
    key = np.arange(S)
    kstat = np.zeros((35, S), np.float32)
    kstat[key // 256, key] = 1.0
    kstat[32] = key % 256
    kstat[33] = key // 256
    kstat[34] = 1.0
    kstat = kstat.astype(bf)
    p = np.arange(128)[:, None, None]
    kt = np.arange(2)[None, :, None]
    a = np.arange(256)[None, None, :]
    cmask = np.where(kt * 128 + p <= a, 0.0, NEG).astype(np.float32).reshape(128, 512).astype(bf)
    gvb = np.zeros((NQB, 32), np.float32)
    gvb2 = np.zeros((NQB, 32), np.float32)
    for qb in range(NQB):
        sB = 15 + qb
        for j in range(32):
            valid = (j < sB) and (half == 1 or j >= 16)
            gvb[qb, j] = 0.0 if valid else -1.0e9
            gvb2[qb, j] = NEG if valid else 2 * NEG
        gvb[qb, sB] = -3.0e9
        gvb2[qb, sB] = 0.0
    gvb = np.ascontiguousarray(np.broadcast_to(gvb.reshape(1, -1), (128, NQB * 32))).astype(np.float32)
    gvb2 = np.ascontiguousarray(np.broadcast_to(gvb2.reshape(1, -1), (128, NQB * 32))).astype(np.float32)
    qstat = np.zeros((NH, 128, NQT, 99), np.float32)
    for h in range(NH):
        slope = 2.0 ** (-(h + 1))
        for qt in range(NQT):
            sB = 15 + qt // 2
            apos = (qt % 2) * 128 + np.arange(128)
            qstat[h, :, qt, 96] = slope
            qstat[h, :, qt, 97] = 256.0 * slope
            qstat[h, :, qt, 98] = -slope * (256.0 * sB + apos)
    qstat = qstat.reshape(NH, 128, NQT * 99).astype(bf)
    hv = np.full((128, 1), float(half), np.float32)
    return dict(ident=ident, kstat=kstat, cmask=cmask, gvb=gvb, gvb2=gvb2, qstat=qstat, hv=hv)


def _pc(v, nchunk):
    return np.ascontiguousarray(np.asarray(v, np.float32).reshape(nchunk, 128).T)


def kernel(x, c, w_ada, b_ada, g_mix, w_in, conv_w, conv_b, g_attn_out, g_conv_out, w_out, g_ffn,
           w_up, ffn_conv_w, ffn_conv_b, w_down, g_final):
    f = lambda a: np.ascontiguousarray(np.asarray(a, dtype=np.float32))
    x = f(x)
    c = f(c)
    if "nc" not in _NC_CACHE:
        _NC_CACHE["nc"] = build_program()
    nc = _NC_CACHE["nc"]
    shared = dict(
        w_ada=f(w_ada[0]), b_ada=f(b_ada[0]).reshape(1, -1), gmix=_pc(g_mix[0], 8), w_in=f(w_in[0]),
        convw=np.ascontiguousarray(np.concatenate([_pc(conv_w[0][kk], 4) for kk in range(3)], axis=1)),
        convb=_pc(conv_b[0], 4),
        gattn=np.ascontiguousarray(f(g_attn_out[0]).reshape(8, 64).T),
        gconv=_pc(g_conv_out[0], 4), w_out=f(w_out[0]), gffn=_pc(g_ffn[0], 8), w_up=f(w_up[0]),
        fcw=np.ascontiguousarray(np.concatenate([_pc(ffn_conv_w[0][kk], 44) for kk in range(3)], axis=1)),
        fcb=_pc(ffn_conv_b[0], 44), w_down=f(w_down[0]),
        gfin=np.ascontiguousarray(np.broadcast_to(f(g_final).reshape(1, -1), (128, D))),
    )
    cst = [_consts(0), _consts(1)]
    in_maps = []
    for i in range(NCORES):
        b, half = i // 2, i % 2
        if half == 1:
            xs = x[b]
        else:
            xs = np.concatenate([np.zeros((4096, D), np.float32), x[b, :4096]], axis=0)
        m = dict(shared)
        m.update(cst[half])
        m["xs"] = np.ascontiguousarray(xs)
        m["cT"] = _pc(c[b], 8)
        in_maps.append(m)
    res = run_bass_kernel_spmd(nc, in_maps, core_ids=list(range(NCORES)))
    outp = np.empty((4, S, D), np.float32)
    for i in range(NCORES):
        b, half = i // 2, i % 2
        outp[b, half * 4096:(half + 1) * 4096] = res.results[i]["out"]
    return outp
```

```python
import numpy as np
from contextlib import ExitStack
import ml_dtypes
import concourse.bass as bass
import concourse.mybir as mybir
from concourse.bass_utils import run_bass_kernel_spmd

F32 = mybir.dt.float32
BF16 = mybir.dt.bfloat16
AF = mybir.ActivationFunctionType
ALU = mybir.AluOpType
AX = mybir.AxisListType

D = 1024
S = 8192
NH = 8
DFF = 2816
NQB = 17
NQT = 34
QTOK = NQB * 256
EPS = 1e-6
NEG = -30000.0
NCORES = 8
PHASES = 3
DEBUG = False


class Tok:
    __slots__ = ("sem", "val")

    def __init__(self, sem, val):
        self.sem = sem
        self.val = val


class Slot:
    def __init__(self, name=""):
        self.name = name
        self.w = None
        self.r = {}
        self.dsem = None
        self.dcnt = 0


class Builder:
    ENG = ("pe", "act", "dve", "pool", "sp")

    def __init__(self, nc, es):
        self.nc = nc
        self.es = es
        self.q = {}
        for n in self.ENG:
            sem = es.enter_context(nc.semaphore("sem_" + n))
            self.q[n] = dict(sem=sem, cnt=0, ops=[], waited={})
        self.dslots = []

    def _waits(self, en, deps):
        q = self.q[en]
        need = {}
        for t in deps:
            if t is None:
                continue
            if en == "pe" and t.sem is q["sem"]:
                continue
            k = id(t.sem)
            if q["waited"].get(k, 0) >= t.val:
                continue
            if k not in need or need[k].val < t.val:
                need[k] = t
        for k, t in need.items():
            q["waited"][k] = t.val
        return [(t.sem, t.val) for t in need.values()]

    @staticmethod
    def _deps(reads, writes, deps):
        d = list(deps)
        for s in reads:
            d.append(s.w)
        for s in writes:
            d.extend(s.r.values())
            d.append(s.w)
        return d

    @staticmethod
    def _update(tok, reads, writes):
        for s in writes:
            s.w = tok
            s.r = {}
        for s in reads:
            k = id(tok.sem)
            if k not in s.r or s.r[k].val < tok.val:
                s.r[k] = tok

    def op(self, en, fn, reads=(), writes=(), deps=(), inc=True):
        q = self.q[en]
        waits = self._waits(en, self._deps(reads, writes, deps))
        tok = None
        if inc:
            q["cnt"] += 1
            tok = Tok(q["sem"], q["cnt"])
        else:
            assert not reads and not writes
        sem = q["sem"]

        def emit(e, waits=waits, fn=fn, inc=inc, sem=sem):
            for (sm, v) in waits:
                e.wait_ge(sm, v)
            ins = fn(e)
            if inc:
                ins.then_inc(sem, 1)

        q["ops"].append(emit)
        if tok is not None:
            self._update(tok, reads, writes)
        return tok

    def pe_group(self, fns, reads=(), writes=()):
        q = self.q["pe"]
        waits = self._waits("pe", self._deps(reads, writes, ()))
        q["cnt"] += 1
        tok = Tok(q["sem"], q["cnt"])
        sem = q["sem"]
        n = len(fns)

        def emit(e, waits=waits, fns=fns, sem=sem, n=n):
            for (sm, v) in waits:
                e.wait_ge(sm, v)
            for i, f in enumerate(fns):
                ins = f(e)
                if i == n - 1:
                    ins.then_inc(sem, 1)

        q["ops"].append(emit)
        self._update(tok, reads, writes)
        return tok

    def dma(self, en, out, in_, sem_slot, reads=(), writes=(), deps=(), **kw):
        q = self.q[en]
        sl = sem_slot
        if sl.dsem is None:
            sl.dsem = self.es.enter_context(self.nc.semaphore("dsem%d" % len(self.dslots)))
            self.dslots.append(sl)
        waits = self._waits(en, self._deps(reads, writes, deps))
        sl.dcnt += 16
        tok = Tok(sl.dsem, sl.dcnt)
        dsem = sl.dsem

        def emit(e, waits=waits, out=out, in_=in_, dsem=dsem, kw=kw):
            for (sm, v) in waits:
                e.wait_ge(sm, v)
            e.dma_start(out=out, in_=in_, **kw).then_inc(dsem, 16)

        q["ops"].append(emit)
        self._update(tok, reads, writes)
        return tok

    def emit_block(self):
        nc = self.nc
        finals = [(s.dsem, s.dcnt) for s in self.dslots if s.dcnt > 0]
        pe_fin = (self.q["pe"]["sem"], self.q["pe"]["cnt"])

        def fin(e, finals=finals):
            for sm, v in finals:
                e.wait_ge(sm, v)

        self.q["sp"]["ops"].append(fin)
        with nc.Block() as blk:
            for n, meth in (("pe", blk.tensor), ("act", blk.scalar), ("dve", blk.vector),
                            ("pool", blk.gpsimd), ("sp", blk.sync)):
                ops = self.q[n]["ops"]
                if ops:
                    def body(e, ops=ops):
                        for f in ops:
                            f(e)
                    meth(body)
                self.q[n]["ops"] = []


def mm(out, lhsT, rhs, start, stop):
    return lambda e: e.matmul(out, lhsT=lhsT, rhs=rhs, start=start, stop=stop)


def tr(out, in_, ident):
    return lambda e: e.transpose(out, in_, ident)


def build_program():
    nc = bass.Bass("TRN2", target_bir_lowering=False)

    def din(name, shape, dt=F32):
        return nc.dram_tensor(name, list(shape), dt, kind="ExternalInput").ap()

    def dscr(name, shape, dt):
        return nc.dram_tensor(name, list(shape), dt, kind="ExternalOutput" if DEBUG else "Internal").ap()

    xs = din("xs", [S, D])
    cT = din("cT", [128, 8])
    w_ada = din("w_ada", [D, 6 * D])
    b_ada = din("b_ada", [1, 6 * D])
    gmix = din("gmix", [128, 8])
    w_in = din("w_in", [D, 3072])
    convw = din("convw", [128, 12])
    convb = din("convb", [128, 4])
    gattn = din("gattn", [64, 8])
    gconv = din("gconv", [128, 4])
    w_out = din("w_out", [D, D])
    gffn = din("gffn", [128, 8])
    w_up = din("w_up", [D, 2 * DFF])
    fcw = din("fcw", [128, 132])
    fcb = din("fcb", [128, 44])
    w_down = din("w_down", [DFF, D])
    gfin = din("gfin", [128, D])
    ident_d = din("ident", [128, 128], BF16)
    kstat = din("kstat", [35, S], BF16)
    cmask = din("cmask", [128, 512], BF16)
    gvb_d = din("gvb", [128, NQB * 32])
    gvb2_d = din("gvb2", [128, NQB * 32])
    qstat = din("qstat", [NH, 128, NQT * 99], BF16)
    hv_d = din("hv", [128, 1])
    out = nc.dram_tensor("out", [4096, D], F32, kind="ExternalOutput").ap()

    kT_s = dscr("kT_s", [NH, 64, S], BF16)
    qT_s = dscr("qT_s", [NH, 64, QTOK], BF16)
    at_s = dscr("at_s", [NH, 64, QTOK], BF16)
    cg_s = dscr("cg_s", [4, 128, QTOK], BF16)
    mod_s = dscr("mod_s", [1, 6 * D], F32)
    km_s = dscr("km_s", [NH, 64, 32], F32)

    es = ExitStack()
    with es:
        B = Builder(nc, es)

        def sb(st, name, shape, dt):
            return st.enter_context(nc.sbuf_tensor("sb_" + name, list(shape), dt))

        def ps(st, name, shape, dt):
            return st.enter_context(nc.psum_tensor("ps_" + name, list(shape), dt))

        ident = sb(es, "ident", [128, 128], BF16)
        ones_b = sb(es, "ones_b", [128, 1], BF16)
        ones_f = sb(es, "ones_f", [128, 64], F32)
        epsc = sb(es, "epsc", [128, 1], F32)
        hv = sb(es, "hv", [128, 1], F32)
        G1 = sb(es, "G1", [128, 8], F32)
        G2 = sb(es, "G2", [128, 8], F32)
        modT = sb(es, "modT", [128, 48], F32)
        gmix_sb = sb(es, "gmix_sb", [128, 8], F32)
        gffn_sb = sb(es, "gffn_sb", [128, 8], F32)
        convw_sb = sb(es, "convw_sb", [128, 12], F32)
        convb_sb = sb(es, "convb_sb", [128, 4], F32)
        gconv_sb = sb(es, "gconv_sb", [128, 4], F32)
        gattn_sb = sb(es, "gattn_sb", [64, 8], F32)
        fcw_sb = sb(es, "fcw_sb", [128, 132], F32)
        fcb_sb = sb(es, "fcb_sb", [128, 44], F32)
        ssa = sb(es, "ssa", [128, NQT], F32)
        ssc = sb(es, "ssc", [128, NQT], F32)
        wo = sb(es, "wo", [128, 8, D], BF16)
        S_wo = Slot("wo")
        S_const = Slot("const")
        S_ssa = Slot("ssa")
        S_ssc = Slot("ssc")
        S_G = Slot("G")
        S_mods = Slot("mods")

        for dst, src in ((ident, ident_d), (hv, hv_d), (gmix_sb, gmix), (gffn_sb, gffn), (convw_sb, convw),
                         (convb_sb, convb), (gconv_sb, gconv), (gattn_sb, gattn), (fcw_sb, fcw), (fcb_sb, fcb)):
            B.dma("sp", dst[:], src, S_const, writes=[S_const])
        B.op("dve", lambda e: e.memset(ones_b[:], 1.0), writes=[S_const])
        B.op("dve", lambda e: e.memset(ones_f[:], 1.0), writes=[S_const])
        B.op("dve", lambda e: e.memset(epsc[:], EPS), writes=[S_const])

        with ExitStack() as p0:
            c_sb = sb(p0, "c_sb", [128, 8], F32)
            sc_b = sb(p0, "sc_b", [128, 8], BF16)
            brow = sb(p0, "brow", [1, 6 * D], F32)
            modrow = sb(p0, "modrow", [1, 6 * D], F32)
            wa = [sb(p0, "wa%d" % i, [128, 8, 512], BF16) for i in range(2)]
            pm = [ps(p0, "pm%d" % i, [128, 512], F32) for i in range(2)]
            S_c, S_scb, S_brow, S_modrow, S_modT = Slot(), Slot(), Slot(), Slot(), Slot()
            S_wa = [Slot(), Slot()]
            S_pm = [Slot(), Slot()]
            B.dma("sp", c_sb[:], cT, S_c, writes=[S_c])
            B.dma("sp", brow[:], b_ada, S_brow, writes=[S_brow])
            B.op("act", lambda e: e.activation(out=sc_b[:], in_=c_sb[:], func=AF.Silu), reads=[S_c], writes=[S_scb])
            wav = w_ada.rearrange("(c p) n -> p c n", p=128)
            for g in range(12):
                k = g % 2
                B.dma("pool", wa[k][:], wav[:, :, g * 512:(g + 1) * 512], S_wa[k], writes=[S_wa[k]])
                B.pe_group([mm(pm[k][0:1, :], sc_b[:, c:c + 1], wa[k][:, c, :], c == 0, c == 7) for c in range(8)],
                           reads=[S_wa[k], S_scb], writes=[S_pm[k]])
                B.op("dve", lambda e, k=k, g=g: e.tensor_tensor(out=modrow[0:1, g * 512:(g + 1) * 512], in0=pm[k][0:1, :],
                                                                 in1=brow[0:1, g * 512:(g + 1) * 512], op=ALU.add),
                     reads=[S_pm[k], S_brow], writes=[S_modrow])
            B.dma("sp", mod_s, modrow[:], S_mods, reads=[S_modrow], writes=[S_mods])
            B.dma("sp", modT[:], mod_s.rearrange("o (k p) -> (o p) k", p=128), S_modT, reads=[S_mods], writes=[S_modT],
                  allow_slow_non_contiguous=True)
            B.op("dve", lambda e: e.scalar_tensor_tensor(out=G1[:], in0=modT[:, 8:16], scalar=1.0, in1=gmix_sb[:],
                                                         op0=ALU.add, op1=ALU.mult), reads=[S_modT, S_const], writes=[S_G])
            B.op("dve", lambda e: e.scalar_tensor_tensor(out=G2[:], in0=modT[:, 32:40], scalar=1.0, in1=gffn_sb[:],
                                                         op0=ALU.add, op1=ALU.mult), reads=[S_modT, S_const], writes=[S_G])
            B.emit_block()
        sh1 = modT[:, 0:8]
        sh2 = modT[:, 24:32]

        with ExitStack() as p12:
          if PHASES >= 1:
            V_all = sb(p12, "V_all", [128, 64, NH, 65], BF16)
            S_V = Slot("V")
            with ExitStack() as p1:
                w_in_sb = sb(p1, "w_in_sb", [128, 8, 3072], BF16)
                S_win = Slot("win")
                wiv = w_in.rearrange("(c p) n -> p c n", p=128)
                for c in range(8):
                    B.dma("pool", w_in_sb[:, c, :], wiv[:, c, :], S_win, writes=[S_win])
                wov = w_out.rearrange("(c p) n -> p c n", p=128)
                for c in range(8):
                    B.dma("pool", wo[:, c, :], wov[:, c, :], S_wo, writes=[S_wo])
                xt = [sb(p1, "xt%d" % i, [128, 2, D], F32) for i in range(2)]
                xn = [sb(p1, "xn%d" % i, [128, 2, D], BF16) for i in range(2)]
                hT = [sb(p1, "hT%d" % i, [128, 8, 258], BF16) for i in range(2)]
                junk = sb(p1, "junk", [128, D], BF16)
                ss = [sb(p1, "ss%d" % i, [128, 2], F32) for i in range(2)]
                rt = [sb(p1, "rt%d" % i, [128, 2], F32) for i in range(2)]
                rstd = [sb(p1, "rstd%d" % i, [128, 2], F32) for i in range(2)]
                kst = [sb(p1, "kst%d" % i, [128, 4, 256], BF16) for i in range(2)]
                qst = [sb(p1, "qst%d" % i, [128, 4, 256], BF16) for i in range(2)]
                cgs = [sb(p1, "cgs%d" % i, [128, 4, 256], BF16) for i in range(2)]
                sqc = sb(p1, "sqc", [128, 4, 256], BF16)
                kmst = sb(p1, "kmst", [128, 4, 32], F32)
                S_kmst = Slot()
                S_kmd = Slot("km_s")
                u_sb = [sb(p1, "u_sb%d" % i, [128, 258], F32) for i in range(2)]
                zb = [sb(p1, "zb%d" % i, [128, 258], F32) for i in range(2)]
                y0 = [sb(p1, "y0%d" % i, [128, 256], F32) for i in range(2)]
                y1 = [sb(p1, "y1%d" % i, [128, 256], F32) for i in range(2)]
                y2 = [sb(p1, "y2%d" % i, [128, 256], F32) for i in range(2)]
                cv = [sb(p1, "cv%d" % i, [128, 256], F32) for i in range(2)]
                tp = ps(p1, "tp", [128, 8, 256], BF16)
                pp = [ps(p1, "pp%d" % i, [128, 512], F32) for i in range(5)]
                pss = ps(p1, "pss", [128, 512], F32)
                S_xt = [Slot(), Slot()]
                S_xn = [Slot(), Slot()]
                S_hT = [Slot(), Slot()]
                S_junk = Slot()
                S_ss = [Slot(), Slot()]
                S_rt = [Slot(), Slot()]
                S_rstd = [Slot(), Slot()]
                S_kst = [Slot(), Slot()]
                S_qst = [Slot(), Slot()]
                S_cgs = [Slot(), Slot()]
                S_sqc = Slot()
                S_u = [Slot(), Slot()]
                S_z = [Slot(), Slot()]
                S_y0 = [Slot(), Slot()]
                S_y1 = [Slot(), Slot()]
                S_y2 = [Slot(), Slot()]
                S_cv = [Slot(), Slot()]
                S_tp = Slot()
                S_pp = [Slot() for _ in range(5)]
                S_pss = Slot()
                S_kT = Slot("kT_s")
                S_qT = Slot("qT_s")
                S_cgd = Slot("cg_s")
                ppi = [0]

                def next_pp():
                    i = ppi[0] % 5
                    ppi[0] += 1
                    return pp[i], S_pp[i]

                B.op("pool", lambda e: e.memset(V_all[:, :, :, 64:65], 1.0), writes=[S_V])
                for i in range(2):
                    B.op("pool", lambda e, i=i: e.memset(hT[i][:, :, 0:2], 0.0), writes=[S_hT[i]])
                xsv = xs.rearrange("(n t p) d -> n p t d", t=2, p=128)
                kTv = kT_s.rearrange("(pr two) d t -> (two d) pr t", two=2)
                qTv = qT_s.rearrange("(pr two) d t -> (two d) pr t", two=2)
                cgv = cg_s.rearrange("c p t -> p c t")
                evac_rr = [0]

                def evac(out_ap, in_ap, reads, writes, scale=None):
                    evac_rr[0] += 1
                    if evac_rr[0] % 2 == 0:
                        if scale is None:
                            return B.op("act", lambda e: e.activation(out=out_ap, in_=in_ap, func=AF.Copy), reads=reads, writes=writes)
                        return B.op("act", lambda e: e.activation(out=out_ap, in_=in_ap, func=AF.Copy, scale=scale), reads=reads, writes=writes)
                    if scale is None:
                        return B.op("dve", lambda e: e.tensor_copy(out=out_ap, in_=in_ap), reads=reads, writes=writes)
                    return B.op("dve", lambda e: e.tensor_scalar(out=out_ap, in0=in_ap, scalar1=scale, scalar2=None, op0=ALU.mult),
                                reads=reads, writes=writes)

                def f1a(s):
                    k = s % 2
                    B.dma("sp", xt[k][:], xsv[s], S_xt[k], writes=[S_xt[k]])
                    for t in range(2):
                        B.op("act", lambda e, k=k, t=t: e.activation(out=junk[:], in_=xt[k][:, t, :], func=AF.Square,
                                                                     accum_out=ss[k][:, t:t + 1]),
                             reads=[S_xt[k]], writes=[S_junk, S_ss[k]])
                    B.op("act", lambda e, k=k: e.activation(out=rt[k][:], in_=ss[k][:], func=AF.Sqrt, scale=1.0 / D, bias=epsc[:]),
                         reads=[S_ss[k], S_const], writes=[S_rt[k]])
                    B.op("dve", lambda e, k=k: e.reciprocal(out=rstd[k][:], in_=rt[k][:]), reads=[S_rt[k]], writes=[S_rstd[k]])
                    for t in range(2):
                        B.op("dve", lambda e, k=k, t=t: e.tensor_scalar(out=xn[k][:, t, :], in0=xt[k][:, t, :], scalar1=rstd[k][:, t:t + 1],
                                                                        scalar2=None, op0=ALU.mult),
                             reads=[S_xt[k], S_rstd[k]], writes=[S_xn[k]])

                def f1b(s):
                    k = s % 2
                    B.pe_group([tr(tp[:, c, t * 128:(t + 1) * 128], xn[k][:, t, c * 128:(c + 1) * 128], ident[:])
                                for t in range(2) for c in range(8)], reads=[S_xn[k], S_const], writes=[S_tp])
                    for c in range(8):
                        if c % 2 == 0:
                            B.op("act", lambda e, k=k, c=c: e.activation(out=hT[k][:, c, 2:258], in_=tp[:, c, :], func=AF.Identity,
                                                                         scale=G1[:, c:c + 1], bias=sh1[:, c:c + 1]),
                                 reads=[S_tp, S_G], writes=[S_hT[k]])
                        else:
                            B.op("dve", lambda e, k=k, c=c: e.tensor_scalar(out=hT[k][:, c, 2:258], in0=tp[:, c, :], scalar1=G1[:, c:c + 1],
                                                                            scalar2=sh1[:, c:c + 1], op0=ALU.mult, op1=ALU.add),
                                 reads=[S_tp, S_G], writes=[S_hT[k]])
                    if s == 16:
                        B.op("dve", lambda e, k=k: e.tensor_scalar(out=hT[k][:, :, 0:2], in0=hT[1 - k][:, :, 256:258], scalar1=hv[:, 0:1],
                                                                   scalar2=None, op0=ALU.mult),
                             reads=[S_hT[1 - k], S_const], writes=[S_hT[k]])
                    elif s > 16:
                        B.op("dve", lambda e, k=k: e.tensor_copy(out=hT[k][:, :, 0:2], in_=hT[1 - k][:, :, 256:258]),
                             reads=[S_hT[1 - k]], writes=[S_hT[k]])

                def back1a(s):
                    k = s % 2
                    for pr in range(4):
                        pt_, sp_ = next_pp()
                        B.pe_group([mm(pt_[:, 0:256], w_in_sb[:, c, 512 + pr * 128:512 + (pr + 1) * 128], hT[k][:, c, 2:258], c == 0, c == 7)
                                    for c in range(8)], reads=[S_win, S_hT[k]], writes=[sp_])
                        evac(kst[k][:, pr, :], pt_[:, 0:256], [sp_], [S_kst[k]])
                        B.op("dve", lambda e, k=k, pr=pr, s=s: e.tensor_reduce(out=kmst[:, pr, s:s + 1], in_=kst[k][:, pr, :], axis=AX.X, op=ALU.add),
                             reads=[S_kst[k]], writes=[S_kmst])
                    B.dma("sp", kTv[:, :, s * 256:(s + 1) * 256], kst[k][:], S_kst[k], reads=[S_kst[k]], writes=[S_kT])
                    for t in range(2):
                        pt_, sp_ = next_pp()
                        B.pe_group([mm(pt_[:, :], hT[k][:, c, 2 + t * 128:2 + (t + 1) * 128], w_in_sb[:, c, 1024:1536], c == 0, c == 7)
                                    for c in range(8)], reads=[S_win, S_hT[k]], writes=[sp_])
                        evac(V_all[:, 2 * s + t, :, 0:64], pt_[:, :].rearrange("p (h d) -> p h d", d=64), [sp_], [S_V])

                def back1b(s):
                    k = s % 2
                    if s < 15:
                        return
                    qb = s - 15
                    for pr in range(4):
                        pt_, sp_ = next_pp()
                        B.pe_group([mm(pt_[:, 0:256], w_in_sb[:, c, pr * 128:(pr + 1) * 128], hT[k][:, c, 2:258], c == 0, c == 7)
                                    for c in range(8)], reads=[S_win, S_hT[k]], writes=[sp_])
                        evac(qst[k][:, pr, :], pt_[:, 0:256], [sp_], [S_qst[k]], scale=0.125)
                    B.dma("sp", qTv[:, :, qb * 256:(qb + 1) * 256], qst[k][:], S_qst[k], reads=[S_qst[k]], writes=[S_qT])
                    for ch in range(4):
                        j = ch % 2
                        pu_, spu = next_pp()
                        B.pe_group([mm(pu_[:, 0:258], w_in_sb[:, c, 1536 + ch * 128:1536 + (ch + 1) * 128], hT[k][:, c, 0:258], c == 0, c == 7)
                                    for c in range(8)], reads=[S_win, S_hT[k]], writes=[spu])
                        pc_, spc = next_pp()
                        B.pe_group([mm(pc_[:, 0:258], w_in_sb[:, c, 2048 + ch * 128:2048 + (ch + 1) * 128], hT[k][:, c, 0:258], c == 0, c == 7)
                                    for c in range(8)], reads=[S_win, S_hT[k]], writes=[spc])
                        pb_, spb = next_pp()
                        B.pe_group([mm(pb_[:, 0:256], w_in_sb[:, c, 2560 + ch * 128:2560 + (ch + 1) * 128], hT[k][:, c, 2:258], c == 0, c == 7)
                                    for c in range(8)], reads=[S_win, S_hT[k]], writes=[spb])
                        B.op("act", lambda e, j=j, pu_=pu_: e.activation(out=u_sb[j][:], in_=pu_[:, 0:258], func=AF.Copy),
                             reads=[spu], writes=[S_u[j]])
                        B.op("dve", lambda e, j=j, pc_=pc_: e.tensor_tensor(out=zb[j][:], in0=pc_[:, 0:258], in1=u_sb[j][:], op=ALU.mult),
                             reads=[spc, S_u[j]], writes=[S_z[j]])
                        B.op("dve", lambda e, j=j, ch=ch: e.tensor_scalar(out=y0[j][:], in0=zb[j][:, 2:258], scalar1=convw_sb[:, 8 + ch:9 + ch],
                                                                          scalar2=convb_sb[:, ch:ch + 1], op0=ALU.mult, op1=ALU.add),
                             reads=[S_z[j], S_const], writes=[S_y0[j]])
                        B.op("dve", lambda e, j=j, ch=ch: e.scalar_tensor_tensor(out=y1[j][:], in0=zb[j][:, 1:257], scalar=convw_sb[:, 4 + ch:5 + ch],
                                                                                 in1=y0[j][:], op0=ALU.mult, op1=ALU.add),
                             reads=[S_z[j], S_y0[j], S_const], writes=[S_y1[j]])
                        B.op("dve", lambda e, j=j, ch=ch: e.scalar_tensor_tensor(out=y2[j][:], in0=zb[j][:, 0:256], scalar=convw_sb[:, ch:ch + 1],
                                                                                 in1=y1[j][:], op0=ALU.mult, op1=ALU.add),
                             reads=[S_z[j], S_y1[j], S_const], writes=[S_y2[j]])
                        B.op("dve", lambda e, j=j, pb_=pb_: e.tensor_tensor(out=cv[j][:], in0=pb_[:, 0:256], in1=y2[j][:], op=ALU.mult),
                             reads=[spb, S_y2[j]], writes=[S_cv[j]])
                        B.op("act", lambda e, j=j, ch=ch: e.activation(out=sqc[:, ch, :], in_=cv[j][:], func=AF.Square),
                             reads=[S_cv[j]], writes=[S_sqc])
                        B.op("act", lambda e, j=j, ch=ch, k=k: e.activation(out=cgs[k][:, ch, :], in_=cv[j][:], func=AF.Copy,
                                                                           scale=gconv_sb[:, ch:ch + 1]),
                             reads=[S_cv[j], S_const], writes=[S_cgs[k]])
                    for t in range(2):
                        B.pe_group([mm(pss[:, t:t + 1], sqc[:, ch, t * 128:(t + 1) * 128], ones_b[:, 0:1], ch == 0, ch == 3) for ch in range(4)],
                                   reads=[S_sqc, S_const], writes=[S_pss])
                    B.op("dve", lambda e, qb=qb: e.tensor_copy(out=ssc[:, 2 * qb:2 * qb + 2], in_=pss[:, 0:2]), reads=[S_pss], writes=[S_ssc])
                    B.dma("sp", cgv[:, :, qb * 256:(qb + 1) * 256], cgs[k][:], S_cgs[k], reads=[S_cgs[k]], writes=[S_cgd])

                f1a(0)
                f1b(0)
                f1a(1)
                for s in range(32):
                    if s + 1 < 32:
                        f1b(s + 1)
                    if s + 2 < 32:
                        f1a(s + 2)
                    back1a(s)
                    back1b(s)
                B.dma("sp", km_s.rearrange("(pr two) d j -> (two d) pr j", two=2), kmst[:], S_kmst, reads=[S_kmst], writes=[S_kmd])
                B.emit_block()

            with ExitStack() as p2:
              if PHASES >= 2:
                kaug = [sb(p2, "kaug%d" % i, [99, S], BF16) for i in range(2)]
                qaug = [sb(p2, "qaug%d" % i, [99, QTOK], BF16) for i in range(2)]
                qa = [sb(p2, "qa%d" % i, [128, NQT, 99], BF16) for i in range(2)]
                atst = [sb(p2, "atst%d" % i, [64, QTOK], BF16) for i in range(2)]
                gvb = sb(p2, "gvb", [128, NQB, 32], F32)
                gvb2 = sb(p2, "gvb2", [128, NQB, 32], F32)
                cm = sb(p2, "cm", [128, 2, 256], BF16)
                km_f = [sb(p2, "km_f%d" % i, [64, 32], F32) for i in range(2)]
                km_b = [sb(p2, "km_b%d" % i, [64, 32], BF16) for i in range(2)]
                gm = [sb(p2, "gm%d" % i, [128, 32], F32) for i in range(2)]
                t8 = [sb(p2, "t8%d" % i, [128, 8], F32) for i in range(2)]
                tsel = [sb(p2, "tsel%d" % i, [128, 32], F32) for i in range(2)]
                pT = [sb(p2, "pT%d" % i, [128, 2, 256], BF16) for i in range(3)]
                rc = [sb(p2, "rc%d" % i, [128, 256], F32) for i in range(4)]
                pos = [sb(p2, "pos%d" % i, [128, 256], F32) for i in range(4)]
                atf = [sb(p2, "atf%d" % i, [64, 256], F32) for i in range(4)]
                sqa = [sb(p2, "sqa%d" % i, [64, 256], BF16) for i in range(4)]
                sp = [ps(p2, "sp%d" % i, [128, 2, 256], F32) for i in range(3)]
                po = [ps(p2, "po%d" % i, [128, 512], F32) for i in range(2)]
                pgt = ps(p2, "pgt", [128, 512], F32)
                pbc = ps(p2, "pbc", [128, 512], F32)
                ptt_t = ps(p2, "ptt", [128, 1024], BF16)
                ptt = ptt_t[:, 0:128]
                S_kaug = [Slot(), Slot()]
                S_kstat = Slot()
                S_qq = [Slot(), Slot()]
                S_qm = [Slot(), Slot()]
                S_qa = [Slot(), Slot()]
                S_atst = [Slot(), Slot()]
                S_c2 = Slot()
                S_kmf = [Slot(), Slot()]
                S_kmb = [Slot(), Slot()]
                S_gm = [Slot(), Slot()]
                S_t8 = [Slot(), Slot()]
                S_tsel = [Slot(), Slot()]
                S_pT = [Slot() for _ in range(3)]
                S_rc = [Slot() for _ in range(4)]
                S_pos = [Slot() for _ in range(4)]
                S_atf = [Slot() for _ in range(4)]
                S_sqa = [Slot() for _ in range(4)]
                S_sp = [Slot() for _ in range(3)]
                S_po = [Slot(), Slot()]
                S_pgt, S_ptt, S_pbc = Slot(), Slot(), Slot()
                S_pss2 = S_pbc
                S_atd = Slot("at_s")

                B.dma("sp", gvb[:].rearrange("p a b -> p (a b)"), gvb_d, S_c2, writes=[S_c2])
                B.dma("sp", gvb2[:].rearrange("p a b -> p (a b)"), gvb2_d, S_c2, writes=[S_c2])
                B.dma("sp", cm[:].rearrange("p a b -> p (a b)"), cmask, S_c2, writes=[S_c2])
                for i in range(2):
                    B.dma("sp", kaug[i][64:99, :], kstat, S_kstat, writes=[S_kstat])

                def load_head(h):
                    hb = h % 2
                    B.dma("sp", kaug[hb][0:64, :], kT_s[h], S_kaug[hb], reads=[S_kT], writes=[S_kaug[hb]])
                    B.dma("sp", qaug[hb][0:64, :], qT_s[h], S_qq[hb], reads=[S_qT], writes=[S_qq[hb]])
                    B.dma("sp", qa[hb][:].rearrange("p a b -> p (a b)"), qstat[h], S_qa[hb], writes=[S_qa[hb]])

                def prep_head(h):
                    hb = h % 2
                    B.dma("sp", km_f[hb][:], km_s[h], S_kmf[hb], reads=[S_kmd], writes=[S_kmf[hb]])
                    B.op("dve", lambda e: e.tensor_scalar(out=km_b[hb][:], in0=km_f[hb][:], scalar1=1.0 / 256, scalar2=None, op0=ALU.mult),
                         reads=[S_kmf[hb]], writes=[S_kmb[hb]])

                def mask_a(h, qt):
                    hb = h % 2
                    qb = qt // 2
                    g2 = qt % 2
                    B.pe_group([mm(pgt[:, 0:32], qaug[hb][0:64, qt * 128:(qt + 1) * 128], km_b[hb][:, :], True, True)],
                               reads=[S_qq[hb], S_kmb[hb]], writes=[S_pgt])
                    B.op("dve", lambda e: e.tensor_tensor(out=gm[g2][:], in0=pgt[:, 0:32], in1=gvb[:, qb, :], op=ALU.add),
                         reads=[S_pgt, S_c2], writes=[S_gm[g2]])
                    B.op("dve", lambda e: e.max(out=t8[g2][:], in_=gm[g2][:]), reads=[S_gm[g2]], writes=[S_t8[g2]])
                    B.op("dve", lambda e: e.tensor_scalar(out=tsel[g2][:], in0=gm[g2][:], scalar1=t8[g2][:, 2:3], scalar2=-NEG,
                                                          op0=ALU.is_ge, op1=ALU.mult),
                         reads=[S_gm[g2], S_t8[g2]], writes=[S_tsel[g2]])
                    B.op("dve", lambda e: e.tensor_tensor(out=qa[hb][:, qt, 64:96], in0=tsel[g2][:], in1=gvb2[:, qb, :], op=ALU.add),
                         reads=[S_tsel[g2], S_c2], writes=[S_qa[hb]])

                def mask_b(h, qt):
                    hb = h % 2
                    B.pe_group([tr(ptt[0:99, :], qa[hb][:, qt, :], ident[:])], reads=[S_qa[hb], S_const], writes=[S_ptt])
                    B.op("dve", lambda e: e.tensor_copy(out=qaug[hb][64:99, qt * 128:(qt + 1) * 128], in_=ptt[64:99, :]),
                         reads=[S_ptt], writes=[S_qm[hb]])

                sctr = [0]
                KEEP = []
                for h_ in range(NH):
                    slope_ = 2.0 ** (-(h_ + 1))
                    kk_ = 0
                    while kk_ < 31 and slope_ * (kk_ * 256 + 1) < 80.0:
                        kk_ += 1
                    KEEP.append(kk_)

                def s_mm(h, qb, j):
                    hb = h % 2
                    sB = 15 + qb
                    qc = slice(qb * 256, (qb + 1) * 256)
                    i = sctr[0] % 3
                    sctr[0] += 1
                    fns = []
                    for kt in range(2):
                        fns.append(mm(sp[i][:, kt, :], kaug[hb][0:99, (2 * j + kt) * 128:(2 * j + kt + 1) * 128], qaug[hb][0:99, qc],
                                      True, j != sB))
                        if j == sB:
                            fns.append(mm(sp[i][:, kt, :], ident[:], cm[:, kt, :], False, True))
                    B.pe_group(fns, reads=[S_kaug[hb], S_kstat, S_qq[hb], S_qm[hb], S_c2, S_const], writes=[S_sp[i]])
                    B.op("act", lambda e: e.activation(out=pT[i][:], in_=sp[i][:], func=AF.Exp), reads=[S_sp[i]], writes=[S_pT[i]])
                    return i

                def pv_mm(h, qb, j, i, first, last):
                    ob = qb % 2
                    B.pe_group([mm(po[ob][0:65, 0:256], V_all[:, 2 * j + kt, h, :], pT[i][:, kt, :], (first and kt == 0), (last and kt == 1))
                                for kt in range(2)], reads=[S_V, S_pT[i]], writes=[S_po[ob]])

                tctr = [0]

                def tail_a(h, qb):
                    ob = qb % 2
                    r = tctr[0] % 4
                    tctr[0] += 1
                    B.op("act", lambda e: e.activation(out=pos[r][0:65, :], in_=po[ob][0:65, 0:256], func=AF.Copy), reads=[S_po[ob]], writes=[S_pos[r]])
                    B.op("dve", lambda e: e.reciprocal(out=rc[r][64:65, :], in_=pos[r][64:65, :]), reads=[S_pos[r]], writes=[S_rc[r]])
                    return r

                def tail_b(h, qb, r):
                    hb = h % 2
                    qc = slice(qb * 256, (qb + 1) * 256)
                    B.pe_group([mm(pbc[0:64, 0:256], ones_f[64:65, 0:64], rc[r][64:65, :], True, True)],
                               reads=[S_rc[r], S_const], writes=[S_pbc])
                    B.op("dve", lambda e: e.tensor_tensor(out=atf[r][:], in0=pbc[0:64, 0:256], in1=pos[r][0:64, :], op=ALU.mult),
                         reads=[S_pbc, S_pos[r]], writes=[S_atf[r]])
                    B.op("pool", lambda e: e.tensor_tensor(out=sqa[r][:], in0=atf[r][:], in1=atf[r][:], op=ALU.mult), reads=[S_atf[r]], writes=[S_sqa[r]])
                    B.op("pool", lambda e: e.tensor_scalar(out=atst[hb][:, qc], in0=atf[r][:], scalar1=gattn_sb[:, h:h + 1], scalar2=None, op0=ALU.mult),
                         reads=[S_atf[r], S_const], writes=[S_atst[hb]])

                def tail_c(h, qb, r):
                    B.pe_group([mm(pbc[:, 256 + t:257 + t], sqa[r][:, t * 128:(t + 1) * 128], ones_b[0:64, 0:1], True, True) for t in range(2)],
                               reads=[S_sqa[r], S_const], writes=[S_pss2])
                    if h == 0:
                        B.op("dve", lambda e: e.tensor_copy(out=ssa[:, 2 * qb:2 * qb + 2], in_=pbc[:, 256:258]), reads=[S_pss2], writes=[S_ssa])
                    else:
                        B.op("dve", lambda e: e.tensor_tensor(out=ssa[:, 2 * qb:2 * qb + 2], in0=pbc[:, 256:258], in1=ssa[:, 2 * qb:2 * qb + 2],
                                                              op=ALU.add),
                             reads=[S_pss2, S_ssa], writes=[S_ssa])

                def blocks_of(h, qb):
                    sB = 15 + qb
                    return list(range(max(0, sB - KEEP[h]), sB + 1))

                load_head(0)
                prep_head(0)
                for qt in range(NQT):
                    mask_a(0, qt)
                    mask_b(0, qt)
                for h in range(NH):
                    hb = h % 2
                    if h + 1 < NH:
                        load_head(h + 1)
                        prep_head(h + 1)
                    total_iter = sum(len(blocks_of(h, qb)) for qb in range(NQB))
                    sched = {}
                    if h + 1 < NH:
                        step = max(1, (total_iter - 8) // NQT)
                        for qt in range(NQT):
                            ia = min(total_iter - 1, 1 + qt * step)
                            ib = min(total_iter - 1, ia + 3)
                            sched.setdefault(ia, []).append(lambda qt=qt: mask_a(h + 1, qt))
                            sched.setdefault(ib, []).append(lambda qt=qt: mask_b(h + 1, qt))
                    it = 0
                    tails = []
                    for qb in range(NQB):
                        js = blocks_of(h, qb)
                        n = len(js)
                        ids = {0: s_mm(h, qb, js[0])}
                        if n > 1:
                            ids[1] = s_mm(h, qb, js[1])
                        for i in range(n):
                            if i + 2 < n:
                                ids[i + 2] = s_mm(h, qb, js[i + 2])
                            pv_mm(h, qb, js[i], ids[i], i == 0, i == n - 1)
                            for fn in sched.pop(it, []):
                                fn()
                            for item in list(tails):
                                if item[0] <= it:
                                    item[1]()
                                    tails.remove(item)
                            it += 1
                        r = tail_a(h, qb)
                        tails.append((it + 5, lambda h=h, qb=qb, r=r: tail_b(h, qb, r)))
                        tails.append((it + 10, lambda h=h, qb=qb, r=r: tail_c(h, qb, r)))
                    for key in sorted(sched):
                        for fn in sched[key]:
                            fn()
                    for item in tails:
                        item[1]()
                    B.dma("sp", at_s[h], atst[hb][:], S_atst[hb], reads=[S_atst[hb]], writes=[S_atd])
                B.emit_block()

        with ExitStack() as p3:
          if PHASES >= 3:
            wu = sb(p3, "wu", [128, 8, 2 * DFF], BF16)
            wd = sb(p3, "wd", [128, 22, D], BF16)
            x1 = [sb(p3, "x1%d" % i, [128, 2, D], F32) for i in range(2)]
            atb = sb(p3, "atb", [128, 4, 256], BF16)
            cgb = sb(p3, "cgb", [128, 4, 256], BF16)
            xn2 = sb(p3, "xn2", [128, 2, D], BF16)
            h2T = [sb(p3, "h2T%d" % i, [128, 8, 258], BF16) for i in range(2)]
            YA = [sb(p3, "YA%d" % i, [128, 256], F32) for i in range(2)]
            YB = [sb(p3, "YB%d" % i, [128, 256], F32) for i in range(2)]
            YV = [sb(p3, "YV%d" % i, [128, 256], F32) for i in range(2)]
            YG = [sb(p3, "YG%d" % i, [128, 256], F32) for i in range(2)]
            actT = sb(p3, "actT", [128, 22, 256], BF16)
            gfb = sb(p3, "gfb", [128, D], F32)
            rsa = sb(p3, "rsa", [128, NQT], F32)
            rsc = sb(p3, "rsc", [128, NQT], F32)
            ssf = sb(p3, "ssf", [128, 2], F32)
            rtf = sb(p3, "rtf", [128, 2], F32)
            rsf = sb(p3, "rsf", [128, 2], F32)
            ssb = sb(p3, "ssb", [128, 2], F32)
            rtb = sb(p3, "rtb", [128, 2], F32)
            rsb = sb(p3, "rsb", [128, 2], F32)
            pu = [ps(p3, "pu%d" % i, [128, 512], F32) for i in range(4)]
            pd = [ps(p3, "pd%d" % i, [128, 512], F32) for i in range(2)]
            pac = [ps(p3, "pac%d" % i, [128, 512], F32) for i in range(2)]
            S_wu, S_wd = Slot(), Slot()
            S_x1 = [Slot(), Slot()]
            S_atb, S_cgb, S_xn2 = Slot(), Slot(), Slot()
            S_h2T = [Slot(), Slot()]
            S_YA = [Slot() for _ in range(2)]
            S_YB = [Slot() for _ in range(2)]
            S_YV = [Slot(), Slot()]
            S_YG = [Slot(), Slot()]
            S_actT, S_gfb, S_rs = Slot(), Slot(), Slot()
            S_ssf, S_rtf, S_rsf, S_ssb, S_rtb, S_rsb = Slot(), Slot(), Slot(), Slot(), Slot(), Slot()
            S_pu = [Slot() for _ in range(4)]
            S_pd = [Slot(), Slot()]
            S_pac = [Slot(), Slot()]
            gtb = x1[1][:, 0, :]
            S_gtb = S_x1[1]

            wov = w_out.rearrange("(c p) n -> p c n", p=128)
            wuv = w_up.rearrange("(c p) n -> p c n", p=128)
            wdv = w_down.rearrange("(c p) n -> p c n", p=128)
            for c in range(8):
                B.dma("pool", wu[:, c, :], wuv[:, c, :], S_wu, writes=[S_wu])
            for c0 in range(0, 22, 2):
                B.dma("pool", wd[:, c0:c0 + 2, :], wdv[:, c0:c0 + 2, :], S_wd, writes=[S_wd])
            B.dma("sp", gfb[:], gfin, S_gfb, writes=[S_gfb])
            mod_t = mod_s.tensor
            B.dma("sp", gtb, bass.AP(mod_t, 2048, [[0, 128], [1, D]]), S_gtb, reads=[S_mods], writes=[S_gtb])
            for c in range(8):
                B.op("pool", lambda e, c=c: e.tensor_tensor(out=wo[:, c, :], in0=wo[:, c, :], in1=gtb, op=ALU.mult),
                     reads=[S_gtb], writes=[S_wo])
            B.dma("sp", gtb, bass.AP(mod_t, 5120, [[0, 128], [1, D]]), S_gtb, reads=[S_mods], writes=[S_gtb])
            for c in range(22):
                B.op("pool", lambda e, c=c: e.tensor_tensor(out=wd[:, c, :], in0=wd[:, c, :], in1=gtb, op=ALU.mult),
                     reads=[S_gtb], writes=[S_wd])
            for src_, dst, ssl in ((ssa, rsa, S_ssa), (ssc, rsc, S_ssc)):
                B.op("act", lambda e, src_=src_, dst=dst: e.activation(out=dst[:], in_=src_[:], func=AF.Sqrt, scale=1.0 / 512, bias=epsc[:]),
                     reads=[ssl, S_const], writes=[S_rs])
                B.op("dve", lambda e, dst=dst: e.reciprocal(out=dst[:], in_=dst[:]), reads=[S_rs], writes=[S_rs])
            for i in range(2):
                B.op("dve", lambda e, i=i: e.memset(h2T[i][:, :, 0:2], 0.0), writes=[S_h2T[i]])

            xsv = xs.rearrange("(n t p) d -> n p t d", t=2, p=128)
            outv = out.rearrange("(n t p) d -> n p t d", t=2, p=128)
            atv = at_s.rearrange("(pr two) d t -> (two d) pr t", two=2)
            cgv = cg_s.rearrange("c p t -> p c t")
            puc = [0]
            ybc = [0]
            yac = [0]
            pend_fin = []
            tpv = [pd[i][:, :].bitcast(BF16).rearrange("p (c t) -> p c t", t=256) for i in range(2)]

            def next_yb():
                i = ybc[0] % 6
                ybc[0] += 1
                return yb[i], S_yb[i]

            def f3a(qb):
                k = qb % 2
                sl = 15 + qb
                qc = slice(qb * 256, (qb + 1) * 256)
                B.dma("sp", x1[k][:], xsv[sl], S_x1[k], writes=[S_x1[k]])
                B.dma("sp", atb[:], atv[:, :, qc], S_atb, reads=[S_atd], writes=[S_atb])
                B.dma("sp", cgb[:], cgv[:, :, qc], S_cgb, reads=[S_cgd], writes=[S_cgb])
                for t in range(2):
                    tile = 2 * qb + t
                    for n in range(2):
                        ns = slice(n * 512, (n + 1) * 512)
                        B.pe_group([mm(pac[0][:, :], atb[:, c, t * 128:(t + 1) * 128], wo[:, c, ns], c == 0, c == 3) for c in range(4)],
                                   reads=[S_atb, S_wo], writes=[S_pac[0]])
                        B.pe_group([mm(pac[1][:, :], cgb[:, c, t * 128:(t + 1) * 128], wo[:, 4 + c, ns], c == 0, c == 3) for c in range(4)],
                                   reads=[S_cgb, S_wo], writes=[S_pac[1]])
                        B.op("dve", lambda e, t=t, ns=ns, tile=tile: e.scalar_tensor_tensor(
                            out=x1[k][:, t, ns], in0=pac[0][:, :], scalar=rsa[:, tile:tile + 1], in1=x1[k][:, t, ns], op0=ALU.mult, op1=ALU.add),
                            reads=[S_pac[0], S_rs], writes=[S_x1[k]])
                        B.op("dve", lambda e, t=t, ns=ns, tile=tile: e.scalar_tensor_tensor(
                            out=x1[k][:, t, ns], in0=pac[1][:, :], scalar=rsc[:, tile:tile + 1], in1=x1[k][:, t, ns], op0=ALU.mult, op1=ALU.add),
                            reads=[S_pac[1], S_rs], writes=[S_x1[k]])
                for t in range(2):
                    B.op("act", lambda e, t=t: e.activation(out=xn2[:, t, :], in_=x1[k][:, t, :], func=AF.Square, accum_out=ssf[:, t:t + 1]),
                         reads=[S_x1[k]], writes=[S_xn2, S_ssf])
                B.op("act", lambda e: e.activation(out=rtf[:], in_=ssf[:], func=AF.Sqrt, scale=1.0 / D, bias=epsc[:]),
                     reads=[S_ssf, S_const], writes=[S_rtf])
                B.op("dve", lambda e: e.reciprocal(out=rsf[:], in_=rtf[:]), reads=[S_rtf], writes=[S_rsf])
                for t in range(2):
                    B.op("dve", lambda e, t=t: e.tensor_scalar(out=xn2[:, t, :], in0=x1[k][:, t, :], scalar1=rsf[:, t:t + 1], scalar2=None,
                                                               op0=ALU.mult),
                         reads=[S_x1[k], S_rsf], writes=[S_xn2])

            def f3b(qb):
                k = qb % 2
                for half in range(2):
                    B.pe_group([tr(tpv[half][:, c, t * 128:(t + 1) * 128], xn2[:, t, (half * 4 + c) * 128:(half * 4 + c + 1) * 128], ident[:])
                                for t in range(2) for c in range(4)], reads=[S_xn2, S_const], writes=[S_pd[half]])
                    for c in range(4):
                        cc = half * 4 + c
                        B.op("act", lambda e, c=c, cc=cc, half=half: e.activation(out=h2T[k][:, cc, 2:258], in_=tpv[half][:, c, :],
                                                                                 func=AF.Identity, scale=G2[:, cc:cc + 1], bias=sh2[:, cc:cc + 1]),
                             reads=[S_pd[half], S_G], writes=[S_h2T[k]])
                if qb == 1:
                    B.op("dve", lambda e: e.tensor_scalar(out=h2T[k][:, :, 0:2], in0=h2T[1 - k][:, :, 256:258], scalar1=hv[:, 0:1],
                                                          scalar2=None, op0=ALU.mult),
                         reads=[S_h2T[1 - k], S_const], writes=[S_h2T[k]])
                elif qb > 1:
                    B.op("dve", lambda e: e.tensor_copy(out=h2T[k][:, :, 0:2], in_=h2T[1 - k][:, :, 256:258]),
                         reads=[S_h2T[1 - k]], writes=[S_h2T[k]])

            def back3(qb):
                k = qb % 2
                for v in range(22):
                    if qb + 1 < NQB and v == 3:
                        f3a(qb + 1)
                    if qb + 1 < NQB and v == 14:
                        f3b(qb + 1)
                    p2_ = v % 2
                    chs = (v, 22 + v)
                    pis = []
                    for ch in chs:
                        i = puc[0] % 4
                        puc[0] += 1
                        pis.append(i)
                        B.pe_group([mm(pu[i][:, 0:258], wu[:, c, ch * 128:(ch + 1) * 128], h2T[k][:, c, 0:258], c == 0, c == 7) for c in range(8)],
                                   reads=[S_wu, S_h2T[k]], writes=[S_pu[i]])
                    yas = []
                    for w_, (ch, i) in enumerate(zip(chs, pis)):
                        ai = yac[0] % 2
                        yac[0] += 1
                        yas.append(ai)
                        B.op("act", lambda e, i=i, ai=ai, ch=ch: e.activation(out=YA[ai][:], in_=pu[i][:, 2:258], func=AF.Identity,
                                                                             scale=fcw_sb[:, 88 + ch:89 + ch], bias=fcb_sb[:, ch:ch + 1]),
                             reads=[S_pu[i], S_const], writes=[S_YA[ai]])
                    if pend_fin:
                        pend_fin.pop(0)()
                    ybs = []
                    for w_, (ch, i) in enumerate(zip(chs, pis)):
                        bi = ybc[0] % 2
                        ybc[0] += 1
                        ybs.append(bi)
                        ai = yas[w_]
                        B.op("dve", lambda e, i=i, ai=ai, bi=bi, ch=ch: e.scalar_tensor_tensor(
                            out=YB[bi][:], in0=pu[i][:, 1:257], scalar=fcw_sb[:, 44 + ch:45 + ch], in1=YA[ai][:], op0=ALU.mult, op1=ALU.add),
                            reads=[S_pu[i], S_YA[ai], S_const], writes=[S_YB[bi]])
                    outs = ((YV[p2_], S_YV[p2_]), (YG[p2_], S_YG[p2_]))
                    for w_, (ch, i) in enumerate(zip(chs, pis)):
                        bi = ybs[w_]
                        yo, syo = outs[w_]
                        B.op("dve", lambda e, i=i, bi=bi, yo=yo, ch=ch: e.scalar_tensor_tensor(
                            out=yo[:], in0=pu[i][:, 0:256], scalar=fcw_sb[:, ch:ch + 1], in1=YB[bi][:], op0=ALU.mult, op1=ALU.add),
                            reads=[S_pu[i], S_YB[bi], S_const], writes=[syo])
                    def fin_pair(p2_=p2_, v=v):
                        B.op("act", lambda e: e.activation(out=YG[p2_][:], in_=YG[p2_][:], func=AF.Silu), reads=[S_YG[p2_]], writes=[S_YG[p2_]])
                        B.op("pool", lambda e: e.tensor_tensor(out=actT[:, v, :], in0=YV[p2_][:], in1=YG[p2_][:], op=ALU.mult),
                             reads=[S_YV[p2_], S_YG[p2_]], writes=[S_actT])
                    pend_fin.append(fin_pair)
                if pend_fin:
                    pend_fin.pop(0)()
                for t in range(2):
                    for n in range(2):
                        B.pe_group([mm(pd[n][:, :], actT[:, v, t * 128:(t + 1) * 128], wd[:, v, n * 512:(n + 1) * 512], v == 0, v == 21)
                                    for v in range(22)], reads=[S_actT, S_wd], writes=[S_pd[n]])
                        B.op("dve", lambda e, t=t, n=n: e.tensor_tensor(out=x1[k][:, t, n * 512:(n + 1) * 512], in0=pd[n][:, :],
                                                                        in1=x1[k][:, t, n * 512:(n + 1) * 512], op=ALU.add),
                             reads=[S_pd[n]], writes=[S_x1[k]])
                for t in range(2):
                    B.op("act", lambda e, t=t: e.activation(out=xn2[:, t, :], in_=x1[k][:, t, :], func=AF.Square, accum_out=ssb[:, t:t + 1]),
                         reads=[S_x1[k]], writes=[S_xn2, S_ssb])
                B.op("act", lambda e: e.activation(out=rtb[:], in_=ssb[:], func=AF.Sqrt, scale=1.0 / D, bias=epsc[:]),
                     reads=[S_ssb, S_const], writes=[S_rtb])
                B.op("dve", lambda e: e.reciprocal(out=rsb[:], in_=rtb[:]), reads=[S_rtb], writes=[S_rsb])
                for t in range(2):
                    B.op("dve", lambda e, t=t: e.scalar_tensor_tensor(out=x1[k][:, t, :], in0=x1[k][:, t, :], scalar=rsb[:, t:t + 1],
                                                                      in1=gfb[:], op0=ALU.mult, op1=ALU.mult),
                         reads=[S_rsb, S_gfb], writes=[S_x1[k]])
                B.dma("sp", outv[qb - 1], x1[k][:], S_x1[k], reads=[S_x1[k]])

            f3a(0)
            f3b(0)
            f3a(1)
            f3b(1)
            for qb in range(1, NQB):
                back3(qb)
            B.emit_block()
    return nc


_NC_CACHE = {}


def _consts(half):
    bf = ml_dtypes.bfloat16
    ident = np.eye(128, dtype=np.float32).astype(bf)
    key = np.arange(S)
    kstat = np.zeros((35, S), np.float32)
    kstat[key // 256, key] = 1.0
    kstat[32] = key % 256
    kstat[33] = key // 256
    kstat[34] = 1.0
    kstat = kstat.astype(bf)
    p = np.arange(128)[:, None, None]
    kt = np.arange(2)[None, :, None]
    a = np.arange(256)[None, None, :]
    cmask = np.where(kt * 128 + p <= a, 0.0, NEG).astype(np.float32).reshape(128, 512).astype(bf)
    gvb = np.zeros((NQB, 32), np.float32)
    gvb2 = np.zeros((NQB, 32), np.float32)
    for qb in range(NQB):
        sB = 15 + qb
        for j in range(32):
            valid = (j < sB) and (half == 1 or j >= 16)
            gvb[qb, j] = 0.0 if valid else -1.0e9
            gvb2[qb, j] = NEG if valid else 2 * NEG
        gvb[qb, sB] = -3.0e9
        gvb2[qb, sB] = 0.0
    gvb = np.ascontiguousarray(np.broadcast_to(gvb.reshape(1, -1), (128, NQB * 32))).astype(np.float32)
    gvb2 = np.ascontiguousarray(np.broadcast_to(gvb2.reshape(1, -1), (128, NQB * 32))).astype(np.float32)
    qstat = np.zeros((NH, 128, NQT, 99), np.float32)
    for h in range(NH):
        slope = 2.0 ** (-(h + 1))
        for qt in range(NQT):
            sB = 15 + qt // 2
            apos = (qt % 2) * 128 + np.arange(128)
            qstat[h, :, qt, 96] = slope
            qstat[h, :, qt, 97] = 256.0 * slope
            qstat[h, :, qt, 98] = -slope * (256.0 * sB + apos)
    qstat = qstat.reshape(NH, 128, NQT * 99).astype(bf)
    hv = np.full((128, 1), float(half), np.float32)
    return dict(ident=ident, kstat=kstat, cmask=cmask, gvb=gvb, gvb2=gvb2, qstat=qstat, hv=hv)


def _pc(v, nchunk):
    return np.ascontiguousarray(np.asarray(v, np.float32).reshape(nchunk, 128).T)


def kernel(x, c, w_ada, b_ada, g_mix, w_in, conv_w, conv_b, g_attn_out, g_conv_out, w_out, g_ffn,
           w_up, ffn_conv_w, ffn_conv_b, w_down, g_final):
    f = lambda a: np.ascontiguousarray(np.asarray(a, dtype=np.float32))
    x = f(x)
    c = f(c)
    if "nc" not in _NC_CACHE:
        _NC_CACHE["nc"] = build_program()
    nc = _NC_CACHE["nc"]
    shared = dict(
        w_ada=f(w_ada[0]), b_ada=f(b_ada[0]).reshape(1, -1), gmix=_pc(g_mix[0], 8), w_in=f(w_in[0]),
        convw=np.ascontiguousarray(np.concatenate([_pc(conv_w[0][kk], 4) for kk in range(3)], axis=1)),
        convb=_pc(conv_b[0], 4),
        gattn=np.ascontiguousarray(f(g_attn_out[0]).reshape(8, 64).T),
        gconv=_pc(g_conv_out[0], 4), w_out=f(w_out[0]), gffn=_pc(g_ffn[0], 8), w_up=f(w_up[0]),
        fcw=np.ascontiguousarray(np.concatenate([_pc(ffn_conv_w[0][kk], 44) for kk in range(3)], axis=1)),
        fcb=_pc(ffn_conv_b[0], 44), w_down=f(w_down[0]),
        gfin=np.ascontiguousarray(np.broadcast_to(f(g_final).reshape(1, -1), (128, D))),
    )
    cst = [_consts(0), _consts(1)]
    in_maps = []
    for i in range(NCORES):
        b, half = i // 2, i % 2
        if half == 1:
            xs = x[b]
        else:
            xs = np.concatenate([np.zeros((4096, D), np.float32), x[b, :4096]], axis=0)
        m = dict(shared)
        m.update(cst[half])
        m["xs"] = np.ascontiguousarray(xs)
        m["cT"] = _pc(c[b], 8)
        in_maps.append(m)
    res = run_bass_kernel_spmd(nc, in_maps, core_ids=list(range(NCORES)))
    outp = np.empty((4, S, D), np.float32)
    for i in range(NCORES):
        b, half = i // 2, i % 2
        outp[b, half * 4096:(half + 1) * 4096] = res.results[i]["out"]
    return outp
```

```python
import numpy as np
from contextlib import ExitStack
import ml_dtypes
import concourse.bass as bass
import concourse.mybir as mybir
from concourse.bass_utils import run_bass_kernel_spmd

F32 = mybir.dt.float32
BF16 = mybir.dt.bfloat16
AF = mybir.ActivationFunctionType
ALU = mybir.AluOpType
AX = mybir.AxisListType

D = 1024
S = 8192
NH = 8
DFF = 2816
NQB = 17
NQT = 34
QTOK = NQB * 256
EPS = 1e-6
NEG = -30000.0
NCORES = 8
PHASES = 3
DEBUG = False


class Tok:
    __slots__ = ("sem", "val")

    def __init__(self, sem, val):
        self.sem = sem
        self.val = val


class Slot:
    def __init__(self, name=""):
        self.name = name
        self.w = None
        self.r = {}
        self.dsem = None
        self.dcnt = 0


class Builder:
    ENG = ("pe", "act", "dve", "pool", "sp")

    def __init__(self, nc, es):
        self.nc = nc
        self.es = es
        self.q = {}
        for n in self.ENG:
            sem = es.enter_context(nc.semaphore("sem_" + n))
            self.q[n] = dict(sem=sem, cnt=0, ops=[], waited={})
        self.dslots = []

    def _waits(self, en, deps):
        q = self.q[en]
        need = {}
        for t in deps:
            if t is None:
                continue
            if en == "pe" and t.sem is q["sem"]:
                continue
            k = id(t.sem)
            if q["waited"].get(k, 0) >= t.val:
                continue
            if k not in need or need[k].val < t.val:
                need[k] = t
        for k, t in need.items():
            q["waited"][k] = t.val
        return [(t.sem, t.val) for t in need.values()]

    @staticmethod
    def _deps(reads, writes, deps):
        d = list(deps)
        for s in reads:
            d.append(s.w)
        for s in writes:
            d.extend(s.r.values())
            d.append(s.w)
        return d

    @staticmethod
    def _update(tok, reads, writes):
        for s in writes:
            s.w = tok
            s.r = {}
        for s in reads:
            k = id(tok.sem)
            if k not in s.r or s.r[k].val < tok.val:
                s.r[k] = tok

    def op(self, en, fn, reads=(), writes=(), deps=(), inc=True):
        q = self.q[en]
        waits = self._waits(en, self._deps(reads, writes, deps))
        tok = None
        if inc:
            q["cnt"] += 1
            tok = Tok(q["sem"], q["cnt"])
        else:
            assert not reads and not writes
        sem = q["sem"]

        def emit(e, waits=waits, fn=fn, inc=inc, sem=sem):
            for (sm, v) in waits:
                e.wait_ge(sm, v)
            ins = fn(e)
            if inc:
                ins.then_inc(sem, 1)

        q["ops"].append(emit)
        if tok is not None:
            self._update(tok, reads, writes)
        return tok

    def pe_group(self, fns, reads=(), writes=()):
        q = self.q["pe"]
        waits = self._waits("pe", self._deps(reads, writes, ()))
        q["cnt"] += 1
        tok = Tok(q["sem"], q["cnt"])
        sem = q["sem"]
        n = len(fns)

        def emit(e, waits=waits, fns=fns, sem=sem, n=n):
            for (sm, v) in waits:
                e.wait_ge(sm, v)
            for i, f in enumerate(fns):
                ins = f(e)
                if i == n - 1:
                    ins.then_inc(sem, 1)

        q["ops"].append(emit)
        self._update(tok, reads, writes)
        return tok

    def dma(self, en, out, in_, sem_slot, reads=(), writes=(), deps=(), **kw):
        q = self.q[en]
        sl = sem_slot
        if sl.dsem is None:
            sl.dsem = self.es.enter_context(self.nc.semaphore("dsem%d" % len(self.dslots)))
            self.dslots.append(sl)
        waits = self._waits(en, self._deps(reads, writes, deps))
        sl.dcnt += 16
        tok = Tok(sl.dsem, sl.dcnt)
        dsem = sl.dsem

        def emit(e, waits=waits, out=out, in_=in_, dsem=dsem, kw=kw):
            for (sm, v) in waits:
                e.wait_ge(sm, v)
            e.dma_start(out=out, in_=in_, **kw).then_inc(dsem, 16)

        q["ops"].append(emit)
        self._update(tok, reads, writes)
        return tok

    def emit_block(self):
        nc = self.nc
        finals = [(s.dsem, s.dcnt) for s in self.dslots if s.dcnt > 0]
        pe_fin = (self.q["pe"]["sem"], self.q["pe"]["cnt"])

        def fin(e, finals=finals):
            for sm, v in finals:
                e.wait_ge(sm, v)

        self.q["sp"]["ops"].append(fin)
        with nc.Block() as blk:
            for n, meth in (("pe", blk.tensor), ("act", blk.scalar), ("dve", blk.vector),
                            ("pool", blk.gpsimd), ("sp", blk.sync)):
                ops = self.q[n]["ops"]
                if ops:
                    def body(e, ops=ops):
                        for f in ops:
                            f(e)
                    meth(body)
                self.q[n]["ops"] = []


def mm(out, lhsT, rhs, start, stop):
    return lambda e: e.matmul(out, lhsT=lhsT, rhs=rhs, start=start, stop=stop)


def tr(out, in_, ident):
    return lambda e: e.transpose(out, in_, ident)


def build_program():
    nc = bass.Bass("TRN2", target_bir_lowering=False)

    def din(name, shape, dt=F32):
        return nc.dram_tensor(name, list(shape), dt, kind="ExternalInput").ap()

    def dscr(name, shape, dt):
        return nc.dram_tensor(name, list(shape), dt, kind="ExternalOutput" if DEBUG else "Internal").ap()

    xs = din("xs", [S, D])
    cT = din("cT", [128, 8])
    w_ada = din("w_ada", [D, 6 * D])
    b_ada = din("b_ada", [1, 6 * D])
    gmix = din("gmix", [128, 8])
    w_in = din("w_in", [D, 3072])
    convw = din("convw", [128, 12])
    convb = din("convb", [128, 4])
    gattn = din("gattn", [64, 8])
    gconv = din("gconv", [128, 4])
    w_out = din("w_out", [D, D])
    gffn = din("gffn", [128, 8])
    w_up = din("w_up", [D, 2 * DFF])
    fcw = din("fcw", [128, 132])
    fcb = din("fcb", [128, 44])
    w_down = din("w_down", [DFF, D])
    gfin = din("gfin", [128, D])
    ident_d = din("ident", [128, 128], BF16)
    kstat = din("kstat", [35, S], BF16)
    cmask = din("cmask", [128, 512], BF16)
    gvb_d = din("gvb", [128, NQB * 32])
    gvb2_d = din("gvb2", [128, NQB * 32])
    qstat = din("qstat", [NH, 128, NQT * 99], BF16)
    hv_d = din("hv", [128, 1])
    out = nc.dram_tensor("out", [4096, D], F32, kind="ExternalOutput").ap()

    kT_s = dscr("kT_s", [NH, 64, S], BF16)
    qT_s = dscr("qT_s", [NH, 64, QTOK], BF16)
    at_s = dscr("at_s", [NH, 64, QTOK], BF16)
    cg_s = dscr("cg_s", [4, 128, QTOK], BF16)
    mod_s = dscr("mod_s", [1, 6 * D], F32)
    km_s = dscr("km_s", [NH, 64, 32], F32)

    es = ExitStack()
    with es:
        B = Builder(nc, es)

        def sb(st, name, shape, dt):
            return st.enter_context(nc.sbuf_tensor("sb_" + name, list(shape), dt))

        def ps(st, name, shape, dt):
            return st.enter_context(nc.psum_tensor("ps_" + name, list(shape), dt))

        ident = sb(es, "ident", [128, 128], BF16)
        ones_b = sb(es, "ones_b", [128, 1], BF16)
        ones_f = sb(es, "ones_f", [128, 64], F32)
        epsc = sb(es, "epsc", [128, 1], F32)
        hv = sb(es, "hv", [128, 1], F32)
        G1 = sb(es, "G1", [128, 8], F32)
        G2 = sb(es, "G2", [128, 8], F32)
        modT = sb(es, "modT", [128, 48], F32)
        gmix_sb = sb(es, "gmix_sb", [128, 8], F32)
        gffn_sb = sb(es, "gffn_sb", [128, 8], F32)
        convw_sb = sb(es, "convw_sb", [128, 12], F32)
        convb_sb = sb(es, "convb_sb", [128, 4], F32)
        gconv_sb = sb(es, "gconv_sb", [128, 4], F32)
        gattn_sb = sb(es, "gattn_sb", [64, 8], F32)
        fcw_sb = sb(es, "fcw_sb", [128, 132], F32)
        fcb_sb = sb(es, "fcb_sb", [128, 44], F32)
        ssa = sb(es, "ssa", [128, NQT], F32)
        ssc = sb(es, "ssc", [128, NQT], F32)
        wo = sb(es, "wo", [128, 8, D], BF16)
        S_wo = Slot("wo")
        S_const = Slot("const")
        S_ssa = Slot("ssa")
        S_ssc = Slot("ssc")
        S_G = Slot("G")
        S_mods = Slot("mods")

        for dst, src in ((ident, ident_d), (hv, hv_d), (gmix_sb, gmix), (gffn_sb, gffn), (convw_sb, convw),
                         (convb_sb, convb), (gconv_sb, gconv), (gattn_sb, gattn), (fcw_sb, fcw), (fcb_sb, fcb)):
            B.dma("sp", dst[:], src, S_const, writes=[S_const])
        B.op("dve", lambda e: e.memset(ones_b[:], 1.0), writes=[S_const])
        B.op("dve", lambda e: e.memset(ones_f[:], 1.0), writes=[S_const])
        B.op("dve", lambda e: e.memset(epsc[:], EPS), writes=[S_const])

        with ExitStack() as p0:
            c_sb = sb(p0, "c_sb", [128, 8], F32)
            sc_b = sb(p0, "sc_b", [128, 8], BF16)
            brow = sb(p0, "brow", [1, 6 * D], F32)
            modrow = sb(p0, "modrow", [1, 6 * D], F32)
            wa = [sb(p0, "wa%d" % i, [128, 8, 512], BF16) for i in range(2)]
            pm = [ps(p0, "pm%d" % i, [128, 512], F32) for i in range(2)]
            S_c, S_scb, S_brow, S_modrow, S_modT = Slot(), Slot(), Slot(), Slot(), Slot()
            S_wa = [Slot(), Slot()]
            S_pm = [Slot(), Slot()]
            B.dma("sp", c_sb[:], cT, S_c, writes=[S_c])
            B.dma("sp", brow[:], b_ada, S_brow, writes=[S_brow])
            B.op("act", lambda e: e.activation(out=sc_b[:], in_=c_sb[:], func=AF.Silu), reads=[S_c], writes=[S_scb])
            wav = w_ada.rearrange("(c p) n -> p c n", p=128)
            for g in range(12):
                k = g % 2
                B.dma("pool", wa[k][:], wav[:, :, g * 512:(g + 1) * 512], S_wa[k], writes=[S_wa[k]])
                B.pe_group([mm(pm[k][0:1, :], sc_b[:, c:c + 1], wa[k][:, c, :], c == 0, c == 7) for c in range(8)],
                           reads=[S_wa[k], S_scb], writes=[S_pm[k]])
                B.op("dve", lambda e, k=k, g=g: e.tensor_tensor(out=modrow[0:1, g * 512:(g + 1) * 512], in0=pm[k][0:1, :],
                                                                 in1=brow[0:1, g * 512:(g + 1) * 512], op=ALU.add),
                     reads=[S_pm[k], S_brow], writes=[S_modrow])
            B.dma("sp", mod_s, modrow[:], S_mods, reads=[S_modrow], writes=[S_mods])
            B.dma("sp", modT[:], mod_s.rearrange("o (k p) -> (o p) k", p=128), S_modT, reads=[S_mods], writes=[S_modT],
                  allow_slow_non_contiguous=True)
            B.op("dve", lambda e: e.scalar_tensor_tensor(out=G1[:], in0=modT[:, 8:16], scalar=1.0, in1=gmix_sb[:],
                                                         op0=ALU.add, op1=ALU.mult), reads=[S_modT, S_const], writes=[S_G])
            B.op("dve", lambda e: e.scalar_tensor_tensor(out=G2[:], in0=modT[:, 32:40], scalar=1.0, in1=gffn_sb[:],
                                                         op0=ALU.add, op1=ALU.mult), reads=[S_modT, S_const], writes=[S_G])
            B.emit_block()
        sh1 = modT[:, 0:8]
        sh2 = modT[:, 24:32]

        with ExitStack() as p12:
          if PHASES >= 1:
            V_all = sb(p12, "V_all", [128, 64, NH, 65], BF16)
            S_V = Slot("V")
            with ExitStack() as p1:
                w_in_sb = sb(p1, "w_in_sb", [128, 8, 3072], BF16)
                S_win = Slot("win")
                wiv = w_in.rearrange("(c p) n -> p c n", p=128)
                for c in range(8):
                    B.dma("pool", w_in_sb[:, c, :], wiv[:, c, :], S_win, writes=[S_win])
                pend_stats = []
                xt = [sb(p1, "xt%d" % i, [128, 2, D], F32) for i in range(2)]
                xn = [sb(p1, "xn%d" % i, [128, 2, D], BF16) for i in range(2)]
                hT = [sb(p1, "hT%d" % i, [128, 8, 258], BF16) for i in range(2)]
                junk = sb(p1, "junk", [128, D], BF16)
                ss = [sb(p1, "ss%d" % i, [128, 2], F32) for i in range(2)]
                rt = [sb(p1, "rt%d" % i, [128, 2], F32) for i in range(2)]
                rstd = [sb(p1, "rstd%d" % i, [128, 2], F32) for i in range(2)]
                kst = [sb(p1, "kst%d" % i, [128, 4, 256], BF16) for i in range(2)]
                qst = [sb(p1, "qst%d" % i, [128, 4, 256], BF16) for i in range(2)]
                cgs = [sb(p1, "cgs%d" % i, [128, 4, 256], BF16) for i in range(2)]
                sqc = sb(p1, "sqc", [128, 4, 256], BF16)
                kmst = sb(p1, "kmst", [128, 4, 32], F32)
                S_kmst = Slot()
                S_kmd = Slot("km_s")
                u_sb = [sb(p1, "u_sb%d" % i, [128, 258], F32) for i in range(2)]
                zb = [sb(p1, "zb%d" % i, [128, 258], F32) for i in range(2)]
                y0 = [sb(p1, "y0%d" % i, [128, 256], F32) for i in range(2)]
                y1 = [sb(p1, "y1%d" % i, [128, 256], F32) for i in range(2)]
                y2 = [sb(p1, "y2%d" % i, [128, 256], F32) for i in range(2)]
                cv = [sb(p1, "cv%d" % i, [128, 256], F32) for i in range(2)]
                tp = ps(p1, "tp", [128, 8, 256], BF16)
                pp = [ps(p1, "pp%d" % i, [128, 512], F32) for i in range(5)]
                pss = ps(p1, "pss", [128, 512], F32)
                S_xt = [Slot(), Slot()]
                S_xn = [Slot(), Slot()]
                S_hT = [Slot(), Slot()]
                S_junk = Slot()
                S_ss = [Slot(), Slot()]
                S_rt = [Slot(), Slot()]
                S_rstd = [Slot(), Slot()]
                S_kst = [Slot(), Slot()]
                S_qst = [Slot(), Slot()]
                S_cgs = [Slot(), Slot()]
                S_sqc = Slot()
                S_u = [Slot(), Slot()]
                S_z = [Slot(), Slot()]
                S_y0 = [Slot(), Slot()]
                S_y1 = [Slot(), Slot()]
                S_y2 = [Slot(), Slot()]
                S_cv = [Slot(), Slot()]
                S_tp = Slot()
                S_pp = [Slot() for _ in range(5)]
                S_pss = Slot()
                S_kT = Slot("kT_s")
                S_qT = Slot("qT_s")
                S_cgd = Slot("cg_s")
                ppi = [0]

                def next_pp():
                    i = ppi[0] % 5
                    ppi[0] += 1
                    return pp[i], S_pp[i]

                B.op("pool", lambda e: e.memset(V_all[:, :, :, 64:65], 1.0), writes=[S_V])
                for i in range(2):
                    B.op("pool", lambda e, i=i: e.memset(hT[i][:, :, 0:2], 0.0), writes=[S_hT[i]])
                xsv = xs.rearrange("(n t p) d -> n p t d", t=2, p=128)
                kTv = kT_s.rearrange("(pr two) d t -> (two d) pr t", two=2)
                qTv = qT_s.rearrange("(pr two) d t -> (two d) pr t", two=2)
                cgv = cg_s.rearrange("c p t -> p c t")
                evac_rr = [0]

                def evac(out_ap, in_ap, reads, writes, scale=None):
                    evac_rr[0] += 1
                    if evac_rr[0] % 2 == 0:
                        if scale is None:
                            return B.op("act", lambda e: e.activation(out=out_ap, in_=in_ap, func=AF.Copy), reads=reads, writes=writes)
                        return B.op("act", lambda e: e.activation(out=out_ap, in_=in_ap, func=AF.Copy, scale=scale), reads=reads, writes=writes)
                    if scale is None:
                        return B.op("dve", lambda e: e.tensor_copy(out=out_ap, in_=in_ap), reads=reads, writes=writes)
                    return B.op("dve", lambda e: e.tensor_scalar(out=out_ap, in0=in_ap, scalar1=scale, scalar2=None, op0=ALU.mult),
                                reads=reads, writes=writes)

                def f1a(s):
                    k = s % 2
                    B.dma("sp", xt[k][:], xsv[s], S_xt[k], writes=[S_xt[k]])
                    for t in range(2):
                        B.op("act", lambda e, k=k, t=t: e.activation(out=junk[:], in_=xt[k][:, t, :], func=AF.Square,
                                                                     accum_out=ss[k][:, t:t + 1]),
                             reads=[S_xt[k]], writes=[S_junk, S_ss[k]])
                    B.op("act", lambda e, k=k: e.activation(out=rt[k][:], in_=ss[k][:], func=AF.Sqrt, scale=1.0 / D, bias=epsc[:]),
                         reads=[S_ss[k], S_const], writes=[S_rt[k]])
                    B.op("dve", lambda e, k=k: e.reciprocal(out=rstd[k][:], in_=rt[k][:]), reads=[S_rt[k]], writes=[S_rstd[k]])
                    for t in range(2):
                        B.op("dve", lambda e, k=k, t=t: e.tensor_scalar(out=xn[k][:, t, :], in0=xt[k][:, t, :], scalar1=rstd[k][:, t:t + 1],
                                                                        scalar2=None, op0=ALU.mult),
                             reads=[S_xt[k], S_rstd[k]], writes=[S_xn[k]])

                def f1b(s):
                    k = s % 2
                    B.pe_group([tr(tp[:, c, t * 128:(t + 1) * 128], xn[k][:, t, c * 128:(c + 1) * 128], ident[:])
                                for t in range(2) for c in range(8)], reads=[S_xn[k], S_const], writes=[S_tp])
                    for c in range(8):
                        if c % 2 == 0:
                            B.op("act", lambda e, k=k, c=c: e.activation(out=hT[k][:, c, 2:258], in_=tp[:, c, :], func=AF.Identity,
                                                                         scale=G1[:, c:c + 1], bias=sh1[:, c:c + 1]),
                                 reads=[S_tp, S_G], writes=[S_hT[k]])
                        else:
                            B.op("dve", lambda e, k=k, c=c: e.tensor_scalar(out=hT[k][:, c, 2:258], in0=tp[:, c, :], scalar1=G1[:, c:c + 1],
                                                                            scalar2=sh1[:, c:c + 1], op0=ALU.mult, op1=ALU.add),
                                 reads=[S_tp, S_G], writes=[S_hT[k]])
                    if s == 16:
                        B.op("dve", lambda e, k=k: e.tensor_scalar(out=hT[k][:, :, 0:2], in0=hT[1 - k][:, :, 256:258], scalar1=hv[:, 0:1],
                                                                   scalar2=None, op0=ALU.mult),
                             reads=[S_hT[1 - k], S_const], writes=[S_hT[k]])
                    elif s > 16:
                        B.op("dve", lambda e, k=k: e.tensor_copy(out=hT[k][:, :, 0:2], in_=hT[1 - k][:, :, 256:258]),
                             reads=[S_hT[1 - k]], writes=[S_hT[k]])

                def back1a(s):
                    k = s % 2
                    for pr in range(4):
                        pt_, sp_ = next_pp()
                        B.pe_group([mm(pt_[:, 0:256], w_in_sb[:, c, 512 + pr * 128:512 + (pr + 1) * 128], hT[k][:, c, 2:258], c == 0, c == 7)
                                    for c in range(8)], reads=[S_win, S_hT[k]], writes=[sp_])
                        evac(kst[k][:, pr, :], pt_[:, 0:256], [sp_], [S_kst[k]])
                        B.op("dve", lambda e, k=k, pr=pr, s=s: e.tensor_reduce(out=kmst[:, pr, s:s + 1], in_=kst[k][:, pr, :], axis=AX.X, op=ALU.add),
                             reads=[S_kst[k]], writes=[S_kmst])
                    B.dma("sp", kTv[:, :, s * 256:(s + 1) * 256], kst[k][:], S_kst[k], reads=[S_kst[k]], writes=[S_kT])
                    for t in range(2):
                        pt_, sp_ = next_pp()
                        B.pe_group([mm(pt_[:, :], hT[k][:, c, 2 + t * 128:2 + (t + 1) * 128], w_in_sb[:, c, 1024:1536], c == 0, c == 7)
                                    for c in range(8)], reads=[S_win, S_hT[k]], writes=[sp_])
                        evac(V_all[:, 2 * s + t, :, 0:64], pt_[:, :].rearrange("p (h d) -> p h d", d=64), [sp_], [S_V])

                def back1b(s):
                    k = s % 2
                    if s < 15:
                        return
                    qb = s - 15
                    for pr in range(4):
                        pt_, sp_ = next_pp()
                        B.pe_group([mm(pt_[:, 0:256], w_in_sb[:, c, pr * 128:(pr + 1) * 128], hT[k][:, c, 2:258], c == 0, c == 7)
                                    for c in range(8)], reads=[S_win, S_hT[k]], writes=[sp_])
                        evac(qst[k][:, pr, :], pt_[:, 0:256], [sp_], [S_qst[k]], scale=0.125)
                    B.dma("sp", qTv[:, :, qb * 256:(qb + 1) * 256], qst[k][:], S_qst[k], reads=[S_qst[k]], writes=[S_qT])
                    for ch in range(4):
                        j = ch % 2
                        pu_, spu = next_pp()
                        B.pe_group([mm(pu_[:, 0:258], w_in_sb[:, c, 1536 + ch * 128:1536 + (ch + 1) * 128], hT[k][:, c, 0:258], c == 0, c == 7)
                                    for c in range(8)], reads=[S_win, S_hT[k]], writes=[spu])
                        pc_, spc = next_pp()
                        B.pe_group([mm(pc_[:, 0:258], w_in_sb[:, c, 2048 + ch * 128:2048 + (ch + 1) * 128], hT[k][:, c, 0:258], c == 0, c == 7)
                                    for c in range(8)], reads=[S_win, S_hT[k]], writes=[spc])
                        pb_, spb = next_pp()
                        B.pe_group([mm(pb_[:, 0:256], w_in_sb[:, c, 2560 + ch * 128:2560 + (ch + 1) * 128], hT[k][:, c, 2:258], c == 0, c == 7)
                                    for c in range(8)], reads=[S_win, S_hT[k]], writes=[spb])
                        B.op("act", lambda e, j=j, pu_=pu_: e.activation(out=u_sb[j][:], in_=pu_[:, 0:258], func=AF.Copy),
                             reads=[spu], writes=[S_u[j]])
                        B.op("dve", lambda e, j=j, pc_=pc_: e.tensor_tensor(out=zb[j][:], in0=pc_[:, 0:258], in1=u_sb[j][:], op=ALU.mult),
                             reads=[spc, S_u[j]], writes=[S_z[j]])
                        B.op("act", lambda e, j=j, ch=ch: e.activation(out=y0[j][:], in_=zb[j][:, 2:258], func=AF.Identity,
                                                                       scale=convw_sb[:, 8 + ch:9 + ch], bias=convb_sb[:, ch:ch + 1]),
                             reads=[S_z[j], S_const], writes=[S_y0[j]])
                        B.op("dve", lambda e, j=j, ch=ch: e.scalar_tensor_tensor(out=y1[j][:], in0=zb[j][:, 1:257], scalar=convw_sb[:, 4 + ch:5 + ch],
                                                                                 in1=y0[j][:], op0=ALU.mult, op1=ALU.add),
                             reads=[S_z[j], S_y0[j], S_const], writes=[S_y1[j]])
                        B.op("dve", lambda e, j=j, ch=ch: e.scalar_tensor_tensor(out=y2[j][:], in0=zb[j][:, 0:256], scalar=convw_sb[:, ch:ch + 1],
                                                                                 in1=y1[j][:], op0=ALU.mult, op1=ALU.add),
                             reads=[S_z[j], S_y1[j], S_const], writes=[S_y2[j]])
                        B.op("dve", lambda e, j=j, pb_=pb_: e.tensor_tensor(out=cv[j][:], in0=pb_[:, 0:256], in1=y2[j][:], op=ALU.mult),
                             reads=[spb, S_y2[j]], writes=[S_cv[j]])
                        B.op("act", lambda e, j=j, ch=ch: e.activation(out=sqc[:, ch, :], in_=cv[j][:], func=AF.Square),
                             reads=[S_cv[j]], writes=[S_sqc])
                        B.op("act", lambda e, j=j, ch=ch, k=k: e.activation(out=cgs[k][:, ch, :], in_=cv[j][:], func=AF.Copy,
                                                                           scale=gconv_sb[:, ch:ch + 1]),
                             reads=[S_cv[j], S_const], writes=[S_cgs[k]])
                    B.dma("sp", cgv[:, :, qb * 256:(qb + 1) * 256], cgs[k][:], S_cgs[k], reads=[S_cgs[k]], writes=[S_cgd])

                    def stats(qb=qb):
                        for t in range(2):
                            B.pe_group([mm(pss[:, t:t + 1], sqc[:, ch, t * 128:(t + 1) * 128], ones_b[:, 0:1], ch == 0, ch == 3) for ch in range(4)],
                                       reads=[S_sqc, S_const], writes=[S_pss])
                        B.op("dve", lambda e: e.tensor_copy(out=ssc[:, 2 * qb:2 * qb + 2], in_=pss[:, 0:2]), reads=[S_pss], writes=[S_ssc])
                    pend_stats.append(stats)

                f1a(0)
                f1b(0)
                f1a(1)
                for s in range(32):
                    if s + 1 < 32:
                        f1b(s + 1)
                    if s + 2 < 32:
                        f1a(s + 2)
                    back1a(s)
                    while pend_stats:
                        pend_stats.pop(0)()
                    if s == 5:
                        wov = w_out.rearrange("(c p) n -> p c n", p=128)
                        for c in range(8):
                            B.dma("pool", wo[:, c, :], wov[:, c, :], S_wo, writes=[S_wo])
                    back1b(s)
                while pend_stats:
                    pend_stats.pop(0)()
                B.dma("sp", km_s.rearrange("(pr two) d j -> (two d) pr j", two=2), kmst[:], S_kmst, reads=[S_kmst], writes=[S_kmd])
                B.emit_block()

            with ExitStack() as p2:
              if PHASES >= 2:
                kaug = [sb(p2, "kaug%d" % i, [99, S], BF16) for i in range(2)]
                qaug = [sb(p2, "qaug%d" % i, [99, QTOK], BF16) for i in range(2)]
                qa = [sb(p2, "qa%d" % i, [128, NQT, 99], BF16) for i in range(2)]
                atst = [sb(p2, "atst%d" % i, [64, QTOK], BF16) for i in range(2)]
                gvb = sb(p2, "gvb", [128, NQB, 32], F32)
                gvb2 = sb(p2, "gvb2", [128, NQB, 32], F32)
                cm = sb(p2, "cm", [128, 2, 256], BF16)
                km_f = [sb(p2, "km_f%d" % i, [64, 32], F32) for i in range(2)]
                km_b = [sb(p2, "km_b%d" % i, [64, 32], BF16) for i in range(2)]
                gm = [sb(p2, "gm%d" % i, [128, 32], F32) for i in range(2)]
                t8 = [sb(p2, "t8%d" % i, [128, 8], F32) for i in range(2)]
                tsel = [sb(p2, "tsel%d" % i, [128, 32], F32) for i in range(2)]
                pT = [sb(p2, "pT%d" % i, [128, 2, 256], BF16) for i in range(3)]
                rc = [sb(p2, "rc%d" % i, [128, 256], F32) for i in range(4)]
                pos = [sb(p2, "pos%d" % i, [128, 256], F32) for i in range(4)]
                atf = [sb(p2, "atf%d" % i, [64, 256], F32) for i in range(4)]
                sqa = [sb(p2, "sqa%d" % i, [64, 256], BF16) for i in range(4)]
                sp = [ps(p2, "sp%d" % i, [128, 2, 256], F32) for i in range(3)]
                po = [ps(p2, "po%d" % i, [128, 512], F32) for i in range(2)]
                pgt = ps(p2, "pgt", [128, 512], F32)
                pbc = ps(p2, "pbc", [128, 512], F32)
                ptt_t = ps(p2, "ptt", [128, 1024], BF16)
                ptt = ptt_t[:, 0:128]
                S_kaug = [Slot(), Slot()]
                S_kstat = Slot()
                S_qq = [Slot(), Slot()]
                S_qm = [Slot(), Slot()]
                S_qa = [Slot(), Slot()]
                S_atst = [Slot(), Slot()]
                S_c2 = Slot()
                S_kmf = [Slot(), Slot()]
                S_kmb = [Slot(), Slot()]
                S_gm = [Slot(), Slot()]
                S_t8 = [Slot(), Slot()]
                S_tsel = [Slot(), Slot()]
                S_pT = [Slot() for _ in range(3)]
                S_rc = [Slot() for _ in range(4)]
                S_pos = [Slot() for _ in range(4)]
                S_atf = [Slot() for _ in range(4)]
                S_sqa = [Slot() for _ in range(4)]
                S_sp = [Slot() for _ in range(3)]
                S_po = [Slot(), Slot()]
                S_pgt, S_ptt, S_pbc = Slot(), Slot(), Slot()
                S_pss2 = S_pbc
                S_atd = Slot("at_s")

                B.dma("sp", gvb[:].rearrange("p a b -> p (a b)"), gvb_d, S_c2, writes=[S_c2])
                B.dma("sp", gvb2[:].rearrange("p a b -> p (a b)"), gvb2_d, S_c2, writes=[S_c2])
                B.dma("sp", cm[:].rearrange("p a b -> p (a b)"), cmask, S_c2, writes=[S_c2])
                for i in range(2):
                    B.dma("sp", kaug[i][64:99, :], kstat, S_kstat, writes=[S_kstat])

                def load_head(h):
                    hb = h % 2
                    B.dma("sp", kaug[hb][0:64, :], kT_s[h], S_kaug[hb], reads=[S_kT], writes=[S_kaug[hb]])
                    B.dma("sp", qaug[hb][0:64, :], qT_s[h], S_qq[hb], reads=[S_qT], writes=[S_qq[hb]])
                    B.dma("sp", qa[hb][:].rearrange("p a b -> p (a b)"), qstat[h], S_qa[hb], writes=[S_qa[hb]])

                def prep_head(h):
                    hb = h % 2
                    B.dma("sp", km_f[hb][:], km_s[h], S_kmf[hb], reads=[S_kmd], writes=[S_kmf[hb]])
                    B.op("dve", lambda e: e.tensor_scalar(out=km_b[hb][:], in0=km_f[hb][:], scalar1=1.0 / 256, scalar2=None, op0=ALU.mult),
                         reads=[S_kmf[hb]], writes=[S_kmb[hb]])

                def mask_a(h, qt):
                    hb = h % 2
                    qb = qt // 2
                    g2 = qt % 2
                    B.pe_group([mm(pgt[:, 0:32], qaug[hb][0:64, qt * 128:(qt + 1) * 128], km_b[hb][:, :], True, True)],
                               reads=[S_qq[hb], S_kmb[hb]], writes=[S_pgt])
                    B.op("dve", lambda e: e.tensor_tensor(out=gm[g2][:], in0=pgt[:, 0:32], in1=gvb[:, qb, :], op=ALU.add),
                         reads=[S_pgt, S_c2], writes=[S_gm[g2]])
                    B.op("dve", lambda e: e.max(out=t8[g2][:], in_=gm[g2][:]), reads=[S_gm[g2]], writes=[S_t8[g2]])
                    B.op("dve", lambda e: e.tensor_scalar(out=tsel[g2][:], in0=gm[g2][:], scalar1=t8[g2][:, 2:3], scalar2=-NEG,
                                                          op0=ALU.is_ge, op1=ALU.mult),
                         reads=[S_gm[g2], S_t8[g2]], writes=[S_tsel[g2]])
                    B.op("dve", lambda e: e.tensor_tensor(out=qa[hb][:, qt, 64:96], in0=tsel[g2][:], in1=gvb2[:, qb, :], op=ALU.add),
                         reads=[S_tsel[g2], S_c2], writes=[S_qa[hb]])

                def mask_b(h, qt):
                    hb = h % 2
                    B.pe_group([tr(ptt[0:99, :], qa[hb][:, qt, :], ident[:])], reads=[S_qa[hb], S_const], writes=[S_ptt])
                    B.op("dve", lambda e: e.tensor_copy(out=qaug[hb][64:99, qt * 128:(qt + 1) * 128], in_=ptt[64:99, :]),
                         reads=[S_ptt], writes=[S_qm[hb]])

                sctr = [0]
                KEEP = []
                for h_ in range(NH):
                    slope_ = 2.0 ** (-(h_ + 1))
                    kk_ = 0
                    while kk_ < 31 and slope_ * (kk_ * 256 + 1) < 80.0:
                        kk_ += 1
                    KEEP.append(kk_)

                def s_mm(h, qb, j):
                    hb = h % 2
                    sB = 15 + qb
                    qc = slice(qb * 256, (qb + 1) * 256)
                    i = sctr[0] % 3
                    sctr[0] += 1
                    fns = []
                    for kt in range(2):
                        fns.append(mm(sp[i][:, kt, :], kaug[hb][0:99, (2 * j + kt) * 128:(2 * j + kt + 1) * 128], qaug[hb][0:99, qc],
                                      True, j != sB))
                        if j == sB:
                            fns.append(mm(sp[i][:, kt, :], ident[:], cm[:, kt, :], False, True))
                    B.pe_group(fns, reads=[S_kaug[hb], S_kstat, S_qq[hb], S_qm[hb], S_c2, S_const], writes=[S_sp[i]])
                    B.op("act", lambda e: e.activation(out=pT[i][:], in_=sp[i][:], func=AF.Exp), reads=[S_sp[i]], writes=[S_pT[i]])
                    return i

                def pv_mm(h, qb, j, i, first, last):
                    ob = qb % 2
                    B.pe_group([mm(po[ob][0:65, 0:256], V_all[:, 2 * j + kt, h, :], pT[i][:, kt, :], (first and kt == 0), (last and kt == 1))
                                for kt in range(2)], reads=[S_V, S_pT[i]], writes=[S_po[ob]])

                tctr = [0]

                def tail_a(h, qb):
                    ob = qb % 2
                    r = tctr[0] % 4
                    tctr[0] += 1
                    B.op("act", lambda e: e.activation(out=pos[r][0:65, :], in_=po[ob][0:65, 0:256], func=AF.Copy), reads=[S_po[ob]], writes=[S_pos[r]])
                    B.op("dve", lambda e: e.reciprocal(out=rc[r][64:65, :], in_=pos[r][64:65, :]), reads=[S_pos[r]], writes=[S_rc[r]])
                    return r

                def tail_b(h, qb, r):
                    hb = h % 2
                    qc = slice(qb * 256, (qb + 1) * 256)
                    B.pe_group([mm(pbc[0:64, 0:256], ones_f[64:65, 0:64], rc[r][64:65, :], True, True)],
                               reads=[S_rc[r], S_const], writes=[S_pbc])
                    B.op("dve", lambda e: e.tensor_tensor(out=atf[r][:], in0=pbc[0:64, 0:256], in1=pos[r][0:64, :], op=ALU.mult),
                         reads=[S_pbc, S_pos[r]], writes=[S_atf[r]])
                    B.op("pool", lambda e: e.tensor_tensor(out=sqa[r][:], in0=atf[r][:], in1=atf[r][:], op=ALU.mult), reads=[S_atf[r]], writes=[S_sqa[r]])
                    B.op("pool", lambda e: e.tensor_scalar(out=atst[hb][:, qc], in0=atf[r][:], scalar1=gattn_sb[:, h:h + 1], scalar2=None, op0=ALU.mult),
                         reads=[S_atf[r], S_const], writes=[S_atst[hb]])

                def tail_c(h, qb, r):
                    B.pe_group([mm(pbc[:, 256 + t:257 + t], sqa[r][:, t * 128:(t + 1) * 128], ones_b[0:64, 0:1], True, True) for t in range(2)],
                               reads=[S_sqa[r], S_const], writes=[S_pss2])
                    if h == 0:
                        B.op("dve", lambda e: e.tensor_copy(out=ssa[:, 2 * qb:2 * qb + 2], in_=pbc[:, 256:258]), reads=[S_pss2], writes=[S_ssa])
                    else:
                        B.op("dve", lambda e: e.tensor_tensor(out=ssa[:, 2 * qb:2 * qb + 2], in0=pbc[:, 256:258], in1=ssa[:, 2 * qb:2 * qb + 2],
                                                              op=ALU.add),
                             reads=[S_pss2, S_ssa], writes=[S_ssa])

                def blocks_of(h, qb):
                    sB = 15 + qb
                    return list(range(max(0, sB - KEEP[h]), sB + 1))

                load_head(0)
                prep_head(0)
                for qt in range(NQT):
                    mask_a(0, qt)
                    mask_b(0, qt)
                for h in range(NH):
                    hb = h % 2
                    if h + 1 < NH:
                        load_head(h + 1)
                        prep_head(h + 1)
                    total_iter = sum(len(blocks_of(h, qb)) for qb in range(NQB))
                    sched = {}
                    if h + 1 < NH:
                        step = max(1, (total_iter - 8) // NQT)
                        for qt in range(NQT):
                            ia = min(total_iter - 1, 1 + qt * step)
                            ib = min(total_iter - 1, ia + 3)
                            sched.setdefault(ia, []).append(lambda qt=qt: mask_a(h + 1, qt))
                            sched.setdefault(ib, []).append(lambda qt=qt: mask_b(h + 1, qt))
                    it = 0
                    tails = []
                    for qb in range(NQB):
                        js = blocks_of(h, qb)
                        n = len(js)
                        ids = {0: s_mm(h, qb, js[0])}
                        if n > 1:
                            ids[1] = s_mm(h, qb, js[1])
                        for i in range(n):
                            if i + 2 < n:
                                ids[i + 2] = s_mm(h, qb, js[i + 2])
                            pv_mm(h, qb, js[i], ids[i], i == 0, i == n - 1)
                            for fn in sched.pop(it, []):
                                fn()
                            for item in list(tails):
                                if item[0] <= it:
                                    item[1]()
                                    tails.remove(item)
                            it += 1
                        r = tail_a(h, qb)
                        tails.append((it + 5, lambda h=h, qb=qb, r=r: tail_b(h, qb, r)))
                        tails.append((it + 10, lambda h=h, qb=qb, r=r: tail_c(h, qb, r)))
                    for key in sorted(sched):
                        for fn in sched[key]:
                            fn()
                    for item in tails:
                        item[1]()
                    B.dma("sp", at_s[h], atst[hb][:], S_atst[hb], reads=[S_atst[hb]], writes=[S_atd])
                B.emit_block()

        with ExitStack() as p3:
          if PHASES >= 3:
            wu = sb(p3, "wu", [128, 8, 2 * DFF], BF16)
            wd = sb(p3, "wd", [128, 22, D], BF16)
            x1 = [sb(p3, "x1%d" % i, [128, 2, D], F32) for i in range(2)]
            atb = sb(p3, "atb", [128, 4, 256], BF16)
            cgb = sb(p3, "cgb", [128, 4, 256], BF16)
            xn2 = sb(p3, "xn2", [128, 2, D], BF16)
            h2T = [sb(p3, "h2T%d" % i, [128, 8, 258], BF16) for i in range(2)]
            YA = [sb(p3, "YA%d" % i, [128, 256], F32) for i in range(2)]
            YB = [sb(p3, "YB%d" % i, [128, 256], F32) for i in range(2)]
            YV = [sb(p3, "YV%d" % i, [128, 256], F32) for i in range(2)]
            YG = [sb(p3, "YG%d" % i, [128, 256], F32) for i in range(2)]
            actT = sb(p3, "actT", [128, 22, 256], BF16)
            gfb = sb(p3, "gfb", [128, D], F32)
            rsa = sb(p3, "rsa", [128, NQT], F32)
            rsc = sb(p3, "rsc", [128, NQT], F32)
            ssf = sb(p3, "ssf", [128, 2], F32)
            rtf = sb(p3, "rtf", [128, 2], F32)
            rsf = sb(p3, "rsf", [128, 2], F32)
            ssb = sb(p3, "ssb", [128, 2], F32)
            rtb = sb(p3, "rtb", [128, 2], F32)
            rsb = sb(p3, "rsb", [128, 2], F32)
            pu = [ps(p3, "pu%d" % i, [128, 512], F32) for i in range(4)]
            pd = [ps(p3, "pd%d" % i, [128, 512], F32) for i in range(2)]
            pac = [ps(p3, "pac%d" % i, [128, 512], F32) for i in range(2)]
            S_wu, S_wd = Slot(), Slot()
            S_x1 = [Slot(), Slot()]
            S_atb, S_cgb, S_xn2 = Slot(), Slot(), Slot()
            S_h2T = [Slot(), Slot()]
            S_YA = [Slot() for _ in range(2)]
            S_YB = [Slot() for _ in range(2)]
            S_YV = [Slot(), Slot()]
            S_YG = [Slot(), Slot()]
            S_actT, S_gfb, S_rs = Slot(), Slot(), Slot()
            S_ssf, S_rtf, S_rsf, S_ssb, S_rtb, S_rsb = Slot(), Slot(), Slot(), Slot(), Slot(), Slot()
            S_pu = [Slot() for _ in range(4)]
            S_pd = [Slot(), Slot()]
            S_pac = [Slot(), Slot()]
            gtb = x1[1][:, 0, :]
            S_gtb = S_x1[1]

            wov = w_out.rearrange("(c p) n -> p c n", p=128)
            wuv = w_up.rearrange("(c p) n -> p c n", p=128)
            wdv = w_down.rearrange("(c p) n -> p c n", p=128)
            for c in range(8):
                B.dma("pool", wu[:, c, :], wuv[:, c, :], S_wu, writes=[S_wu])
            for c0 in range(0, 22, 2):
                B.dma("pool", wd[:, c0:c0 + 2, :], wdv[:, c0:c0 + 2, :], S_wd, writes=[S_wd])
            B.dma("sp", gfb[:], gfin, S_gfb, writes=[S_gfb])
            mod_t = mod_s.tensor
            B.dma("sp", gtb, bass.AP(mod_t, 2048, [[0, 128], [1, D]]), S_gtb, reads=[S_mods], writes=[S_gtb])
            for c in range(8):
                B.op("pool", lambda e, c=c: e.tensor_tensor(out=wo[:, c, :], in0=wo[:, c, :], in1=gtb, op=ALU.mult),
                     reads=[S_gtb], writes=[S_wo])
            B.dma("sp", gtb, bass.AP(mod_t, 5120, [[0, 128], [1, D]]), S_gtb, reads=[S_mods], writes=[S_gtb])
            for c in range(22):
                B.op("pool", lambda e, c=c: e.tensor_tensor(out=wd[:, c, :], in0=wd[:, c, :], in1=gtb, op=ALU.mult),
                     reads=[S_gtb], writes=[S_wd])
            for src_, dst, ssl in ((ssa, rsa, S_ssa), (ssc, rsc, S_ssc)):
                B.op("act", lambda e, src_=src_, dst=dst: e.activation(out=dst[:], in_=src_[:], func=AF.Sqrt, scale=1.0 / 512, bias=epsc[:]),
                     reads=[ssl, S_const], writes=[S_rs])
                B.op("dve", lambda e, dst=dst: e.reciprocal(out=dst[:], in_=dst[:]), reads=[S_rs], writes=[S_rs])
            for i in range(2):
                B.op("dve", lambda e, i=i: e.memset(h2T[i][:, :, 0:2], 0.0), writes=[S_h2T[i]])

            xsv = xs.rearrange("(n t p) d -> n p t d", t=2, p=128)
            outv = out.rearrange("(n t p) d -> n p t d", t=2, p=128)
            atv = at_s.rearrange("(pr two) d t -> (two d) pr t", two=2)
            cgv = cg_s.rearrange("c p t -> p c t")
            puc = [0]
            ybc = [0]
            yac = [0]
            pend_fin = []
            tpv = [pac[i][:, :].bitcast(BF16).rearrange("p (c t) -> p c t", t=256) for i in range(2)]

            def next_yb():
                i = ybc[0] % 6
                ybc[0] += 1
                return yb[i], S_yb[i]

            def f3a_dma(qb):
                k = qb % 2
                sl = 15 + qb
                qc = slice(qb * 256, (qb + 1) * 256)
                B.dma("sp", x1[k][:], xsv[sl], S_x1[k], writes=[S_x1[k]])
                B.dma("sp", atb[:], atv[:, :, qc], S_atb, reads=[S_atd], writes=[S_atb])
                B.dma("sp", cgb[:], cgv[:, :, qc], S_cgb, reads=[S_cgd], writes=[S_cgb])

            def f3a(qb):
                k = qb % 2
                for t in range(2):
                    tile = 2 * qb + t
                    for n in range(2):
                        ns = slice(n * 512, (n + 1) * 512)
                        B.pe_group([mm(pac[0][:, :], atb[:, c, t * 128:(t + 1) * 128], wo[:, c, ns], c == 0, c == 3) for c in range(4)],
                                   reads=[S_atb, S_wo], writes=[S_pac[0]])
                        B.pe_group([mm(pac[1][:, :], cgb[:, c, t * 128:(t + 1) * 128], wo[:, 4 + c, ns], c == 0, c == 3) for c in range(4)],
                                   reads=[S_cgb, S_wo], writes=[S_pac[1]])
                        B.op("dve", lambda e, t=t, ns=ns, tile=tile: e.scalar_tensor_tensor(
                            out=x1[k][:, t, ns], in0=pac[0][:, :], scalar=rsa[:, tile:tile + 1], in1=x1[k][:, t, ns], op0=ALU.mult, op1=ALU.add),
                            reads=[S_pac[0], S_rs], writes=[S_x1[k]])
                        B.op("dve", lambda e, t=t, ns=ns, tile=tile: e.scalar_tensor_tensor(
                            out=x1[k][:, t, ns], in0=pac[1][:, :], scalar=rsc[:, tile:tile + 1], in1=x1[k][:, t, ns], op0=ALU.mult, op1=ALU.add),
                            reads=[S_pac[1], S_rs], writes=[S_x1[k]])
                for t in range(2):
                    B.op("act", lambda e, t=t: e.activation(out=xn2[:, t, :], in_=x1[k][:, t, :], func=AF.Square, accum_out=ssf[:, t:t + 1]),
                         reads=[S_x1[k]], writes=[S_xn2, S_ssf])
                B.op("act", lambda e: e.activation(out=rtf[:], in_=ssf[:], func=AF.Sqrt, scale=1.0 / D, bias=epsc[:]),
                     reads=[S_ssf, S_const], writes=[S_rtf])
                B.op("dve", lambda e: e.reciprocal(out=rsf[:], in_=rtf[:]), reads=[S_rtf], writes=[S_rsf])
                for t in range(2):
                    B.op("dve", lambda e, t=t: e.tensor_scalar(out=xn2[:, t, :], in0=x1[k][:, t, :], scalar1=rsf[:, t:t + 1], scalar2=None,
                                                               op0=ALU.mult),
                         reads=[S_x1[k], S_rsf], writes=[S_xn2])

            def f3b(qb):
                k = qb % 2
                for half in range(2):
                    B.pe_group([tr(tpv[half][:, c, t * 128:(t + 1) * 128], xn2[:, t, (half * 4 + c) * 128:(half * 4 + c + 1) * 128], ident[:])
                                for t in range(2) for c in range(4)], reads=[S_xn2, S_const], writes=[S_pac[half]])
                    for c in range(4):
                        cc = half * 4 + c
                        B.op("act", lambda e, c=c, cc=cc, half=half: e.activation(out=h2T[k][:, cc, 2:258], in_=tpv[half][:, c, :],
                                                                                 func=AF.Identity, scale=G2[:, cc:cc + 1], bias=sh2[:, cc:cc + 1]),
                             reads=[S_pac[half], S_G], writes=[S_h2T[k]])
                if qb == 1:
                    B.op("dve", lambda e: e.tensor_scalar(out=h2T[k][:, :, 0:2], in0=h2T[1 - k][:, :, 256:258], scalar1=hv[:, 0:1],
                                                          scalar2=None, op0=ALU.mult),
                         reads=[S_h2T[1 - k], S_const], writes=[S_h2T[k]])
                elif qb > 1:
                    B.op("dve", lambda e: e.tensor_copy(out=h2T[k][:, :, 0:2], in_=h2T[1 - k][:, :, 256:258]),
                         reads=[S_h2T[1 - k]], writes=[S_h2T[k]])

            def back3(qb):
                k = qb % 2
                for v in range(22):
                    if qb + 1 < NQB and v == 3:
                        f3a_dma(qb + 1)
                    p2_ = v % 2
                    chs = (v, 22 + v)
                    pis = []
                    for ch in chs:
                        i = puc[0] % 4
                        puc[0] += 1
                        pis.append(i)
                        B.pe_group([mm(pu[i][:, 0:258], wu[:, c, ch * 128:(ch + 1) * 128], h2T[k][:, c, 0:258], c == 0, c == 7) for c in range(8)],
                                   reads=[S_wu, S_h2T[k]], writes=[S_pu[i]])
                    yas = []
                    for w_, (ch, i) in enumerate(zip(chs, pis)):
                        ai = yac[0] % 2
                        yac[0] += 1
                        yas.append(ai)
                        B.op("act", lambda e, i=i, ai=ai, ch=ch: e.activation(out=YA[ai][:], in_=pu[i][:, 2:258], func=AF.Identity,
                                                                             scale=fcw_sb[:, 88 + ch:89 + ch], bias=fcb_sb[:, ch:ch + 1]),
                             reads=[S_pu[i], S_const], writes=[S_YA[ai]])
                    if pend_fin:
                        pend_fin.pop(0)()
                    ybs = []
                    for w_, (ch, i) in enumerate(zip(chs, pis)):
                        bi = ybc[0] % 2
                        ybc[0] += 1
                        ybs.append(bi)
                        ai = yas[w_]
                        B.op("dve", lambda e, i=i, ai=ai, bi=bi, ch=ch: e.scalar_tensor_tensor(
                            out=YB[bi][:], in0=pu[i][:, 1:257], scalar=fcw_sb[:, 44 + ch:45 + ch], in1=YA[ai][:], op0=ALU.mult, op1=ALU.add),
                            reads=[S_pu[i], S_YA[ai], S_const], writes=[S_YB[bi]])
                    outs = ((YV[p2_], S_YV[p2_]), (YG[p2_], S_YG[p2_]))
                    for w_, (ch, i) in enumerate(zip(chs, pis)):
                        bi = ybs[w_]
                        yo, syo = outs[w_]
                        B.op("dve", lambda e, i=i, bi=bi, yo=yo, ch=ch: e.scalar_tensor_tensor(
                            out=yo[:], in0=pu[i][:, 0:256], scalar=fcw_sb[:, ch:ch + 1], in1=YB[bi][:], op0=ALU.mult, op1=ALU.add),
                            reads=[S_pu[i], S_YB[bi], S_const], writes=[syo])
                    def fin_pair(p2_=p2_, v=v):
                        B.op("act", lambda e: e.activation(out=YG[p2_][:], in_=YG[p2_][:], func=AF.Silu), reads=[S_YG[p2_]], writes=[S_YG[p2_]])
                        B.op("pool", lambda e: e.tensor_tensor(out=actT[:, v, :], in0=YV[p2_][:], in1=YG[p2_][:], op=ALU.mult),
                             reads=[S_YV[p2_], S_YG[p2_]], writes=[S_actT])
                    pend_fin.append(fin_pair)
                if pend_fin:
                    pend_fin.pop(0)()
                if qb + 1 < NQB:
                    f3a(qb + 1)
                for t in range(2):
                    if t == 1 and qb + 1 < NQB:
                        f3b(qb + 1)
                    for n in range(2):
                        B.pe_group([mm(pd[n][:, :], actT[:, v, t * 128:(t + 1) * 128], wd[:, v, n * 512:(n + 1) * 512], v == 0, v == 21)
                                    for v in range(22)], reads=[S_actT, S_wd], writes=[S_pd[n]])
                        B.op("dve", lambda e, t=t, n=n: e.tensor_tensor(out=x1[k][:, t, n * 512:(n + 1) * 512], in0=pd[n][:, :],
                                                                        in1=x1[k][:, t, n * 512:(n + 1) * 512], op=ALU.add),
                             reads=[S_pd[n]], writes=[S_x1[k]])
                for t in range(2):
                    B.op("act", lambda e, t=t: e.activation(out=xn2[:, t, :], in_=x1[k][:, t, :], func=AF.Square, accum_out=ssb[:, t:t + 1]),
                         reads=[S_x1[k]], writes=[S_xn2, S_ssb])
                B.op("act", lambda e: e.activation(out=rtb[:], in_=ssb[:], func=AF.Sqrt, scale=1.0 / D, bias=epsc[:]),
                     reads=[S_ssb, S_const], writes=[S_rtb])
                B.op("dve", lambda e: e.reciprocal(out=rsb[:], in_=rtb[:]), reads=[S_rtb], writes=[S_rsb])
                for t in range(2):
                    B.op("dve", lambda e, t=t: e.scalar_tensor_tensor(out=x1[k][:, t, :], in0=x1[k][:, t, :], scalar=rsb[:, t:t + 1],
                                                                      in1=gfb[:], op0=ALU.mult, op1=ALU.mult),
                         reads=[S_rsb, S_gfb], writes=[S_x1[k]])
                B.dma("sp", outv[qb - 1], x1[k][:], S_x1[k], reads=[S_x1[k]])

            f3a_dma(0)
            f3a(0)
            f3b(0)
            f3a_dma(1)
            f3a(1)
            f3b(1)
            for qb in range(1, NQB):
                back3(qb)
            B.emit_block()
    return nc


_NC_CACHE = {}


def _consts(half):
    bf = ml_dtypes.bfloat16
    ident = np.eye(128, dtype=np.float32).astype(bf)
    key = np.arange(S)
    kstat = np.zeros((35, S), np.float32)
    kstat[key // 256, key] = 1.0
    kstat[32] = key % 256
    kstat[33] = key // 256
    kstat[34] = 1.0
    kstat = kstat.astype(bf)
    p = np.arange(128)[:, None, None]
    kt = np.arange(2)[None, :, None]
    a = np.arange(256)[None, None, :]
    cmask = np.where(kt * 128 + p <= a, 0.0, NEG).astype(np.float32).reshape(128, 512).astype(bf)
    gvb = np.zeros((NQB, 32), np.float32)
    gvb2 = np.zeros((NQB, 32), np.float32)
    for qb in range(NQB):
        sB = 15 + qb
        for j in range(32):
            valid = (j < sB) and (half == 1 or j >= 16)
            gvb[qb, j] = 0.0 if valid else -1.0e9
            gvb2[qb, j] = NEG if valid else 2 * NEG
        gvb[qb, sB] = -3.0e9
        gvb2[qb, sB] = 0.0
    gvb = np.ascontiguousarray(np.broadcast_to(gvb.reshape(1, -1), (128, NQB * 32))).astype(np.float32)
    gvb2 = np.ascontiguousarray(np.broadcast_to(gvb2.reshape(1, -1), (128, NQB * 32))).astype(np.float32)
    qstat = np.zeros((NH, 128, NQT, 99), np.float32)
    for h in range(NH):
        slope = 2.0 ** (-(h + 1))
        for qt in range(NQT):
            sB = 15 + qt // 2
            apos = (qt % 2) * 128 + np.arange(128)
            qstat[h, :, qt, 96] = slope
            qstat[h, :, qt, 97] = 256.0 * slope
            qstat[h, :, qt, 98] = -slope * (256.0 * sB + apos)
    qstat = qstat.reshape(NH, 128, NQT * 99).astype(bf)
    hv = np.full((128, 1), float(half), np.float32)
    return dict(ident=ident, kstat=kstat, cmask=cmask, gvb=gvb, gvb2=gvb2, qstat=qstat, hv=hv)


def _pc(v, nchunk):
    return np.ascontiguousarray(np.asarray(v, np.float32).reshape(nchunk, 128).T)


def kernel(x, c, w_ada, b_ada, g_mix, w_in, conv_w, conv_b, g_attn_out, g_conv_out, w_out, g_ffn,
           w_up, ffn_conv_w, ffn_conv_b, w_down, g_final):
    f = lambda a: np.ascontiguousarray(np.asarray(a, dtype=np.float32))
    x = f(x)
    c = f(c)
    if "nc" not in _NC_CACHE:
        _NC_CACHE["nc"] = build_program()
    nc = _NC_CACHE["nc"]
    shared = dict(
        w_ada=f(w_ada[0]), b_ada=f(b_ada[0]).reshape(1, -1), gmix=_pc(g_mix[0], 8), w_in=f(w_in[0]),
        convw=np.ascontiguousarray(np.concatenate([_pc(conv_w[0][kk], 4) for kk in range(3)], axis=1)),
        convb=_pc(conv_b[0], 4),
        gattn=np.ascontiguousarray(f(g_attn_out[0]).reshape(8, 64).T),
        gconv=_pc(g_conv_out[0], 4), w_out=f(w_out[0]), gffn=_pc(g_ffn[0], 8), w_up=f(w_up[0]),
        fcw=np.ascontiguousarray(np.concatenate([_pc(ffn_conv_w[0][kk], 44) for kk in range(3)], axis=1)),
        fcb=_pc(ffn_conv_b[0], 44), w_down=f(w_down[0]),
        gfin=np.ascontiguousarray(np.broadcast_to(f(g_final).reshape(1, -1), (128, D))),
    )
    cst = [_consts(0), _consts(1)]
    in_maps = []
    for i in range(NCORES):
        b, half = i // 2, i % 2
        if half == 1:
            xs = x[b]
        else:
            xs = np.concatenate([np.zeros((4096, D), np.float32), x[b, :4096]], axis=0)
        m = dict(shared)
        m.update(cst[half])
        m["xs"] = np.ascontiguousarray(xs)
        m["cT"] = _pc(c[b], 8)
        in_maps.append(m)
    res = run_bass_kernel_spmd(nc, in_maps, core_ids=list(range(NCORES)))
    outp = np.empty((4, S, D), np.float32)
    for i in range(NCORES):
        b, half = i // 2, i % 2
        outp[b, half * 4096:(half + 1) * 4096] = res.results[i]["out"]
    return outp
```

```python
import numpy as np
from contextlib import ExitStack
import ml_dtypes
import concourse.bass as bass
import concourse.mybir as mybir
from concourse.bass_utils import run_bass_kernel_spmd

F32 = mybir.dt.float32
BF16 = mybir.dt.bfloat16
AF = mybir.ActivationFunctionType
ALU = mybir.AluOpType
AX = mybir.AxisListType

D = 1024
S = 8192
NH = 8
DFF = 2816
NQB = 17
NQT = 34
QTOK = NQB * 256
EPS = 1e-6
NEG = -30000.0
NCORES = 8
PHASES = 3
DEBUG = False


class Tok:
    __slots__ = ("sem", "val")

    def __init__(self, sem, val):
        self.sem = sem
        self.val = val


class Slot:
    def __init__(self, name=""):
        self.name = name
        self.w = None
        self.r = {}
        self.dsem = None
        self.dcnt = 0


class Builder:
    ENG = ("pe", "act", "dve", "pool", "sp")

    def __init__(self, nc, es):
        self.nc = nc
        self.es = es
        self.q = {}
        for n in self.ENG:
            sem = es.enter_context(nc.semaphore("sem_" + n))
            self.q[n] = dict(sem=sem, cnt=0, ops=[], waited={})
        self.dslots = []

    def _waits(self, en, deps):
        q = self.q[en]
        need = {}
        for t in deps:
            if t is None:
                continue
            if en == "pe" and t.sem is q["sem"]:
                continue
            k = id(t.sem)
            if q["waited"].get(k, 0) >= t.val:
                continue
            if k not in need or need[k].val < t.val:
                need[k] = t
        for k, t in need.items():
            q["waited"][k] = t.val
        return [(t.sem, t.val) for t in need.values()]

    @staticmethod
    def _deps(reads, writes, deps):
        d = list(deps)
        for s in reads:
            d.append(s.w)
        for s in writes:
            d.extend(s.r.values())
            d.append(s.w)
        return d

    @staticmethod
    def _update(tok, reads, writes):
        for s in writes:
            s.w = tok
            s.r = {}
        for s in reads:
            k = id(tok.sem)
            if k not in s.r or s.r[k].val < tok.val:
                s.r[k] = tok

    def op(self, en, fn, reads=(), writes=(), deps=(), inc=True):
        q = self.q[en]
        waits = self._waits(en, self._deps(reads, writes, deps))
        tok = None
        if inc:
            q["cnt"] += 1
            tok = Tok(q["sem"], q["cnt"])
        else:
            assert not reads and not writes
        sem = q["sem"]

        def emit(e, waits=waits, fn=fn, inc=inc, sem=sem):
            for (sm, v) in waits:
                e.wait_ge(sm, v)
            ins = fn(e)
            if inc:
                ins.then_inc(sem, 1)

        q["ops"].append(emit)
        if tok is not None:
            self._update(tok, reads, writes)
        return tok

    def pe_group(self, fns, reads=(), writes=()):
        q = self.q["pe"]
        waits = self._waits("pe", self._deps(reads, writes, ()))
        q["cnt"] += 1
        tok = Tok(q["sem"], q["cnt"])
        sem = q["sem"]
        n = len(fns)

        def emit(e, waits=waits, fns=fns, sem=sem, n=n):
            for (sm, v) in waits:
                e.wait_ge(sm, v)
            for i, f in enumerate(fns):
                ins = f(e)
                if i == n - 1:
                    ins.then_inc(sem, 1)

        q["ops"].append(emit)
        self._update(tok, reads, writes)
        return tok

    def dma(self, en, out, in_, sem_slot, reads=(), writes=(), deps=(), **kw):
        q = self.q[en]
        sl = sem_slot
        if sl.dsem is None:
            sl.dsem = self.es.enter_context(self.nc.semaphore("dsem%d" % len(self.dslots)))
            self.dslots.append(sl)
        waits = self._waits(en, self._deps(reads, writes, deps))
        sl.dcnt += 16
        tok = Tok(sl.dsem, sl.dcnt)
        dsem = sl.dsem

        def emit(e, waits=waits, out=out, in_=in_, dsem=dsem, kw=kw):
            for (sm, v) in waits:
                e.wait_ge(sm, v)
            e.dma_start(out=out, in_=in_, **kw).then_inc(dsem, 16)

        q["ops"].append(emit)
        self._update(tok, reads, writes)
        return tok

    def emit_block(self):
        nc = self.nc
        finals = [(s.dsem, s.dcnt) for s in self.dslots if s.dcnt > 0]
        pe_fin = (self.q["pe"]["sem"], self.q["pe"]["cnt"])

        def fin(e, finals=finals):
            for sm, v in finals:
                e.wait_ge(sm, v)

        self.q["sp"]["ops"].append(fin)
        with nc.Block() as blk:
            for n, meth in (("pe", blk.tensor), ("act", blk.scalar), ("dve", blk.vector),
                            ("pool", blk.gpsimd), ("sp", blk.sync)):
                ops = self.q[n]["ops"]
                if ops:
                    def body(e, ops=ops):
                        for f in ops:
                            f(e)
                    meth(body)
                self.q[n]["ops"] = []


def mm(out, lhsT, rhs, start, stop):
    return lambda e: e.matmul(out, lhsT=lhsT, rhs=rhs, start=start, stop=stop)


def tr(out, in_, ident):
    return lambda e: e.transpose(out, in_, ident)


def build_program():
    nc = bass.Bass("TRN2", target_bir_lowering=False)

    def din(name, shape, dt=F32):
        return nc.dram_tensor(name, list(shape), dt, kind="ExternalInput").ap()

    def dscr(name, shape, dt):
        return nc.dram_tensor(name, list(shape), dt, kind="ExternalOutput" if DEBUG else "Internal").ap()

    xs = din("xs", [S, D])
    cT = din("cT", [128, 8])
    w_ada = din("w_ada", [D, 6 * D])
    b_ada = din("b_ada", [1, 6 * D])
    gmix = din("gmix", [128, 8])
    w_in = din("w_in", [D, 3072])
    convw = din("convw", [128, 12])
    convb = din("convb", [128, 4])
    gattn = din("gattn", [64, 8])
    gconv = din("gconv", [128, 4])
    w_out = din("w_out", [D, D])
    gffn = din("gffn", [128, 8])
    w_up = din("w_up", [D, 2 * DFF])
    fcw = din("fcw", [128, 132])
    fcb = din("fcb", [128, 44])
    w_down = din("w_down", [DFF, D])
    gfin = din("gfin", [128, D])
    ident_d = din("ident", [128, 128], BF16)
    kstat = din("kstat", [35, S], BF16)
    cmask = din("cmask", [128, 512], BF16)
    gvb_d = din("gvb", [128, NQB * 32])
    gvb2_d = din("gvb2", [128, NQB * 32])
    qstat = din("qstat", [NH, 128, NQT * 99], BF16)
    hv_d = din("hv", [128, 1])
    out = nc.dram_tensor("out", [4096, D], F32, kind="ExternalOutput").ap()

    kT_s = dscr("kT_s", [NH, 64, S], BF16)
    qT_s = dscr("qT_s", [NH, 64, QTOK], BF16)
    at_s = dscr("at_s", [NH, 64, QTOK], BF16)
    cg_s = dscr("cg_s", [4, 128, QTOK], BF16)
    mod_s = dscr("mod_s", [1, 6 * D], F32)
    km_s = dscr("km_s", [NH, 64, 32], F32)

    es = ExitStack()
    with es:
        B = Builder(nc, es)

        def sb(st, name, shape, dt):
            return st.enter_context(nc.sbuf_tensor("sb_" + name, list(shape), dt))

        def ps(st, name, shape, dt):
            return st.enter_context(nc.psum_tensor("ps_" + name, list(shape), dt))

        ident = sb(es, "ident", [128, 128], BF16)
        ones_b = sb(es, "ones_b", [128, 1], BF16)
        ones_f = sb(es, "ones_f", [128, 64], F32)
        epsc = sb(es, "epsc", [128, 1], F32)
        hv = sb(es, "hv", [128, 1], F32)
        G1 = sb(es, "G1", [128, 8], F32)
        G2 = sb(es, "G2", [128, 8], F32)
        modT = sb(es, "modT", [128, 48], F32)
        gmix_sb = sb(es, "gmix_sb", [128, 8], F32)
        gffn_sb = sb(es, "gffn_sb", [128, 8], F32)
        convw_sb = sb(es, "convw_sb", [128, 12], F32)
        convb_sb = sb(es, "convb_sb", [128, 4], F32)
        gconv_sb = sb(es, "gconv_sb", [128, 4], F32)
        gattn_sb = sb(es, "gattn_sb", [64, 8], F32)
        fcw_sb = sb(es, "fcw_sb", [128, 132], F32)
        fcb_sb = sb(es, "fcb_sb", [128, 44], F32)
        ssa = sb(es, "ssa", [128, NQT], F32)
        ssc = sb(es, "ssc", [128, NQT], F32)
        wo = sb(es, "wo", [128, 8, D], BF16)
        S_wo = Slot("wo")
        S_const = Slot("const")
        S_ssa = Slot("ssa")
        S_ssc = Slot("ssc")
        S_G = Slot("G")
        S_mods = Slot("mods")

        for dst, src in ((ident, ident_d), (hv, hv_d), (gmix_sb, gmix), (gffn_sb, gffn), (convw_sb, convw),
                         (convb_sb, convb), (gconv_sb, gconv), (gattn_sb, gattn), (fcw_sb, fcw), (fcb_sb, fcb)):
            B.dma("sp", dst[:], src, S_const, writes=[S_const])
        B.op("dve", lambda e: e.memset(ones_b[:], 1.0), writes=[S_const])
        B.op("dve", lambda e: e.memset(ones_f[:], 1.0), writes=[S_const])
        B.op("dve", lambda e: e.memset(epsc[:], EPS), writes=[S_const])

        with ExitStack() as p0:
            c_sb = sb(p0, "c_sb", [128, 8], F32)
            sc_b = sb(p0, "sc_b", [128, 8], BF16)
            brow = sb(p0, "brow", [1, 6 * D], F32)
            modrow = sb(p0, "modrow", [1, 6 * D], F32)
            wa = [sb(p0, "wa%d" % i, [128, 8, 512], BF16) for i in range(2)]
            pm = [ps(p0, "pm%d" % i, [128, 512], F32) for i in range(2)]
            S_c, S_scb, S_brow, S_modrow, S_modT = Slot(), Slot(), Slot(), Slot(), Slot()
            S_wa = [Slot(), Slot()]
            S_pm = [Slot(), Slot()]
            B.dma("sp", c_sb[:], cT, S_c, writes=[S_c])
            B.dma("sp", brow[:], b_ada, S_brow, writes=[S_brow])
            B.op("act", lambda e: e.activation(out=sc_b[:], in_=c_sb[:], func=AF.Silu), reads=[S_c], writes=[S_scb])
            wav = w_ada.rearrange("(c p) n -> p c n", p=128)
            for g in range(12):
                k = g % 2
                B.dma("pool", wa[k][:], wav[:, :, g * 512:(g + 1) * 512], S_wa[k], writes=[S_wa[k]])
                B.pe_group([mm(pm[k][0:1, :], sc_b[:, c:c + 1], wa[k][:, c, :], c == 0, c == 7) for c in range(8)],
                           reads=[S_wa[k], S_scb], writes=[S_pm[k]])
                B.op("dve", lambda e, k=k, g=g: e.tensor_tensor(out=modrow[0:1, g * 512:(g + 1) * 512], in0=pm[k][0:1, :],
                                                                 in1=brow[0:1, g * 512:(g + 1) * 512], op=ALU.add),
                     reads=[S_pm[k], S_brow], writes=[S_modrow])
            B.dma("sp", mod_s, modrow[:], S_mods, reads=[S_modrow], writes=[S_mods])
            B.dma("sp", modT[:], mod_s.rearrange("o (k p) -> (o p) k", p=128), S_modT, reads=[S_mods], writes=[S_modT],
                  allow_slow_non_contiguous=True)
            B.op("dve", lambda e: e.scalar_tensor_tensor(out=G1[:], in0=modT[:, 8:16], scalar=1.0, in1=gmix_sb[:],
                                                         op0=ALU.add, op1=ALU.mult), reads=[S_modT, S_const], writes=[S_G])
            B.op("dve", lambda e: e.scalar_tensor_tensor(out=G2[:], in0=modT[:, 32:40], scalar=1.0, in1=gffn_sb[:],
                                                         op0=ALU.add, op1=ALU.mult), reads=[S_modT, S_const], writes=[S_G])
            B.emit_block()
        sh1 = modT[:, 0:8]
        sh2 = modT[:, 24:32]

        with ExitStack() as p12:
          if PHASES >= 1:
            V_all = sb(p12, "V_all", [128, 64, NH, 65], BF16)
            S_V = Slot("V")
            with ExitStack() as p1:
                w_in_sb = sb(p1, "w_in_sb", [128, 8, 3072], BF16)
                S_win = Slot("win")
                wiv = w_in.rearrange("(c p) n -> p c n", p=128)
                for c in range(8):
                    B.dma("pool", w_in_sb[:, c, :], wiv[:, c, :], S_win, writes=[S_win])
                pend_stats = []
                xt = [sb(p1, "xt%d" % i, [128, 2, D], F32) for i in range(3)]
                xn = [sb(p1, "xnp%d" % i, [128, 2, D], BF16) for i in range(3)]
                hT = [sb(p1, "hT%d" % i, [128, 8, 258], BF16) for i in range(2)]
                junk = sb(p1, "junk", [128, D], BF16)
                ss = [sb(p1, "ssp%d" % i, [128, 2], F32) for i in range(3)]
                rt = [sb(p1, "rt%d" % i, [128, 2], F32) for i in range(3)]
                rstd = [sb(p1, "rstd%d" % i, [128, 2], F32) for i in range(3)]
                kst = [sb(p1, "kst%d" % i, [128, 4, 256], BF16) for i in range(2)]
                qst = [sb(p1, "qst%d" % i, [128, 4, 256], BF16) for i in range(2)]
                cgs = [sb(p1, "cgs%d" % i, [128, 4, 256], BF16) for i in range(2)]
                sqc = sb(p1, "sqc", [128, 4, 256], BF16)
                kmst = sb(p1, "kmst", [128, 4, 32], F32)
                S_kmst = Slot()
                S_kmd = Slot("km_s")
                u_sb = [sb(p1, "u_sb%d" % i, [128, 258], F32) for i in range(2)]
                zb = [sb(p1, "zb%d" % i, [128, 258], F32) for i in range(2)]
                y0 = [sb(p1, "y0%d" % i, [128, 256], F32) for i in range(2)]
                y1 = [sb(p1, "y1%d" % i, [128, 256], F32) for i in range(2)]
                y2 = [sb(p1, "y2%d" % i, [128, 256], F32) for i in range(2)]
                cv = [sb(p1, "cv%d" % i, [128, 256], F32) for i in range(2)]
                tp = ps(p1, "tp", [128, 8, 256], BF16)
                pp = [ps(p1, "pp%d" % i, [128, 512], F32) for i in range(5)]
                pss = ps(p1, "pss", [128, 512], F32)
                S_xt = [Slot(), Slot(), Slot()]
                S_xn = [Slot(), Slot(), Slot()]
                S_hT = [Slot(), Slot()]
                S_junk = Slot()
                S_ss = [Slot(), Slot(), Slot()]
                S_rt = [Slot(), Slot(), Slot()]
                S_rstd = [Slot(), Slot(), Slot()]
                S_kst = [Slot(), Slot()]
                S_qst = [Slot(), Slot()]
                S_cgs = [Slot(), Slot()]
                S_sqc = Slot()
                S_u = [Slot(), Slot()]
                S_z = [Slot(), Slot()]
                S_y0 = [Slot(), Slot()]
                S_y1 = [Slot(), Slot()]
                S_y2 = [Slot(), Slot()]
                S_cv = [Slot(), Slot()]
                S_tp = Slot()
                S_pp = [Slot() for _ in range(5)]
                S_pss = Slot()
                S_kT = Slot("kT_s")
                S_qT = Slot("qT_s")
                S_cgd = Slot("cg_s")
                ppi = [0]

                def next_pp():
                    i = ppi[0] % 5
                    ppi[0] += 1
                    return pp[i], S_pp[i]

                B.op("pool", lambda e: e.memset(V_all[:, :, :, 64:65], 1.0), writes=[S_V])
                for i in range(2):
                    B.op("pool", lambda e, i=i: e.memset(hT[i][:, :, 0:2], 0.0), writes=[S_hT[i]])
                xsv = xs.rearrange("(n t p) d -> n p t d", t=2, p=128)
                kTv = kT_s.rearrange("(pr two) d t -> (two d) pr t", two=2)
                qTv = qT_s.rearrange("(pr two) d t -> (two d) pr t", two=2)
                cgv = cg_s.rearrange("c p t -> p c t")
                evac_rr = [0]

                def evac(out_ap, in_ap, reads, writes, scale=None):
                    evac_rr[0] += 1
                    if evac_rr[0] % 2 == 0:
                        if scale is None:
                            return B.op("act", lambda e: e.activation(out=out_ap, in_=in_ap, func=AF.Copy), reads=reads, writes=writes)
                        return B.op("act", lambda e: e.activation(out=out_ap, in_=in_ap, func=AF.Copy, scale=scale), reads=reads, writes=writes)
                    if scale is None:
                        return B.op("dve", lambda e: e.tensor_copy(out=out_ap, in_=in_ap), reads=reads, writes=writes)
                    return B.op("dve", lambda e: e.tensor_scalar(out=out_ap, in0=in_ap, scalar1=scale, scalar2=None, op0=ALU.mult),
                                reads=reads, writes=writes)

                def f1a(s):
                    k = s % 3
                    B.dma("sp", xt[k][:], xsv[s], S_xt[k], writes=[S_xt[k]])
                    for t in range(2):
                        B.op("act", lambda e, k=k, t=t: e.activation(out=junk[:], in_=xt[k][:, t, :], func=AF.Square,
                                                                     accum_out=ss[k][:, t:t + 1]),
                             reads=[S_xt[k]], writes=[S_junk, S_ss[k]])
                    B.op("act", lambda e, k=k: e.activation(out=rt[k][:], in_=ss[k][:], func=AF.Sqrt, scale=1.0 / D, bias=epsc[:]),
                         reads=[S_ss[k], S_const], writes=[S_rt[k]])
                    B.op("dve", lambda e, k=k: e.reciprocal(out=rstd[k][:], in_=rt[k][:]), reads=[S_rt[k]], writes=[S_rstd[k]])
                    for t in range(2):
                        B.op("dve", lambda e, k=k, t=t: e.tensor_scalar(out=xn[k][:, t, :], in0=xt[k][:, t, :], scalar1=rstd[k][:, t:t + 1],
                                                                        scalar2=None, op0=ALU.mult),
                             reads=[S_xt[k], S_rstd[k]], writes=[S_xn[k]])

                def f1b(s):
                    k = s % 2
                    B.pe_group([tr(tp[:, c, t * 128:(t + 1) * 128], xn[s % 3][:, t, c * 128:(c + 1) * 128], ident[:])
                                for t in range(2) for c in range(8)], reads=[S_xn[s % 3], S_const], writes=[S_tp])
                    for c in range(8):
                        if c % 2 == 0:
                            B.op("act", lambda e, k=k, c=c: e.activation(out=hT[k][:, c, 2:258], in_=tp[:, c, :], func=AF.Identity,
                                                                         scale=G1[:, c:c + 1], bias=sh1[:, c:c + 1]),
                                 reads=[S_tp, S_G], writes=[S_hT[k]])
                        else:
                            B.op("dve", lambda e, k=k, c=c: e.tensor_scalar(out=hT[k][:, c, 2:258], in0=tp[:, c, :], scalar1=G1[:, c:c + 1],
                                                                            scalar2=sh1[:, c:c + 1], op0=ALU.mult, op1=ALU.add),
                                 reads=[S_tp, S_G], writes=[S_hT[k]])
                    if s == 16:
                        B.op("dve", lambda e, k=k: e.tensor_scalar(out=hT[k][:, :, 0:2], in0=hT[1 - k][:, :, 256:258], scalar1=hv[:, 0:1],
                                                                   scalar2=None, op0=ALU.mult),
                             reads=[S_hT[1 - k], S_const], writes=[S_hT[k]])
                    elif s > 16:
                        B.op("dve", lambda e, k=k: e.tensor_copy(out=hT[k][:, :, 0:2], in_=hT[1 - k][:, :, 256:258]),
                             reads=[S_hT[1 - k]], writes=[S_hT[k]])

                def back1a(s):
                    k = s % 2
                    for pr in range(4):
                        pt_, sp_ = next_pp()
                        B.pe_group([mm(pt_[:, 0:256], w_in_sb[:, c, 512 + pr * 128:512 + (pr + 1) * 128], hT[k][:, c, 2:258], c == 0, c == 7)
                                    for c in range(8)], reads=[S_win, S_hT[k]], writes=[sp_])
                        evac(kst[k][:, pr, :], pt_[:, 0:256], [sp_], [S_kst[k]])
                        B.op("dve", lambda e, k=k, pr=pr, s=s: e.tensor_reduce(out=kmst[:, pr, s:s + 1], in_=kst[k][:, pr, :], axis=AX.X, op=ALU.add),
                             reads=[S_kst[k]], writes=[S_kmst])
                    B.dma("sp", kTv[:, :, s * 256:(s + 1) * 256], kst[k][:], S_kst[k], reads=[S_kst[k]], writes=[S_kT])
                    for t in range(2):
                        pt_, sp_ = next_pp()
                        B.pe_group([mm(pt_[:, :], hT[k][:, c, 2 + t * 128:2 + (t + 1) * 128], w_in_sb[:, c, 1024:1536], c == 0, c == 7)
                                    for c in range(8)], reads=[S_win, S_hT[k]], writes=[sp_])
                        evac(V_all[:, 2 * s + t, :, 0:64], pt_[:, :].rearrange("p (h d) -> p h d", d=64), [sp_], [S_V])

                def back1b(s):
                    k = s % 2
                    if s < 15:
                        return
                    qb = s - 15
                    for pr in range(4):
                        pt_, sp_ = next_pp()
                        B.pe_group([mm(pt_[:, 0:256], w_in_sb[:, c, pr * 128:(pr + 1) * 128], hT[k][:, c, 2:258], c == 0, c == 7)
                                    for c in range(8)], reads=[S_win, S_hT[k]], writes=[sp_])
                        evac(qst[k][:, pr, :], pt_[:, 0:256], [sp_], [S_qst[k]], scale=0.125)
                    B.dma("sp", qTv[:, :, qb * 256:(qb + 1) * 256], qst[k][:], S_qst[k], reads=[S_qst[k]], writes=[S_qT])
                    for ch in range(4):
                        j = ch % 2
                        pu_, spu = next_pp()
                        B.pe_group([mm(pu_[:, 0:258], w_in_sb[:, c, 1536 + ch * 128:1536 + (ch + 1) * 128], hT[k][:, c, 0:258], c == 0, c == 7)
                                    for c in range(8)], reads=[S_win, S_hT[k]], writes=[spu])
                        pc_, spc = next_pp()
                        B.pe_group([mm(pc_[:, 0:258], w_in_sb[:, c, 2048 + ch * 128:2048 + (ch + 1) * 128], hT[k][:, c, 0:258], c == 0, c == 7)
                                    for c in range(8)], reads=[S_win, S_hT[k]], writes=[spc])
                        pb_, spb = next_pp()
                        B.pe_group([mm(pb_[:, 0:256], w_in_sb[:, c, 2560 + ch * 128:2560 + (ch + 1) * 128], hT[k][:, c, 2:258], c == 0, c == 7)
                                    for c in range(8)], reads=[S_win, S_hT[k]], writes=[spb])
                        B.op("act", lambda e, j=j, pu_=pu_: e.activation(out=u_sb[j][:], in_=pu_[:, 0:258], func=AF.Copy),
                             reads=[spu], writes=[S_u[j]])
                        B.op("dve", lambda e, j=j, pc_=pc_: e.tensor_tensor(out=zb[j][:], in0=pc_[:, 0:258], in1=u_sb[j][:], op=ALU.mult),
                             reads=[spc, S_u[j]], writes=[S_z[j]])
                        B.op("act", lambda e, j=j, ch=ch: e.activation(out=y0[j][:], in_=zb[j][:, 2:258], func=AF.Identity,
                                                                       scale=convw_sb[:, 8 + ch:9 + ch], bias=convb_sb[:, ch:ch + 1]),
                             reads=[S_z[j], S_const], writes=[S_y0[j]])
                        B.op("dve", lambda e, j=j, ch=ch: e.scalar_tensor_tensor(out=y1[j][:], in0=zb[j][:, 1:257], scalar=convw_sb[:, 4 + ch:5 + ch],
                                                                                 in1=y0[j][:], op0=ALU.mult, op1=ALU.add),
                             reads=[S_z[j], S_y0[j], S_const], writes=[S_y1[j]])
                        B.op("dve", lambda e, j=j, ch=ch: e.scalar_tensor_tensor(out=y2[j][:], in0=zb[j][:, 0:256], scalar=convw_sb[:, ch:ch + 1],
                                                                                 in1=y1[j][:], op0=ALU.mult, op1=ALU.add),
                             reads=[S_z[j], S_y1[j], S_const], writes=[S_y2[j]])
                        B.op("dve", lambda e, j=j, pb_=pb_: e.tensor_tensor(out=cv[j][:], in0=pb_[:, 0:256], in1=y2[j][:], op=ALU.mult),
                             reads=[spb, S_y2[j]], writes=[S_cv[j]])
                        B.op("act", lambda e, j=j, ch=ch: e.activation(out=sqc[:, ch, :], in_=cv[j][:], func=AF.Square),
                             reads=[S_cv[j]], writes=[S_sqc])
                        B.op("act", lambda e, j=j, ch=ch, k=k: e.activation(out=cgs[k][:, ch, :], in_=cv[j][:], func=AF.Copy,
                                                                           scale=gconv_sb[:, ch:ch + 1]),
                             reads=[S_cv[j], S_const], writes=[S_cgs[k]])
                    B.dma("sp", cgv[:, :, qb * 256:(qb + 1) * 256], cgs[k][:], S_cgs[k], reads=[S_cgs[k]], writes=[S_cgd])

                    def stats(qb=qb):
                        for t in range(2):
                            B.pe_group([mm(pss[:, t:t + 1], sqc[:, ch, t * 128:(t + 1) * 128], ones_b[:, 0:1], ch == 0, ch == 3) for ch in range(4)],
                                       reads=[S_sqc, S_const], writes=[S_pss])
                        B.op("dve", lambda e: e.tensor_copy(out=ssc[:, 2 * qb:2 * qb + 2], in_=pss[:, 0:2]), reads=[S_pss], writes=[S_ssc])
                    pend_stats.append(stats)

                f1a(0)
                f1b(0)
                f1a(1)
                f1a(2)
                for s in range(32):
                    if s + 1 < 32:
                        f1b(s + 1)
                    if s + 3 < 32:
                        f1a(s + 3)
                    back1a(s)
                    while pend_stats:
                        pend_stats.pop(0)()
                    if s == 5:
                        wov = w_out.rearrange("(c p) n -> p c n", p=128)
                        for c in range(8):
                            B.dma("pool", wo[:, c, :], wov[:, c, :], S_wo, writes=[S_wo])
                    back1b(s)
                while pend_stats:
                    pend_stats.pop(0)()
                B.dma("sp", km_s.rearrange("(pr two) d j -> (two d) pr j", two=2), kmst[:], S_kmst, reads=[S_kmst], writes=[S_kmd])
                B.emit_block()

            with ExitStack() as p2:
              if PHASES >= 2:
                kaug = [sb(p2, "kaug%d" % i, [99, S], BF16) for i in range(2)]
                qaug = [sb(p2, "qaug%d" % i, [99, QTOK], BF16) for i in range(2)]
                qa = [sb(p2, "qa%d" % i, [128, NQT, 99], BF16) for i in range(2)]
                atst = [sb(p2, "atst%d" % i, [64, QTOK], BF16) for i in range(2)]
                gvb = sb(p2, "gvb", [128, NQB, 32], F32)
                gvb2 = sb(p2, "gvb2", [128, NQB, 32], F32)
                cm = sb(p2, "cm", [128, 2, 256], BF16)
                km_f = [sb(p2, "km_f%d" % i, [64, 32], F32) for i in range(2)]
                km_b = [sb(p2, "km_b%d" % i, [64, 32], BF16) for i in range(2)]
                gm = [sb(p2, "gm%d" % i, [128, 32], F32) for i in range(2)]
                t8 = [sb(p2, "t8%d" % i, [128, 8], F32) for i in range(2)]
                tsel = [sb(p2, "tsel%d" % i, [128, 32], F32) for i in range(2)]
                pT = [sb(p2, "pT%d" % i, [128, 2, 256], BF16) for i in range(3)]
                rc = [sb(p2, "rc%d" % i, [128, 256], F32) for i in range(4)]
                pos = [sb(p2, "pos%d" % i, [128, 256], F32) for i in range(4)]
                atf = [sb(p2, "atf%d" % i, [64, 256], F32) for i in range(4)]
                sqa = [sb(p2, "sqa%d" % i, [64, 256], BF16) for i in range(4)]
                sp = [ps(p2, "sp%d" % i, [128, 2, 256], F32) for i in range(3)]
                po = [ps(p2, "po%d" % i, [128, 512], F32) for i in range(2)]
                pgt = ps(p2, "pgt", [128, 512], F32)
                pbc = ps(p2, "pbc", [128, 512], F32)
                ptt_t = ps(p2, "ptt", [128, 1024], BF16)
                ptt = ptt_t[:, 0:128]
                S_kaug = [Slot(), Slot()]
                S_kstat = Slot()
                S_qq = [Slot(), Slot()]
                S_qm = [Slot(), Slot()]
                S_qa = [Slot(), Slot()]
                S_atst = [Slot(), Slot()]
                S_c2 = Slot()
                S_kmf = [Slot(), Slot()]
                S_kmb = [Slot(), Slot()]
                S_gm = [Slot(), Slot()]
                S_t8 = [Slot(), Slot()]
                S_tsel = [Slot(), Slot()]
                S_pT = [Slot() for _ in range(3)]
                S_rc = [Slot() for _ in range(4)]
                S_pos = [Slot() for _ in range(4)]
                S_atf = [Slot() for _ in range(4)]
                S_sqa = [Slot() for _ in range(4)]
                S_sp = [Slot() for _ in range(3)]
                S_po = [Slot(), Slot()]
                S_pgt, S_ptt, S_pbc = Slot(), Slot(), Slot()
                S_pss2 = S_pbc
                S_atd = Slot("at_s")

                B.dma("sp", gvb[:].rearrange("p a b -> p (a b)"), gvb_d, S_c2, writes=[S_c2])
                B.dma("sp", gvb2[:].rearrange("p a b -> p (a b)"), gvb2_d, S_c2, writes=[S_c2])
                B.dma("sp", cm[:].rearrange("p a b -> p (a b)"), cmask, S_c2, writes=[S_c2])
                for i in range(2):
                    B.dma("sp", kaug[i][64:99, :], kstat, S_kstat, writes=[S_kstat])

                def load_head(h):
                    hb = h % 2
                    B.dma("sp", kaug[hb][0:64, :], kT_s[h], S_kaug[hb], reads=[S_kT], writes=[S_kaug[hb]])
                    B.dma("sp", qaug[hb][0:64, :], qT_s[h], S_qq[hb], reads=[S_qT], writes=[S_qq[hb]])
                    B.dma("sp", qa[hb][:].rearrange("p a b -> p (a b)"), qstat[h], S_qa[hb], writes=[S_qa[hb]])

                def prep_head(h):
                    hb = h % 2
                    B.dma("sp", km_f[hb][:], km_s[h], S_kmf[hb], reads=[S_kmd], writes=[S_kmf[hb]])
                    B.op("dve", lambda e: e.tensor_scalar(out=km_b[hb][:], in0=km_f[hb][:], scalar1=1.0 / 256, scalar2=None, op0=ALU.mult),
                         reads=[S_kmf[hb]], writes=[S_kmb[hb]])

                def mask_a(h, qt):
                    hb = h % 2
                    qb = qt // 2
                    g2 = qt % 2
                    B.pe_group([mm(pgt[:, 0:32], qaug[hb][0:64, qt * 128:(qt + 1) * 128], km_b[hb][:, :], True, True)],
                               reads=[S_qq[hb], S_kmb[hb]], writes=[S_pgt])
                    B.op("dve", lambda e: e.tensor_tensor(out=gm[g2][:], in0=pgt[:, 0:32], in1=gvb[:, qb, :], op=ALU.add),
                         reads=[S_pgt, S_c2], writes=[S_gm[g2]])
                    B.op("dve", lambda e: e.max(out=t8[g2][:], in_=gm[g2][:]), reads=[S_gm[g2]], writes=[S_t8[g2]])
                    B.op("dve", lambda e: e.tensor_scalar(out=tsel[g2][:], in0=gm[g2][:], scalar1=t8[g2][:, 2:3], scalar2=-NEG,
                                                          op0=ALU.is_ge, op1=ALU.mult),
                         reads=[S_gm[g2], S_t8[g2]], writes=[S_tsel[g2]])
                    B.op("dve", lambda e: e.tensor_tensor(out=qa[hb][:, qt, 64:96], in0=tsel[g2][:], in1=gvb2[:, qb, :], op=ALU.add),
                         reads=[S_tsel[g2], S_c2], writes=[S_qa[hb]])

                def mask_b(h, qt):
                    hb = h % 2
                    B.pe_group([tr(ptt[0:99, :], qa[hb][:, qt, :], ident[:])], reads=[S_qa[hb], S_const], writes=[S_ptt])
                    B.op("dve", lambda e: e.tensor_copy(out=qaug[hb][64:99, qt * 128:(qt + 1) * 128], in_=ptt[64:99, :]),
                         reads=[S_ptt], writes=[S_qm[hb]])

                sctr = [0]
                KEEP = []
                for h_ in range(NH):
                    slope_ = 2.0 ** (-(h_ + 1))
                    kk_ = 0
                    while kk_ < 31 and slope_ * (kk_ * 256 + 1) < 80.0:
                        kk_ += 1
                    KEEP.append(kk_)

                def s_mm(h, qb, j):
                    hb = h % 2
                    sB = 15 + qb
                    qc = slice(qb * 256, (qb + 1) * 256)
                    i = sctr[0] % 3
                    sctr[0] += 1
                    fns = []
                    for kt in range(2):
                        fns.append(mm(sp[i][:, kt, :], kaug[hb][0:99, (2 * j + kt) * 128:(2 * j + kt + 1) * 128], qaug[hb][0:99, qc],
                                      True, j != sB))
                        if j == sB:
                            fns.append(mm(sp[i][:, kt, :], ident[:], cm[:, kt, :], False, True))
                    B.pe_group(fns, reads=[S_kaug[hb], S_kstat, S_qq[hb], S_qm[hb], S_c2, S_const], writes=[S_sp[i]])
                    B.op("act", lambda e: e.activation(out=pT[i][:], in_=sp[i][:], func=AF.Exp), reads=[S_sp[i]], writes=[S_pT[i]])
                    return i

                def pv_mm(h, qb, j, i, first, last):
                    ob = qb % 2
                    B.pe_group([mm(po[ob][0:65, 0:256], V_all[:, 2 * j + kt, h, :], pT[i][:, kt, :], (first and kt == 0), (last and kt == 1))
                                for kt in range(2)], reads=[S_V, S_pT[i]], writes=[S_po[ob]])

                tctr = [0]

                def tail_a(h, qb):
                    ob = qb % 2
                    r = tctr[0] % 4
                    tctr[0] += 1
                    B.op("act", lambda e: e.activation(out=pos[r][0:65, :], in_=po[ob][0:65, 0:256], func=AF.Copy), reads=[S_po[ob]], writes=[S_pos[r]])
                    B.op("dve", lambda e: e.reciprocal(out=rc[r][64:65, :], in_=pos[r][64:65, :]), reads=[S_pos[r]], writes=[S_rc[r]])
                    return r

                def tail_b(h, qb, r):
                    hb = h % 2
                    qc = slice(qb * 256, (qb + 1) * 256)
                    B.pe_group([mm(pbc[0:64, 0:256], ones_f[64:65, 0:64], rc[r][64:65, :], True, True)],
                               reads=[S_rc[r], S_const], writes=[S_pbc])
                    B.op("dve", lambda e: e.tensor_tensor(out=atf[r][:], in0=pbc[0:64, 0:256], in1=pos[r][0:64, :], op=ALU.mult),
                         reads=[S_pbc, S_pos[r]], writes=[S_atf[r]])
                    B.op("pool", lambda e: e.tensor_tensor(out=sqa[r][:], in0=atf[r][:], in1=atf[r][:], op=ALU.mult), reads=[S_atf[r]], writes=[S_sqa[r]])
                    B.op("pool", lambda e: e.tensor_scalar(out=atst[hb][:, qc], in0=atf[r][:], scalar1=gattn_sb[:, h:h + 1], scalar2=None, op0=ALU.mult),
                         reads=[S_atf[r], S_const], writes=[S_atst[hb]])

                def tail_c(h, qb, r):
                    B.pe_group([mm(pbc[:, 256 + t:257 + t], sqa[r][:, t * 128:(t + 1) * 128], ones_b[0:64, 0:1], True, True) for t in range(2)],
                               reads=[S_sqa[r], S_const], writes=[S_pss2])
                    if h == 0:
                        B.op("dve", lambda e: e.tensor_copy(out=ssa[:, 2 * qb:2 * qb + 2], in_=pbc[:, 256:258]), reads=[S_pss2], writes=[S_ssa])
                    else:
                        B.op("dve", lambda e: e.tensor_tensor(out=ssa[:, 2 * qb:2 * qb + 2], in0=pbc[:, 256:258], in1=ssa[:, 2 * qb:2 * qb + 2],
                                                              op=ALU.add),
                             reads=[S_pss2, S_ssa], writes=[S_ssa])

                def blocks_of(h, qb):
                    sB = 15 + qb
                    return list(range(max(0, sB - KEEP[h]), sB + 1))

                load_head(0)
                prep_head(0)
                for qt in range(NQT):
                    mask_a(0, qt)
                    mask_b(0, qt)
                for h in range(NH):
                    hb = h % 2
                    if h + 1 < NH:
                        load_head(h + 1)
                        prep_head(h + 1)
                    total_iter = sum(len(blocks_of(h, qb)) for qb in range(NQB))
                    sched = {}
                    if h + 1 < NH:
                        step = max(1, (total_iter - 8) // NQT)
                        for qt in range(NQT):
                            ia = min(total_iter - 1, 1 + qt * step)
                            ib = min(total_iter - 1, ia + 3)
                            sched.setdefault(ia, []).append(lambda qt=qt: mask_a(h + 1, qt))
                            sched.setdefault(ib, []).append(lambda qt=qt: mask_b(h + 1, qt))
                    it = 0
                    tails = []
                    for qb in range(NQB):
                        js = blocks_of(h, qb)
                        n = len(js)
                        ids = {0: s_mm(h, qb, js[0])}
                        if n > 1:
                            ids[1] = s_mm(h, qb, js[1])
                        for i in range(n):
                            if i + 2 < n:
                                ids[i + 2] = s_mm(h, qb, js[i + 2])
                            pv_mm(h, qb, js[i], ids[i], i == 0, i == n - 1)
                            for fn in sched.pop(it, []):
                                fn()
                            for item in list(tails):
                                if item[0] <= it:
                                    item[1]()
                                    tails.remove(item)
                            it += 1
                        r = tail_a(h, qb)
                        tails.append((it + 5, lambda h=h, qb=qb, r=r: tail_b(h, qb, r)))
                        tails.append((it + 10, lambda h=h, qb=qb, r=r: tail_c(h, qb, r)))
                    for key in sorted(sched):
                        for fn in sched[key]:
                            fn()
                    for item in tails:
                        item[1]()
                    B.dma("sp", at_s[h], atst[hb][:], S_atst[hb], reads=[S_atst[hb]], writes=[S_atd])
                B.emit_block()

        with ExitStack() as p3:
          if PHASES >= 3:
            wu = sb(p3, "wu", [128, 8, 2 * DFF], BF16)
            wd = sb(p3, "wd", [128, 22, D], BF16)
            x1 = [sb(p3, "x1%d" % i, [128, 2, D], F32) for i in range(2)]
            atb = sb(p3, "atb", [128, 4, 256], BF16)
            cgb = sb(p3, "cgb", [128, 4, 256], BF16)
            xn2 = sb(p3, "xn2", [128, 2, D], BF16)
            h2T = [sb(p3, "h2T%d" % i, [128, 8, 258], BF16) for i in range(2)]
            YA = [sb(p3, "YA%d" % i, [128, 256], F32) for i in range(2)]
            YB = [sb(p3, "YB%d" % i, [128, 256], F32) for i in range(2)]
            YV = [sb(p3, "YV%d" % i, [128, 256], F32) for i in range(2)]
            YG = [sb(p3, "YG%d" % i, [128, 256], F32) for i in range(2)]
            actT = sb(p3, "actT", [128, 22, 256], BF16)
            gfb = sb(p3, "gfb", [128, D], F32)
            rsa = sb(p3, "rsa", [128, NQT], F32)
            rsc = sb(p3, "rsc", [128, NQT], F32)
            ssf = sb(p3, "ssf", [128, 2], F32)
            rtf = sb(p3, "rtf", [128, 2], F32)
            rsf = sb(p3, "rsf", [128, 2], F32)
            ssb = sb(p3, "ssb", [128, 2], F32)
            rtb = sb(p3, "rtb", [128, 2], F32)
            rsb = sb(p3, "rsb", [128, 2], F32)
            pu = [ps(p3, "pu%d" % i, [128, 512], F32) for i in range(4)]
            pd = [ps(p3, "pd%d" % i, [128, 512], F32) for i in range(2)]
            pac = [ps(p3, "pac%d" % i, [128, 512], F32) for i in range(2)]
            S_wu, S_wd = Slot(), Slot()
            S_x1 = [Slot(), Slot()]
            S_atb, S_cgb, S_xn2 = Slot(), Slot(), Slot()
            S_h2T = [Slot(), Slot()]
            S_YA = [Slot() for _ in range(2)]
            S_YB = [Slot() for _ in range(2)]
            S_YV = [Slot(), Slot()]
            S_YG = [Slot(), Slot()]
            S_actT, S_gfb, S_rs = Slot(), Slot(), Slot()
            S_ssf, S_rtf, S_rsf, S_ssb, S_rtb, S_rsb = Slot(), Slot(), Slot(), Slot(), Slot(), Slot()
            S_pu = [Slot() for _ in range(4)]
            S_pd = [Slot(), Slot()]
            S_pac = [Slot(), Slot()]
            gtb = x1[1][:, 0, :]
            S_gtb = S_x1[1]

            wov = w_out.rearrange("(c p) n -> p c n", p=128)
            wuv = w_up.rearrange("(c p) n -> p c n", p=128)
            wdv = w_down.rearrange("(c p) n -> p c n", p=128)
            for c in range(8):
                B.dma("pool", wu[:, c, :], wuv[:, c, :], S_wu, writes=[S_wu])
            for c0 in range(0, 22, 2):
                B.dma("pool", wd[:, c0:c0 + 2, :], wdv[:, c0:c0 + 2, :], S_wd, writes=[S_wd])
            B.dma("sp", gfb[:], gfin, S_gfb, writes=[S_gfb])
            mod_t = mod_s.tensor
            B.dma("sp", gtb, bass.AP(mod_t, 2048, [[0, 128], [1, D]]), S_gtb, reads=[S_mods], writes=[S_gtb])
            for c in range(8):
                B.op("pool", lambda e, c=c: e.tensor_tensor(out=wo[:, c, :], in0=wo[:, c, :], in1=gtb, op=ALU.mult),
                     reads=[S_gtb], writes=[S_wo])
            B.dma("sp", gtb, bass.AP(mod_t, 5120, [[0, 128], [1, D]]), S_gtb, reads=[S_mods], writes=[S_gtb])
            for c in range(22):
                B.op("pool", lambda e, c=c: e.tensor_tensor(out=wd[:, c, :], in0=wd[:, c, :], in1=gtb, op=ALU.mult),
                     reads=[S_gtb], writes=[S_wd])
            for src_, dst, ssl in ((ssa, rsa, S_ssa), (ssc, rsc, S_ssc)):
                B.op("act", lambda e, src_=src_, dst=dst: e.activation(out=dst[:], in_=src_[:], func=AF.Sqrt, scale=1.0 / 512, bias=epsc[:]),
                     reads=[ssl, S_const], writes=[S_rs])
                B.op("dve", lambda e, dst=dst: e.reciprocal(out=dst[:], in_=dst[:]), reads=[S_rs], writes=[S_rs])
            for i in range(2):
                B.op("dve", lambda e, i=i: e.memset(h2T[i][:, :, 0:2], 0.0), writes=[S_h2T[i]])

            xsv = xs.rearrange("(n t p) d -> n p t d", t=2, p=128)
            outv = out.rearrange("(n t p) d -> n p t d", t=2, p=128)
            atv = at_s.rearrange("(pr two) d t -> (two d) pr t", two=2)
            cgv = cg_s.rearrange("c p t -> p c t")
            puc = [0]
            ybc = [0]
            yac = [0]
            pend_fin = []
            tpv = [pac[i][:, :].bitcast(BF16).rearrange("p (c t) -> p c t", t=256) for i in range(2)]

            def next_yb():
                i = ybc[0] % 6
                ybc[0] += 1
                return yb[i], S_yb[i]

            def f3a_dma(qb):
                k = qb % 2
                sl = 15 + qb
                qc = slice(qb * 256, (qb + 1) * 256)
                B.dma("sp", x1[k][:], xsv[sl], S_x1[k], writes=[S_x1[k]])
                B.dma("sp", atb[:], atv[:, :, qc], S_atb, reads=[S_atd], writes=[S_atb])
                B.dma("sp", cgb[:], cgv[:, :, qc], S_cgb, reads=[S_cgd], writes=[S_cgb])

            def f3a(qb):
                k = qb % 2
                for t in range(2):
                    tile = 2 * qb + t
                    for n in range(2):
                        ns = slice(n * 512, (n + 1) * 512)
                        B.pe_group([mm(pac[0][:, :], atb[:, c, t * 128:(t + 1) * 128], wo[:, c, ns], c == 0, c == 3) for c in range(4)],
                                   reads=[S_atb, S_wo], writes=[S_pac[0]])
                        B.pe_group([mm(pac[1][:, :], cgb[:, c, t * 128:(t + 1) * 128], wo[:, 4 + c, ns], c == 0, c == 3) for c in range(4)],
                                   reads=[S_cgb, S_wo], writes=[S_pac[1]])
                        B.op("dve", lambda e, t=t, ns=ns, tile=tile: e.scalar_tensor_tensor(
                            out=x1[k][:, t, ns], in0=pac[0][:, :], scalar=rsa[:, tile:tile + 1], in1=x1[k][:, t, ns], op0=ALU.mult, op1=ALU.add),
                            reads=[S_pac[0], S_rs], writes=[S_x1[k]])
                        B.op("dve", lambda e, t=t, ns=ns, tile=tile: e.scalar_tensor_tensor(
                            out=x1[k][:, t, ns], in0=pac[1][:, :], scalar=rsc[:, tile:tile + 1], in1=x1[k][:, t, ns], op0=ALU.mult, op1=ALU.add),
                            reads=[S_pac[1], S_rs], writes=[S_x1[k]])
                for t in range(2):
                    B.op("act", lambda e, t=t: e.activation(out=xn2[:, t, :], in_=x1[k][:, t, :], func=AF.Square, accum_out=ssf[:, t:t + 1]),
                         reads=[S_x1[k]], writes=[S_xn2, S_ssf])
                B.op("act", lambda e: e.activation(out=rtf[:], in_=ssf[:], func=AF.Sqrt, scale=1.0 / D, bias=epsc[:]),
                     reads=[S_ssf, S_const], writes=[S_rtf])
                B.op("dve", lambda e: e.reciprocal(out=rsf[:], in_=rtf[:]), reads=[S_rtf], writes=[S_rsf])
                for t in range(2):
                    B.op("dve", lambda e, t=t: e.tensor_scalar(out=xn2[:, t, :], in0=x1[k][:, t, :], scalar1=rsf[:, t:t + 1], scalar2=None,
                                                               op0=ALU.mult),
                         reads=[S_x1[k], S_rsf], writes=[S_xn2])

            def f3b(qb):
                k = qb % 2
                for half in range(2):
                    B.pe_group([tr(tpv[half][:, c, t * 128:(t + 1) * 128], xn2[:, t, (half * 4 + c) * 128:(half * 4 + c + 1) * 128], ident[:])
                                for t in range(2) for c in range(4)], reads=[S_xn2, S_const], writes=[S_pac[half]])
                    for c in range(4):
                        cc = half * 4 + c
                        B.op("act", lambda e, c=c, cc=cc, half=half: e.activation(out=h2T[k][:, cc, 2:258], in_=tpv[half][:, c, :],
                                                                                 func=AF.Identity, scale=G2[:, cc:cc + 1], bias=sh2[:, cc:cc + 1]),
                             reads=[S_pac[half], S_G], writes=[S_h2T[k]])
                if qb == 1:
                    B.op("dve", lambda e: e.tensor_scalar(out=h2T[k][:, :, 0:2], in0=h2T[1 - k][:, :, 256:258], scalar1=hv[:, 0:1],
                                                          scalar2=None, op0=ALU.mult),
                         reads=[S_h2T[1 - k], S_const], writes=[S_h2T[k]])
                elif qb > 1:
                    B.op("dve", lambda e: e.tensor_copy(out=h2T[k][:, :, 0:2], in_=h2T[1 - k][:, :, 256:258]),
                         reads=[S_h2T[1 - k]], writes=[S_h2T[k]])

            def back3(qb):
                k = qb % 2
                for v in range(22):
                    if qb + 1 < NQB and v == 3:
                        f3a_dma(qb + 1)
                    p2_ = v % 2
                    chs = (v, 22 + v)
                    pis = []
                    for ch in chs:
                        i = puc[0] % 4
                        puc[0] += 1
                        pis.append(i)
                        B.pe_group([mm(pu[i][:, 0:258], wu[:, c, ch * 128:(ch + 1) * 128], h2T[k][:, c, 0:258], c == 0, c == 7) for c in range(8)],
                                   reads=[S_wu, S_h2T[k]], writes=[S_pu[i]])
                    yas = []
                    for w_, (ch, i) in enumerate(zip(chs, pis)):
                        ai = yac[0] % 2
                        yac[0] += 1
                        yas.append(ai)
                        B.op("act", lambda e, i=i, ai=ai, ch=ch: e.activation(out=YA[ai][:], in_=pu[i][:, 2:258], func=AF.Identity,
                                                                             scale=fcw_sb[:, 88 + ch:89 + ch], bias=fcb_sb[:, ch:ch + 1]),
                             reads=[S_pu[i], S_const], writes=[S_YA[ai]])
                    if pend_fin:
                        pend_fin.pop(0)()
                    ybs = []
                    for w_, (ch, i) in enumerate(zip(chs, pis)):
                        bi = ybc[0] % 2
                        ybc[0] += 1
                        ybs.append(bi)
                        ai = yas[w_]
                        B.op("dve", lambda e, i=i, ai=ai, bi=bi, ch=ch: e.scalar_tensor_tensor(
                            out=YB[bi][:], in0=pu[i][:, 1:257], scalar=fcw_sb[:, 44 + ch:45 + ch], in1=YA[ai][:], op0=ALU.mult, op1=ALU.add),
                            reads=[S_pu[i], S_YA[ai], S_const], writes=[S_YB[bi]])
                    outs = ((YV[p2_], S_YV[p2_]), (YG[p2_], S_YG[p2_]))
                    for w_, (ch, i) in enumerate(zip(chs, pis)):
                        bi = ybs[w_]
                        yo, syo = outs[w_]
                        B.op("dve", lambda e, i=i, bi=bi, yo=yo, ch=ch: e.scalar_tensor_tensor(
                            out=yo[:], in0=pu[i][:, 0:256], scalar=fcw_sb[:, ch:ch + 1], in1=YB[bi][:], op0=ALU.mult, op1=ALU.add),
                            reads=[S_pu[i], S_YB[bi], S_const], writes=[syo])
                    def fin_pair(p2_=p2_, v=v):
                        B.op("act", lambda e: e.activation(out=YG[p2_][:], in_=YG[p2_][:], func=AF.Silu), reads=[S_YG[p2_]], writes=[S_YG[p2_]])
                        B.op("pool", lambda e: e.tensor_tensor(out=actT[:, v, :], in0=YV[p2_][:], in1=YG[p2_][:], op=ALU.mult),
                             reads=[S_YV[p2_], S_YG[p2_]], writes=[S_actT])
                    pend_fin.append(fin_pair)
                if pend_fin:
                    pend_fin.pop(0)()
                if qb + 1 < NQB:
                    f3a(qb + 1)
                for t in range(2):
                    if t == 1 and qb + 1 < NQB:
                        f3b(qb + 1)
                    for n in range(2):
                        B.pe_group([mm(pd[n][:, :], actT[:, v, t * 128:(t + 1) * 128], wd[:, v, n * 512:(n + 1) * 512], v == 0, v == 21)
                                    for v in range(22)], reads=[S_actT, S_wd], writes=[S_pd[n]])
                        B.op("dve", lambda e, t=t, n=n: e.tensor_tensor(out=x1[k][:, t, n * 512:(n + 1) * 512], in0=pd[n][:, :],
                                                                        in1=x1[k][:, t, n * 512:(n + 1) * 512], op=ALU.add),
                             reads=[S_pd[n]], writes=[S_x1[k]])
                for t in range(2):
                    B.op("act", lambda e, t=t: e.activation(out=xn2[:, t, :], in_=x1[k][:, t, :], func=AF.Square, accum_out=ssb[:, t:t + 1]),
                         reads=[S_x1[k]], writes=[S_xn2, S_ssb])
                B.op("act", lambda e: e.activation(out=rtb[:], in_=ssb[:], func=AF.Sqrt, scale=1.0 / D, bias=epsc[:]),
                     reads=[S_ssb, S_const], writes=[S_rtb])
                B.op("dve", lambda e: e.reciprocal(out=rsb[:], in_=rtb[:]), reads=[S_rtb], writes=[S_rsb])
                for t in range(2):
                    B.op("dve", lambda e, t=t: e.scalar_tensor_tensor(out=x1[k][:, t, :], in0=x1[k][:, t, :], scalar=rsb[:, t:t + 1],
                                                                      in1=gfb[:], op0=ALU.mult, op1=ALU.mult),
                         reads=[S_rsb, S_gfb], writes=[S_x1[k]])
                B.dma("sp", outv[qb - 1], x1[k][:], S_x1[k], reads=[S_x1[k]])

            f3a_dma(0)
            f3a(0)
            f3b(0)
            f3a_dma(1)
            f3a(1)
            f3b(1)
            for qb in range(1, NQB):
                back3(qb)
            B.emit_block()
    return nc


_NC_CACHE = {}


def _consts(half):
    bf = ml_dtypes.bfloat16
    ident = np.eye(128, dtype=np.float32).astype(bf)
    key = np.arange(S)
    kstat = np.zeros((35, S), np.float32)
    kstat[key // 256, key] = 1.0
    kstat[32] = key % 256
    kstat[33] = key // 256
    kstat[34] = 1.0
    kstat = kstat.astype(bf)
    p = np.arange(128)[:, None, None]
    kt = np.arange(2)[None, :, None]
    a = np.arange(256)[None, None, :]
    cmask = np.where(kt * 128 + p <= a, 0.0, NEG).astype(np.float32).reshape(128, 512).astype(bf)
    gvb = np.zeros((NQB, 32), np.float32)
    gvb2 = np.zeros((NQB, 32), np.float32)
    for qb in range(NQB):
        sB = 15 + qb
        for j in range(32):
            valid = (j < sB) and (half == 1 or j >= 16)
            gvb[qb, j] = 0.0 if valid else -1.0e9
            gvb2[qb, j] = NEG if valid else 2 * NEG
        gvb[qb, sB] = -3.0e9
        gvb2[qb, sB] = 0.0
    gvb = np.ascontiguousarray(np.broadcast_to(gvb.reshape(1, -1), (128, NQB * 32))).astype(np.float32)
    gvb2 = np.ascontiguousarray(np.broadcast_to(gvb2.reshape(1, -1), (128, NQB * 32))).astype(np.float32)
    qstat = np.zeros((NH, 128, NQT, 99), np.float32)
    for h in range(NH):
        slope = 2.0 ** (-(h + 1))
        for qt in range(NQT):
            sB = 15 + qt // 2
            apos = (qt % 2) * 128 + np.arange(128)
            qstat[h, :, qt, 96] = slope
            qstat[h, :, qt, 97] = 256.0 * slope
            qstat[h, :, qt, 98] = -slope * (256.0 * sB + apos)
    qstat = qstat.reshape(NH, 128, NQT * 99).astype(bf)
    hv = np.full((128, 1), float(half), np.float32)
    return dict(ident=ident, kstat=kstat, cmask=cmask, gvb=gvb, gvb2=gvb2, qstat=qstat, hv=hv)


def _pc(v, nchunk):
    return np.ascontiguousarray(np.asarray(v, np.float32).reshape(nchunk, 128).T)


def kernel(x, c, w_ada, b_ada, g_mix, w_in, conv_w, conv_b, g_attn_out, g_conv_out, w_out, g_ffn,
           w_up, ffn_conv_w, ffn_conv_b, w_down, g_final):
    f = lambda a: np.ascontiguousarray(np.asarray(a, dtype=np.float32))
    x = f(x)
    c = f(c)
    if "nc" not in _NC_CACHE:
        _NC_CACHE["nc"] = build_program()
    nc = _NC_CACHE["nc"]
    shared = dict(
        w_ada=f(w_ada[0]), b_ada=f(b_ada[0]).reshape(1, -1), gmix=_pc(g_mix[0], 8), w_in=f(w_in[0]),
        convw=np.ascontiguousarray(np.concatenate([_pc(conv_w[0][kk], 4) for kk in range(3)], axis=1)),
        convb=_pc(conv_b[0], 4),
        gattn=np.ascontiguousarray(f(g_attn_out[0]).reshape(8, 64).T),
        gconv=_pc(g_conv_out[0], 4), w_out=f(w_out[0]), gffn=_pc(g_ffn[0], 8), w_up=f(w_up[0]),
        fcw=np.ascontiguousarray(np.concatenate([_pc(ffn_conv_w[0][kk], 44) for kk in range(3)], axis=1)),
        fcb=_pc(ffn_conv_b[0], 44), w_down=f(w_down[0]),
        gfin=np.ascontiguousarray(np.broadcast_to(f(g_final).reshape(1, -1), (128, D))),
    )
    cst = [_consts(0), _consts(1)]
    in_maps = []
    for i in range(NCORES):
        b, half = i // 2, i % 2
        if half == 1:
            xs = x[b]
        else:
            xs = np.concatenate([np.zeros((4096, D), np.float32), x[b, :4096]], axis=0)
        m = dict(shared)
        m.update(cst[half])
        m["xs"] = np.ascontiguousarray(xs)
        m["cT"] = _pc(c[b], 8)
        in_maps.append(m)
    res = run_bass_kernel_spmd(nc, in_maps, core_ids=list(range(NCORES)))
    outp = np.empty((4, S, D), np.float32)
    for i in range(NCORES):
        b, half = i // 2, i % 2
        outp[b, half * 4096:(half + 1) * 4096] = res.results[i]["out"]
    return outp
```

```python
import numpy as np
from contextlib import ExitStack
import ml_dtypes
import concourse.bass as bass
import concourse.mybir as mybir
from concourse.bass_utils import run_bass_kernel_spmd

F32 = mybir.dt.float32
BF16 = mybir.dt.bfloat16
AF = mybir.ActivationFunctionType
ALU = mybir.AluOpType
AX = mybir.AxisListType

D = 1024
S = 8192
NH = 8
DFF = 2816
NQB = 17
NQT = 34
QTOK = NQB * 256
EPS = 1e-6
NEG = -30000.0
NCORES = 8
PHASES = 3
DEBUG = False


class Tok:
    __slots__ = ("sem", "val")

    def __init__(self, sem, val):
        self.sem = sem
        self.val = val


class Slot:
    def __init__(self, name=""):
        self.name = name
        self.w = None
        self.r = {}
        self.dsem = None
        self.dcnt = 0


class Builder:
    ENG = ("pe", "act", "dve", "pool", "sp")

    def __init__(self, nc, es):
        self.nc = nc
        self.es = es
        self.q = {}
        for n in self.ENG:
            sem = es.enter_context(nc.semaphore("sem_" + n))
            self.q[n] = dict(sem=sem, cnt=0, ops=[], waited={})
        self.dslots = []

    def _waits(self, en, deps):
        q = self.q[en]
        need = {}
        for t in deps:
            if t is None:
                continue
            if en == "pe" and t.sem is q["sem"]:
                continue
            k = id(t.sem)
            if q["waited"].get(k, 0) >= t.val:
                continue
            if k not in need or need[k].val < t.val:
                need[k] = t
        for k, t in need.items():
            q["waited"][k] = t.val
        return [(t.sem, t.val) for t in need.values()]

    @staticmethod
    def _deps(reads, writes, deps):
        d = list(deps)
        for s in reads:
            d.append(s.w)
        for s in writes:
            d.extend(s.r.values())
            d.append(s.w)
        return d

    @staticmethod
    def _update(tok, reads, writes):
        for s in writes:
            s.w = tok
            s.r = {}
        for s in reads:
            k = id(tok.sem)
            if k not in s.r or s.r[k].val < tok.val:
                s.r[k] = tok

    def op(self, en, fn, reads=(), writes=(), deps=(), inc=True):
        q = self.q[en]
        waits = self._waits(en, self._deps(reads, writes, deps))
        tok = None
        if inc:
            q["cnt"] += 1
            tok = Tok(q["sem"], q["cnt"])
        else:
            assert not reads and not writes
        sem = q["sem"]

        def emit(e, waits=waits, fn=fn, inc=inc, sem=sem):
            for (sm, v) in waits:
                e.wait_ge(sm, v)
            ins = fn(e)
            if inc:
                ins.then_inc(sem, 1)

        q["ops"].append(emit)
        if tok is not None:
            self._update(tok, reads, writes)
        return tok

    def pe_group(self, fns, reads=(), writes=()):
        q = self.q["pe"]
        waits = self._waits("pe", self._deps(reads, writes, ()))
        q["cnt"] += 1
        tok = Tok(q["sem"], q["cnt"])
        sem = q["sem"]
        n = len(fns)

        def emit(e, waits=waits, fns=fns, sem=sem, n=n):
            for (sm, v) in waits:
                e.wait_ge(sm, v)
            for i, f in enumerate(fns):
                ins = f(e)
                if i == n - 1:
                    ins.then_inc(sem, 1)

        q["ops"].append(emit)
        self._update(tok, reads, writes)
        return tok

    def dma(self, en, out, in_, sem_slot, reads=(), writes=(), deps=(), **kw):
        q = self.q[en]
        sl = sem_slot
        if sl.dsem is None:
            sl.dsem = self.es.enter_context(self.nc.semaphore("dsem%d" % len(self.dslots)))
            self.dslots.append(sl)
        waits = self._waits(en, self._deps(reads, writes, deps))
        sl.dcnt += 16
        tok = Tok(sl.dsem, sl.dcnt)
        dsem = sl.dsem

        def emit(e, waits=waits, out=out, in_=in_, dsem=dsem, kw=kw):
            for (sm, v) in waits:
                e.wait_ge(sm, v)
            e.dma_start(out=out, in_=in_, **kw).then_inc(dsem, 16)

        q["ops"].append(emit)
        self._update(tok, reads, writes)
        return tok

    def emit_block(self):
        nc = self.nc
        finals = [(s.dsem, s.dcnt) for s in self.dslots if s.dcnt > 0]
        pe_fin = (self.q["pe"]["sem"], self.q["pe"]["cnt"])

        def fin(e, finals=finals):
            for sm, v in finals:
                e.wait_ge(sm, v)

        self.q["sp"]["ops"].append(fin)
        with nc.Block() as blk:
            for n, meth in (("pe", blk.tensor), ("act", blk.scalar), ("dve", blk.vector),
                            ("pool", blk.gpsimd), ("sp", blk.sync)):
                ops = self.q[n]["ops"]
                if ops:
                    def body(e, ops=ops):
                        for f in ops:
                            f(e)
                    meth(body)
                self.q[n]["ops"] = []


def mm(out, lhsT, rhs, start, stop):
    return lambda e: e.matmul(out, lhsT=lhsT, rhs=rhs, start=start, stop=stop)


def tr(out, in_, ident):
    return lambda e: e.transpose(out, in_, ident)


def build_program():
    nc = bass.Bass("TRN2", target_bir_lowering=False)

    def din(name, shape, dt=F32):
        return nc.dram_tensor(name, list(shape), dt, kind="ExternalInput").ap()

    def dscr(name, shape, dt):
        return nc.dram_tensor(name, list(shape), dt, kind="ExternalOutput" if DEBUG else "Internal").ap()

    xs = din("xs", [S, D])
    cT = din("cT", [128, 8])
    w_ada = din("w_ada", [D, 6 * D])
    b_ada = din("b_ada", [1, 6 * D])
    gmix = din("gmix", [128, 8])
    w_in = din("w_in", [D, 3072])
    convw = din("convw", [128, 12])
    convb = din("convb", [128, 4])
    gattn = din("gattn", [64, 8])
    gconv = din("gconv", [128, 4])
    w_out = din("w_out", [D, D])
    gffn = din("gffn", [128, 8])
    w_up = din("w_up", [D, 2 * DFF])
    fcw = din("fcw", [128, 132])
    fcb = din("fcb", [128, 44])
    w_down = din("w_down", [DFF, D])
    gfin = din("gfin", [128, D])
    ident_d = din("ident", [128, 128], BF16)
    kstat = din("kstat", [35, S], BF16)
    cmask = din("cmask", [128, 512], BF16)
    gvb_d = din("gvb", [128, NQB * 32])
    gvb2_d = din("gvb2", [128, NQB * 32])
    qstat = din("qstat", [NH, 128, NQT * 99], BF16)
    hv_d = din("hv", [128, 1])
    out = nc.dram_tensor("out", [4096, D], F32, kind="ExternalOutput").ap()

    kT_s = dscr("kT_s", [NH, 64, S], BF16)
    qT_s = dscr("qT_s", [NH, 64, QTOK], BF16)
    at_s = dscr("at_s", [NH, 64, QTOK], BF16)
    cg_s = dscr("cg_s", [4, 128, QTOK], BF16)
    mod_s = dscr("mod_s", [1, 6 * D], F32)
    km_s = dscr("km_s", [NH, 64, 32], F32)

    es = ExitStack()
    with es:
        B = Builder(nc, es)

        def sb(st, name, shape, dt):
            return st.enter_context(nc.sbuf_tensor("sb_" + name, list(shape), dt))

        def ps(st, name, shape, dt):
            return st.enter_context(nc.psum_tensor("ps_" + name, list(shape), dt))

        ident = sb(es, "ident", [128, 128], BF16)
        ones_b = sb(es, "ones_b", [128, 1], BF16)
        ones_f = sb(es, "ones_f", [128, 64], F32)
        epsc = sb(es, "epsc", [128, 1], F32)
        hv = sb(es, "hv", [128, 1], F32)
        G1 = sb(es, "G1", [128, 8], F32)
        G2 = sb(es, "G2", [128, 8], F32)
        modT = sb(es, "modT", [128, 48], F32)
        gmix_sb = sb(es, "gmix_sb", [128, 8], F32)
        gffn_sb = sb(es, "gffn_sb", [128, 8], F32)
        convw_sb = sb(es, "convw_sb", [128, 12], F32)
        convb_sb = sb(es, "convb_sb", [128, 4], F32)
        gconv_sb = sb(es, "gconv_sb", [128, 4], F32)
        gattn_sb = sb(es, "gattn_sb", [64, 8], F32)
        fcw_sb = sb(es, "fcw_sb", [128, 132], F32)
        fcb_sb = sb(es, "fcb_sb", [128, 44], F32)
        ssa = sb(es, "ssa", [128, NQT], F32)
        ssc = sb(es, "ssc", [128, NQT], F32)
        wo = sb(es, "wo", [128, 8, D], BF16)
        S_wo = Slot("wo")
        S_const = Slot("const")
        S_ssa = Slot("ssa")
        S_ssc = Slot("ssc")
        S_G = Slot("G")
        S_mods = Slot("mods")

        for dst, src in ((ident, ident_d), (hv, hv_d), (gmix_sb, gmix), (gffn_sb, gffn), (convw_sb, convw),
                         (convb_sb, convb), (gconv_sb, gconv), (gattn_sb, gattn), (fcw_sb, fcw), (fcb_sb, fcb)):
            B.dma("sp", dst[:], src, S_const, writes=[S_const])
        B.op("dve", lambda e: e.memset(ones_b[:], 1.0), writes=[S_const])
        B.op("dve", lambda e: e.memset(ones_f[:], 1.0), writes=[S_const])
        B.op("dve", lambda e: e.memset(epsc[:], EPS), writes=[S_const])

        with ExitStack() as p0:
            c_sb = sb(p0, "c_sb", [128, 8], F32)
            sc_b = sb(p0, "sc_b", [128, 8], BF16)
            brow = sb(p0, "brow", [1, 6 * D], F32)
            modrow = sb(p0, "modrow", [1, 6 * D], F32)
            wa = [sb(p0, "wa%d" % i, [128, 8, 512], BF16) for i in range(2)]
            pm = [ps(p0, "pm%d" % i, [128, 512], F32) for i in range(2)]
            S_c, S_scb, S_brow, S_modrow, S_modT = Slot(), Slot(), Slot(), Slot(), Slot()
            S_wa = [Slot(), Slot()]
            S_pm = [Slot(), Slot()]
            B.dma("sp", c_sb[:], cT, S_c, writes=[S_c])
            B.dma("sp", brow[:], b_ada, S_brow, writes=[S_brow])
            B.op("act", lambda e: e.activation(out=sc_b[:], in_=c_sb[:], func=AF.Silu), reads=[S_c], writes=[S_scb])
            wav = w_ada.rearrange("(c p) n -> p c n", p=128)
            for g in range(12):
                k = g % 2
                B.dma("pool", wa[k][:], wav[:, :, g * 512:(g + 1) * 512], S_wa[k], writes=[S_wa[k]])
                B.pe_group([mm(pm[k][0:1, :], sc_b[:, c:c + 1], wa[k][:, c, :], c == 0, c == 7) for c in range(8)],
                           reads=[S_wa[k], S_scb], writes=[S_pm[k]])
                B.op("dve", lambda e, k=k, g=g: e.tensor_tensor(out=modrow[0:1, g * 512:(g + 1) * 512], in0=pm[k][0:1, :],
                                                                 in1=brow[0:1, g * 512:(g + 1) * 512], op=ALU.add),
                     reads=[S_pm[k], S_brow], writes=[S_modrow])
            B.dma("sp", mod_s, modrow[:], S_mods, reads=[S_modrow], writes=[S_mods])
            B.dma("sp", modT[:], mod_s.rearrange("o (k p) -> (o p) k", p=128), S_modT, reads=[S_mods], writes=[S_modT],
                  allow_slow_non_contiguous=True)
            B.op("dve", lambda e: e.scalar_tensor_tensor(out=G1[:], in0=modT[:, 8:16], scalar=1.0, in1=gmix_sb[:],
                                                         op0=ALU.add, op1=ALU.mult), reads=[S_modT, S_const], writes=[S_G])
            B.op("dve", lambda e: e.scalar_tensor_tensor(out=G2[:], in0=modT[:, 32:40], scalar=1.0, in1=gffn_sb[:],
                                                         op0=ALU.add, op1=ALU.mult), reads=[S_modT, S_const], writes=[S_G])
            B.emit_block()
        sh1 = modT[:, 0:8]
        sh2 = modT[:, 24:32]

        with ExitStack() as p12:
          if PHASES >= 1:
            V_all = sb(p12, "V_all", [128, 64, NH, 65], BF16)
            S_V = Slot("V")
            with ExitStack() as p1:
                w_in_sb = sb(p1, "w_in_sb", [128, 8, 3072], BF16)
                S_win = Slot("win")
                wiv = w_in.rearrange("(c p) n -> p c n", p=128)
                for c in range(8):
                    B.dma("pool", w_in_sb[:, c, :], wiv[:, c, :], S_win, writes=[S_win])
                pend_stats = []
                xt = [sb(p1, "xt%d" % i, [128, 2, D], F32) for i in range(3)]
                xn = [sb(p1, "xnp%d" % i, [128, 2, D], BF16) for i in range(3)]
                hT = [sb(p1, "hT%d" % i, [128, 8, 258], BF16) for i in range(2)]
                junk = sb(p1, "junk", [128, D], BF16)
                ss = [sb(p1, "ssp%d" % i, [128, 2], F32) for i in range(3)]
                rt = [sb(p1, "rt%d" % i, [128, 2], F32) for i in range(3)]
                rstd = [sb(p1, "rstd%d" % i, [128, 2], F32) for i in range(3)]
                kst = [sb(p1, "kst%d" % i, [128, 4, 256], BF16) for i in range(2)]
                qst = [sb(p1, "qst%d" % i, [128, 4, 256], BF16) for i in range(2)]
                cgs = [sb(p1, "cgs%d" % i, [128, 4, 256], BF16) for i in range(2)]
                sqc = sb(p1, "sqc", [128, 4, 256], BF16)
                kmst = sb(p1, "kmst", [128, 4, 32], F32)
                S_kmst = Slot()
                S_kmd = Slot("km_s")
                u_sb = [sb(p1, "u_sb%d" % i, [128, 258], F32) for i in range(2)]
                zb = [sb(p1, "zb%d" % i, [128, 258], F32) for i in range(2)]
                y0 = [sb(p1, "y0%d" % i, [128, 256], F32) for i in range(2)]
                y1 = [sb(p1, "y1%d" % i, [128, 256], F32) for i in range(2)]
                y2 = [sb(p1, "y2%d" % i, [128, 256], F32) for i in range(2)]
                cv = [sb(p1, "cv%d" % i, [128, 256], F32) for i in range(2)]
                tp = ps(p1, "tp", [128, 8, 256], BF16)
                pp = [ps(p1, "pp%d" % i, [128, 512], F32) for i in range(5)]
                pss = ps(p1, "pss", [128, 512], F32)
                S_xt = [Slot(), Slot(), Slot()]
                S_xn = [Slot(), Slot(), Slot()]
                S_hT = [Slot(), Slot()]
                S_junk = Slot()
                S_ss = [Slot(), Slot(), Slot()]
                S_rt = [Slot(), Slot(), Slot()]
                S_rstd = [Slot(), Slot(), Slot()]
                S_kst = [Slot(), Slot()]
                S_qst = [Slot(), Slot()]
                S_cgs = [Slot(), Slot()]
                S_sqc = Slot()
                S_u = [Slot(), Slot()]
                S_z = [Slot(), Slot()]
                S_y0 = [Slot(), Slot()]
                S_y1 = [Slot(), Slot()]
                S_y2 = [Slot(), Slot()]
                S_cv = [Slot(), Slot()]
                S_tp = Slot()
                S_pp = [Slot() for _ in range(5)]
                S_pss = Slot()
                S_kT = Slot("kT_s")
                S_qT = Slot("qT_s")
                S_cgd = Slot("cg_s")
                ppi = [0]

                def next_pp():
                    i = ppi[0] % 5
                    ppi[0] += 1
                    return pp[i], S_pp[i]

                B.op("pool", lambda e: e.memset(V_all[:, :, :, 64:65], 1.0), writes=[S_V])
                for i in range(2):
                    B.op("pool", lambda e, i=i: e.memset(hT[i][:, :, 0:2], 0.0), writes=[S_hT[i]])
                xsv = xs.rearrange("(n t p) d -> n p t d", t=2, p=128)
                kTv = kT_s.rearrange("(pr two) d t -> (two d) pr t", two=2)
                qTv = qT_s.rearrange("(pr two) d t -> (two d) pr t", two=2)
                cgv = cg_s.rearrange("c p t -> p c t")
                evac_rr = [0]

                def evac(out_ap, in_ap, reads, writes, scale=None):
                    evac_rr[0] += 1
                    if evac_rr[0] % 2 == 0:
                        if scale is None:
                            return B.op("act", lambda e: e.activation(out=out_ap, in_=in_ap, func=AF.Copy), reads=reads, writes=writes)
                        return B.op("act", lambda e: e.activation(out=out_ap, in_=in_ap, func=AF.Copy, scale=scale), reads=reads, writes=writes)
                    if scale is None:
                        return B.op("dve", lambda e: e.tensor_copy(out=out_ap, in_=in_ap), reads=reads, writes=writes)
                    return B.op("dve", lambda e: e.tensor_scalar(out=out_ap, in0=in_ap, scalar1=scale, scalar2=None, op0=ALU.mult),
                                reads=reads, writes=writes)

                def f1a(s):
                    k = s % 3
                    B.dma("sp", xt[k][:], xsv[s], S_xt[k], writes=[S_xt[k]])
                    for t in range(2):
                        B.op("act", lambda e, k=k, t=t: e.activation(out=junk[:], in_=xt[k][:, t, :], func=AF.Square,
                                                                     accum_out=ss[k][:, t:t + 1]),
                             reads=[S_xt[k]], writes=[S_junk, S_ss[k]])
                    B.op("act", lambda e, k=k: e.activation(out=rt[k][:], in_=ss[k][:], func=AF.Sqrt, scale=1.0 / D, bias=epsc[:]),
                         reads=[S_ss[k], S_const], writes=[S_rt[k]])
                    B.op("dve", lambda e, k=k: e.reciprocal(out=rstd[k][:], in_=rt[k][:]), reads=[S_rt[k]], writes=[S_rstd[k]])
                    for t in range(2):
                        B.op("dve", lambda e, k=k, t=t: e.tensor_scalar(out=xn[k][:, t, :], in0=xt[k][:, t, :], scalar1=rstd[k][:, t:t + 1],
                                                                        scalar2=None, op0=ALU.mult),
                             reads=[S_xt[k], S_rstd[k]], writes=[S_xn[k]])

                def f1b(s):
                    k = s % 2
                    B.pe_group([tr(tp[:, c, t * 128:(t + 1) * 128], xn[s % 3][:, t, c * 128:(c + 1) * 128], ident[:])
                                for t in range(2) for c in range(8)], reads=[S_xn[s % 3], S_const], writes=[S_tp])
                    for c in range(8):
                        if c % 2 == 0:
                            B.op("act", lambda e, k=k, c=c: e.activation(out=hT[k][:, c, 2:258], in_=tp[:, c, :], func=AF.Identity,
                                                                         scale=G1[:, c:c + 1], bias=sh1[:, c:c + 1]),
                                 reads=[S_tp, S_G], writes=[S_hT[k]])
                        else:
                            B.op("dve", lambda e, k=k, c=c: e.tensor_scalar(out=hT[k][:, c, 2:258], in0=tp[:, c, :], scalar1=G1[:, c:c + 1],
                                                                            scalar2=sh1[:, c:c + 1], op0=ALU.mult, op1=ALU.add),
                                 reads=[S_tp, S_G], writes=[S_hT[k]])
                    if s == 16:
                        B.op("dve", lambda e, k=k: e.tensor_scalar(out=hT[k][:, :, 0:2], in0=hT[1 - k][:, :, 256:258], scalar1=hv[:, 0:1],
                                                                   scalar2=None, op0=ALU.mult),
                             reads=[S_hT[1 - k], S_const], writes=[S_hT[k]])
                    elif s > 16:
                        B.op("dve", lambda e, k=k: e.tensor_copy(out=hT[k][:, :, 0:2], in_=hT[1 - k][:, :, 256:258]),
                             reads=[S_hT[1 - k]], writes=[S_hT[k]])

                def back1a(s):
                    k = s % 2
                    for pr in range(4):
                        pt_, sp_ = next_pp()
                        B.pe_group([mm(pt_[:, 0:256], w_in_sb[:, c, 512 + pr * 128:512 + (pr + 1) * 128], hT[k][:, c, 2:258], c == 0, c == 7)
                                    for c in range(8)], reads=[S_win, S_hT[k]], writes=[sp_])
                        evac(kst[k][:, pr, :], pt_[:, 0:256], [sp_], [S_kst[k]])
                        B.op("dve", lambda e, k=k, pr=pr, s=s: e.tensor_reduce(out=kmst[:, pr, s:s + 1], in_=kst[k][:, pr, :], axis=AX.X, op=ALU.add),
                             reads=[S_kst[k]], writes=[S_kmst])
                    B.dma("sp", kTv[:, :, s * 256:(s + 1) * 256], kst[k][:], S_kst[k], reads=[S_kst[k]], writes=[S_kT])
                    for t in range(2):
                        pt_, sp_ = next_pp()
                        B.pe_group([mm(pt_[:, :], hT[k][:, c, 2 + t * 128:2 + (t + 1) * 128], w_in_sb[:, c, 1024:1536], c == 0, c == 7)
                                    for c in range(8)], reads=[S_win, S_hT[k]], writes=[sp_])
                        evac(V_all[:, 2 * s + t, :, 0:64], pt_[:, :].rearrange("p (h d) -> p h d", d=64), [sp_], [S_V])

                def back1b(s):
                    k = s % 2
                    if s < 15:
                        return
                    qb = s - 15
                    for pr in range(4):
                        pt_, sp_ = next_pp()
                        B.pe_group([mm(pt_[:, 0:256], w_in_sb[:, c, pr * 128:(pr + 1) * 128], hT[k][:, c, 2:258], c == 0, c == 7)
                                    for c in range(8)], reads=[S_win, S_hT[k]], writes=[sp_])
                        evac(qst[k][:, pr, :], pt_[:, 0:256], [sp_], [S_qst[k]], scale=0.125)
                    B.dma("sp", qTv[:, :, qb * 256:(qb + 1) * 256], qst[k][:], S_qst[k], reads=[S_qst[k]], writes=[S_qT])
                    for ch in range(4):
                        j = ch % 2
                        pu_, spu = next_pp()
                        B.pe_group([mm(pu_[:, 0:258], w_in_sb[:, c, 1536 + ch * 128:1536 + (ch + 1) * 128], hT[k][:, c, 0:258], c == 0, c == 7)
                                    for c in range(8)], reads=[S_win, S_hT[k]], writes=[spu])
                        pc_, spc = next_pp()
                        B.pe_group([mm(pc_[:, 0:258], w_in_sb[:, c, 2048 + ch * 128:2048 + (ch + 1) * 128], hT[k][:, c, 0:258], c == 0, c == 7)
                                    for c in range(8)], reads=[S_win, S_hT[k]], writes=[spc])
                        pb_, spb = next_pp()
                        B.pe_group([mm(pb_[:, 0:256], w_in_sb[:, c, 2560 + ch * 128:2560 + (ch + 1) * 128], hT[k][:, c, 2:258], c == 0, c == 7)
                                    for c in range(8)], reads=[S_win, S_hT[k]], writes=[spb])
                        B.op("act", lambda e, j=j, pu_=pu_: e.activation(out=u_sb[j][:], in_=pu_[:, 0:258], func=AF.Copy),
                             reads=[spu], writes=[S_u[j]])
                        B.op("dve", lambda e, j=j, pc_=pc_: e.tensor_tensor(out=zb[j][:], in0=pc_[:, 0:258], in1=u_sb[j][:], op=ALU.mult),
                             reads=[spc, S_u[j]], writes=[S_z[j]])
                        B.op("dve", lambda e, j=j, ch=ch: e.tensor_scalar(out=y0[j][:], in0=zb[j][:, 2:258], scalar1=convw_sb[:, 8 + ch:9 + ch],
                                                                          scalar2=convb_sb[:, ch:ch + 1], op0=ALU.mult, op1=ALU.add),
                             reads=[S_z[j], S_const], writes=[S_y0[j]])
                        B.op("dve", lambda e, j=j, ch=ch: e.scalar_tensor_tensor(out=y1[j][:], in0=zb[j][:, 1:257], scalar=convw_sb[:, 4 + ch:5 + ch],
                                                                                 in1=y0[j][:], op0=ALU.mult, op1=ALU.add),
                             reads=[S_z[j], S_y0[j], S_const], writes=[S_y1[j]])
                        B.op("dve", lambda e, j=j, ch=ch: e.scalar_tensor_tensor(out=y2[j][:], in0=zb[j][:, 0:256], scalar=convw_sb[:, ch:ch + 1],
                                                                                 in1=y1[j][:], op0=ALU.mult, op1=ALU.add),
                             reads=[S_z[j], S_y1[j], S_const], writes=[S_y2[j]])
                        B.op("dve", lambda e, j=j, pb_=pb_: e.tensor_tensor(out=cv[j][:], in0=pb_[:, 0:256], in1=y2[j][:], op=ALU.mult),
                             reads=[spb, S_y2[j]], writes=[S_cv[j]])
                        B.op("act", lambda e, j=j, ch=ch: e.activation(out=sqc[:, ch, :], in_=cv[j][:], func=AF.Square),
                             reads=[S_cv[j]], writes=[S_sqc])
                        B.op("act", lambda e, j=j, ch=ch, k=k: e.activation(out=cgs[k][:, ch, :], in_=cv[j][:], func=AF.Copy,
                                                                           scale=gconv_sb[:, ch:ch + 1]),
                             reads=[S_cv[j], S_const], writes=[S_cgs[k]])
                    B.dma("sp", cgv[:, :, qb * 256:(qb + 1) * 256], cgs[k][:], S_cgs[k], reads=[S_cgs[k]], writes=[S_cgd])

                    def stats(qb=qb):
                        for t in range(2):
                            B.pe_group([mm(pss[:, t:t + 1], sqc[:, ch, t * 128:(t + 1) * 128], ones_b[:, 0:1], ch == 0, ch == 3) for ch in range(4)],
                                       reads=[S_sqc, S_const], writes=[S_pss])
                        B.op("dve", lambda e: e.tensor_copy(out=ssc[:, 2 * qb:2 * qb + 2], in_=pss[:, 0:2]), reads=[S_pss], writes=[S_ssc])
                    pend_stats.append(stats)

                f1a(0)
                f1b(0)
                f1a(1)
                f1a(2)
                for s in range(32):
                    if s + 1 < 32:
                        f1b(s + 1)
                    if s + 3 < 32:
                        f1a(s + 3)
                    back1a(s)
                    while pend_stats:
                        pend_stats.pop(0)()
                    if s == 5:
                        wov = w_out.rearrange("(c p) n -> p c n", p=128)
                        for c in range(8):
                            B.dma("pool", wo[:, c, :], wov[:, c, :], S_wo, writes=[S_wo])
                    back1b(s)
                while pend_stats:
                    pend_stats.pop(0)()
                B.dma("sp", km_s.rearrange("(pr two) d j -> (two d) pr j", two=2), kmst[:], S_kmst, reads=[S_kmst], writes=[S_kmd])
                B.emit_block()

            with ExitStack() as p2:
              if PHASES >= 2:
                kaug = [sb(p2, "kaug%d" % i, [99, S], BF16) for i in range(2)]
                qaug = [sb(p2, "qaug%d" % i, [99, QTOK], BF16) for i in range(2)]
                qa = [sb(p2, "qa%d" % i, [128, NQT, 99], BF16) for i in range(2)]
                atst = [sb(p2, "atst%d" % i, [64, QTOK], BF16) for i in range(2)]
                gvb = sb(p2, "gvb", [128, NQB, 32], F32)
                gvb2 = sb(p2, "gvb2", [128, NQB, 32], F32)
                cm = sb(p2, "cm", [128, 2, 256], BF16)
                km_f = [sb(p2, "km_f%d" % i, [64, 32], F32) for i in range(2)]
                km_b = [sb(p2, "km_b%d" % i, [64, 32], BF16) for i in range(2)]
                gm = [sb(p2, "gm%d" % i, [128, 32], F32) for i in range(2)]
                t8 = [sb(p2, "t8%d" % i, [128, 8], F32) for i in range(2)]
                tsel = [sb(p2, "tsel%d" % i, [128, 32], F32) for i in range(2)]
                pT = [sb(p2, "pT%d" % i, [128, 2, 256], BF16) for i in range(3)]
                rc = [sb(p2, "rc%d" % i, [128, 256], F32) for i in range(4)]
                pos = [sb(p2, "pos%d" % i, [128, 256], F32) for i in range(4)]
                atf = [sb(p2, "atf%d" % i, [64, 256], F32) for i in range(4)]
                sqa = [sb(p2, "sqa%d" % i, [64, 256], BF16) for i in range(4)]
                sp = [ps(p2, "sp%d" % i, [128, 2, 256], F32) for i in range(3)]
                po = [ps(p2, "po%d" % i, [128, 512], F32) for i in range(2)]
                pgt = ps(p2, "pgt", [128, 512], F32)
                pbc = ps(p2, "pbc", [128, 512], F32)
                ptt_t = ps(p2, "ptt", [128, 1024], BF16)
                ptt = ptt_t[:, 0:128]
                S_kaug = [Slot(), Slot()]
                S_kstat = Slot()
                S_qq = [Slot(), Slot()]
                S_qm = [Slot(), Slot()]
                S_qa = [Slot(), Slot()]
                S_atst = [Slot(), Slot()]
                S_c2 = Slot()
                S_kmf = [Slot(), Slot()]
                S_kmb = [Slot(), Slot()]
                S_gm = [Slot(), Slot()]
                S_t8 = [Slot(), Slot()]
                S_tsel = [Slot(), Slot()]
                S_pT = [Slot() for _ in range(3)]
                S_rc = [Slot() for _ in range(4)]
                S_pos = [Slot() for _ in range(4)]
                S_atf = [Slot() for _ in range(4)]
                S_sqa = [Slot() for _ in range(4)]
                S_sp = [Slot() for _ in range(3)]
                S_po = [Slot(), Slot()]
                S_pgt, S_ptt, S_pbc = Slot(), Slot(), Slot()
                S_pss2 = S_pbc
                S_atd = Slot("at_s")

                B.dma("sp", gvb[:].rearrange("p a b -> p (a b)"), gvb_d, S_c2, writes=[S_c2])
                B.dma("sp", gvb2[:].rearrange("p a b -> p (a b)"), gvb2_d, S_c2, writes=[S_c2])
                B.dma("sp", cm[:].rearrange("p a b -> p (a b)"), cmask, S_c2, writes=[S_c2])
                for i in range(2):
                    B.dma("sp", kaug[i][64:99, :], kstat, S_kstat, writes=[S_kstat])

                def load_head(h):
                    hb = h % 2
                    B.dma("sp", kaug[hb][0:64, :], kT_s[h], S_kaug[hb], reads=[S_kT], writes=[S_kaug[hb]])
                    B.dma("sp", qaug[hb][0:64, :], qT_s[h], S_qq[hb], reads=[S_qT], writes=[S_qq[hb]])
                    B.dma("sp", qa[hb][:].rearrange("p a b -> p (a b)"), qstat[h], S_qa[hb], writes=[S_qa[hb]])

                def prep_head(h):
                    hb = h % 2
                    B.dma("sp", km_f[hb][:], km_s[h], S_kmf[hb], reads=[S_kmd], writes=[S_kmf[hb]])
                    B.op("dve", lambda e: e.tensor_scalar(out=km_b[hb][:], in0=km_f[hb][:], scalar1=1.0 / 256, scalar2=None, op0=ALU.mult),
                         reads=[S_kmf[hb]], writes=[S_kmb[hb]])

                def mask_a(h, qt):
                    hb = h % 2
                    qb = qt // 2
                    g2 = qt % 2
                    B.pe_group([mm(pgt[:, 0:32], qaug[hb][0:64, qt * 128:(qt + 1) * 128], km_b[hb][:, :], True, True)],
                               reads=[S_qq[hb], S_kmb[hb]], writes=[S_pgt])
                    B.op("dve", lambda e: e.tensor_tensor(out=gm[g2][:], in0=pgt[:, 0:32], in1=gvb[:, qb, :], op=ALU.add),
                         reads=[S_pgt, S_c2], writes=[S_gm[g2]])
                    B.op("dve", lambda e: e.max(out=t8[g2][:], in_=gm[g2][:]), reads=[S_gm[g2]], writes=[S_t8[g2]])
                    B.op("dve", lambda e: e.tensor_scalar(out=tsel[g2][:], in0=gm[g2][:], scalar1=t8[g2][:, 2:3], scalar2=-NEG,
                                                          op0=ALU.is_ge, op1=ALU.mult),
                         reads=[S_gm[g2], S_t8[g2]], writes=[S_tsel[g2]])
                    B.op("dve", lambda e: e.tensor_tensor(out=qa[hb][:, qt, 64:96], in0=tsel[g2][:], in1=gvb2[:, qb, :], op=ALU.add),
                         reads=[S_tsel[g2], S_c2], writes=[S_qa[hb]])

                def mask_b(h, qt):
                    hb = h % 2
                    B.pe_group([tr(ptt[0:99, :], qa[hb][:, qt, :], ident[:])], reads=[S_qa[hb], S_const], writes=[S_ptt])
                    B.op("dve", lambda e: e.tensor_copy(out=qaug[hb][64:99, qt * 128:(qt + 1) * 128], in_=ptt[64:99, :]),
                         reads=[S_ptt], writes=[S_qm[hb]])

                sctr = [0]
                HORDER = list(range(NH - 1, -1, -1))
                KEEP = []
                for h_ in range(NH):
                    slope_ = 2.0 ** (-(h_ + 1))
                    kk_ = 0
                    while kk_ < 31 and slope_ * (kk_ * 256 + 1) < 80.0:
                        kk_ += 1
                    KEEP.append(kk_)

                def s_mm(h, qb, j):
                    hb = h % 2
                    sB = 15 + qb
                    qc = slice(qb * 256, (qb + 1) * 256)
                    i = sctr[0] % 3
                    sctr[0] += 1
                    fns = []
                    for kt in range(2):
                        fns.append(mm(sp[i][:, kt, :], kaug[hb][0:99, (2 * j + kt) * 128:(2 * j + kt + 1) * 128], qaug[hb][0:99, qc],
                                      True, j != sB))
                        if j == sB:
                            fns.append(mm(sp[i][:, kt, :], ident[:], cm[:, kt, :], False, True))
                    B.pe_group(fns, reads=[S_kaug[hb], S_kstat, S_qq[hb], S_qm[hb], S_c2, S_const], writes=[S_sp[i]])
                    B.op("act", lambda e: e.activation(out=pT[i][:], in_=sp[i][:], func=AF.Exp), reads=[S_sp[i]], writes=[S_pT[i]])
                    return i

                def pv_mm(h, qb, j, i, first, last):
                    ob = qb % 2
                    B.pe_group([mm(po[ob][0:65, 0:256], V_all[:, 2 * j + kt, h, :], pT[i][:, kt, :], (first and kt == 0), (last and kt == 1))
                                for kt in range(2)], reads=[S_V, S_pT[i]], writes=[S_po[ob]])

                tctr = [0]

                def tail_a(h, qb):
                    ob = qb % 2
                    r = tctr[0] % 4
                    tctr[0] += 1
                    B.op("act", lambda e: e.activation(out=pos[r][0:65, :], in_=po[ob][0:65, 0:256], func=AF.Copy), reads=[S_po[ob]], writes=[S_pos[r]])
                    B.op("dve", lambda e: e.reciprocal(out=rc[r][64:65, :], in_=pos[r][64:65, :]), reads=[S_pos[r]], writes=[S_rc[r]])
                    return r

                def tail_b(h, qb, r):
                    hb = h % 2
                    qc = slice(qb * 256, (qb + 1) * 256)
                    B.pe_group([mm(pbc[0:64, 0:256], ones_f[64:65, 0:64], rc[r][64:65, :], True, True)],
                               reads=[S_rc[r], S_const], writes=[S_pbc])
                    B.op("dve", lambda e: e.tensor_tensor(out=atf[r][:], in0=pbc[0:64, 0:256], in1=pos[r][0:64, :], op=ALU.mult),
                         reads=[S_pbc, S_pos[r]], writes=[S_atf[r]])
                    B.op("pool", lambda e: e.tensor_tensor(out=sqa[r][:], in0=atf[r][:], in1=atf[r][:], op=ALU.mult), reads=[S_atf[r]], writes=[S_sqa[r]])
                    B.op("pool", lambda e: e.tensor_scalar(out=atst[hb][:, qc], in0=atf[r][:], scalar1=gattn_sb[:, h:h + 1], scalar2=None, op0=ALU.mult),
                         reads=[S_atf[r], S_const], writes=[S_atst[hb]])

                def tail_c(h, qb, r):
                    B.pe_group([mm(pbc[:, 256 + t:257 + t], sqa[r][:, t * 128:(t + 1) * 128], ones_b[0:64, 0:1], True, True) for t in range(2)],
                               reads=[S_sqa[r], S_const], writes=[S_pss2])
                    if h == HORDER[0]:
                        B.op("dve", lambda e: e.tensor_copy(out=ssa[:, 2 * qb:2 * qb + 2], in_=pbc[:, 256:258]), reads=[S_pss2], writes=[S_ssa])
                    else:
                        B.op("dve", lambda e: e.tensor_tensor(out=ssa[:, 2 * qb:2 * qb + 2], in0=pbc[:, 256:258], in1=ssa[:, 2 * qb:2 * qb + 2],
                                                              op=ALU.add),
                             reads=[S_pss2, S_ssa], writes=[S_ssa])

                def blocks_of(h, qb):
                    sB = 15 + qb
                    return list(range(max(0, sB - KEEP[h]), sB + 1))

                load_head(HORDER[0])
                prep_head(HORDER[0])
                for qt in range(NQT):
                    mask_a(HORDER[0], qt)
                    mask_b(HORDER[0], qt)
                for hi, h in enumerate(HORDER):
                    hb = h % 2
                    hn = HORDER[hi + 1] if hi + 1 < NH else None
                    if hn is not None:
                        load_head(hn)
                        prep_head(hn)
                    total_iter = sum(len(blocks_of(h, qb)) for qb in range(NQB))
                    sched = {}
                    if hn is not None:
                        step = max(1, (total_iter - 8) // NQT)
                        for qt in range(NQT):
                            ia = min(total_iter - 1, 1 + qt * step)
                            ib = min(total_iter - 1, ia + 3)
                            sched.setdefault(ia, []).append(lambda qt=qt, hn=hn: mask_a(hn, qt))
                            sched.setdefault(ib, []).append(lambda qt=qt, hn=hn: mask_b(hn, qt))
                    it = 0
                    tails = []
                    for qb in range(NQB):
                        js = blocks_of(h, qb)
                        n = len(js)
                        ids = {0: s_mm(h, qb, js[0])}
                        if n > 1:
                            ids[1] = s_mm(h, qb, js[1])
                        for i in range(n):
                            if i + 2 < n:
                                ids[i + 2] = s_mm(h, qb, js[i + 2])
                            pv_mm(h, qb, js[i], ids[i], i == 0, i == n - 1)
                            for fn in sched.pop(it, []):
                                fn()
                            for item in list(tails):
                                if item[0] <= it:
                                    item[1]()
                                    tails.remove(item)
                            it += 1
                        r = tail_a(h, qb)
                        tails.append((it + 5, lambda h=h, qb=qb, r=r: tail_b(h, qb, r)))
                        tails.append((it + 10, lambda h=h, qb=qb, r=r: tail_c(h, qb, r)))
                    for key in sorted(sched):
                        for fn in sched[key]:
                            fn()
                    for item in tails:
                        item[1]()
                    B.dma("sp", at_s[h], atst[hb][:], S_atst[hb], reads=[S_atst[hb]], writes=[S_atd])
                B.emit_block()

        with ExitStack() as p3:
          if PHASES >= 3:
            wu = sb(p3, "wu", [128, 8, 2 * DFF], BF16)
            wd = sb(p3, "wd", [128, 22, D], BF16)
            x1 = [sb(p3, "x1%d" % i, [128, 2, D], F32) for i in range(2)]
            atb = sb(p3, "atb", [128, 4, 256], BF16)
            cgb = sb(p3, "cgb", [128, 4, 256], BF16)
            xn2 = sb(p3, "xn2", [128, 2, D], BF16)
            h2T = [sb(p3, "h2T%d" % i, [128, 8, 258], BF16) for i in range(2)]
            YA = [sb(p3, "YA%d" % i, [128, 256], F32) for i in range(2)]
            YB = [sb(p3, "YB%d" % i, [128, 256], F32) for i in range(2)]
            YV = [sb(p3, "YV%d" % i, [128, 256], F32) for i in range(2)]
            YG = [sb(p3, "YG%d" % i, [128, 256], F32) for i in range(2)]
            actT = sb(p3, "actT", [128, 22, 256], BF16)
            gfb = sb(p3, "gfb", [128, D], F32)
            rsa = sb(p3, "rsa", [128, NQT], F32)
            rsc = sb(p3, "rsc", [128, NQT], F32)
            ssf = sb(p3, "ssf", [128, 2], F32)
            rtf = sb(p3, "rtf", [128, 2], F32)
            rsf = sb(p3, "rsf", [128, 2], F32)
            ssb = sb(p3, "ssb", [128, 2], F32)
            rtb = sb(p3, "rtb", [128, 2], F32)
            rsb = sb(p3, "rsb", [128, 2], F32)
            pu = [ps(p3, "pu%d" % i, [128, 512], F32) for i in range(4)]
            pd = [ps(p3, "pd%d" % i, [128, 512], F32) for i in range(2)]
            pac = [ps(p3, "pac%d" % i, [128, 512], F32) for i in range(2)]
            S_wd = Slot()
            S_wuq = [Slot() for _ in range(4)]
            S_x1 = [Slot(), Slot()]
            S_atb, S_cgb, S_xn2 = Slot(), Slot(), Slot()
            S_h2T = [Slot(), Slot()]
            S_YA = [Slot() for _ in range(2)]
            S_YB = [Slot() for _ in range(2)]
            S_YV = [Slot(), Slot()]
            S_YG = [Slot(), Slot()]
            S_actT, S_gfb, S_rs = Slot(), Slot(), Slot()
            S_ssf, S_rtf, S_rsf, S_ssb, S_rtb, S_rsb = Slot(), Slot(), Slot(), Slot(), Slot(), Slot()
            S_pu = [Slot() for _ in range(4)]
            S_pd = [Slot(), Slot()]
            S_pac = [Slot(), Slot()]
            gtb = x1[1][:, 0, :]
            S_gtb = S_x1[1]

            wov = w_out.rearrange("(c p) n -> p c n", p=128)
            wuv = w_up.rearrange("(c p) n -> p c n", p=128)
            wdv = w_down.rearrange("(c p) n -> p c n", p=128)
            mod_t = mod_s.tensor
            B.dma("sp", gtb, bass.AP(mod_t, 2048, [[0, 128], [1, D]]), S_gtb, reads=[S_mods], writes=[S_gtb])
            B.dma("sp", gfb[:], gfin, S_gfb, writes=[S_gfb])
            WQ = [(0, 6), (6, 12), (12, 17), (17, 22)]
            for qi, (v0, v1) in enumerate(WQ):
                for base in (0, DFF):
                    B.dma("pool", wu[:, :, base + v0 * 128:base + v1 * 128], wuv[:, :, base + v0 * 128:base + v1 * 128], S_wuq[qi], writes=[S_wuq[qi]])
                if qi == 0:
                    for c in range(8):
                        B.op("pool", lambda e, c=c: e.tensor_tensor(out=wo[:, c, :], in0=wo[:, c, :], in1=gtb, op=ALU.mult),
                             reads=[S_gtb], writes=[S_wo])
            for c0 in range(0, 22, 2):
                B.dma("pool", wd[:, c0:c0 + 2, :], wdv[:, c0:c0 + 2, :], S_wd, writes=[S_wd])
            B.dma("sp", gtb, bass.AP(mod_t, 5120, [[0, 128], [1, D]]), S_gtb, reads=[S_mods], writes=[S_gtb])
            for c in range(22):
                B.op("pool", lambda e, c=c: e.tensor_tensor(out=wd[:, c, :], in0=wd[:, c, :], in1=gtb, op=ALU.mult),
                     reads=[S_gtb], writes=[S_wd])
            for src_, dst, ssl in ((ssa, rsa, S_ssa), (ssc, rsc, S_ssc)):
                B.op("act", lambda e, src_=src_, dst=dst: e.activation(out=dst[:], in_=src_[:], func=AF.Sqrt, scale=1.0 / 512, bias=epsc[:]),
                     reads=[ssl, S_const], writes=[S_rs])
                B.op("dve", lambda e, dst=dst: e.reciprocal(out=dst[:], in_=dst[:]), reads=[S_rs], writes=[S_rs])
            for i in range(2):
                B.op("dve", lambda e, i=i: e.memset(h2T[i][:, :, 0:2], 0.0), writes=[S_h2T[i]])

            xsv = xs.rearrange("(n t p) d -> n p t d", t=2, p=128)
            outv = out.rearrange("(n t p) d -> n p t d", t=2, p=128)
            atv = at_s.rearrange("(pr two) d t -> (two d) pr t", two=2)
            cgv = cg_s.rearrange("c p t -> p c t")
            puc = [0]
            ybc = [0]
            yac = [0]
            pend_fin = []
            tpv = [pac[i][:, :].bitcast(BF16).rearrange("p (c t) -> p c t", t=256) for i in range(2)]

            def next_yb():
                i = ybc[0] % 6
                ybc[0] += 1
                return yb[i], S_yb[i]

            def f3a_dma(qb):
                k = qb % 2
                sl = 15 + qb
                qc = slice(qb * 256, (qb + 1) * 256)
                B.dma("sp", x1[k][:], xsv[sl], S_x1[k], writes=[S_x1[k]])
                B.dma("sp", atb[:], atv[:, :, qc], S_atb, reads=[S_atd], writes=[S_atb])
                B.dma("sp", cgb[:], cgv[:, :, qc], S_cgb, reads=[S_cgd], writes=[S_cgb])

            def f3a(qb):
                k = qb % 2
                for t in range(2):
                    tile = 2 * qb + t
                    for n in range(2):
                        ns = slice(n * 512, (n + 1) * 512)
                        B.pe_group([mm(pac[0][:, :], atb[:, c, t * 128:(t + 1) * 128], wo[:, c, ns], c == 0, c == 3) for c in range(4)],
                                   reads=[S_atb, S_wo], writes=[S_pac[0]])
                        B.pe_group([mm(pac[1][:, :], cgb[:, c, t * 128:(t + 1) * 128], wo[:, 4 + c, ns], c == 0, c == 3) for c in range(4)],
                                   reads=[S_cgb, S_wo], writes=[S_pac[1]])
                        B.op("dve", lambda e, t=t, ns=ns, tile=tile: e.scalar_tensor_tensor(
                            out=x1[k][:, t, ns], in0=pac[0][:, :], scalar=rsa[:, tile:tile + 1], in1=x1[k][:, t, ns], op0=ALU.mult, op1=ALU.add),
                            reads=[S_pac[0], S_rs], writes=[S_x1[k]])
                        B.op("dve", lambda e, t=t, ns=ns, tile=tile: e.scalar_tensor_tensor(
                            out=x1[k][:, t, ns], in0=pac[1][:, :], scalar=rsc[:, tile:tile + 1], in1=x1[k][:, t, ns], op0=ALU.mult, op1=ALU.add),
                            reads=[S_pac[1], S_rs], writes=[S_x1[k]])
                for t in range(2):
                    B.op("act", lambda e, t=t: e.activation(out=xn2[:, t, :], in_=x1[k][:, t, :], func=AF.Square, accum_out=ssf[:, t:t + 1]),
                         reads=[S_x1[k]], writes=[S_xn2, S_ssf])
                B.op("act", lambda e: e.activation(out=rtf[:], in_=ssf[:], func=AF.Sqrt, scale=1.0 / D, bias=epsc[:]),
                     reads=[S_ssf, S_const], writes=[S_rtf])
                B.op("dve", lambda e: e.reciprocal(out=rsf[:], in_=rtf[:]), reads=[S_rtf], writes=[S_rsf])
                for t in range(2):
                    B.op("dve", lambda e, t=t: e.tensor_scalar(out=xn2[:, t, :], in0=x1[k][:, t, :], scalar1=rsf[:, t:t + 1], scalar2=None,
                                                               op0=ALU.mult),
                         reads=[S_x1[k], S_rsf], writes=[S_xn2])

            def f3b(qb):
                k = qb % 2
                for half in range(2):
                    B.pe_group([tr(tpv[half][:, c, t * 128:(t + 1) * 128], xn2[:, t, (half * 4 + c) * 128:(half * 4 + c + 1) * 128], ident[:])
                                for t in range(2) for c in range(4)], reads=[S_xn2, S_const], writes=[S_pac[half]])
                    for c in range(4):
                        cc = half * 4 + c
                        B.op("act", lambda e, c=c, cc=cc, half=half: e.activation(out=h2T[k][:, cc, 2:258], in_=tpv[half][:, c, :],
                                                                                 func=AF.Identity, scale=G2[:, cc:cc + 1], bias=sh2[:, cc:cc + 1]),
                             reads=[S_pac[half], S_G], writes=[S_h2T[k]])
                if qb == 1:
                    B.op("dve", lambda e: e.tensor_scalar(out=h2T[k][:, :, 0:2], in0=h2T[1 - k][:, :, 256:258], scalar1=hv[:, 0:1],
                                                          scalar2=None, op0=ALU.mult),
                         reads=[S_h2T[1 - k], S_const], writes=[S_h2T[k]])
                elif qb > 1:
                    B.op("dve", lambda e: e.tensor_copy(out=h2T[k][:, :, 0:2], in_=h2T[1 - k][:, :, 256:258]),
                         reads=[S_h2T[1 - k]], writes=[S_h2T[k]])

            def back3(qb):
                k = qb % 2
                for v in range(22):
                    if qb + 1 < NQB and v == 3:
                        f3a_dma(qb + 1)
                    p2_ = v % 2
                    chs = (v, 22 + v)
                    pis = []
                    for ch in chs:
                        i = puc[0] % 4
                        puc[0] += 1
                        pis.append(i)
                        B.pe_group([mm(pu[i][:, 0:258], wu[:, c, ch * 128:(ch + 1) * 128], h2T[k][:, c, 0:258], c == 0, c == 7) for c in range(8)],
                                   reads=[S_wuq[0 if v < 6 else 1 if v < 12 else 2 if v < 17 else 3], S_h2T[k]], writes=[S_pu[i]])
                    yas = []
                    for w_, (ch, i) in enumerate(zip(chs, pis)):
                        ai = yac[0] % 2
                        yac[0] += 1
                        yas.append(ai)
                        B.op("act", lambda e, i=i, ai=ai, ch=ch: e.activation(out=YA[ai][:], in_=pu[i][:, 2:258], func=AF.Identity,
                                                                             scale=fcw_sb[:, 88 + ch:89 + ch], bias=fcb_sb[:, ch:ch + 1]),
                             reads=[S_pu[i], S_const], writes=[S_YA[ai]])
                    if pend_fin:
                        pend_fin.pop(0)()
                    ybs = []
                    for w_, (ch, i) in enumerate(zip(chs, pis)):
                        bi = ybc[0] % 2
                        ybc[0] += 1
                        ybs.append(bi)
                        ai = yas[w_]
                        B.op("dve", lambda e, i=i, ai=ai, bi=bi, ch=ch: e.scalar_tensor_tensor(
                            out=YB[bi][:], in0=pu[i][:, 1:257], scalar=fcw_sb[:, 44 + ch:45 + ch], in1=YA[ai][:], op0=ALU.mult, op1=ALU.add),
                            reads=[S_pu[i], S_YA[ai], S_const], writes=[S_YB[bi]])
                    outs = ((YV[p2_], S_YV[p2_]), (YG[p2_], S_YG[p2_]))
                    for w_, (ch, i) in enumerate(zip(chs, pis)):
                        bi = ybs[w_]
                        yo, syo = outs[w_]
                        B.op("dve", lambda e, i=i, bi=bi, yo=yo, ch=ch: e.scalar_tensor_tensor(
                            out=yo[:], in0=pu[i][:, 0:256], scalar=fcw_sb[:, ch:ch + 1], in1=YB[bi][:], op0=ALU.mult, op1=ALU.add),
                            reads=[S_pu[i], S_YB[bi], S_const], writes=[syo])
                    def fin_pair(p2_=p2_, v=v):
                        B.op("act", lambda e: e.activation(out=YG[p2_][:], in_=YG[p2_][:], func=AF.Silu), reads=[S_YG[p2_]], writes=[S_YG[p2_]])
                        B.op("pool", lambda e: e.tensor_tensor(out=actT[:, v, :], in0=YV[p2_][:], in1=YG[p2_][:], op=ALU.mult),
                             reads=[S_YV[p2_], S_YG[p2_]], writes=[S_actT])
                    pend_fin.append(fin_pair)
                if pend_fin:
                    pend_fin.pop(0)()
                if qb + 1 < NQB:
                    f3a(qb + 1)
                for t in range(2):
                    if t == 1 and qb + 1 < NQB:
                        f3b(qb + 1)
                    for n in range(2):
                        B.pe_group([mm(pd[n][:, :], actT[:, v, t * 128:(t + 1) * 128], wd[:, v, n * 512:(n + 1) * 512], v == 0, v == 21)
                                    for v in range(22)], reads=[S_actT, S_wd], writes=[S_pd[n]])
                        B.op("dve", lambda e, t=t, n=n: e.tensor_tensor(out=x1[k][:, t, n * 512:(n + 1) * 512], in0=pd[n][:, :],
                                                                        in1=x1[k][:, t, n * 512:(n + 1) * 512], op=ALU.add),
                             reads=[S_pd[n]], writes=[S_x1[k]])
                for t in range(2):
                    B.op("act", lambda e, t=t: e.activation(out=xn2[:, t, :], in_=x1[k][:, t, :], func=AF.Square, accum_out=ssb[:, t:t + 1]),
                         reads=[S_x1[k]], writes=[S_xn2, S_ssb])
                B.op("act", lambda e: e.activation(out=rtb[:], in_=ssb[:], func=AF.Sqrt, scale=1.0 / D, bias=epsc[:]),
                     reads=[S_ssb, S_const], writes=[S_rtb])
                B.op("dve", lambda e: e.reciprocal(out=rsb[:], in_=rtb[:]), reads=[S_rtb], writes=[S_rsb])
                for t in range(2):
                    B.op("dve", lambda e, t=t: e.scalar_tensor_tensor(out=x1[k][:, t, :], in0=x1[k][:, t, :], scalar=rsb[:, t:t + 1],
                                                                      in1=gfb[:], op0=ALU.mult, op1=ALU.mult),
                         reads=[S_rsb, S_gfb], writes=[S_x1[k]])
                B.dma("sp", outv[qb - 1], x1[k][:], S_x1[k], reads=[S_x1[k]])

            f3a_dma(0)
            f3a(0)
            f3b(0)
            f3a_dma(1)
            f3a(1)
            f3b(1)
            for qb in range(1, NQB):
                back3(qb)
            B.emit_block()
    return nc


_NC_CACHE = {}


def _consts(half):
    bf = ml_dtypes.bfloat16
    ident = np.eye(128, dtype=np.float32).astype(bf)
    key = np.arange(S)
    kstat = np.zeros((35, S), np.float32)
    kstat[key // 256, key] = 1.0
    kstat[32] = key % 256
    kstat[33] = key // 256
    kstat[34] = 1.0
    kstat = kstat.astype(bf)
    p = np.arange(128)[:, None, None]
    kt = np.arange(2)[None, :, None]
    a = np.arange(256)[None, None, :]
    cmask = np.where(kt * 128 + p <= a, 0.0, NEG).astype(np.float32).reshape(128, 512).astype(bf)
    gvb = np.zeros((NQB, 32), np.float32)
    gvb2 = np.zeros((NQB, 32), np.float32)
    for qb in range(NQB):
        sB = 15 + qb
        for j in range(32):
            valid = (j < sB) and (half == 1 or j >= 16)
            gvb[qb, j] = 0.0 if valid else -1.0e9
            gvb2[qb, j] = NEG if valid else 2 * NEG
        gvb[qb, sB] = -3.0e9
        gvb2[qb, sB] = 0.0
    gvb = np.ascontiguousarray(np.broadcast_to(gvb.reshape(1, -1), (128, NQB * 32))).astype(np.float32)
    gvb2 = np.ascontiguousarray(np.broadcast_to(gvb2.reshape(1, -1), (128, NQB * 32))).astype(np.float32)
    qstat = np.zeros((NH, 128, NQT, 99), np.float32)
    for h in range(NH):
        slope = 2.0 ** (-(h + 1))
        for qt in range(NQT):
            sB = 15 + qt // 2
            apos = (qt % 2) * 128 + np.arange(128)
            qstat[h, :, qt, 96] = slope
            qstat[h, :, qt, 97] = 256.0 * slope
            qstat[h, :, qt, 98] = -slope * (256.0 * sB + apos)
    qstat = qstat.reshape(NH, 128, NQT * 99).astype(bf)
    hv = np.full((128, 1), float(half), np.float32)
    return dict(ident=ident, kstat=kstat, cmask=cmask, gvb=gvb, gvb2=gvb2, qstat=qstat, hv=hv)


def _pc(v, nchunk):
    return np.ascontiguousarray(np.asarray(v, np.float32).reshape(nchunk, 128).T)


def kernel(x, c, w_ada, b_ada, g_mix, w_in, conv_w, conv_b, g_attn_out, g_conv_out, w_out, g_ffn,
           w_up, ffn_conv_w, ffn_conv_b, w_down, g_final):
    f = lambda a: np.ascontiguousarray(np.asarray(a, dtype=np.float32))
    x = f(x)
    c = f(c)
    if "nc" not in _NC_CACHE:
        _NC_CACHE["nc"] = build_program()
    nc = _NC_CACHE["nc"]
    shared = dict(
        w_ada=f(w_ada[0]), b_ada=f(b_ada[0]).reshape(1, -1), gmix=_pc(g_mix[0], 8), w_in=f(w_in[0]),
        convw=np.ascontiguousarray(np.concatenate([_pc(conv_w[0][kk], 4) for kk in range(3)], axis=1)),
        convb=_pc(conv_b[0], 4),
        gattn=np.ascontiguousarray(f(g_attn_out[0]).reshape(8, 64).T),
        gconv=_pc(g_conv_out[0], 4), w_out=f(w_out[0]), gffn=_pc(g_ffn[0], 8), w_up=f(w_up[0]),
        fcw=np.ascontiguousarray(np.concatenate([_pc(ffn_conv_w[0][kk], 44) for kk in range(3)], axis=1)),
        fcb=_pc(ffn_conv_b[0], 44), w_down=f(w_down[0]),
        gfin=np.ascontiguousarray(np.broadcast_to(f(g_final).reshape(1, -1), (128, D))),
    )
    cst = [_consts(0), _consts(1)]
    in_maps = []
    for i in range(NCORES):
        b, half = i // 2, i % 2
        if half == 1:
            xs = x[b]
        else:
            xs = np.concatenate([np.zeros((4096, D), np.float32), x[b, :4096]], axis=0)
        m = dict(shared)
        m.update(cst[half])
        m["xs"] = np.ascontiguousarray(xs)
        m["cT"] = _pc(c[b], 8)
        in_maps.append(m)
    res = run_bass_kernel_spmd(nc, in_maps, core_ids=list(range(NCORES)))
    outp = np.empty((4, S, D), np.float32)
    for i in range(NCORES):
        b, half = i // 2, i % 2
        outp[b, half * 4096:(half + 1) * 4096] = res.results[i]["out"]
    return outp
```

```python
import numpy as np
from contextlib import ExitStack
import ml_dtypes
import concourse.bass as bass
import concourse.mybir as mybir
from concourse.bass_utils import run_bass_kernel_spmd

F32 = mybir.dt.float32
BF16 = mybir.dt.bfloat16
AF = mybir.ActivationFunctionType
ALU = mybir.AluOpType
AX = mybir.AxisListType

D = 1024
S = 8192
NH = 8
DFF = 2816
NQB = 17
NQT = 34
QTOK = NQB * 256
EPS = 1e-6
NEG = -30000.0
NCORES = 8
PHASES = 3
DEBUG = False


class Tok:
    __slots__ = ("sem", "val")

    def __init__(self, sem, val):
        self.sem = sem
        self.val = val


class Slot:
    def __init__(self, name=""):
        self.name = name
        self.w = None
        self.r = {}
        self.dsem = None
        self.dcnt = 0


class Builder:
    ENG = ("pe", "act", "dve", "pool", "sp")

    def __init__(self, nc, es):
        self.nc = nc
        self.es = es
        self.q = {}
        for n in self.ENG:
            sem = es.enter_context(nc.semaphore("sem_" + n))
            self.q[n] = dict(sem=sem, cnt=0, ops=[], waited={})
        self.dslots = []

    def _waits(self, en, deps):
        q = self.q[en]
        need = {}
        for t in deps:
            if t is None:
                continue
            if en == "pe" and t.sem is q["sem"]:
                continue
            k = id(t.sem)
            if q["waited"].get(k, 0) >= t.val:
                continue
            if k not in need or need[k].val < t.val:
                need[k] = t
        for k, t in need.items():
            q["waited"][k] = t.val
        return [(t.sem, t.val) for t in need.values()]

    @staticmethod
    def _deps(reads, writes, deps):
        d = list(deps)
        for s in reads:
            d.append(s.w)
        for s in writes:
            d.extend(s.r.values())
            d.append(s.w)
        return d

    @staticmethod
    def _update(tok, reads, writes):
        for s in writes:
            s.w = tok
            s.r = {}
        for s in reads:
            k = id(tok.sem)
            if k not in s.r or s.r[k].val < tok.val:
                s.r[k] = tok

    def op(self, en, fn, reads=(), writes=(), deps=(), inc=True):
        q = self.q[en]
        waits = self._waits(en, self._deps(reads, writes, deps))
        tok = None
        if inc:
            q["cnt"] += 1
            tok = Tok(q["sem"], q["cnt"])
        else:
            assert not reads and not writes
        sem = q["sem"]

        def emit(e, waits=waits, fn=fn, inc=inc, sem=sem):
            for (sm, v) in waits:
                e.wait_ge(sm, v)
            ins = fn(e)
            if inc:
                ins.then_inc(sem, 1)

        q["ops"].append(emit)
        if tok is not None:
            self._update(tok, reads, writes)
        return tok

    def pe_group(self, fns, reads=(), writes=()):
        q = self.q["pe"]
        waits = self._waits("pe", self._deps(reads, writes, ()))
        q["cnt"] += 1
        tok = Tok(q["sem"], q["cnt"])
        sem = q["sem"]
        n = len(fns)

        def emit(e, waits=waits, fns=fns, sem=sem, n=n):
            for (sm, v) in waits:
                e.wait_ge(sm, v)
            for i, f in enumerate(fns):
                ins = f(e)
                if i == n - 1:
                    ins.then_inc(sem, 1)

        q["ops"].append(emit)
        self._update(tok, reads, writes)
        return tok

    def dma(self, en, out, in_, sem_slot, reads=(), writes=(), deps=(), **kw):
        q = self.q[en]
        sl = sem_slot
        if sl.dsem is None:
            sl.dsem = self.es.enter_context(self.nc.semaphore("dsem%d" % len(self.dslots)))
            self.dslots.append(sl)
        waits = self._waits(en, self._deps(reads, writes, deps))
        sl.dcnt += 16
        tok = Tok(sl.dsem, sl.dcnt)
        dsem = sl.dsem

        def emit(e, waits=waits, out=out, in_=in_, dsem=dsem, kw=kw):
            for (sm, v) in waits:
                e.wait_ge(sm, v)
            e.dma_start(out=out, in_=in_, **kw).then_inc(dsem, 16)

        q["ops"].append(emit)
        self._update(tok, reads, writes)
        return tok

    def emit_block(self):
        nc = self.nc
        finals = [(s.dsem, s.dcnt) for s in self.dslots if s.dcnt > 0]
        pe_fin = (self.q["pe"]["sem"], self.q["pe"]["cnt"])

        def fin(e, finals=finals):
            for sm, v in finals:
                e.wait_ge(sm, v)

        self.q["sp"]["ops"].append(fin)
        with nc.Block() as blk:
            for n, meth in (("pe", blk.tensor), ("act", blk.scalar), ("dve", blk.vector),
                            ("pool", blk.gpsimd), ("sp", blk.sync)):
                ops = self.q[n]["ops"]
                if ops:
                    def body(e, ops=ops):
                        for f in ops:
                            f(e)
                    meth(body)
                self.q[n]["ops"] = []


def mm(out, lhsT, rhs, start, stop):
    return lambda e: e.matmul(out, lhsT=lhsT, rhs=rhs, start=start, stop=stop)


def tr(out, in_, ident):
    return lambda e: e.transpose(out, in_, ident)


def build_program():
    nc = bass.Bass("TRN2", target_bir_lowering=False)

    def din(name, shape, dt=F32):
        return nc.dram_tensor(name, list(shape), dt, kind="ExternalInput").ap()

    def dscr(name, shape, dt):
        return nc.dram_tensor(name, list(shape), dt, kind="ExternalOutput" if DEBUG else "Internal").ap()

    xs = din("xs", [S, D])
    cT = din("cT", [128, 8])
    w_ada = din("w_ada", [D, 6 * D])
    b_ada = din("b_ada", [1, 6 * D])
    gmix = din("gmix", [128, 8])
    w_in = din("w_in", [D, 3072])
    convw = din("convw", [128, 12])
    convb = din("convb", [128, 4])
    gattn = din("gattn", [64, 8])
    gconv = din("gconv", [128, 4])
    w_out = din("w_out", [D, D])
    gffn = din("gffn", [128, 8])
    w_up = din("w_up", [D, 2 * DFF])
    fcw = din("fcw", [128, 132])
    fcb = din("fcb", [128, 44])
    w_down = din("w_down", [DFF, D])
    gfin = din("gfin", [128, D])
    ident_d = din("ident", [128, 128], BF16)
    kstat = din("kstat", [35, S], BF16)
    cmask = din("cmask", [128, 512], BF16)
    gvb_d = din("gvb", [128, NQB * 32])
    gvb2_d = din("gvb2", [128, NQB * 32])
    qstat = din("qstat", [NH, 128, NQT * 99], BF16)
    hv_d = din("hv", [128, 1])
    out = nc.dram_tensor("out", [4096, D], F32, kind="ExternalOutput").ap()

    kT_s = dscr("kT_s", [NH, 64, S], BF16)
    qT_s = dscr("qT_s", [NH, 64, QTOK], BF16)
    at_s = dscr("at_s", [NH, 64, QTOK], BF16)
    cg_s = dscr("cg_s", [4, 128, QTOK], BF16)
    mod_s = dscr("mod_s", [1, 6 * D], F32)
    km_s = dscr("km_s", [NH, 64, 32], F32)

    es = ExitStack()
    with es:
        B = Builder(nc, es)

        def sb(st, name, shape, dt):
            return st.enter_context(nc.sbuf_tensor("sb_" + name, list(shape), dt))

        def ps(st, name, shape, dt):
            return st.enter_context(nc.psum_tensor("ps_" + name, list(shape), dt))

        ident = sb(es, "ident", [128, 128], BF16)
        ones_b = sb(es, "ones_b", [128, 1], BF16)
        ones_f = sb(es, "ones_f", [128, 64], F32)
        epsc = sb(es, "epsc", [128, 1], F32)
        hv = sb(es, "hv", [128, 1], F32)
        G1 = sb(es, "G1", [128, 8], F32)
        G2 = sb(es, "G2", [128, 8], F32)
        modT = sb(es, "modT", [128, 48], F32)
        gmix_sb = sb(es, "gmix_sb", [128, 8], F32)
        gffn_sb = sb(es, "gffn_sb", [128, 8], F32)
        convw_sb = sb(es, "convw_sb", [128, 12], F32)
        convb_sb = sb(es, "convb_sb", [128, 4], F32)
        gconv_sb = sb(es, "gconv_sb", [128, 4], F32)
        gattn_sb = sb(es, "gattn_sb", [64, 8], F32)
        fcw_sb = sb(es, "fcw_sb", [128, 132], F32)
        fcb_sb = sb(es, "fcb_sb", [128, 44], F32)
        ssa = sb(es, "ssa", [128, NQT], F32)
        ssc = sb(es, "ssc", [128, NQT], F32)
        wo = sb(es, "wo", [128, 8, D], BF16)
        S_wo = Slot("wo")
        S_const = Slot("const")
        S_ssa = Slot("ssa")
        S_ssc = Slot("ssc")
        S_G = Slot("G")
        S_mods = Slot("mods")

        for dst, src in ((ident, ident_d), (hv, hv_d), (gmix_sb, gmix), (gffn_sb, gffn), (convw_sb, convw),
                         (convb_sb, convb), (gconv_sb, gconv), (gattn_sb, gattn), (fcw_sb, fcw), (fcb_sb, fcb)):
            B.dma("sp", dst[:], src, S_const, writes=[S_const])
        B.op("dve", lambda e: e.memset(ones_b[:], 1.0), writes=[S_const])
        B.op("dve", lambda e: e.memset(ones_f[:], 1.0), writes=[S_const])
        B.op("dve", lambda e: e.memset(epsc[:], EPS), writes=[S_const])

        with ExitStack() as p0:
            c_sb = sb(p0, "c_sb", [128, 8], F32)
            sc_b = sb(p0, "sc_b", [128, 8], BF16)
            brow = sb(p0, "brow", [1, 6 * D], F32)
            modrow = sb(p0, "modrow", [1, 6 * D], F32)
            wa = [sb(p0, "wa%d" % i, [128, 8, 512], BF16) for i in range(2)]
            pm = [ps(p0, "pm%d" % i, [128, 512], F32) for i in range(2)]
            S_c, S_scb, S_brow, S_modrow, S_modT = Slot(), Slot(), Slot(), Slot(), Slot()
            S_wa = [Slot(), Slot()]
            S_pm = [Slot(), Slot()]
            B.dma("sp", c_sb[:], cT, S_c, writes=[S_c])
            B.dma("sp", brow[:], b_ada, S_brow, writes=[S_brow])
            B.op("act", lambda e: e.activation(out=sc_b[:], in_=c_sb[:], func=AF.Silu), reads=[S_c], writes=[S_scb])
            wav = w_ada.rearrange("(c p) n -> p c n", p=128)
            for g in range(12):
                k = g % 2
                B.dma("pool", wa[k][:], wav[:, :, g * 512:(g + 1) * 512], S_wa[k], writes=[S_wa[k]])
                B.pe_group([mm(pm[k][0:1, :], sc_b[:, c:c + 1], wa[k][:, c, :], c == 0, c == 7) for c in range(8)],
                           reads=[S_wa[k], S_scb], writes=[S_pm[k]])
                B.op("dve", lambda e, k=k, g=g: e.tensor_tensor(out=modrow[0:1, g * 512:(g + 1) * 512], in0=pm[k][0:1, :],
                                                                 in1=brow[0:1, g * 512:(g + 1) * 512], op=ALU.add),
                     reads=[S_pm[k], S_brow], writes=[S_modrow])
            B.dma("sp", mod_s, modrow[:], S_mods, reads=[S_modrow], writes=[S_mods])
            B.dma("sp", modT[:], mod_s.rearrange("o (k p) -> (o p) k", p=128), S_modT, reads=[S_mods], writes=[S_modT],
                  allow_slow_non_contiguous=True)
            B.op("dve", lambda e: e.scalar_tensor_tensor(out=G1[:], in0=modT[:, 8:16], scalar=1.0, in1=gmix_sb[:],
                                                         op0=ALU.add, op1=ALU.mult), reads=[S_modT, S_const], writes=[S_G])
            B.op("dve", lambda e: e.scalar_tensor_tensor(out=G2[:], in0=modT[:, 32:40], scalar=1.0, in1=gffn_sb[:],
                                                         op0=ALU.add, op1=ALU.mult), reads=[S_modT, S_const], writes=[S_G])
            B.emit_block()
        sh1 = modT[:, 0:8]
        sh2 = modT[:, 24:32]

        with ExitStack() as p12:
          if PHASES >= 1:
            V_all = sb(p12, "V_all", [128, 64, NH, 65], BF16)
            S_V = Slot("V")
            with ExitStack() as p1:
                w_in_sb = sb(p1, "w_in_sb", [128, 8, 3072], BF16)
                S_win = Slot("win")
                wiv = w_in.rearrange("(c p) n -> p c n", p=128)
                for c in range(8):
                    B.dma("pool", w_in_sb[:, c, :], wiv[:, c, :], S_win, writes=[S_win])
                pend_stats = []
                xt = [sb(p1, "xt%d" % i, [128, 2, D], F32) for i in range(3)]
                xn = [sb(p1, "xnp%d" % i, [128, 2, D], BF16) for i in range(3)]
                hT = [sb(p1, "hT%d" % i, [128, 8, 258], BF16) for i in range(2)]
                junk = sb(p1, "junk", [128, D], BF16)
                ss = [sb(p1, "ssp%d" % i, [128, 2], F32) for i in range(3)]
                rt = [sb(p1, "rt%d" % i, [128, 2], F32) for i in range(3)]
                rstd = [sb(p1, "rstd%d" % i, [128, 2], F32) for i in range(3)]
                kst = [sb(p1, "kst%d" % i, [128, 4, 256], BF16) for i in range(2)]
                qst = [sb(p1, "qst%d" % i, [128, 4, 256], BF16) for i in range(2)]
                cgs = [sb(p1, "cgs%d" % i, [128, 4, 256], BF16) for i in range(2)]
                sqc = sb(p1, "sqc", [128, 4, 256], BF16)
                kmst = sb(p1, "kmst", [128, 4, 32], F32)
                S_kmst = Slot()
                S_kmd = Slot("km_s")
                u_sb = [sb(p1, "u_sb%d" % i, [128, 258], F32) for i in range(2)]
                zb = [sb(p1, "zb%d" % i, [128, 258], F32) for i in range(2)]
                y0 = [sb(p1, "y0%d" % i, [128, 256], F32) for i in range(2)]
                y1 = [sb(p1, "y1%d" % i, [128, 256], F32) for i in range(2)]
                y2 = [sb(p1, "y2%d" % i, [128, 256], F32) for i in range(2)]
                cv = [sb(p1, "cv%d" % i, [128, 256], F32) for i in range(2)]
                tp = ps(p1, "tp", [128, 8, 256], BF16)
                pp = [ps(p1, "pp%d" % i, [128, 512], F32) for i in range(5)]
                pss = ps(p1, "pss", [128, 512], F32)
                S_xt = [Slot(), Slot(), Slot()]
                S_xn = [Slot(), Slot(), Slot()]
                S_hT = [Slot(), Slot()]
                S_junk = Slot()
                S_ss = [Slot(), Slot(), Slot()]
                S_rt = [Slot(), Slot(), Slot()]
                S_rstd = [Slot(), Slot(), Slot()]
                S_kst = [Slot(), Slot()]
                S_qst = [Slot(), Slot()]
                S_cgs = [Slot(), Slot()]
                S_sqc = Slot()
                S_u = [Slot(), Slot()]
                S_z = [Slot(), Slot()]
                S_y0 = [Slot(), Slot()]
                S_y1 = [Slot(), Slot()]
                S_y2 = [Slot(), Slot()]
                S_cv = [Slot(), Slot()]
                S_tp = Slot()
                S_pp = [Slot() for _ in range(5)]
                S_pss = Slot()
                S_kT = Slot("kT_s")
                S_qT = Slot("qT_s")
                S_cgd = Slot("cg_s")
                ppi = [0]

                def next_pp():
                    i = ppi[0] % 5
                    ppi[0] += 1
                    return pp[i], S_pp[i]

                B.op("pool", lambda e: e.memset(V_all[:, :, :, 64:65], 1.0), writes=[S_V])
                for i in range(2):
                    B.op("pool", lambda e, i=i: e.memset(hT[i][:, :, 0:2], 0.0), writes=[S_hT[i]])
                xsv = xs.rearrange("(n t p) d -> n p t d", t=2, p=128)
                kTv = kT_s.rearrange("(pr two) d t -> (two d) pr t", two=2)
                qTv = qT_s.rearrange("(pr two) d t -> (two d) pr t", two=2)
                cgv = cg_s.rearrange("c p t -> p c t")
                evac_rr = [0]

                def evac(out_ap, in_ap, reads, writes, scale=None):
                    evac_rr[0] += 1
                    if evac_rr[0] % 2 == 0:
                        if scale is None:
                            return B.op("act", lambda e: e.activation(out=out_ap, in_=in_ap, func=AF.Copy), reads=reads, writes=writes)
                        return B.op("act", lambda e: e.activation(out=out_ap, in_=in_ap, func=AF.Copy, scale=scale), reads=reads, writes=writes)
                    if scale is None:
                        return B.op("dve", lambda e: e.tensor_copy(out=out_ap, in_=in_ap), reads=reads, writes=writes)
                    return B.op("dve", lambda e: e.tensor_scalar(out=out_ap, in0=in_ap, scalar1=scale, scalar2=None, op0=ALU.mult),
                                reads=reads, writes=writes)

                def f1a(s):
                    k = s % 3
                    B.dma("sp", xt[k][:], xsv[s], S_xt[k], writes=[S_xt[k]])
                    for t in range(2):
                        B.op("act", lambda e, k=k, t=t: e.activation(out=junk[:], in_=xt[k][:, t, :], func=AF.Square,
                                                                     accum_out=ss[k][:, t:t + 1]),
                             reads=[S_xt[k]], writes=[S_junk, S_ss[k]])
                    B.op("act", lambda e, k=k: e.activation(out=rt[k][:], in_=ss[k][:], func=AF.Sqrt, scale=1.0 / D, bias=epsc[:]),
                         reads=[S_ss[k], S_const], writes=[S_rt[k]])
                    B.op("dve", lambda e, k=k: e.reciprocal(out=rstd[k][:], in_=rt[k][:]), reads=[S_rt[k]], writes=[S_rstd[k]])
                    for t in range(2):
                        B.op("dve", lambda e, k=k, t=t: e.tensor_scalar(out=xn[k][:, t, :], in0=xt[k][:, t, :], scalar1=rstd[k][:, t:t + 1],
                                                                        scalar2=None, op0=ALU.mult),
                             reads=[S_xt[k], S_rstd[k]], writes=[S_xn[k]])

                def f1b(s):
                    k = s % 2
                    B.pe_group([tr(tp[:, c, t * 128:(t + 1) * 128], xn[s % 3][:, t, c * 128:(c + 1) * 128], ident[:])
                                for t in range(2) for c in range(8)], reads=[S_xn[s % 3], S_const], writes=[S_tp])
                    for c in range(8):
                        if c % 2 == 0:
                            B.op("act", lambda e, k=k, c=c: e.activation(out=hT[k][:, c, 2:258], in_=tp[:, c, :], func=AF.Identity,
                                                                         scale=G1[:, c:c + 1], bias=sh1[:, c:c + 1]),
                                 reads=[S_tp, S_G], writes=[S_hT[k]])
                        else:
                            B.op("dve", lambda e, k=k, c=c: e.tensor_scalar(out=hT[k][:, c, 2:258], in0=tp[:, c, :], scalar1=G1[:, c:c + 1],
                                                                            scalar2=sh1[:, c:c + 1], op0=ALU.mult, op1=ALU.add),
                                 reads=[S_tp, S_G], writes=[S_hT[k]])
                    if s == 16:
                        B.op("dve", lambda e, k=k: e.tensor_scalar(out=hT[k][:, :, 0:2], in0=hT[1 - k][:, :, 256:258], scalar1=hv[:, 0:1],
                                                                   scalar2=None, op0=ALU.mult),
                             reads=[S_hT[1 - k], S_const], writes=[S_hT[k]])
                    elif s > 16:
                        B.op("dve", lambda e, k=k: e.tensor_copy(out=hT[k][:, :, 0:2], in_=hT[1 - k][:, :, 256:258]),
                             reads=[S_hT[1 - k]], writes=[S_hT[k]])

                def back1a(s):
                    k = s % 2
                    for pr in range(4):
                        pt_, sp_ = next_pp()
                        B.pe_group([mm(pt_[:, 0:256], w_in_sb[:, c, 512 + pr * 128:512 + (pr + 1) * 128], hT[k][:, c, 2:258], c == 0, c == 7)
                                    for c in range(8)], reads=[S_win, S_hT[k]], writes=[sp_])
                        evac(kst[k][:, pr, :], pt_[:, 0:256], [sp_], [S_kst[k]])
                        B.op("dve", lambda e, k=k, pr=pr, s=s: e.tensor_reduce(out=kmst[:, pr, s:s + 1], in_=kst[k][:, pr, :], axis=AX.X, op=ALU.add),
                             reads=[S_kst[k]], writes=[S_kmst])
                    B.dma("sp", kTv[:, :, s * 256:(s + 1) * 256], kst[k][:], S_kst[k], reads=[S_kst[k]], writes=[S_kT])
                    for t in range(2):
                        pt_, sp_ = next_pp()
                        B.pe_group([mm(pt_[:, :], hT[k][:, c, 2 + t * 128:2 + (t + 1) * 128], w_in_sb[:, c, 1024:1536], c == 0, c == 7)
                                    for c in range(8)], reads=[S_win, S_hT[k]], writes=[sp_])
                        evac(V_all[:, 2 * s + t, :, 0:64], pt_[:, :].rearrange("p (h d) -> p h d", d=64), [sp_], [S_V])

                def back1b(s):
                    k = s % 2
                    if s < 15:
                        return
                    qb = s - 15
                    for pr in range(4):
                        pt_, sp_ = next_pp()
                        B.pe_group([mm(pt_[:, 0:256], w_in_sb[:, c, pr * 128:(pr + 1) * 128], hT[k][:, c, 2:258], c == 0, c == 7)
                                    for c in range(8)], reads=[S_win, S_hT[k]], writes=[sp_])
                        evac(qst[k][:, pr, :], pt_[:, 0:256], [sp_], [S_qst[k]], scale=0.125)
                    B.dma("sp", qTv[:, :, qb * 256:(qb + 1) * 256], qst[k][:], S_qst[k], reads=[S_qst[k]], writes=[S_qT])
                    for ch in range(4):
                        j = ch % 2
                        pu_, spu = next_pp()
                        B.pe_group([mm(pu_[:, 0:258], w_in_sb[:, c, 1536 + ch * 128:1536 + (ch + 1) * 128], hT[k][:, c, 0:258], c == 0, c == 7)
                                    for c in range(8)], reads=[S_win, S_hT[k]], writes=[spu])
                        pc_, spc = next_pp()
                        B.pe_group([mm(pc_[:, 0:258], w_in_sb[:, c, 2048 + ch * 128:2048 + (ch + 1) * 128], hT[k][:, c, 0:258], c == 0, c == 7)
                                    for c in range(8)], reads=[S_win, S_hT[k]], writes=[spc])
                        pb_, spb = next_pp()
                        B.pe_group([mm(pb_[:, 0:256], w_in_sb[:, c, 2560 + ch * 128:2560 + (ch + 1) * 128], hT[k][:, c, 2:258], c == 0, c == 7)
                                    for c in range(8)], reads=[S_win, S_hT[k]], writes=[spb])
                        B.op("act", lambda e, j=j, pu_=pu_: e.activation(out=u_sb[j][:], in_=pu_[:, 0:258], func=AF.Copy),
                             reads=[spu], writes=[S_u[j]])
                        B.op("dve", lambda e, j=j, pc_=pc_: e.tensor_tensor(out=zb[j][:], in0=pc_[:, 0:258], in1=u_sb[j][:], op=ALU.mult),
                             reads=[spc, S_u[j]], writes=[S_z[j]])
                        B.op("dve", lambda e, j=j, ch=ch: e.tensor_scalar(out=y0[j][:], in0=zb[j][:, 2:258], scalar1=convw_sb[:, 8 + ch:9 + ch],
                                                                          scalar2=convb_sb[:, ch:ch + 1], op0=ALU.mult, op1=ALU.add),
                             reads=[S_z[j], S_const], writes=[S_y0[j]])
                        B.op("dve", lambda e, j=j, ch=ch: e.scalar_tensor_tensor(out=y1[j][:], in0=zb[j][:, 1:257], scalar=convw_sb[:, 4 + ch:5 + ch],
                                                                                 in1=y0[j][:], op0=ALU.mult, op1=ALU.add),
                             reads=[S_z[j], S_y0[j], S_const], writes=[S_y1[j]])
                        B.op("dve", lambda e, j=j, ch=ch: e.scalar_tensor_tensor(out=y2[j][:], in0=zb[j][:, 0:256], scalar=convw_sb[:, ch:ch + 1],
                                                                                 in1=y1[j][:], op0=ALU.mult, op1=ALU.add),
                             reads=[S_z[j], S_y1[j], S_const], writes=[S_y2[j]])
                        B.op("dve", lambda e, j=j, pb_=pb_: e.tensor_tensor(out=cv[j][:], in0=pb_[:, 0:256], in1=y2[j][:], op=ALU.mult),
                             reads=[spb, S_y2[j]], writes=[S_cv[j]])
                        B.op("act", lambda e, j=j, ch=ch: e.activation(out=sqc[:, ch, :], in_=cv[j][:], func=AF.Square),
                             reads=[S_cv[j]], writes=[S_sqc])
                        B.op("act", lambda e, j=j, ch=ch, k=k: e.activation(out=cgs[k][:, ch, :], in_=cv[j][:], func=AF.Copy,
                                                                           scale=gconv_sb[:, ch:ch + 1]),
                             reads=[S_cv[j], S_const], writes=[S_cgs[k]])
                    B.dma("sp", cgv[:, :, qb * 256:(qb + 1) * 256], cgs[k][:], S_cgs[k], reads=[S_cgs[k]], writes=[S_cgd])

                    def stats(qb=qb):
                        for t in range(2):
                            B.pe_group([mm(pss[:, t:t + 1], sqc[:, ch, t * 128:(t + 1) * 128], ones_b[:, 0:1], ch == 0, ch == 3) for ch in range(4)],
                                       reads=[S_sqc, S_const], writes=[S_pss])
                        B.op("dve", lambda e: e.tensor_copy(out=ssc[:, 2 * qb:2 * qb + 2], in_=pss[:, 0:2]), reads=[S_pss], writes=[S_ssc])
                    pend_stats.append(stats)

                f1a(0)
                f1b(0)
                f1a(1)
                f1a(2)
                for s in range(32):
                    if s + 1 < 32:
                        f1b(s + 1)
                    if s + 3 < 32:
                        f1a(s + 3)
                    back1a(s)
                    while pend_stats:
                        pend_stats.pop(0)()
                    if s == 5:
                        wov = w_out.rearrange("(c p) n -> p c n", p=128)
                        for c in range(8):
                            B.dma("pool", wo[:, c, :], wov[:, c, :], S_wo, writes=[S_wo])
                    back1b(s)
                while pend_stats:
                    pend_stats.pop(0)()
                B.dma("sp", km_s.rearrange("(pr two) d j -> (two d) pr j", two=2), kmst[:], S_kmst, reads=[S_kmst], writes=[S_kmd])
                B.emit_block()

            with ExitStack() as p2:
              if PHASES >= 2:
                kaug = [sb(p2, "kaug%d" % i, [99, S], BF16) for i in range(2)]
                qaug = [sb(p2, "qaug%d" % i, [99, QTOK], BF16) for i in range(2)]
                qa = [sb(p2, "qa%d" % i, [128, NQT, 99], BF16) for i in range(2)]
                atst = [sb(p2, "atst%d" % i, [64, QTOK], BF16) for i in range(2)]
                gvb = sb(p2, "gvb", [128, NQB, 32], F32)
                gvb2 = sb(p2, "gvb2", [128, NQB, 32], F32)
                cm = sb(p2, "cm", [128, 2, 256], BF16)
                km_f = [sb(p2, "km_f%d" % i, [64, 32], F32) for i in range(2)]
                km_b = [sb(p2, "km_b%d" % i, [64, 32], BF16) for i in range(2)]
                gm = [sb(p2, "gm%d" % i, [128, 32], F32) for i in range(2)]
                t8 = [sb(p2, "t8%d" % i, [128, 8], F32) for i in range(2)]
                tsel = [sb(p2, "tsel%d" % i, [128, 32], F32) for i in range(2)]
                pT = [sb(p2, "pT%d" % i, [128, 2, 256], BF16) for i in range(3)]
                rc = [sb(p2, "rc%d" % i, [128, 256], F32) for i in range(4)]
                pos = [sb(p2, "pos%d" % i, [128, 256], F32) for i in range(4)]
                atf = [sb(p2, "atf%d" % i, [64, 256], F32) for i in range(4)]
                sqa = [sb(p2, "sqa%d" % i, [64, 256], BF16) for i in range(4)]
                sp = [ps(p2, "sp%d" % i, [128, 2, 256], F32) for i in range(3)]
                po = [ps(p2, "po%d" % i, [128, 512], F32) for i in range(2)]
                pgt = ps(p2, "pgt", [128, 512], F32)
                pbc = ps(p2, "pbc", [128, 512], F32)
                ptt_t = ps(p2, "ptt", [128, 1024], BF16)
                ptt = ptt_t[:, 0:128]
                S_kaug = [Slot(), Slot()]
                S_kstat = Slot()
                S_qq = [Slot(), Slot()]
                S_qm = [Slot(), Slot()]
                S_qa = [Slot(), Slot()]
                S_atst = [Slot(), Slot()]
                S_c2 = Slot()
                S_kmf = [Slot(), Slot()]
                S_kmb = [Slot(), Slot()]
                S_gm = [Slot(), Slot()]
                S_t8 = [Slot(), Slot()]
                S_tsel = [Slot(), Slot()]
                S_pT = [Slot() for _ in range(3)]
                S_rc = [Slot() for _ in range(4)]
                S_pos = [Slot() for _ in range(4)]
                S_atf = [Slot() for _ in range(4)]
                S_sqa = [Slot() for _ in range(4)]
                S_sp = [Slot() for _ in range(3)]
                S_po = [Slot(), Slot()]
                S_pgt, S_ptt, S_pbc = Slot(), Slot(), Slot()
                S_pss2 = S_pbc
                S_atd = Slot("at_s")

                B.dma("sp", gvb[:].rearrange("p a b -> p (a b)"), gvb_d, S_c2, writes=[S_c2])
                B.dma("sp", gvb2[:].rearrange("p a b -> p (a b)"), gvb2_d, S_c2, writes=[S_c2])
                B.dma("sp", cm[:].rearrange("p a b -> p (a b)"), cmask, S_c2, writes=[S_c2])
                for i in range(2):
                    B.dma("sp", kaug[i][64:99, :], kstat, S_kstat, writes=[S_kstat])

                def load_head(h):
                    hb = h % 2
                    B.dma("sp", kaug[hb][0:64, :], kT_s[h], S_kaug[hb], reads=[S_kT], writes=[S_kaug[hb]])
                    B.dma("sp", qaug[hb][0:64, :], qT_s[h], S_qq[hb], reads=[S_qT], writes=[S_qq[hb]])
                    B.dma("sp", qa[hb][:].rearrange("p a b -> p (a b)"), qstat[h], S_qa[hb], writes=[S_qa[hb]])

                def prep_head(h):
                    hb = h % 2
                    B.dma("sp", km_f[hb][:], km_s[h], S_kmf[hb], reads=[S_kmd], writes=[S_kmf[hb]])
                    B.op("dve", lambda e: e.tensor_scalar(out=km_b[hb][:], in0=km_f[hb][:], scalar1=1.0 / 256, scalar2=None, op0=ALU.mult),
                         reads=[S_kmf[hb]], writes=[S_kmb[hb]])

                def mask_a(h, qt):
                    hb = h % 2
                    qb = qt // 2
                    g2 = qt % 2
                    B.pe_group([mm(pgt[:, 0:32], qaug[hb][0:64, qt * 128:(qt + 1) * 128], km_b[hb][:, :], True, True)],
                               reads=[S_qq[hb], S_kmb[hb]], writes=[S_pgt])
                    B.op("dve", lambda e: e.tensor_tensor(out=gm[g2][:], in0=pgt[:, 0:32], in1=gvb[:, qb, :], op=ALU.add),
                         reads=[S_pgt, S_c2], writes=[S_gm[g2]])
                    B.op("dve", lambda e: e.max(out=t8[g2][:], in_=gm[g2][:]), reads=[S_gm[g2]], writes=[S_t8[g2]])
                    B.op("dve", lambda e: e.tensor_scalar(out=tsel[g2][:], in0=gm[g2][:], scalar1=t8[g2][:, 2:3], scalar2=-NEG,
                                                          op0=ALU.is_ge, op1=ALU.mult),
                         reads=[S_gm[g2], S_t8[g2]], writes=[S_tsel[g2]])
                    B.op("dve", lambda e: e.tensor_tensor(out=qa[hb][:, qt, 64:96], in0=tsel[g2][:], in1=gvb2[:, qb, :], op=ALU.add),
                         reads=[S_tsel[g2], S_c2], writes=[S_qa[hb]])

                def mask_b(h, qt):
                    hb = h % 2
                    B.pe_group([tr(ptt[0:99, :], qa[hb][:, qt, :], ident[:])], reads=[S_qa[hb], S_const], writes=[S_ptt])
                    B.op("dve", lambda e: e.tensor_copy(out=qaug[hb][64:99, qt * 128:(qt + 1) * 128], in_=ptt[64:99, :]),
                         reads=[S_ptt], writes=[S_qm[hb]])

                sctr = [0]
                HORDER = list(range(NH - 1, -1, -1))
                KEEP = []
                for h_ in range(NH):
                    slope_ = 2.0 ** (-(h_ + 1))
                    kk_ = 0
                    while kk_ < 31 and slope_ * (kk_ * 256 + 1) < 80.0:
                        kk_ += 1
                    KEEP.append(kk_)

                def s_mm(h, qb, j):
                    hb = h % 2
                    sB = 15 + qb
                    qc = slice(qb * 256, (qb + 1) * 256)
                    i = sctr[0] % 3
                    sctr[0] += 1
                    fns = []
                    for kt in range(2):
                        fns.append(mm(sp[i][:, kt, :], kaug[hb][0:99, (2 * j + kt) * 128:(2 * j + kt + 1) * 128], qaug[hb][0:99, qc],
                                      True, j != sB))
                        if j == sB:
                            fns.append(mm(sp[i][:, kt, :], ident[:], cm[:, kt, :], False, True))
                    B.pe_group(fns, reads=[S_kaug[hb], S_kstat, S_qq[hb], S_qm[hb], S_c2, S_const], writes=[S_sp[i]])
                    B.op("act", lambda e: e.activation(out=pT[i][:], in_=sp[i][:], func=AF.Exp), reads=[S_sp[i]], writes=[S_pT[i]])
                    return i

                def pv_mm(h, qb, j, i, first, last):
                    ob = qb % 2
                    B.pe_group([mm(po[ob][0:65, 0:256], V_all[:, 2 * j + kt, h, :], pT[i][:, kt, :], (first and kt == 0), (last and kt == 1))
                                for kt in range(2)], reads=[S_V, S_pT[i]], writes=[S_po[ob]])

                tctr = [0]

                def tail_a(h, qb):
                    ob = qb % 2
                    r = tctr[0] % 4
                    tctr[0] += 1
                    B.op("act", lambda e: e.activation(out=pos[r][0:65, :], in_=po[ob][0:65, 0:256], func=AF.Copy), reads=[S_po[ob]], writes=[S_pos[r]])
                    B.op("dve", lambda e: e.reciprocal(out=rc[r][64:65, :], in_=pos[r][64:65, :]), reads=[S_pos[r]], writes=[S_rc[r]])
                    return r

                def tail_b(h, qb, r):
                    hb = h % 2
                    qc = slice(qb * 256, (qb + 1) * 256)
                    B.pe_group([mm(pbc[0:64, 0:256], ones_f[64:65, 0:64], rc[r][64:65, :], True, True)],
                               reads=[S_rc[r], S_const], writes=[S_pbc])
                    B.op("dve", lambda e: e.tensor_tensor(out=atf[r][:], in0=pbc[0:64, 0:256], in1=pos[r][0:64, :], op=ALU.mult),
                         reads=[S_pbc, S_pos[r]], writes=[S_atf[r]])
                    B.op("pool", lambda e: e.tensor_tensor(out=sqa[r][:], in0=atf[r][:], in1=atf[r][:], op=ALU.mult), reads=[S_atf[r]], writes=[S_sqa[r]])
                    B.op("pool", lambda e: e.tensor_scalar(out=atst[hb][:, qc], in0=atf[r][:], scalar1=gattn_sb[:, h:h + 1], scalar2=None, op0=ALU.mult),
                         reads=[S_atf[r], S_const], writes=[S_atst[hb]])

                def tail_c(h, qb, r):
                    B.pe_group([mm(pbc[:, 256 + t:257 + t], sqa[r][:, t * 128:(t + 1) * 128], ones_b[0:64, 0:1], True, True) for t in range(2)],
                               reads=[S_sqa[r], S_const], writes=[S_pss2])
                    if h == HORDER[0]:
                        B.op("dve", lambda e: e.tensor_copy(out=ssa[:, 2 * qb:2 * qb + 2], in_=pbc[:, 256:258]), reads=[S_pss2], writes=[S_ssa])
                    else:
                        B.op("dve", lambda e: e.tensor_tensor(out=ssa[:, 2 * qb:2 * qb + 2], in0=pbc[:, 256:258], in1=ssa[:, 2 * qb:2 * qb + 2],
                                                              op=ALU.add),
                             reads=[S_pss2, S_ssa], writes=[S_ssa])

                def blocks_of(h, qb):
                    sB = 15 + qb
                    return list(range(max(0, sB - KEEP[h]), sB + 1))

                load_head(HORDER[0])
                prep_head(HORDER[0])
                for qt in range(NQT):
                    mask_a(HORDER[0], qt)
                    mask_b(HORDER[0], qt)
                it = 0
                tails = []
                for hi, h in enumerate(HORDER):
                    hb = h % 2
                    hn = HORDER[hi + 1] if hi + 1 < NH else None
                    if hn is not None:
                        load_head(hn)
                        prep_head(hn)
                    total_iter = sum(len(blocks_of(h, qb)) for qb in range(NQB))
                    sched = {}
                    if hn is not None:
                        step = max(1, int(0.8 * total_iter) // NQT)
                        for qt in range(NQT):
                            ia = it + min(total_iter - 1, 1 + qt * step)
                            ib = it + min(total_iter - 1, 1 + qt * step + 3)
                            sched.setdefault(ia, []).append(lambda qt=qt, hn=hn: mask_a(hn, qt))
                            sched.setdefault(ib, []).append(lambda qt=qt, hn=hn: mask_b(hn, qt))
                    for qb in range(NQB):
                        js = blocks_of(h, qb)
                        n = len(js)
                        ids = {0: s_mm(h, qb, js[0])}
                        if n > 1:
                            ids[1] = s_mm(h, qb, js[1])
                        for i in range(n):
                            if i + 2 < n:
                                ids[i + 2] = s_mm(h, qb, js[i + 2])
                            pv_mm(h, qb, js[i], ids[i], i == 0, i == n - 1)
                            for fn in sched.pop(it, []):
                                fn()
                            for item in list(tails):
                                if item[0] <= it:
                                    item[1]()
                                    tails.remove(item)
                            it += 1
                        r = tail_a(h, qb)
                        tails.append((it + 5, lambda h=h, qb=qb, r=r: tail_b(h, qb, r)))
                        tails.append((it + 10, lambda h=h, qb=qb, r=r: tail_c(h, qb, r)))
                    for key in sorted(sched):
                        for fn in sched[key]:
                            fn()
                    tails.append((it + 11, lambda h=h, hb=hb: B.dma("sp", at_s[h], atst[hb][:], S_atst[hb], reads=[S_atst[hb]], writes=[S_atd])))
                for item in tails:
                    item[1]()
                B.emit_block()

        with ExitStack() as p3:
          if PHASES >= 3:
            wu = sb(p3, "wu", [128, 8, 2 * DFF], BF16)
            wd = sb(p3, "wd", [128, 22, D], BF16)
            x1 = [sb(p3, "x1%d" % i, [128, 2, D], F32) for i in range(2)]
            atb = sb(p3, "atb", [128, 4, 256], BF16)
            cgb = sb(p3, "cgb", [128, 4, 256], BF16)
            xn2 = sb(p3, "xn2", [128, 2, D], BF16)
            h2T = [sb(p3, "h2T%d" % i, [128, 8, 258], BF16) for i in range(2)]
            YA = [sb(p3, "YA%d" % i, [128, 256], F32) for i in range(2)]
            YB = [sb(p3, "YB%d" % i, [128, 256], F32) for i in range(2)]
            YV = [sb(p3, "YV%d" % i, [128, 256], F32) for i in range(2)]
            YG = [sb(p3, "YG%d" % i, [128, 256], F32) for i in range(2)]
            actT = sb(p3, "actT", [128, 22, 256], BF16)
            gfb = sb(p3, "gfb", [128, D], F32)
            rsa = sb(p3, "rsa", [128, NQT], F32)
            rsc = sb(p3, "rsc", [128, NQT], F32)
            ssf = sb(p3, "ssf", [128, 2], F32)
            rtf = sb(p3, "rtf", [128, 2], F32)
            rsf = sb(p3, "rsf", [128, 2], F32)
            ssb = sb(p3, "ssb", [128, 2], F32)
            rtb = sb(p3, "rtb", [128, 2], F32)
            rsb = sb(p3, "rsb", [128, 2], F32)
            pu = [ps(p3, "pu%d" % i, [128, 512], F32) for i in range(4)]
            pd = [ps(p3, "pd%d" % i, [128, 512], F32) for i in range(2)]
            pac = [ps(p3, "pac%d" % i, [128, 512], F32) for i in range(2)]
            S_wd = Slot()
            S_wuq = [Slot() for _ in range(4)]
            S_x1 = [Slot(), Slot()]
            S_atb, S_cgb, S_xn2 = Slot(), Slot(), Slot()
            S_h2T = [Slot(), Slot()]
            S_YA = [Slot() for _ in range(2)]
            S_YB = [Slot() for _ in range(2)]
            S_YV = [Slot(), Slot()]
            S_YG = [Slot(), Slot()]
            S_actT, S_gfb, S_rs = Slot(), Slot(), Slot()
            S_ssf, S_rtf, S_rsf, S_ssb, S_rtb, S_rsb = Slot(), Slot(), Slot(), Slot(), Slot(), Slot()
            S_pu = [Slot() for _ in range(4)]
            S_pd = [Slot(), Slot()]
            S_pac = [Slot(), Slot()]
            gtb = x1[1][:, 0, :]
            S_gtb = S_x1[1]

            wov = w_out.rearrange("(c p) n -> p c n", p=128)
            wuv = w_up.rearrange("(c p) n -> p c n", p=128)
            wdv = w_down.rearrange("(c p) n -> p c n", p=128)
            mod_t = mod_s.tensor
            B.dma("sp", gtb, bass.AP(mod_t, 2048, [[0, 128], [1, D]]), S_gtb, reads=[S_mods], writes=[S_gtb])
            B.dma("sp", gfb[:], gfin, S_gfb, writes=[S_gfb])
            WQ = [(0, 6), (6, 12), (12, 17), (17, 22)]
            for qi, (v0, v1) in enumerate(WQ):
                for base in (0, DFF):
                    B.dma("pool", wu[:, :, base + v0 * 128:base + v1 * 128], wuv[:, :, base + v0 * 128:base + v1 * 128], S_wuq[qi], writes=[S_wuq[qi]],
                          deps=[S_wuq[qi - 1].w] if qi > 0 else [])
                if qi == 0:
                    for c in range(8):
                        B.op("pool", lambda e, c=c: e.tensor_tensor(out=wo[:, c, :], in0=wo[:, c, :], in1=gtb, op=ALU.mult),
                             reads=[S_gtb], writes=[S_wo])
            for c0 in range(0, 22, 2):
                B.dma("pool", wd[:, c0:c0 + 2, :], wdv[:, c0:c0 + 2, :], S_wd, writes=[S_wd])
            B.dma("sp", gtb, bass.AP(mod_t, 5120, [[0, 128], [1, D]]), S_gtb, reads=[S_mods], writes=[S_gtb])
            for c in range(22):
                B.op("pool", lambda e, c=c: e.tensor_tensor(out=wd[:, c, :], in0=wd[:, c, :], in1=gtb, op=ALU.mult),
                     reads=[S_gtb], writes=[S_wd])
            for src_, dst, ssl in ((ssa, rsa, S_ssa), (ssc, rsc, S_ssc)):
                B.op("act", lambda e, src_=src_, dst=dst: e.activation(out=dst[:], in_=src_[:], func=AF.Sqrt, scale=1.0 / 512, bias=epsc[:]),
                     reads=[ssl, S_const], writes=[S_rs])
                B.op("dve", lambda e, dst=dst: e.reciprocal(out=dst[:], in_=dst[:]), reads=[S_rs], writes=[S_rs])
            for i in range(2):
                B.op("dve", lambda e, i=i: e.memset(h2T[i][:, :, 0:2], 0.0), writes=[S_h2T[i]])

            xsv = xs.rearrange("(n t p) d -> n p t d", t=2, p=128)
            outv = out.rearrange("(n t p) d -> n p t d", t=2, p=128)
            atv = at_s.rearrange("(pr two) d t -> (two d) pr t", two=2)
            cgv = cg_s.rearrange("c p t -> p c t")
            puc = [0]
            ybc = [0]
            yac = [0]
            pend_fin = []
            tpv = [pac[i][:, :].bitcast(BF16).rearrange("p (c t) -> p c t", t=256) for i in range(2)]

            def next_yb():
                i = ybc[0] % 6
                ybc[0] += 1
                return yb[i], S_yb[i]

            def f3a_dma(qb):
                k = qb % 2
                sl = 15 + qb
                qc = slice(qb * 256, (qb + 1) * 256)
                B.dma("sp", x1[k][:], xsv[sl], S_x1[k], writes=[S_x1[k]])
                B.dma("sp", atb[:], atv[:, :, qc], S_atb, reads=[S_atd], writes=[S_atb])
                B.dma("sp", cgb[:], cgv[:, :, qc], S_cgb, reads=[S_cgd], writes=[S_cgb])

            def f3a(qb):
                k = qb % 2
                for t in range(2):
                    tile = 2 * qb + t
                    for n in range(2):
                        ns = slice(n * 512, (n + 1) * 512)
                        B.pe_group([mm(pac[0][:, :], atb[:, c, t * 128:(t + 1) * 128], wo[:, c, ns], c == 0, c == 3) for c in range(4)],
                                   reads=[S_atb, S_wo], writes=[S_pac[0]])
                        B.pe_group([mm(pac[1][:, :], cgb[:, c, t * 128:(t + 1) * 128], wo[:, 4 + c, ns], c == 0, c == 3) for c in range(4)],
                                   reads=[S_cgb, S_wo], writes=[S_pac[1]])
                        B.op("dve", lambda e, t=t, ns=ns, tile=tile: e.scalar_tensor_tensor(
                            out=x1[k][:, t, ns], in0=pac[0][:, :], scalar=rsa[:, tile:tile + 1], in1=x1[k][:, t, ns], op0=ALU.mult, op1=ALU.add),
                            reads=[S_pac[0], S_rs], writes=[S_x1[k]])
                        B.op("dve", lambda e, t=t, ns=ns, tile=tile: e.scalar_tensor_tensor(
                            out=x1[k][:, t, ns], in0=pac[1][:, :], scalar=rsc[:, tile:tile + 1], in1=x1[k][:, t, ns], op0=ALU.mult, op1=ALU.add),
                            reads=[S_pac[1], S_rs], writes=[S_x1[k]])
                for t in range(2):
                    B.op("act", lambda e, t=t: e.activation(out=xn2[:, t, :], in_=x1[k][:, t, :], func=AF.Square, accum_out=ssf[:, t:t + 1]),
                         reads=[S_x1[k]], writes=[S_xn2, S_ssf])
                B.op("act", lambda e: e.activation(out=rtf[:], in_=ssf[:], func=AF.Sqrt, scale=1.0 / D, bias=epsc[:]),
                     reads=[S_ssf, S_const], writes=[S_rtf])
                B.op("dve", lambda e: e.reciprocal(out=rsf[:], in_=rtf[:]), reads=[S_rtf], writes=[S_rsf])
                for t in range(2):
                    B.op("dve", lambda e, t=t: e.tensor_scalar(out=xn2[:, t, :], in0=x1[k][:, t, :], scalar1=rsf[:, t:t + 1], scalar2=None,
                                                               op0=ALU.mult),
                         reads=[S_x1[k], S_rsf], writes=[S_xn2])

            def f3b(qb):
                k = qb % 2
                for half in range(2):
                    B.pe_group([tr(tpv[half][:, c, t * 128:(t + 1) * 128], xn2[:, t, (half * 4 + c) * 128:(half * 4 + c + 1) * 128], ident[:])
                                for t in range(2) for c in range(4)], reads=[S_xn2, S_const], writes=[S_pac[half]])
                    for c in range(4):
                        cc = half * 4 + c
                        B.op("act", lambda e, c=c, cc=cc, half=half: e.activation(out=h2T[k][:, cc, 2:258], in_=tpv[half][:, c, :],
                                                                                 func=AF.Identity, scale=G2[:, cc:cc + 1], bias=sh2[:, cc:cc + 1]),
                             reads=[S_pac[half], S_G], writes=[S_h2T[k]])
                if qb == 1:
                    B.op("dve", lambda e: e.tensor_scalar(out=h2T[k][:, :, 0:2], in0=h2T[1 - k][:, :, 256:258], scalar1=hv[:, 0:1],
                                                          scalar2=None, op0=ALU.mult),
                         reads=[S_h2T[1 - k], S_const], writes=[S_h2T[k]])
                elif qb > 1:
                    B.op("dve", lambda e: e.tensor_copy(out=h2T[k][:, :, 0:2], in_=h2T[1 - k][:, :, 256:258]),
                         reads=[S_h2T[1 - k]], writes=[S_h2T[k]])

            def back3(qb):
                k = qb % 2
                for v in range(22):
                    if qb + 1 < NQB and v == 3:
                        f3a_dma(qb + 1)
                    p2_ = v % 2
                    chs = (v, 22 + v)
                    pis = []
                    for ch in chs:
                        i = puc[0] % 4
                        puc[0] += 1
                        pis.append(i)
                        B.pe_group([mm(pu[i][:, 0:258], wu[:, c, ch * 128:(ch + 1) * 128], h2T[k][:, c, 0:258], c == 0, c == 7) for c in range(8)],
                                   reads=[S_wuq[0 if v < 6 else 1 if v < 12 else 2 if v < 17 else 3], S_h2T[k]], writes=[S_pu[i]])
                    yas = []
                    for w_, (ch, i) in enumerate(zip(chs, pis)):
                        ai = yac[0] % 2
                        yac[0] += 1
                        yas.append(ai)
                        B.op("act", lambda e, i=i, ai=ai, ch=ch: e.activation(out=YA[ai][:], in_=pu[i][:, 2:258], func=AF.Identity,
                                                                             scale=fcw_sb[:, 88 + ch:89 + ch], bias=fcb_sb[:, ch:ch + 1]),
                             reads=[S_pu[i], S_const], writes=[S_YA[ai]])
                    if pend_fin:
                        pend_fin.pop(0)()
                    ybs = []
                    for w_, (ch, i) in enumerate(zip(chs, pis)):
                        bi = ybc[0] % 2
                        ybc[0] += 1
                        ybs.append(bi)
                        ai = yas[w_]
                        B.op("dve", lambda e, i=i, ai=ai, bi=bi, ch=ch: e.scalar_tensor_tensor(
                            out=YB[bi][:], in0=pu[i][:, 1:257], scalar=fcw_sb[:, 44 + ch:45 + ch], in1=YA[ai][:], op0=ALU.mult, op1=ALU.add),
                            reads=[S_pu[i], S_YA[ai], S_const], writes=[S_YB[bi]])
                    outs = ((YV[p2_], S_YV[p2_]), (YG[p2_], S_YG[p2_]))
                    for w_, (ch, i) in enumerate(zip(chs, pis)):
                        bi = ybs[w_]
                        yo, syo = outs[w_]
                        B.op("dve", lambda e, i=i, bi=bi, yo=yo, ch=ch: e.scalar_tensor_tensor(
                            out=yo[:], in0=pu[i][:, 0:256], scalar=fcw_sb[:, ch:ch + 1], in1=YB[bi][:], op0=ALU.mult, op1=ALU.add),
                            reads=[S_pu[i], S_YB[bi], S_const], writes=[syo])
                    def fin_pair(p2_=p2_, v=v):
                        B.op("act", lambda e: e.activation(out=YG[p2_][:], in_=YG[p2_][:], func=AF.Silu), reads=[S_YG[p2_]], writes=[S_YG[p2_]])
                        B.op("pool", lambda e: e.tensor_tensor(out=actT[:, v, :], in0=YV[p2_][:], in1=YG[p2_][:], op=ALU.mult),
                             reads=[S_YV[p2_], S_YG[p2_]], writes=[S_actT])
                    pend_fin.append(fin_pair)
                if pend_fin:
                    pend_fin.pop(0)()
                if qb + 1 < NQB:
                    f3a(qb + 1)
                for t in range(2):
                    if t == 1 and qb + 1 < NQB:
                        f3b(qb + 1)
                    for n in range(2):
                        B.pe_group([mm(pd[n][:, :], actT[:, v, t * 128:(t + 1) * 128], wd[:, v, n * 512:(n + 1) * 512], v == 0, v == 21)
                                    for v in range(22)], reads=[S_actT, S_wd], writes=[S_pd[n]])
                        B.op("dve", lambda e, t=t, n=n: e.tensor_tensor(out=x1[k][:, t, n * 512:(n + 1) * 512], in0=pd[n][:, :],
                                                                        in1=x1[k][:, t, n * 512:(n + 1) * 512], op=ALU.add),
                             reads=[S_pd[n]], writes=[S_x1[k]])
                for t in range(2):
                    B.op("act", lambda e, t=t: e.activation(out=xn2[:, t, :], in_=x1[k][:, t, :], func=AF.Square, accum_out=ssb[:, t:t + 1]),
                         reads=[S_x1[k]], writes=[S_xn2, S_ssb])
                B.op("act", lambda e: e.activation(out=rtb[:], in_=ssb[:], func=AF.Sqrt, scale=1.0 / D, bias=epsc[:]),
                     reads=[S_ssb, S_const], writes=[S_rtb])
                B.op("dve", lambda e: e.reciprocal(out=rsb[:], in_=rtb[:]), reads=[S_rtb], writes=[S_rsb])
                for t in range(2):
                    B.op("dve", lambda e, t=t: e.scalar_tensor_tensor(out=x1[k][:, t, :], in0=x1[k][:, t, :], scalar=rsb[:, t:t + 1],
                                                                      in1=gfb[:], op0=ALU.mult, op1=ALU.mult),
                         reads=[S_rsb, S_gfb], writes=[S_x1[k]])
                B.dma("sp", outv[qb - 1], x1[k][:], S_x1[k], reads=[S_x1[k]])

            f3a_dma(0)
            f3a(0)
            f3b(0)
            f3a_dma(1)
            f3a(1)
            f3b(1)
            for qb in range(1, NQB):
                back3(qb)
            B.emit_block()
    return nc


_NC_CACHE = {}


def _consts(half):
    bf = ml_dtypes.bfloat16
    ident = np.eye(128, dtype=np.float32).astype(bf)
    key = np.arange(S)
    kstat = np.zeros((35, S), np.float32)
    kstat[key // 256, key] = 1.0
    kstat[32] = key % 256
    kstat[33] = key // 256
    kstat[34] = 1.0
    kstat = kstat.astype(bf)
    p = np.arange(128)[:, None, None]
    kt = np.arange(2)[None, :, None]
    a = np.arange(256)[None, None, :]
    cmask = np.where(kt * 128 + p <= a, 0.0, NEG).astype(np.float32).reshape(128, 512).astype(bf)
    gvb = np.zeros((NQB, 32), np.float32)
    gvb2 = np.zeros((NQB, 32), np.float32)
    for qb in range(NQB):
        sB = 15 + qb
        for j in range(32):
            valid = (j < sB) and (half == 1 or j >= 16)
            gvb[qb, j] = 0.0 if valid else -1.0e9
            gvb2[qb, j] = NEG if valid else 2 * NEG
        gvb[qb, sB] = -3.0e9
        gvb2[qb, sB] = 0.0
    gvb = np.ascontiguousarray(np.broadcast_to(gvb.reshape(1, -1), (128, NQB * 32))).astype(np.float32)
    gvb2 = np.ascontiguousarray(np.broadcast_to(gvb2.reshape(1, -1), (128, NQB * 32))).astype(np.float32)
    qstat = np.zeros((NH, 128, NQT, 99), np.float32)
    for h in range(NH):
        slope = 2.0 ** (-(h + 1))
        for qt in range(NQT):
            sB = 15 + qt // 2
            apos = (qt % 2) * 128 + np.arange(128)
            qstat[h, :, qt, 96] = slope
            qstat[h, :, qt, 97] = 256.0 * slope
            qstat[h, :, qt, 98] = -slope * (256.0 * sB + apos)
    qstat = qstat.reshape(NH, 128, NQT * 99).astype(bf)
    hv = np.full((128, 1), float(half), np.float32)
    return dict(ident=ident, kstat=kstat, cmask=cmask, gvb=gvb, gvb2=gvb2, qstat=qstat, hv=hv)


def _pc(v, nchunk):
    return np.ascontiguousarray(np.asarray(v, np.float32).reshape(nchunk, 128).T)


def kernel(x, c, w_ada, b_ada, g_mix, w_in, conv_w, conv_b, g_attn_out, g_conv_out, w_out, g_ffn,
           w_up, ffn_conv_w, ffn_conv_b, w_down, g_final):
    f = lambda a: np.ascontiguousarray(np.asarray(a, dtype=np.float32))
    x = f(x)
    c = f(c)
    if "nc" not in _NC_CACHE:
        _NC_CACHE["nc"] = build_program()
    nc = _NC_CACHE["nc"]
    shared = dict(
        w_ada=f(w_ada[0]), b_ada=f(b_ada[0]).reshape(1, -1), gmix=_pc(g_mix[0], 8), w_in=f(w_in[0]),
        convw=np.ascontiguousarray(np.concatenate([_pc(conv_w[0][kk], 4) for kk in range(3)], axis=1)),
        convb=_pc(conv_b[0], 4),
        gattn=np.ascontiguousarray(f(g_attn_out[0]).reshape(8, 64).T),
        gconv=_pc(g_conv_out[0], 4), w_out=f(w_out[0]), gffn=_pc(g_ffn[0], 8), w_up=f(w_up[0]),
        fcw=np.ascontiguousarray(np.concatenate([_pc(ffn_conv_w[0][kk], 44) for kk in range(3)], axis=1)),
        fcb=_pc(ffn_conv_b[0], 44), w_down=f(w_down[0]),
        gfin=np.ascontiguousarray(np.broadcast_to(f(g_final).reshape(1, -1), (128, D))),
    )
    cst = [_consts(0), _consts(1)]
    in_maps = []
    for i in range(NCORES):
        b, half = i // 2, i % 2
        if half == 1:
            xs = x[b]
        else:
            xs = np.concatenate([np.zeros((4096, D), np.float32), x[b, :4096]], axis=0)
        m = dict(shared)
        m.update(cst[half])
        m["xs"] = np.ascontiguousarray(xs)
        m["cT"] = _pc(c[b], 8)
        in_maps.append(m)
    res = run_bass_kernel_spmd(nc, in_maps, core_ids=list(range(NCORES)))
    outp = np.empty((4, S, D), np.float32)
    for i in range(NCORES):
        b, half = i // 2, i % 2
        outp[b, half * 4096:(half + 1) * 4096] = res.results[i]["out"]
    return outp
```

```python
import numpy as np
from contextlib import ExitStack
import ml_dtypes
import concourse.bass as bass
import concourse.mybir as mybir
from concourse.bass_utils import run_bass_kernel_spmd

F32 = mybir.dt.float32
BF16 = mybir.dt.bfloat16
AF = mybir.ActivationFunctionType
ALU = mybir.AluOpType
AX = mybir.AxisListType

D = 1024
S = 8192
NH = 8
DFF = 2816
NQB = 17
NQT = 34
QTOK = NQB * 256
EPS = 1e-6
NEG = -30000.0
NCORES = 8
PHASES = 3
DEBUG = False


class Tok:
    __slots__ = ("sem", "val")

    def __init__(self, sem, val):
        self.sem = sem
        self.val = val


class Slot:
    def __init__(self, name=""):
        self.name = name
        self.w = None
        self.r = {}
        self.dsem = None
        self.dcnt = 0


class Builder:
    ENG = ("pe", "act", "dve", "pool", "sp")

    def __init__(self, nc, es):
        self.nc = nc
        self.es = es
        self.q = {}
        for n in self.ENG:
            sem = es.enter_context(nc.semaphore("sem_" + n))
            self.q[n] = dict(sem=sem, cnt=0, ops=[], waited={})
        self.dslots = []

    def _waits(self, en, deps):
        q = self.q[en]
        need = {}
        for t in deps:
            if t is None:
                continue
            if en == "pe" and t.sem is q["sem"]:
                continue
            k = id(t.sem)
            if q["waited"].get(k, 0) >= t.val:
                continue
            if k not in need or need[k].val < t.val:
                need[k] = t
        for k, t in need.items():
            q["waited"][k] = t.val
        return [(t.sem, t.val) for t in need.values()]

    @staticmethod
    def _deps(reads, writes, deps):
        d = list(deps)
        for s in reads:
            d.append(s.w)
        for s in writes:
            d.extend(s.r.values())
            d.append(s.w)
        return d

    @staticmethod
    def _update(tok, reads, writes):
        for s in writes:
            s.w = tok
            s.r = {}
        for s in reads:
            k = id(tok.sem)
            if k not in s.r or s.r[k].val < tok.val:
                s.r[k] = tok

    def op(self, en, fn, reads=(), writes=(), deps=(), inc=True):
        q = self.q[en]
        waits = self._waits(en, self._deps(reads, writes, deps))
        tok = None
        if inc:
            q["cnt"] += 1
            tok = Tok(q["sem"], q["cnt"])
        else:
            assert not reads and not writes
        sem = q["sem"]

        def emit(e, waits=waits, fn=fn, inc=inc, sem=sem):
            for (sm, v) in waits:
                e.wait_ge(sm, v)
            ins = fn(e)
            if inc:
                ins.then_inc(sem, 1)

        q["ops"].append(emit)
        if tok is not None:
            self._update(tok, reads, writes)
        return tok

    def pe_group(self, fns, reads=(), writes=()):
        q = self.q["pe"]
        waits = self._waits("pe", self._deps(reads, writes, ()))
        q["cnt"] += 1
        tok = Tok(q["sem"], q["cnt"])
        sem = q["sem"]
        n = len(fns)

        def emit(e, waits=waits, fns=fns, sem=sem, n=n):
            for (sm, v) in waits:
                e.wait_ge(sm, v)
            for i, f in enumerate(fns):
                ins = f(e)
                if i == n - 1:
                    ins.then_inc(sem, 1)

        q["ops"].append(emit)
        self._update(tok, reads, writes)
        return tok

    def dma(self, en, out, in_, sem_slot, reads=(), writes=(), deps=(), **kw):
        q = self.q[en]
        sl = sem_slot
        if sl.dsem is None:
            sl.dsem = self.es.enter_context(self.nc.semaphore("dsem%d" % len(self.dslots)))
            self.dslots.append(sl)
        waits = self._waits(en, self._deps(reads, writes, deps))
        sl.dcnt += 16
        tok = Tok(sl.dsem, sl.dcnt)
        dsem = sl.dsem

        def emit(e, waits=waits, out=out, in_=in_, dsem=dsem, kw=kw):
            for (sm, v) in waits:
                e.wait_ge(sm, v)
            e.dma_start(out=out, in_=in_, **kw).then_inc(dsem, 16)

        q["ops"].append(emit)
        self._update(tok, reads, writes)
        return tok

    def emit_block(self):
        nc = self.nc
        finals = [(s.dsem, s.dcnt) for s in self.dslots if s.dcnt > 0]
        pe_fin = (self.q["pe"]["sem"], self.q["pe"]["cnt"])

        def fin(e, finals=finals):
            for sm, v in finals:
                e.wait_ge(sm, v)

        self.q["sp"]["ops"].append(fin)
        with nc.Block() as blk:
            for n, meth in (("pe", blk.tensor), ("act", blk.scalar), ("dve", blk.vector),
                            ("pool", blk.gpsimd), ("sp", blk.sync)):
                ops = self.q[n]["ops"]
                if ops:
                    def body(e, ops=ops):
                        for f in ops:
                            f(e)
                    meth(body)
                self.q[n]["ops"] = []


def mm(out, lhsT, rhs, start, stop):
    return lambda e: e.matmul(out, lhsT=lhsT, rhs=rhs, start=start, stop=stop)


def tr(out, in_, ident):
    return lambda e: e.transpose(out, in_, ident)


def build_program():
    nc = bass.Bass("TRN2", target_bir_lowering=False)

    def din(name, shape, dt=F32):
        return nc.dram_tensor(name, list(shape), dt, kind="ExternalInput").ap()

    def dscr(name, shape, dt):
        return nc.dram_tensor(name, list(shape), dt, kind="ExternalOutput" if DEBUG else "Internal").ap()

    xs = din("xs", [S, D])
    cT = din("cT", [128, 8])
    w_ada = din("w_ada", [D, 6 * D])
    b_ada = din("b_ada", [1, 6 * D])
    gmix = din("gmix", [128, 8])
    w_in = din("w_in", [D, 3072])
    convw = din("convw", [128, 12])
    convb = din("convb", [128, 4])
    gattn = din("gattn", [64, 8])
    gconv = din("gconv", [128, 4])
    w_out = din("w_out", [D, D])
    gffn = din("gffn", [128, 8])
    w_up = din("w_up", [D, 2 * DFF])
    fcw = din("fcw", [128, 132])
    fcb = din("fcb", [128, 44])
    w_down = din("w_down", [DFF, D])
    gfin = din("gfin", [128, D])
    ident_d = din("ident", [128, 128], BF16)
    kstat = din("kstat", [35, S], BF16)
    cmask = din("cmask", [128, 512], BF16)
    gvb_d = din("gvb", [128, NQB * 32])
    gvb2_d = din("gvb2", [128, NQB * 32])
    qstat = din("qstat", [NH, 128, NQT * 99], BF16)
    hv_d = din("hv", [128, 1])
    out = nc.dram_tensor("out", [4096, D], F32, kind="ExternalOutput").ap()

    kT_s = dscr("kT_s", [NH, 64, S], BF16)
    qT_s = dscr("qT_s", [NH, 64, QTOK], BF16)
    at_s = dscr("at_s", [NH, 64, QTOK], BF16)
    cg_s = dscr("cg_s", [4, 128, QTOK], BF16)
    mod_s = dscr("mod_s", [1, 6 * D], F32)
    km_s = dscr("km_s", [NH, 64, 32], F32)

    es = ExitStack()
    with es:
        B = Builder(nc, es)

        def sb(st, name, shape, dt):
            return st.enter_context(nc.sbuf_tensor("sb_" + name, list(shape), dt))

        def ps(st, name, shape, dt):
            return st.enter_context(nc.psum_tensor("ps_" + name, list(shape), dt))

        ident = sb(es, "ident", [128, 128], BF16)
        ones_b = sb(es, "ones_b", [128, 1], BF16)
        ones_f = sb(es, "ones_f", [128, 64], F32)
        epsc = sb(es, "epsc", [128, 1], F32)
        hv = sb(es, "hv", [128, 1], F32)
        G1 = sb(es, "G1", [128, 8], F32)
        G2 = sb(es, "G2", [128, 8], F32)
        modT = sb(es, "modT", [128, 48], F32)
        gmix_sb = sb(es, "gmix_sb", [128, 8], F32)
        gffn_sb = sb(es, "gffn_sb", [128, 8], F32)
        convw_sb = sb(es, "convw_sb", [128, 12], F32)
        convb_sb = sb(es, "convb_sb", [128, 4], F32)
        gconv_sb = sb(es, "gconv_sb", [128, 4], F32)
        gattn_sb = sb(es, "gattn_sb", [64, 8], F32)
        fcw_sb = sb(es, "fcw_sb", [128, 132], F32)
        fcb_sb = sb(es, "fcb_sb", [128, 44], F32)
        ssa = sb(es, "ssa", [128, NQT], F32)
        ssc = sb(es, "ssc", [128, NQT], F32)
        wo = sb(es, "wo", [128, 8, D], BF16)
        S_wo = Slot("wo")
        S_const = Slot("const")
        S_ssa = Slot("ssa")
        S_ssc = Slot("ssc")
        S_G = Slot("G")
        S_mods = Slot("mods")

        for dst, src in ((ident, ident_d), (hv, hv_d), (gmix_sb, gmix), (gffn_sb, gffn), (convw_sb, convw),
                         (convb_sb, convb), (gconv_sb, gconv), (gattn_sb, gattn), (fcw_sb, fcw), (fcb_sb, fcb)):
            B.dma("sp", dst[:], src, S_const, writes=[S_const])
        B.op("dve", lambda e: e.memset(ones_b[:], 1.0), writes=[S_const])
        B.op("dve", lambda e: e.memset(ones_f[:], 1.0), writes=[S_const])
        B.op("dve", lambda e: e.memset(epsc[:], EPS), writes=[S_const])

        with ExitStack() as p0:
            c_sb = sb(p0, "c_sb", [128, 8], F32)
            sc_b = sb(p0, "sc_b", [128, 8], BF16)
            brow = sb(p0, "brow", [1, 6 * D], F32)
            modrow = sb(p0, "modrow", [1, 6 * D], F32)
            wa = [sb(p0, "wa%d" % i, [128, 8, 512], BF16) for i in range(2)]
            pm = [ps(p0, "pm%d" % i, [128, 512], F32) for i in range(2)]
            S_c, S_scb, S_brow, S_modrow, S_modT = Slot(), Slot(), Slot(), Slot(), Slot()
            S_wa = [Slot(), Slot()]
            S_pm = [Slot(), Slot()]
            B.dma("sp", c_sb[:], cT, S_c, writes=[S_c])
            B.dma("sp", brow[:], b_ada, S_brow, writes=[S_brow])
            B.op("act", lambda e: e.activation(out=sc_b[:], in_=c_sb[:], func=AF.Silu), reads=[S_c], writes=[S_scb])
            wav = w_ada.rearrange("(c p) n -> p c n", p=128)
            for g in range(12):
                k = g % 2
                B.dma("pool", wa[k][:], wav[:, :, g * 512:(g + 1) * 512], S_wa[k], writes=[S_wa[k]])
                B.pe_group([mm(pm[k][0:1, :], sc_b[:, c:c + 1], wa[k][:, c, :], c == 0, c == 7) for c in range(8)],
                           reads=[S_wa[k], S_scb], writes=[S_pm[k]])
                B.op("dve", lambda e, k=k, g=g: e.tensor_tensor(out=modrow[0:1, g * 512:(g + 1) * 512], in0=pm[k][0:1, :],
                                                                 in1=brow[0:1, g * 512:(g + 1) * 512], op=ALU.add),
                     reads=[S_pm[k], S_brow], writes=[S_modrow])
            B.dma("sp", mod_s, modrow[:], S_mods, reads=[S_modrow], writes=[S_mods])
            B.dma("sp", modT[:], mod_s.rearrange("o (k p) -> (o p) k", p=128), S_modT, reads=[S_mods], writes=[S_modT],
                  allow_slow_non_contiguous=True)
            B.op("dve", lambda e: e.scalar_tensor_tensor(out=G1[:], in0=modT[:, 8:16], scalar=1.0, in1=gmix_sb[:],
                                                         op0=ALU.add, op1=ALU.mult), reads=[S_modT, S_const], writes=[S_G])
            B.op("dve", lambda e: e.scalar_tensor_tensor(out=G2[:], in0=modT[:, 32:40], scalar=1.0, in1=gffn_sb[:],
                                                         op0=ALU.add, op1=ALU.mult), reads=[S_modT, S_const], writes=[S_G])
            B.emit_block()
        sh1 = modT[:, 0:8]
        sh2 = modT[:, 24:32]

        with ExitStack() as p12:
          if PHASES >= 1:
            V_all = sb(p12, "V_all", [128, 64, NH, 65], BF16)
            S_V = Slot("V")
            with ExitStack() as p1:
                w_in_sb = sb(p1, "w_in_sb", [128, 8, 3072], BF16)
                S_win = Slot("win")
                wiv = w_in.rearrange("(c p) n -> p c n", p=128)
                for c in range(8):
                    B.dma("pool", w_in_sb[:, c, :], wiv[:, c, :], S_win, writes=[S_win])
                pend_stats = []
                xt = [sb(p1, "xt%d" % i, [128, 2, D], F32) for i in range(3)]
                xn = [sb(p1, "xnp%d" % i, [128, 2, D], BF16) for i in range(3)]
                hT = [sb(p1, "hT%d" % i, [128, 8, 258], BF16) for i in range(2)]
                junk = sb(p1, "junk", [128, D], BF16)
                ss = [sb(p1, "ssp%d" % i, [128, 2], F32) for i in range(3)]
                rt = [sb(p1, "rt%d" % i, [128, 2], F32) for i in range(3)]
                rstd = [sb(p1, "rstd%d" % i, [128, 2], F32) for i in range(3)]
                kst = [sb(p1, "kst%d" % i, [128, 4, 256], BF16) for i in range(2)]
                qst = [sb(p1, "qst%d" % i, [128, 4, 256], BF16) for i in range(2)]
                cgs = [sb(p1, "cgs%d" % i, [128, 4, 256], BF16) for i in range(2)]
                sqc = sb(p1, "sqc", [128, 4, 256], BF16)
                kmst = sb(p1, "kmst", [128, 4, 32], F32)
                S_kmst = Slot()
                S_kmd = Slot("km_s")
                u_sb = [sb(p1, "u_sb%d" % i, [128, 258], F32) for i in range(2)]
                zb = [sb(p1, "zb%d" % i, [128, 258], F32) for i in range(2)]
                y0 = [sb(p1, "y0%d" % i, [128, 256], F32) for i in range(2)]
                y1 = [sb(p1, "y1%d" % i, [128, 256], F32) for i in range(2)]
                y2 = [sb(p1, "y2%d" % i, [128, 256], F32) for i in range(2)]
                cv = [sb(p1, "cv%d" % i, [128, 256], F32) for i in range(2)]
                tp = ps(p1, "tp", [128, 8, 256], BF16)
                pp = [ps(p1, "pp%d" % i, [128, 512], F32) for i in range(5)]
                pss = ps(p1, "pss", [128, 512], F32)
                S_xt = [Slot(), Slot(), Slot()]
                S_xn = [Slot(), Slot(), Slot()]
                S_hT = [Slot(), Slot()]
                S_junk = Slot()
                S_ss = [Slot(), Slot(), Slot()]
                S_rt = [Slot(), Slot(), Slot()]
                S_rstd = [Slot(), Slot(), Slot()]
                S_kst = [Slot(), Slot()]
                S_qst = [Slot(), Slot()]
                S_cgs = [Slot(), Slot()]
                S_sqc = Slot()
                S_u = [Slot(), Slot()]
                S_z = [Slot(), Slot()]
                S_y0 = [Slot(), Slot()]
                S_y1 = [Slot(), Slot()]
                S_y2 = [Slot(), Slot()]
                S_cv = [Slot(), Slot()]
                S_tp = Slot()
                S_pp = [Slot() for _ in range(5)]
                S_pss = Slot()
                S_kT = Slot("kT_s")
                S_qT = Slot("qT_s")
                S_cgd = Slot("cg_s")
                ppi = [0]

                def next_pp():
                    i = ppi[0] % 5
                    ppi[0] += 1
                    return pp[i], S_pp[i]

                B.op("pool", lambda e: e.memset(V_all[:, :, :, 64:65], 1.0), writes=[S_V])
                for i in range(2):
                    B.op("pool", lambda e, i=i: e.memset(hT[i][:, :, 0:2], 0.0), writes=[S_hT[i]])
                xsv = xs.rearrange("(n t p) d -> n p t d", t=2, p=128)
                kTv = kT_s.rearrange("(pr two) d t -> (two d) pr t", two=2)
                qTv = qT_s.rearrange("(pr two) d t -> (two d) pr t", two=2)
                cgv = cg_s.rearrange("c p t -> p c t")
                evac_rr = [0]

                def evac(out_ap, in_ap, reads, writes, scale=None):
                    evac_rr[0] += 1
                    if evac_rr[0] % 2 == 0:
                        if scale is None:
                            return B.op("act", lambda e: e.activation(out=out_ap, in_=in_ap, func=AF.Copy), reads=reads, writes=writes)
                        return B.op("act", lambda e: e.activation(out=out_ap, in_=in_ap, func=AF.Copy, scale=scale), reads=reads, writes=writes)
                    if scale is None:
                        return B.op("dve", lambda e: e.tensor_copy(out=out_ap, in_=in_ap), reads=reads, writes=writes)
                    return B.op("dve", lambda e: e.tensor_scalar(out=out_ap, in0=in_ap, scalar1=scale, scalar2=None, op0=ALU.mult),
                                reads=reads, writes=writes)

                def f1a(s):
                    k = s % 3
                    B.dma("sp", xt[k][:], xsv[s], S_xt[k], writes=[S_xt[k]])
                    for t in range(2):
                        B.op("act", lambda e, k=k, t=t: e.activation(out=junk[:], in_=xt[k][:, t, :], func=AF.Square,
                                                                     accum_out=ss[k][:, t:t + 1]),
                             reads=[S_xt[k]], writes=[S_junk, S_ss[k]])
                    B.op("act", lambda e, k=k: e.activation(out=rt[k][:], in_=ss[k][:], func=AF.Sqrt, scale=1.0 / D, bias=epsc[:]),
                         reads=[S_ss[k], S_const], writes=[S_rt[k]])
                    B.op("dve", lambda e, k=k: e.reciprocal(out=rstd[k][:], in_=rt[k][:]), reads=[S_rt[k]], writes=[S_rstd[k]])
                    for t in range(2):
                        B.op("dve", lambda e, k=k, t=t: e.tensor_scalar(out=xn[k][:, t, :], in0=xt[k][:, t, :], scalar1=rstd[k][:, t:t + 1],
                                                                        scalar2=None, op0=ALU.mult),
                             reads=[S_xt[k], S_rstd[k]], writes=[S_xn[k]])

                def f1b(s):
                    k = s % 2
                    B.pe_group([tr(tp[:, c, t * 128:(t + 1) * 128], xn[s % 3][:, t, c * 128:(c + 1) * 128], ident[:])
                                for t in range(2) for c in range(8)], reads=[S_xn[s % 3], S_const], writes=[S_tp])
                    for c in range(8):
                        if c % 2 == 0:
                            B.op("act", lambda e, k=k, c=c: e.activation(out=hT[k][:, c, 2:258], in_=tp[:, c, :], func=AF.Identity,
                                                                         scale=G1[:, c:c + 1], bias=sh1[:, c:c + 1]),
                                 reads=[S_tp, S_G], writes=[S_hT[k]])
                        else:
                            B.op("dve", lambda e, k=k, c=c: e.tensor_scalar(out=hT[k][:, c, 2:258], in0=tp[:, c, :], scalar1=G1[:, c:c + 1],
                                                                            scalar2=sh1[:, c:c + 1], op0=ALU.mult, op1=ALU.add),
                                 reads=[S_tp, S_G], writes=[S_hT[k]])
                    if s == 16:
                        B.op("dve", lambda e, k=k: e.tensor_scalar(out=hT[k][:, :, 0:2], in0=hT[1 - k][:, :, 256:258], scalar1=hv[:, 0:1],
                                                                   scalar2=None, op0=ALU.mult),
                             reads=[S_hT[1 - k], S_const], writes=[S_hT[k]])
                    elif s > 16:
                        B.op("dve", lambda e, k=k: e.tensor_copy(out=hT[k][:, :, 0:2], in_=hT[1 - k][:, :, 256:258]),
                             reads=[S_hT[1 - k]], writes=[S_hT[k]])

                def back1a(s):
                    k = s % 2
                    for pr in range(4):
                        pt_, sp_ = next_pp()
                        B.pe_group([mm(pt_[:, 0:256], w_in_sb[:, c, 512 + pr * 128:512 + (pr + 1) * 128], hT[k][:, c, 2:258], c == 0, c == 7)
                                    for c in range(8)], reads=[S_win, S_hT[k]], writes=[sp_])
                        evac(kst[k][:, pr, :], pt_[:, 0:256], [sp_], [S_kst[k]])
                        B.op("dve", lambda e, k=k, pr=pr, s=s: e.tensor_reduce(out=kmst[:, pr, s:s + 1], in_=kst[k][:, pr, :], axis=AX.X, op=ALU.add),
                             reads=[S_kst[k]], writes=[S_kmst])
                    B.dma("sp", kTv[:, :, s * 256:(s + 1) * 256], kst[k][:], S_kst[k], reads=[S_kst[k]], writes=[S_kT])
                    for t in range(2):
                        pt_, sp_ = next_pp()
                        B.pe_group([mm(pt_[:, :], hT[k][:, c, 2 + t * 128:2 + (t + 1) * 128], w_in_sb[:, c, 1024:1536], c == 0, c == 7)
                                    for c in range(8)], reads=[S_win, S_hT[k]], writes=[sp_])
                        evac(V_all[:, 2 * s + t, :, 0:64], pt_[:, :].rearrange("p (h d) -> p h d", d=64), [sp_], [S_V])

                def back1b(s):
                    k = s % 2
                    if s < 15:
                        return
                    qb = s - 15
                    for pr in range(4):
                        pt_, sp_ = next_pp()
                        B.pe_group([mm(pt_[:, 0:256], w_in_sb[:, c, pr * 128:(pr + 1) * 128], hT[k][:, c, 2:258], c == 0, c == 7)
                                    for c in range(8)], reads=[S_win, S_hT[k]], writes=[sp_])
                        evac(qst[k][:, pr, :], pt_[:, 0:256], [sp_], [S_qst[k]], scale=0.125)
                    B.dma("sp", qTv[:, :, qb * 256:(qb + 1) * 256], qst[k][:], S_qst[k], reads=[S_qst[k]], writes=[S_qT])
                    for ch in range(4):
                        j = ch % 2
                        pu_, spu = next_pp()
                        B.pe_group([mm(pu_[:, 0:258], w_in_sb[:, c, 1536 + ch * 128:1536 + (ch + 1) * 128], hT[k][:, c, 0:258], c == 0, c == 7)
                                    for c in range(8)], reads=[S_win, S_hT[k]], writes=[spu])
                        pc_, spc = next_pp()
                        B.pe_group([mm(pc_[:, 0:258], w_in_sb[:, c, 2048 + ch * 128:2048 + (ch + 1) * 128], hT[k][:, c, 0:258], c == 0, c == 7)
                                    for c in range(8)], reads=[S_win, S_hT[k]], writes=[spc])
                        pb_, spb = next_pp()
                        B.pe_group([mm(pb_[:, 0:256], w_in_sb[:, c, 2560 + ch * 128:2560 + (ch + 1) * 128], hT[k][:, c, 2:258], c == 0, c == 7)
                                    for c in range(8)], reads=[S_win, S_hT[k]], writes=[spb])
                        B.op("act", lambda e, j=j, pu_=pu_: e.activation(out=u_sb[j][:], in_=pu_[:, 0:258], func=AF.Copy),
                             reads=[spu], writes=[S_u[j]])
                        B.op("dve", lambda e, j=j, pc_=pc_: e.tensor_tensor(out=zb[j][:], in0=pc_[:, 0:258], in1=u_sb[j][:], op=ALU.mult),
                             reads=[spc, S_u[j]], writes=[S_z[j]])
                        B.op("dve", lambda e, j=j, ch=ch: e.tensor_scalar(out=y0[j][:], in0=zb[j][:, 2:258], scalar1=convw_sb[:, 8 + ch:9 + ch],
                                                                          scalar2=convb_sb[:, ch:ch + 1], op0=ALU.mult, op1=ALU.add),
                             reads=[S_z[j], S_const], writes=[S_y0[j]])
                        B.op("dve", lambda e, j=j, ch=ch: e.scalar_tensor_tensor(out=y1[j][:], in0=zb[j][:, 1:257], scalar=convw_sb[:, 4 + ch:5 + ch],
                                                                                 in1=y0[j][:], op0=ALU.mult, op1=ALU.add),
                             reads=[S_z[j], S_y0[j], S_const], writes=[S_y1[j]])
                        B.op("dve", lambda e, j=j, ch=ch: e.scalar_tensor_tensor(out=y2[j][:], in0=zb[j][:, 0:256], scalar=convw_sb[:, ch:ch + 1],
                                                                                 in1=y1[j][:], op0=ALU.mult, op1=ALU.add),
                             reads=[S_z[j], S_y1[j], S_const], writes=[S_y2[j]])
                        B.op("dve", lambda e, j=j, pb_=pb_: e.tensor_tensor(out=cv[j][:], in0=pb_[:, 0:256], in1=y2[j][:], op=ALU.mult),
                             reads=[spb, S_y2[j]], writes=[S_cv[j]])
                        B.op("act", lambda e, j=j, ch=ch: e.activation(out=sqc[:, ch, :], in_=cv[j][:], func=AF.Square),
                             reads=[S_cv[j]], writes=[S_sqc])
                        B.op("act", lambda e, j=j, ch=ch, k=k: e.activation(out=cgs[k][:, ch, :], in_=cv[j][:], func=AF.Copy,
                                                                           scale=gconv_sb[:, ch:ch + 1]),
                             reads=[S_cv[j], S_const], writes=[S_cgs[k]])
                    B.dma("sp", cgv[:, :, qb * 256:(qb + 1) * 256], cgs[k][:], S_cgs[k], reads=[S_cgs[k]], writes=[S_cgd])

                    def stats(qb=qb):
                        for t in range(2):
                            B.pe_group([mm(pss[:, t:t + 1], sqc[:, ch, t * 128:(t + 1) * 128], ones_b[:, 0:1], ch == 0, ch == 3) for ch in range(4)],
                                       reads=[S_sqc, S_const], writes=[S_pss])
                        B.op("dve", lambda e: e.tensor_copy(out=ssc[:, 2 * qb:2 * qb + 2], in_=pss[:, 0:2]), reads=[S_pss], writes=[S_ssc])
                    pend_stats.append(stats)

                f1a(0)
                f1b(0)
                f1a(1)
                f1a(2)
                for s in range(32):
                    if s + 1 < 32:
                        f1b(s + 1)
                    if s + 3 < 32:
                        f1a(s + 3)
                    back1a(s)
                    while pend_stats:
                        pend_stats.pop(0)()
                    if s == 5:
                        wov = w_out.rearrange("(c p) n -> p c n", p=128)
                        for c in range(8):
                            B.dma("pool", wo[:, c, :], wov[:, c, :], S_wo, writes=[S_wo])
                    back1b(s)
                while pend_stats:
                    pend_stats.pop(0)()
                B.dma("sp", km_s.rearrange("(pr two) d j -> (two d) pr j", two=2), kmst[:], S_kmst, reads=[S_kmst], writes=[S_kmd])
                B.emit_block()

            with ExitStack() as p2:
              if PHASES >= 2:
                kaug = [sb(p2, "kaug%d" % i, [99, S], BF16) for i in range(2)]
                qaug = [sb(p2, "qaug%d" % i, [99, QTOK], BF16) for i in range(2)]
                qa = [sb(p2, "qa%d" % i, [128, NQT, 99], BF16) for i in range(2)]
                atst = [sb(p2, "atst%d" % i, [64, QTOK], BF16) for i in range(2)]
                gvb = sb(p2, "gvb", [128, NQB, 32], F32)
                gvb2 = sb(p2, "gvb2", [128, NQB, 32], F32)
                cm = sb(p2, "cm", [128, 2, 256], BF16)
                km_f = [sb(p2, "km_f%d" % i, [64, 32], F32) for i in range(2)]
                km_b = [sb(p2, "km_b%d" % i, [64, 32], BF16) for i in range(2)]
                gm = [sb(p2, "gm%d" % i, [128, 32], F32) for i in range(2)]
                t8 = [sb(p2, "t8%d" % i, [128, 8], F32) for i in range(2)]
                tsel = [sb(p2, "tsel%d" % i, [128, 32], F32) for i in range(2)]
                pT = [sb(p2, "pT%d" % i, [128, 2, 256], BF16) for i in range(3)]
                rc = [sb(p2, "rc%d" % i, [128, 256], F32) for i in range(4)]
                pos = [sb(p2, "pos%d" % i, [128, 256], F32) for i in range(4)]
                atf = [sb(p2, "atf%d" % i, [64, 256], F32) for i in range(4)]
                sqa = [sb(p2, "sqa%d" % i, [64, 256], BF16) for i in range(4)]
                sp = [ps(p2, "sp%d" % i, [128, 2, 256], F32) for i in range(3)]
                po = [ps(p2, "po%d" % i, [128, 512], F32) for i in range(2)]
                pgt = ps(p2, "pgt", [128, 512], F32)
                pbc = ps(p2, "pbc", [128, 512], F32)
                ptt_t = ps(p2, "ptt", [128, 1024], BF16)
                ptt = ptt_t[:, 0:128]
                S_kaug = [Slot(), Slot()]
                S_kstat = Slot()
                S_qq = [Slot(), Slot()]
                S_qm = [Slot(), Slot()]
                S_qa = [Slot(), Slot()]
                S_atst = [Slot(), Slot()]
                S_c2 = Slot()
                S_kmf = [Slot(), Slot()]
                S_kmb = [Slot(), Slot()]
                S_gm = [Slot(), Slot()]
                S_t8 = [Slot(), Slot()]
                S_tsel = [Slot(), Slot()]
                S_pT = [Slot() for _ in range(3)]
                S_rc = [Slot() for _ in range(4)]
                S_pos = [Slot() for _ in range(4)]
                S_atf = [Slot() for _ in range(4)]
                S_sqa = [Slot() for _ in range(4)]
                S_sp = [Slot() for _ in range(3)]
                S_po = [Slot(), Slot()]
                S_pgt, S_ptt, S_pbc = Slot(), Slot(), Slot()
                S_pss2 = S_pbc
                S_atd = Slot("at_s")

                B.dma("sp", gvb[:].rearrange("p a b -> p (a b)"), gvb_d, S_c2, writes=[S_c2])
                B.dma("sp", gvb2[:].rearrange("p a b -> p (a b)"), gvb2_d, S_c2, writes=[S_c2])
                B.dma("sp", cm[:].rearrange("p a b -> p (a b)"), cmask, S_c2, writes=[S_c2])
                for i in range(2):
                    B.dma("sp", kaug[i][64:99, :], kstat, S_kstat, writes=[S_kstat])

                def load_head(h):
                    hb = h % 2
                    B.dma("sp", kaug[hb][0:64, :], kT_s[h], S_kaug[hb], reads=[S_kT], writes=[S_kaug[hb]])
                    B.dma("sp", qaug[hb][0:64, :], qT_s[h], S_qq[hb], reads=[S_qT], writes=[S_qq[hb]])
                    B.dma("sp", qa[hb][:].rearrange("p a b -> p (a b)"), qstat[h], S_qa[hb], writes=[S_qa[hb]])

                def prep_head(h):
                    hb = h % 2
                    B.dma("sp", km_f[hb][:], km_s[h], S_kmf[hb], reads=[S_kmd], writes=[S_kmf[hb]])
                    B.op("dve", lambda e: e.tensor_scalar(out=km_b[hb][:], in0=km_f[hb][:], scalar1=1.0 / 256, scalar2=None, op0=ALU.mult),
                         reads=[S_kmf[hb]], writes=[S_kmb[hb]])

                def mask_a(h, qt):
                    hb = h % 2
                    qb = qt // 2
                    g2 = qt % 2
                    B.pe_group([mm(pgt[:, 0:32], qaug[hb][0:64, qt * 128:(qt + 1) * 128], km_b[hb][:, :], True, True)],
                               reads=[S_qq[hb], S_kmb[hb]], writes=[S_pgt])
                    B.op("dve", lambda e: e.tensor_tensor(out=gm[g2][:], in0=pgt[:, 0:32], in1=gvb[:, qb, :], op=ALU.add),
                         reads=[S_pgt, S_c2], writes=[S_gm[g2]])
                    B.op("dve", lambda e: e.max(out=t8[g2][:], in_=gm[g2][:]), reads=[S_gm[g2]], writes=[S_t8[g2]])
                    B.op("dve", lambda e: e.tensor_scalar(out=tsel[g2][:], in0=gm[g2][:], scalar1=t8[g2][:, 2:3], scalar2=-NEG,
                                                          op0=ALU.is_ge, op1=ALU.mult),
                         reads=[S_gm[g2], S_t8[g2]], writes=[S_tsel[g2]])
                    B.op("dve", lambda e: e.tensor_tensor(out=qa[hb][:, qt, 64:96], in0=tsel[g2][:], in1=gvb2[:, qb, :], op=ALU.add),
                         reads=[S_tsel[g2], S_c2], writes=[S_qa[hb]])

                def mask_b(h, qt):
                    hb = h % 2
                    B.pe_group([tr(ptt[0:99, :], qa[hb][:, qt, :], ident[:])], reads=[S_qa[hb], S_const], writes=[S_ptt])
                    B.op("dve", lambda e: e.tensor_copy(out=qaug[hb][64:99, qt * 128:(qt + 1) * 128], in_=ptt[64:99, :]),
                         reads=[S_ptt], writes=[S_qm[hb]])

                sctr = [0]
                HORDER = list(range(NH - 1, -1, -1))
                KEEP = []
                for h_ in range(NH):
                    slope_ = 2.0 ** (-(h_ + 1))
                    kk_ = 0
                    while kk_ < 31 and slope_ * (kk_ * 256 + 1) < 80.0:
                        kk_ += 1
                    KEEP.append(kk_)

                def s_mm(h, qb, j):
                    hb = h % 2
                    sB = 15 + qb
                    qc = slice(qb * 256, (qb + 1) * 256)
                    i = sctr[0] % 3
                    sctr[0] += 1
                    fns = []
                    for kt in range(2):
                        fns.append(mm(sp[i][:, kt, :], kaug[hb][0:99, (2 * j + kt) * 128:(2 * j + kt + 1) * 128], qaug[hb][0:99, qc],
                                      True, j != sB))
                        if j == sB:
                            fns.append(mm(sp[i][:, kt, :], ident[:], cm[:, kt, :], False, True))
                    B.pe_group(fns, reads=[S_kaug[hb], S_kstat, S_qq[hb], S_qm[hb], S_c2, S_const], writes=[S_sp[i]])
                    B.op("act", lambda e: e.activation(out=pT[i][:], in_=sp[i][:], func=AF.Exp), reads=[S_sp[i]], writes=[S_pT[i]])
                    return i

                def pv_mm(h, qb, j, i, first, last):
                    ob = qb % 2
                    B.pe_group([mm(po[ob][0:65, 0:256], V_all[:, 2 * j + kt, h, :], pT[i][:, kt, :], (first and kt == 0), (last and kt == 1))
                                for kt in range(2)], reads=[S_V, S_pT[i]], writes=[S_po[ob]])

                tctr = [0]

                def tail_a(h, qb):
                    ob = qb % 2
                    r = tctr[0] % 4
                    tctr[0] += 1
                    B.op("act", lambda e: e.activation(out=pos[r][0:65, :], in_=po[ob][0:65, 0:256], func=AF.Copy), reads=[S_po[ob]], writes=[S_pos[r]])
                    B.op("dve", lambda e: e.reciprocal(out=rc[r][64:65, :], in_=pos[r][64:65, :]), reads=[S_pos[r]], writes=[S_rc[r]])
                    return r

                def tail_b(h, qb, r):
                    hb = h % 2
                    qc = slice(qb * 256, (qb + 1) * 256)
                    B.pe_group([mm(pbc[0:64, 0:256], ones_f[64:65, 0:64], rc[r][64:65, :], True, True)],
                               reads=[S_rc[r], S_const], writes=[S_pbc])
                    B.op("dve", lambda e: e.tensor_tensor(out=atf[r][:], in0=pbc[0:64, 0:256], in1=pos[r][0:64, :], op=ALU.mult),
                         reads=[S_pbc, S_pos[r]], writes=[S_atf[r]])
                    B.op("pool", lambda e: e.tensor_tensor(out=sqa[r][:], in0=atf[r][:], in1=atf[r][:], op=ALU.mult), reads=[S_atf[r]], writes=[S_sqa[r]])
                    B.op("pool", lambda e: e.tensor_scalar(out=atst[hb][:, qc], in0=atf[r][:], scalar1=gattn_sb[:, h:h + 1], scalar2=None, op0=ALU.mult),
                         reads=[S_atf[r], S_const], writes=[S_atst[hb]])

                def tail_c(h, qb, r):
                    B.pe_group([mm(pbc[:, 256 + t:257 + t], sqa[r][:, t * 128:(t + 1) * 128], ones_b[0:64, 0:1], True, True) for t in range(2)],
                               reads=[S_sqa[r], S_const], writes=[S_pss2])
                    if h == HORDER[0]:
                        B.op("dve", lambda e: e.tensor_copy(out=ssa[:, 2 * qb:2 * qb + 2], in_=pbc[:, 256:258]), reads=[S_pss2], writes=[S_ssa])
                    else:
                        B.op("dve", lambda e: e.tensor_tensor(out=ssa[:, 2 * qb:2 * qb + 2], in0=pbc[:, 256:258], in1=ssa[:, 2 * qb:2 * qb + 2],
                                                              op=ALU.add),
                             reads=[S_pss2, S_ssa], writes=[S_ssa])

                def blocks_of(h, qb):
                    sB = 15 + qb
                    return list(range(max(0, sB - KEEP[h]), sB + 1))

                load_head(HORDER[0])
                prep_head(HORDER[0])
                for qt in range(NQT):
                    mask_a(HORDER[0], qt)
                    mask_b(HORDER[0], qt)
                it = 0
                tails = []
                for hi, h in enumerate(HORDER):
                    hb = h % 2
                    hn = HORDER[hi + 1] if hi + 1 < NH else None
                    if hn is not None:
                        load_head(hn)
                        prep_head(hn)
                    total_iter = sum(len(blocks_of(h, qb)) for qb in range(NQB))
                    sched = {}
                    if hn is not None:
                        step = max(1, int(0.8 * total_iter) // NQT)
                        for qt in range(NQT):
                            ia = it + min(total_iter - 1, 1 + qt * step)
                            ib = it + min(total_iter - 1, 1 + qt * step + 3)
                            sched.setdefault(ia, []).append(lambda qt=qt, hn=hn: mask_a(hn, qt))
                            sched.setdefault(ib, []).append(lambda qt=qt, hn=hn: mask_b(hn, qt))
                    for qb in range(NQB):
                        js = blocks_of(h, qb)
                        n = len(js)
                        ids = {0: s_mm(h, qb, js[0])}
                        if n > 1:
                            ids[1] = s_mm(h, qb, js[1])
                        for i in range(n):
                            if i + 2 < n:
                                ids[i + 2] = s_mm(h, qb, js[i + 2])
                            pv_mm(h, qb, js[i], ids[i], i == 0, i == n - 1)
                            for fn in sched.pop(it, []):
                                fn()
                            for item in list(tails):
                                if item[0] <= it:
                                    item[1]()
                                    tails.remove(item)
                            it += 1
                        r = tail_a(h, qb)
                        tails.append((it + 5, lambda h=h, qb=qb, r=r: tail_b(h, qb, r)))
                        tails.append((it + 10, lambda h=h, qb=qb, r=r: tail_c(h, qb, r)))
                    for key in sorted(sched):
                        for fn in sched[key]:
                            fn()
                    tails.append((it + 11, lambda h=h, hb=hb: B.dma("sp", at_s[h], atst[hb][:], S_atst[hb], reads=[S_atst[hb]], writes=[S_atd])))
                for item in tails:
                    item[1]()
                B.emit_block()

        with ExitStack() as p3:
          if PHASES >= 3:
            wu = sb(p3, "wu", [128, 8, 2 * DFF], BF16)
            wd = sb(p3, "wd", [128, 22, D], BF16)
            x1 = [sb(p3, "x1%d" % i, [128, 2, D], F32) for i in range(2)]
            atb = sb(p3, "atb", [128, 4, 256], BF16)
            cgb = sb(p3, "cgb", [128, 4, 256], BF16)
            xn2 = sb(p3, "xn2", [128, 2, D], BF16)
            h2T = [sb(p3, "h2T%d" % i, [128, 8, 258], BF16) for i in range(2)]
            YA = [sb(p3, "YA%d" % i, [128, 256], F32) for i in range(2)]
            YB = [sb(p3, "YB%d" % i, [128, 256], F32) for i in range(2)]
            YV = [sb(p3, "YV%d" % i, [128, 256], F32) for i in range(2)]
            YG = [sb(p3, "YG%d" % i, [128, 256], F32) for i in range(2)]
            actT = sb(p3, "actT", [128, 22, 256], BF16)
            gfb = sb(p3, "gfb", [128, D], F32)
            rsa = sb(p3, "rsa", [128, NQT], F32)
            rsc = sb(p3, "rsc", [128, NQT], F32)
            ssf = sb(p3, "ssf", [128, 2], F32)
            rtf = sb(p3, "rtf", [128, 2], F32)
            rsf = sb(p3, "rsf", [128, 2], F32)
            ssb = sb(p3, "ssb", [128, 2], F32)
            rtb = sb(p3, "rtb", [128, 2], F32)
            rsb = sb(p3, "rsb", [128, 2], F32)
            pu = [ps(p3, "pu%d" % i, [128, 512], F32) for i in range(4)]
            pd = [ps(p3, "pd%d" % i, [128, 512], F32) for i in range(2)]
            pac = [ps(p3, "pac%d" % i, [128, 512], F32) for i in range(2)]
            S_wd = Slot()
            S_wuq = [Slot() for _ in range(4)]
            S_x1 = [Slot(), Slot()]
            S_atb, S_cgb, S_xn2 = Slot(), Slot(), Slot()
            S_h2T = [Slot(), Slot()]
            S_YA = [Slot() for _ in range(2)]
            S_YB = [Slot() for _ in range(2)]
            S_YV = [Slot(), Slot()]
            S_YG = [Slot(), Slot()]
            S_actT, S_gfb, S_rs = Slot(), Slot(), Slot()
            S_ssf, S_rtf, S_rsf, S_ssb, S_rtb, S_rsb = Slot(), Slot(), Slot(), Slot(), Slot(), Slot()
            S_pu = [Slot() for _ in range(4)]
            S_pd = [Slot(), Slot()]
            S_pac = [Slot(), Slot()]
            gtb = x1[1][:, 0, :]
            S_gtb = S_x1[1]

            wov = w_out.rearrange("(c p) n -> p c n", p=128)
            wuv = w_up.rearrange("(c p) n -> p c n", p=128)
            wdv = w_down.rearrange("(c p) n -> p c n", p=128)
            mod_t = mod_s.tensor
            B.dma("sp", gtb, bass.AP(mod_t, 2048, [[0, 128], [1, D]]), S_gtb, reads=[S_mods], writes=[S_gtb])
            B.dma("sp", gfb[:], bass.AP(mod_t, 5120, [[0, 128], [1, D]]), S_gfb, reads=[S_mods], writes=[S_gfb])
            WQ = [(0, 6), (6, 12), (12, 17), (17, 22)]
            for qi, (v0, v1) in enumerate(WQ):
                for base in (0, DFF):
                    B.dma("pool", wu[:, :, base + v0 * 128:base + v1 * 128], wuv[:, :, base + v0 * 128:base + v1 * 128], S_wuq[qi], writes=[S_wuq[qi]],
                          deps=[S_wuq[qi - 1].w] if qi > 0 else [])
                if qi == 0:
                    for c in range(8):
                        B.op("pool", lambda e, c=c: e.tensor_tensor(out=wo[:, c, :], in0=wo[:, c, :], in1=gtb, op=ALU.mult),
                             reads=[S_gtb], writes=[S_wo])
            for c0 in range(0, 22, 2):
                B.dma("pool", wd[:, c0:c0 + 2, :], wdv[:, c0:c0 + 2, :], S_wd, writes=[S_wd])

            def scale_wd_then_load_gfin():
                for c in range(22):
                    B.op("dve", lambda e, c=c: e.tensor_tensor(out=wd[:, c, :], in0=wd[:, c, :], in1=gfb[:], op=ALU.mult),
                         reads=[S_gfb], writes=[S_wd])
                B.dma("sp", gfb[:], gfin, S_gfb, writes=[S_gfb])
            for src_, dst, ssl in ((ssa, rsa, S_ssa), (ssc, rsc, S_ssc)):
                B.op("act", lambda e, src_=src_, dst=dst: e.activation(out=dst[:], in_=src_[:], func=AF.Sqrt, scale=1.0 / 512, bias=epsc[:]),
                     reads=[ssl, S_const], writes=[S_rs])
                B.op("dve", lambda e, dst=dst: e.reciprocal(out=dst[:], in_=dst[:]), reads=[S_rs], writes=[S_rs])
            for i in range(2):
                B.op("dve", lambda e, i=i: e.memset(h2T[i][:, :, 0:2], 0.0), writes=[S_h2T[i]])

            xsv = xs.rearrange("(n t p) d -> n p t d", t=2, p=128)
            outv = out.rearrange("(n t p) d -> n p t d", t=2, p=128)
            atv = at_s.rearrange("(pr two) d t -> (two d) pr t", two=2)
            cgv = cg_s.rearrange("c p t -> p c t")
            puc = [0]
            ybc = [0]
            yac = [0]
            pend_fin = []
            tpv = [pac[i][:, :].bitcast(BF16).rearrange("p (c t) -> p c t", t=256) for i in range(2)]

            def next_yb():
                i = ybc[0] % 6
                ybc[0] += 1
                return yb[i], S_yb[i]

            def f3a_dma(qb):
                k = qb % 2
                sl = 15 + qb
                qc = slice(qb * 256, (qb + 1) * 256)
                B.dma("sp", x1[k][:], xsv[sl], S_x1[k], writes=[S_x1[k]])
                B.dma("sp", atb[:], atv[:, :, qc], S_atb, reads=[S_atd], writes=[S_atb])
                B.dma("sp", cgb[:], cgv[:, :, qc], S_cgb, reads=[S_cgd], writes=[S_cgb])

            def f3a(qb):
                k = qb % 2
                for t in range(2):
                    tile = 2 * qb + t
                    for n in range(2):
                        ns = slice(n * 512, (n + 1) * 512)
                        B.pe_group([mm(pac[0][:, :], atb[:, c, t * 128:(t + 1) * 128], wo[:, c, ns], c == 0, c == 3) for c in range(4)],
                                   reads=[S_atb, S_wo], writes=[S_pac[0]])
                        B.pe_group([mm(pac[1][:, :], cgb[:, c, t * 128:(t + 1) * 128], wo[:, 4 + c, ns], c == 0, c == 3) for c in range(4)],
                                   reads=[S_cgb, S_wo], writes=[S_pac[1]])
                        B.op("dve", lambda e, t=t, ns=ns, tile=tile: e.scalar_tensor_tensor(
                            out=x1[k][:, t, ns], in0=pac[0][:, :], scalar=rsa[:, tile:tile + 1], in1=x1[k][:, t, ns], op0=ALU.mult, op1=ALU.add),
                            reads=[S_pac[0], S_rs], writes=[S_x1[k]])
                        B.op("dve", lambda e, t=t, ns=ns, tile=tile: e.scalar_tensor_tensor(
                            out=x1[k][:, t, ns], in0=pac[1][:, :], scalar=rsc[:, tile:tile + 1], in1=x1[k][:, t, ns], op0=ALU.mult, op1=ALU.add),
                            reads=[S_pac[1], S_rs], writes=[S_x1[k]])
                for t in range(2):
                    B.op("act", lambda e, t=t: e.activation(out=xn2[:, t, :], in_=x1[k][:, t, :], func=AF.Square, accum_out=ssf[:, t:t + 1]),
                         reads=[S_x1[k]], writes=[S_xn2, S_ssf])
                B.op("act", lambda e: e.activation(out=rtf[:], in_=ssf[:], func=AF.Sqrt, scale=1.0 / D, bias=epsc[:]),
                     reads=[S_ssf, S_const], writes=[S_rtf])
                B.op("dve", lambda e: e.reciprocal(out=rsf[:], in_=rtf[:]), reads=[S_rtf], writes=[S_rsf])
                for t in range(2):
                    B.op("dve", lambda e, t=t: e.tensor_scalar(out=xn2[:, t, :], in0=x1[k][:, t, :], scalar1=rsf[:, t:t + 1], scalar2=None,
                                                               op0=ALU.mult),
                         reads=[S_x1[k], S_rsf], writes=[S_xn2])

            def f3b(qb):
                k = qb % 2
                for half in range(2):
                    B.pe_group([tr(tpv[half][:, c, t * 128:(t + 1) * 128], xn2[:, t, (half * 4 + c) * 128:(half * 4 + c + 1) * 128], ident[:])
                                for t in range(2) for c in range(4)], reads=[S_xn2, S_const], writes=[S_pac[half]])
                    for c in range(4):
                        cc = half * 4 + c
                        B.op("act", lambda e, c=c, cc=cc, half=half: e.activation(out=h2T[k][:, cc, 2:258], in_=tpv[half][:, c, :],
                                                                                 func=AF.Identity, scale=G2[:, cc:cc + 1], bias=sh2[:, cc:cc + 1]),
                             reads=[S_pac[half], S_G], writes=[S_h2T[k]])
                if qb == 1:
                    B.op("dve", lambda e: e.tensor_scalar(out=h2T[k][:, :, 0:2], in0=h2T[1 - k][:, :, 256:258], scalar1=hv[:, 0:1],
                                                          scalar2=None, op0=ALU.mult),
                         reads=[S_h2T[1 - k], S_const], writes=[S_h2T[k]])
                elif qb > 1:
                    B.op("dve", lambda e: e.tensor_copy(out=h2T[k][:, :, 0:2], in_=h2T[1 - k][:, :, 256:258]),
                         reads=[S_h2T[1 - k]], writes=[S_h2T[k]])

            def back3(qb):
                k = qb % 2
                for v in range(22):
                    if qb + 1 < NQB and v == 3:
                        f3a_dma(qb + 1)
                    p2_ = v % 2
                    chs = (v, 22 + v)
                    pis = []
                    for ch in chs:
                        i = puc[0] % 4
                        puc[0] += 1
                        pis.append(i)
                        B.pe_group([mm(pu[i][:, 0:258], wu[:, c, ch * 128:(ch + 1) * 128], h2T[k][:, c, 0:258], c == 0, c == 7) for c in range(8)],
                                   reads=[S_wuq[0 if v < 6 else 1 if v < 12 else 2 if v < 17 else 3], S_h2T[k]], writes=[S_pu[i]])
                    yas = []
                    for w_, (ch, i) in enumerate(zip(chs, pis)):
                        ai = yac[0] % 2
                        yac[0] += 1
                        yas.append(ai)
                        B.op("act", lambda e, i=i, ai=ai, ch=ch: e.activation(out=YA[ai][:], in_=pu[i][:, 2:258], func=AF.Identity,
                                                                             scale=fcw_sb[:, 88 + ch:89 + ch], bias=fcb_sb[:, ch:ch + 1]),
                             reads=[S_pu[i], S_const], writes=[S_YA[ai]])
                    if pend_fin:
                        pend_fin.pop(0)()
                    ybs = []
                    for w_, (ch, i) in enumerate(zip(chs, pis)):
                        bi = ybc[0] % 2
                        ybc[0] += 1
                        ybs.append(bi)
                        ai = yas[w_]
                        B.op("dve", lambda e, i=i, ai=ai, bi=bi, ch=ch: e.scalar_tensor_tensor(
                            out=YB[bi][:], in0=pu[i][:, 1:257], scalar=fcw_sb[:, 44 + ch:45 + ch], in1=YA[ai][:], op0=ALU.mult, op1=ALU.add),
                            reads=[S_pu[i], S_YA[ai], S_const], writes=[S_YB[bi]])
                    outs = ((YV[p2_], S_YV[p2_]), (YG[p2_], S_YG[p2_]))
                    for w_, (ch, i) in enumerate(zip(chs, pis)):
                        bi = ybs[w_]
                        yo, syo = outs[w_]
                        B.op("dve", lambda e, i=i, bi=bi, yo=yo, ch=ch: e.scalar_tensor_tensor(
                            out=yo[:], in0=pu[i][:, 0:256], scalar=fcw_sb[:, ch:ch + 1], in1=YB[bi][:], op0=ALU.mult, op1=ALU.add),
                            reads=[S_pu[i], S_YB[bi], S_const], writes=[syo])
                    def fin_pair(p2_=p2_, v=v):
                        B.op("act", lambda e: e.activation(out=YG[p2_][:], in_=YG[p2_][:], func=AF.Silu), reads=[S_YG[p2_]], writes=[S_YG[p2_]])
                        B.op("dve" if qb == 1 else "pool", lambda e: e.tensor_tensor(out=actT[:, v, :], in0=YV[p2_][:], in1=YG[p2_][:], op=ALU.mult),
                             reads=[S_YV[p2_], S_YG[p2_]], writes=[S_actT])
                    pend_fin.append(fin_pair)
                if pend_fin:
                    pend_fin.pop(0)()
                if qb + 1 < NQB:
                    f3a(qb + 1)
                if qb == 1:
                    scale_wd_then_load_gfin()
                for t in range(2):
                    if t == 1 and qb + 1 < NQB:
                        f3b(qb + 1)
                    for n in range(2):
                        B.pe_group([mm(pd[n][:, :], actT[:, v, t * 128:(t + 1) * 128], wd[:, v, n * 512:(n + 1) * 512], v == 0, v == 21)
                                    for v in range(22)], reads=[S_actT, S_wd], writes=[S_pd[n]])
                        B.op("dve", lambda e, t=t, n=n: e.tensor_tensor(out=x1[k][:, t, n * 512:(n + 1) * 512], in0=pd[n][:, :],
                                                                        in1=x1[k][:, t, n * 512:(n + 1) * 512], op=ALU.add),
                             reads=[S_pd[n]], writes=[S_x1[k]])
                for t in range(2):
                    B.op("act", lambda e, t=t: e.activation(out=xn2[:, t, :], in_=x1[k][:, t, :], func=AF.Square, accum_out=ssb[:, t:t + 1]),
                         reads=[S_x1[k]], writes=[S_xn2, S_ssb])
                B.op("act", lambda e: e.activation(out=rtb[:], in_=ssb[:], func=AF.Sqrt, scale=1.0 / D, bias=epsc[:]),
                     reads=[S_ssb, S_const], writes=[S_rtb])
                B.op("dve", lambda e: e.reciprocal(out=rsb[:], in_=rtb[:]), reads=[S_rtb], writes=[S_rsb])
                for t in range(2):
                    B.op("dve", lambda e, t=t: e.scalar_tensor_tensor(out=x1[k][:, t, :], in0=x1[k][:, t, :], scalar=rsb[:, t:t + 1],
                                                                      in1=gfb[:], op0=ALU.mult, op1=ALU.mult),
                         reads=[S_rsb, S_gfb], writes=[S_x1[k]])
                B.dma("sp", outv[qb - 1], x1[k][:], S_x1[k], reads=[S_x1[k]])

            f3a_dma(0)
            f3a(0)
            f3b(0)
            f3a_dma(1)
            f3a(1)
            f3b(1)
            for qb in range(1, NQB):
                back3(qb)
            B.emit_block()
    return nc


_NC_CACHE = {}


def _consts(half):
    bf = ml_dtypes.bfloat16
    ident = np.eye(128, dtype=np.float32).astype(bf)
    key = np.arange(S)
    kstat = np.zeros((35, S), np.float32)
    kstat[key // 256, key] = 1.0
    kstat[32] = key % 256
    kstat[33] = key // 256
    kstat[34] = 1.0
    kstat = kstat.astype(bf)
    p = np.arange(128)[:, None, None]
    kt = np.arange(2)[None, :, None]
    a = np.arange(256)[None, None, :]
    cmask = np.where(kt * 128 + p <= a, 0.0, NEG).astype(np.float32).reshape(128, 512).astype(bf)
    gvb = np.zeros((NQB, 32), np.float32)
    gvb2 = np.zeros((NQB, 32), np.float32)
    for qb in range(NQB):
        sB = 15 + qb
        for j in range(32):
            valid = (j < sB) and (half == 1 or j >= 16)
            gvb[qb, j] = 0.0 if valid else -1.0e9
            gvb2[qb, j] = NEG if valid else 2 * NEG
        gvb[qb, sB] = -3.0e9
        gvb2[qb, sB] = 0.0
    gvb = np.ascontiguousarray(np.broadcast_to(gvb.reshape(1, -1), (128, NQB * 32))).astype(np.float32)
    gvb2 = np.ascontiguousarray(np.broadcast_to(gvb2.reshape(1, -1), (128, NQB * 32))).astype(np.float32)
    qstat = np.zeros((NH, 128, NQT, 99), np.float32)
    for h in range(NH):
        slope = 2.0 ** (-(h + 1))
        for qt in range(NQT):
            sB = 15 + qt // 2
            apos = (qt % 2) * 128 + np.arange(128)
            qstat[h, :, qt, 96] = slope
            qstat[h, :, qt, 97] = 256.0 * slope
            qstat[h, :, qt, 98] = -slope * (256.0 * sB + apos)
    qstat = qstat.reshape(NH, 128, NQT * 99).astype(bf)
    hv = np.full((128, 1), float(half), np.float32)
    return dict(ident=ident, kstat=kstat, cmask=cmask, gvb=gvb, gvb2=gvb2, qstat=qstat, hv=hv)


def _pc(v, nchunk):
    return np.ascontiguousarray(np.asarray(v, np.float32).reshape(nchunk, 128).T)


def kernel(x, c, w_ada, b_ada, g_mix, w_in, conv_w, conv_b, g_attn_out, g_conv_out, w_out, g_ffn,
           w_up, ffn_conv_w, ffn_conv_b, w_down, g_final):
    f = lambda a: np.ascontiguousarray(np.asarray(a, dtype=np.float32))
    x = f(x)
    c = f(c)
    if "nc" not in _NC_CACHE:
        _NC_CACHE["nc"] = build_program()
    nc = _NC_CACHE["nc"]
    shared = dict(
        w_ada=f(w_ada[0]), b_ada=f(b_ada[0]).reshape(1, -1), gmix=_pc(g_mix[0], 8), w_in=f(w_in[0]),
        convw=np.ascontiguousarray(np.concatenate([_pc(conv_w[0][kk], 4) for kk in range(3)], axis=1)),
        convb=_pc(conv_b[0], 4),
        gattn=np.ascontiguousarray(f(g_attn_out[0]).reshape(8, 64).T),
        gconv=_pc(g_conv_out[0], 4), w_out=f(w_out[0]), gffn=_pc(g_ffn[0], 8), w_up=f(w_up[0]),
        fcw=np.ascontiguousarray(np.concatenate([_pc(ffn_conv_w[0][kk], 44) for kk in range(3)], axis=1)),
        fcb=_pc(ffn_conv_b[0], 44), w_down=f(w_down[0]),
        gfin=np.ascontiguousarray(np.broadcast_to(f(g_final).reshape(1, -1), (128, D))),
    )
    cst = [_consts(0), _consts(1)]
    in_maps = []
    for i in range(NCORES):
        b, half = i // 2, i % 2
        if half == 1:
            xs = x[b]
        else:
            xs = np.concatenate([np.zeros((4096, D), np.float32), x[b, :4096]], axis=0)
        m = dict(shared)
        m.update(cst[half])
        m["xs"] = np.ascontiguousarray(xs)
        m["cT"] = _pc(c[b], 8)
        in_maps.append(m)
    res = run_bass_kernel_spmd(nc, in_maps, core_ids=list(range(NCORES)))
    outp = np.empty((4, S, D), np.float32)
    for i in range(NCORES):
        b, half = i // 2, i % 2
        outp[b, half * 4096:(half + 1) * 4096] = res.results[i]["out"]
    return outp
```
